# Optimizing a Trainium2 kernel written in Bass

```python
import jax, jax.numpy as jnp
from jax import lax
import numpy as np

D_MODEL = 1024
BATCH = 8
SEQ = 4096
DEPTH = 2

MEM_LEN = 256
EXPAND = 2
D_MIX = EXPAND * D_MODEL
GLA_HEADS = 4
GLA_HEAD_K = 128
GLA_HEAD_V = 256
GLA_KEY_WIDTH = GLA_HEADS * GLA_HEAD_K
GLA_VAL_WIDTH = GLA_HEADS * GLA_HEAD_V
GLA_LOWRANK = 16
GLA_GATE_TAU = 16.0
HGRN_HEADS = 4
HGRN_HEAD_DIM = 128
HGRN_WIDTH = HGRN_HEADS * HGRN_HEAD_DIM
LB_FLOOR = 1e-30
XATTN_HEADS = 4
XATTN_HEAD_DIM = 128
XATTN_WIDTH = XATTN_HEADS * XATTN_HEAD_DIM
SPLIT_SIZES = (GLA_KEY_WIDTH, GLA_KEY_WIDTH, GLA_VAL_WIDTH, GLA_LOWRANK,
               HGRN_WIDTH, HGRN_WIDTH, HGRN_WIDTH,
               XATTN_WIDTH,
               D_MIX)
D_IN_PROJ = 6160
CHUNK = 16
NORM_EPS = 1e-6

kernel_name = "hybrid_gla_hgrn2_memxattn_parallel_heads"


def rms_norm(x, w):
    xf = x.astype(jnp.float32)
    y = xf * lax.rsqrt(jnp.mean(xf * xf, axis=-1, keepdims=True) + NORM_EPS)
    return (y * w.astype(jnp.float32)).astype(x.dtype)


def split_heads(t, n_heads):
    b, s, w = t.shape
    return t.reshape(b, s, n_heads, w // n_heads).transpose(0, 2, 1, 3)


def merge_heads(t):
    b, h, s, d = t.shape
    return t.transpose(0, 2, 1, 3).reshape(b, s, h * d)


def chunked_gated_linear_attention(q, k, v, log_g):
    b_, h_, s_, dk = q.shape
    dv = v.shape[-1]
    n = s_ // CHUNK
    qf = q.astype(jnp.float32).reshape(b_, h_, n, CHUNK, dk)
    kf = k.astype(jnp.float32).reshape(b_, h_, n, CHUNK, dk)
    vf = v.astype(jnp.float32).reshape(b_, h_, n, CHUNK, dv)
    bcum = jnp.cumsum(log_g.astype(jnp.float32).reshape(b_, h_, n, CHUNK, dk), axis=3)
    b_last = bcum[..., CHUNK - 1:CHUNK, :]
    causal = jnp.tril(jnp.ones((CHUNK, CHUNK), dtype=bool))[:, :, None]
    diff = bcum[..., :, None, :] - bcum[..., None, :, :]
    decay = jnp.where(causal, jnp.exp(jnp.where(causal, diff, 0.0)), 0.0)
    scores = jnp.einsum('bhnid,bhnjd,bhnijd->bhnij', qf, kf, decay)
    o_intra = jnp.einsum('bhnij,bhnjv->bhniv', scores, vf)
    chunk_kv = jnp.einsum('bhncd,bhncv->bhndv', kf * jnp.exp(b_last - bcum), vf)
    chunk_decay = jnp.exp(b_last[..., 0, :])

    def step(state, inp):
        dec, kv = inp
        return dec[..., None] * state + kv, state

    init = jnp.zeros((b_, h_, dk, dv), jnp.float32)
    _, states_prev = lax.scan(step, init, (jnp.moveaxis(chunk_decay, 2, 0), jnp.moveaxis(chunk_kv, 2, 0)))
    o_inter = jnp.einsum('bhncd,nbhdv->bhncv', qf * jnp.exp(bcum), states_prev)
    return (o_intra + o_inter).reshape(b_, h_, s_, dv)


def setup_inputs(seed: int = 0) -> dict:
    key = jax.random.key(seed)
    ks = jax.random.split(key, 14)
    f32 = jnp.float32
    nrm = lambda k, shape, scale: (jax.random.normal(k, shape, f32) * scale).astype(f32)
    return {
        "x": nrm(ks[0], (BATCH, SEQ, D_MODEL), 1.0),
        "mem": nrm(ks[1], (BATCH, MEM_LEN, D_MODEL), 1.0),
        "norm_w": 1.0 + nrm(ks[2], (DEPTH, D_MODEL), 0.1),
        "w_in": nrm(ks[3], (DEPTH, D_MODEL, D_IN_PROJ), D_MODEL ** -0.5),
        "gla_w_gate_up": nrm(ks[4], (DEPTH, GLA_LOWRANK, GLA_KEY_WIDTH), GLA_LOWRANK ** -0.5),
        "gla_b_gate": nrm(ks[5], (DEPTH, GLA_KEY_WIDTH), 0.1),
        "gla_norm_w": 1.0 + nrm(ks[6], (DEPTH, GLA_HEAD_V), 0.1),
        "hgrn_lower_bounds": nrm(ks[7], (DEPTH, HGRN_WIDTH), 0.5),
        "hgrn_norm_w": 1.0 + nrm(ks[8], (DEPTH, HGRN_HEAD_DIM), 0.1),
        "mem_norm_w": 1.0 + nrm(ks[9], (DEPTH, D_MODEL), 0.1),
        "w_mem_kv": nrm(ks[10], (DEPTH, D_MODEL, 2 * XATTN_WIDTH), D_MODEL ** -0.5),
        "xattn_norm_w": 1.0 + nrm(ks[11], (DEPTH, XATTN_HEAD_DIM), 0.1),
        "w_out": nrm(ks[12], (DEPTH, D_MIX, D_MODEL), D_MIX ** -0.5),
        "final_norm_w": 1.0 + nrm(ks[13], (D_MODEL,), 0.1),
    }


def reference(x, mem, norm_w, w_in, gla_w_gate_up, gla_b_gate, gla_norm_w, hgrn_lower_bounds,
              hgrn_norm_w, mem_norm_w, w_mem_kv, xattn_norm_w, w_out, final_norm_w):
    dtype = x.dtype
    split_idx = [int(i) for i in np.cumsum(SPLIT_SIZES)[:-1]]
    lb_p = jax.nn.softmax(hgrn_lower_bounds.astype(jnp.float32), axis=0)
    lower_bounds = jnp.cumsum(lb_p, axis=0) - lb_p[0]

    for layer in range(DEPTH):
        h = rms_norm(x, norm_w[layer])
        proj = jnp.einsum('bsd,de->bse', h, w_in[layer])
        (gq, gk, gv, glr, hq, hf, hi, xq, gate) = jnp.split(proj, split_idx, axis=-1)

        glr_up = jnp.einsum('bsr,rk->bsk', glr, gla_w_gate_up[layer]) + gla_b_gate[layer]
        gla_log_a = jax.nn.log_sigmoid(glr_up.astype(jnp.float32)) / GLA_GATE_TAU
        gla_o = chunked_gated_linear_attention(
            split_heads(gq, GLA_HEADS) * (GLA_HEAD_K ** -0.5), split_heads(gk, GLA_HEADS),
            split_heads(gv, GLA_HEADS), split_heads(gla_log_a, GLA_HEADS))
        gla_o = merge_heads(rms_norm(gla_o, gla_norm_w[layer]))

        lb = lower_bounds[layer]
        zf = hf.astype(jnp.float32)
        hgrn_log_f = jnp.logaddexp(jnp.log(jnp.maximum(lb, LB_FLOOR)),
                                   jnp.log1p(-lb) + jax.nn.log_sigmoid(zf))
        hgrn_k = (1.0 - lb) * jax.nn.sigmoid(-zf)
        hgrn_o = chunked_gated_linear_attention(
            split_heads(hq, HGRN_HEADS), split_heads(hgrn_k, HGRN_HEADS),
            split_heads(hi, HGRN_HEADS), split_heads(hgrn_log_f, HGRN_HEADS))
        hgrn_o = merge_heads(rms_norm(hgrn_o, hgrn_norm_w[layer]))

        m = rms_norm(mem, mem_norm_w[layer])
        mkv = jnp.einsum('bmd,de->bme', m, w_mem_kv[layer])
        mk, mv = jnp.split(mkv, 2, axis=-1)
        s = jnp.einsum('bhsd,bhmd->bhsm', split_heads(xq, XATTN_HEADS).astype(jnp.float32),
                       split_heads(mk, XATTN_HEADS).astype(jnp.float32)) * (XATTN_HEAD_DIM ** -0.5)
        p = jax.nn.softmax(s, axis=-1)
        xo = jnp.einsum('bhsm,bhmd->bhsd', p, split_heads(mv, XATTN_HEADS).astype(jnp.float32))
        xo = merge_heads(rms_norm(xo, xattn_norm_w[layer]))

        mixed = jnp.concatenate([gla_o, hgrn_o, xo], axis=-1).astype(dtype)
        mixed = mixed * jax.nn.silu(gate)
        x = x + jnp.einsum('bse,ed->bsd', mixed, w_out[layer]).astype(dtype)

    return rms_norm(x, final_norm_w)
```

```python
import contextlib
import numpy as np
import ml_dtypes
import concourse.bass as bass
import concourse.mybir as mybir
from concourse.bass_utils import run_bass_kernel_spmd

F32 = mybir.dt.float32
BF16 = mybir.dt.bfloat16
AF = mybir.ActivationFunctionType
ALU = mybir.AluOpType
AX = mybir.AxisListType

D = 1024
DIN = 6160
DMIX = 2048
MEM = 256
EPS = 1e-6
C_GQ, C_GK, C_GV, C_GLR, C_HQ, C_HF, C_HI, C_XQ, C_GATE = 0, 512, 1024, 2048, 2064, 2576, 3088, 3600, 4112


class Prog:
    ENG = ("pe", "act", "dve", "pool", "sp")

    def __init__(self):
        self.q = {e: [] for e in self.ENG}
        self.cnt = {}
        self.res = {}
        self.waited = {e: {} for e in self.ENG}

    def op(self, eng, fn, reads=(), writes=(), dma=None):
        deps = {}

        def add(tok):
            if tok is None:
                return
            k, v = tok
            if deps.get(k, 0) < v:
                deps[k] = v

        for r in reads:
            st = self.res.get(r)
            if st:
                add(st[0])
        for w in writes:
            st = self.res.get(w)
            if st:
                add(st[0])
                for k, v in st[1].items():
                    add((k, v))
        if eng == "pe":
            deps.pop("pe", None)
        waits = []
        wd = self.waited[eng]
        for k, v in deps.items():
            if wd.get(k, 0) < v:
                wd[k] = v
                waits.append((k, v))
        key, amt = (dma, 16) if dma is not None else (eng, 1)
        self.cnt[key] = self.cnt.get(key, 0) + amt
        tok = (key, self.cnt[key])
        self.q[eng].append((waits, fn, key, amt))
        for r in reads:
            st = self.res.setdefault(r, [None, {}])
            if st[1].get(key, 0) < tok[1]:
                st[1][key] = tok[1]
        for w in writes:
            self.res[w] = [tok, {}]
        return tok

    def emit(self, nc):
        with contextlib.ExitStack() as es:
            sems = {k: es.enter_context(nc.semaphore("s_" + k)) for k in self.cnt}
            block = es.enter_context(nc.Block())
            final = [(k, v) for k, v in self.cnt.items()]

            def run(name, e):
                for waits, fn, key, amt in self.q[name]:
                    for k, v in waits:
                        e.wait_ge(sems[k], v)
                    ins = fn(e)
                    ins.then_inc(sems[key], amt)

            @block.tensor
            def _(e):
                run("pe", e)

            @block.scalar
            def _(e):
                run("act", e)

            @block.vector
            def _(e):
                run("dve", e)

            @block.gpsimd
            def _(e):
                run("pool", e)

            @block.sync
            def _(e):
                run("sp", e)
                for k, v in final:
                    e.wait_ge(sems[k], v)


def build(S, layers, final_norm, n_layers_total=2):
    nc = bass.Bass("TRN2", target_bir_lowering=False)
    P = Prog()
    NT = S // 128
    NL = n_layers_total

    def din(name, shape, dt=F32):
        return nc.dram_tensor(name, list(shape), dt, kind="ExternalInput").ap()

    x_d = din("x", [S, D])
    mem_d = din("mem", [MEM, D])
    win_d = din("w_in", [NL, D, DIN])
    wout_d = din("w_out", [NL, DMIX, D])
    wkv_d = din("w_kv", [NL, D, 2 * 512])
    wup_d = din("wup", [NL, 17, 512])
    pcols_d = din("pcols", [NL, 128, 32])
    lbraw_d = din("lbraw", [128, NL * 512])
    fnw_d = din("fnw", [128, D])
    cst_d = din("cst", [128, 6 * 128 + 8])
    msk_d = din("msk", [128, 3 * 128])
    out_d = nc.dram_tensor("out", [S, D], F32, kind="ExternalOutput").ap()
    xmid_d = None
    if len(layers) > 1:
        xmid_d = nc.dram_tensor("xmid", [S, D], F32, kind="Internal").ap()

    es = contextlib.ExitStack()
    with es:
        def sb(name, shape, dt):
            return es.enter_context(nc.sbuf_tensor("sb_" + name, list(shape), dt))

        def ps(name, shape, dt):
            return es.enter_context(nc.psum_tensor("ps_" + name, list(shape), dt))

        win = sb("win", [128, 8, DIN], BF16)
        wout = sb("wout", [128, 16, D], BF16)
        cst = sb("cst", [128, 4 * 128 + 8], F32)
        mskb = sb("mskb", [128, 3 * 128], BF16)
        pcols = sb("pcols", [128, 32], F32)
        wup = sb("wup", [32, 512], BF16)
        lbt = sb("lbt", [128, NL * 512], F32)
        fnw = sb("fnw", [128, D], F32)
        gS = sb("gS", [128, 4, 256], F32)
        gSb = sb("gSb", [128, 4, 256], BF16)
        hS = sb("hS", [128, 4, 128], F32)
        hSb = sb("hSb", [128, 4, 4, 128], BF16)
        mkT = sb("mkT", [128, 4, 256], BF16)
        mv = sb("mv", [128, 2, 512], BF16)
        xt = [sb("xt0", [128, D], F32), sb("xt1", [128, D], F32)]
        h = sb("h", [128, D], BF16)
        hT = sb("hT", [128, 8, 128], BF16)
        Ft = [sb("F%d" % i, [128, 512], F32) for i in range(6)]
        qb = sb("qb", [128, 512], BF16)
        kb = sb("kb", [128, 512], BF16)
        khb = sb("khb", [128, 512], BF16)
        qkT = sb("qkT", [128, 8, 128], BF16)
        v = sb("v", [128, 1024], BF16)
        hi = sb("hi", [128, 512], BF16)
        him = sb("him", [128, 4, 4, 128], BF16)
        AT = sb("AT", [128, 4, 128], BF16)
        glrT = sb("glrT", [32, 128], BF16)
        G = sb("G", [128, DMIX], BF16)
        mixed = sb("mixed", [128, DMIX], BF16)
        outt = sb("outt", [128, D], F32)
        lbraw = outt
        xqs = qb
        xqT = AT
        pb = him[:].rearrange("p a b c -> p (a b c)")[:, 0:1024].rearrange("p (a b) -> p a b", a=4)
        HIM = ["him%d" % c_ for c_ in range(4)]
        pT = qkT
        mT = G[:].rearrange("p (c m) -> p c m", c=16)
        st = sb("st", [128, 128], F32)
        junk = sb("junk", [128, 256], BF16)

        pj = [ps("pj0", [128, 512], F32), ps("pj1", [128, 512], F32)]
        pt = ps("pt", [128, 1024], BF16)
        pcu = [ps("pcu0", [128, 512], F32), ps("pcu1", [128, 512], F32)]
        pss = ps("pss", [128, 512], F32)
        po = [ps("po0", [128, 512], F32), ps("po1", [128, 512], F32)]

        ident = mskb[:, 0:128]
        maskG = mskb[:, 128:256]
        maskH = mskb[:, 256:384]
        triG = cst[:, 0:128]
        triUG = cst[:, 128:256]
        triH = cst[:, 256:384]
        triUH = cst[:, 384:512]
        cind = cst[:, 512:520]
        nwT = pcols[:, 0:8]
        gwT = pcols[:, 8:24]
        mnwT = pcols[:, 24:32]

        pjc = [0]

        def next_pj():
            i = pjc[0] % 2
            pjc[0] += 1
            return pj[i], "pj%d" % i

        def bc(ap2, n):
            return ap2.unsqueeze(2).broadcast_to([128, ap2.shape[1], n])

        def bc_mid(ap2, n):
            return ap2.unsqueeze(1).broadcast_to([128, n, ap2.shape[1]])

        def mm(out_ap, pairs, reads, writes, first_start=True, **kw):
            pairs = list(pairs)

            def fn(e):
                n = len(pairs)
                ins = None
                for i, (a, b) in enumerate(pairs):
                    ins = e.matmul(out_ap, a, b, start=(first_start and i == 0), stop=(i == n - 1), **kw)
                return ins
            P.op("pe", fn, reads, writes)

        def mm_multi(items, reads, writes):
            items = list(items)

            def fn(e):
                ins = None
                for (o, a, b, s0, s1, kw) in items:
                    ins = e.matmul(o, a, b, start=s0, stop=s1, **kw)
                return ins
            P.op("pe", fn, reads, writes)

        def transposes(items, reads, writes):
            items = list(items)

            def fn(e):
                ins = None
                for (o, i_) in items:
                    ins = e.transpose(o, i_, ident)
                return ins
            P.op("pe", fn, list(reads) + ["mskb"], writes)

        def act(out, in_, func, reads, writes, **kw):
            P.op("act", lambda e: e.activation(out, in_, func, **kw), reads, writes)

        def tt(eng, out, in0, in1, op, reads, writes):
            P.op(eng, lambda e: e.tensor_tensor(out, in0, in1, op), reads, writes)

        def ts(eng, out, in0, s1, s2, op0, op1, reads, writes):
            if s2 is None:
                P.op(eng, lambda e: e.tensor_scalar(out, in0, s1, None, op0), reads, writes)
            else:
                P.op(eng, lambda e: e.tensor_scalar(out, in0, s1, s2, op0, op1), reads, writes)

        def stt(out, in0, scalar, in1, op0, op1, reads, writes):
            P.op("dve", lambda e: e.scalar_tensor_tensor(out, in0, scalar, in1, op0, op1), reads, writes)

        def copy(eng, out, in_, reads, writes):
            if eng == "act":
                P.op("act", lambda e: e.activation(out, in_, AF.Identity), reads, writes)
            else:
                P.op(eng, lambda e: e.tensor_copy(out, in_), reads, writes)

        def dma(eng, out, in_, reads, writes, sem, **kw):
            P.op(eng, lambda e: e.dma_start(out=out, in_=in_, **kw), reads, writes, dma=sem)

        def rstd_from(ssq_col, tmp_col, out_col, inv_n, rname, reads=None):
            act(tmp_col, ssq_col, AF.Ln, reads or [rname], [rname + "_t"], scale=inv_n, bias=EPS)
            act(out_col, tmp_col, AF.Exp, [rname + "_t"], [rname + "_r"], scale=-0.5)

        dma("sp", cst[:], cst_d[:, 0:520], [], ["cst"], "d_cst")
        dma("pool", mskb[:], msk_d, [], ["mskb"], "d_msk")
        dma("sp", lbraw[:], lbraw_d, [], ["outt"], "d_lbraw")
        if final_norm:
            dma("sp", fnw[:], fnw_d, [], ["fnw"], "d_fnw")
        P.op("pool", lambda e: e.memset(glrT[:], 1.0), [], ["glrT"])
        lr3 = lbraw[:].rearrange("p (l n) -> p l n", l=NL)
        lb3 = lbt[:].rearrange("p (l n) -> p l n", l=NL)
        mx = Ft[0]
        P.op("dve", lambda e: e.tensor_copy(mx[:], lr3[:, 0, :]), ["outt"], ["F0"])
        for l in range(1, NL):
            tt("dve", mx[:], mx[:], lr3[:, l, :], ALU.max, ["F0", "outt"], ["F0"])
        for l in range(NL):
            tt("dve", lb3[:, l, :], lr3[:, l, :], mx[:], ALU.subtract, ["F0", "outt"], ["lbt"])
        act(lbt[:], lbt[:], AF.Exp, ["lbt"], ["lbt"])
        den = Ft[1]
        P.op("dve", lambda e: e.tensor_copy(den[:], lb3[:, 0, :]), ["lbt"], ["F1"])
        for l in range(1, NL):
            tt("dve", den[:], den[:], lb3[:, l, :], ALU.add, ["F1", "lbt"], ["F1"])
        P.op("dve", lambda e: e.reciprocal(den[:], den[:]), ["F1"], ["F1"])
        for l in range(NL):
            tt("dve", lb3[:, l, :], lb3[:, l, :], den[:], ALU.mult, ["F1", "lbt"], ["lbt"])
        p0 = Ft[2]
        P.op("dve", lambda e: e.tensor_copy(p0[:], lb3[:, 0, :]), ["lbt"], ["F2"])
        for l in range(1, NL):
            tt("dve", lb3[:, l, :], lb3[:, l, :], lb3[:, l - 1, :], ALU.add, ["lbt"], ["lbt"])
        for l in range(NL):
            tt("dve", lb3[:, l, :], lb3[:, l, :], p0[:], ALU.subtract, ["lbt", "F2"], ["lbt"])

        F = ["F%d" % i for i in range(6)]

        for li, L in enumerate(layers):
            last = (li == len(layers) - 1)
            src_d = x_d if li == 0 else xmid_d
            dst_d = out_d if last else xmid_d
            do_final = last and final_norm
            lbL = lbt[:, L * 512:(L + 1) * 512]

            dma("sp", pcols[:], pcols_d[L], [], ["pcols"], "d_pcols")
            dma("pool", wup[0:17, :], wup_d[L], [], ["wup"], "d_wup")
            wkv = wout[:, 0:8, :]
            for hf_ in range(2):
                dma("pool", wout[:, hf_ * 4:(hf_ + 1) * 4, :],
                    wkv_d[L, hf_ * 512:(hf_ + 1) * 512, :].rearrange("(c p) n -> p c n", p=128),
                    [], ["wq%d" % hf_], "d_wkv%d" % hf_, max_dma_last_dim=4096)
            for c in range(8):
                dma("pool", win[:, c, :], win_d[L, c * 128:(c + 1) * 128, :], [], ["win%d" % c], "d_win%d" % c,
                    max_dma_last_dim=4096)
            P.op("dve", lambda e: e.memset(gS[:], 0.0), [], ["gS"])
            P.op("dve", lambda e: e.memset(hS[:], 0.0), [], ["hS"])
            P.op("pool", lambda e: e.memset(gSb[:], 0.0), [], ["gSb"])
            P.op("pool", lambda e: e.memset(hSb[:], 0.0), [], ["hSb"])
            WIN = ["win%d" % c for c in range(8)]

            mnT = mixed[:].rearrange("p (c m) -> p c m", c=8)
            for blk in range(2):
                xb = xt[blk]
                xn = "xt%d" % blk
                dma("sp", xb[:], mem_d[blk * 128:(blk + 1) * 128, :], [], [xn], "d_x%d" % blk)
                act(h[:], xb[:], AF.Square, [xn], ["h", "ssq"], accum_out=st[:, 0:1])
                rstd_from(st[:, 0:1], st[:, 1:2], st[:, 2:3], 1.0 / D, "ssq")
                ts("dve", h[:], xb[:], st[:, 2:3], None, ALU.mult, None, [xn, "ssq_r"], ["h"])
                transposes([(pt[:, c * 128:(c + 1) * 128], h[:, c * 128:(c + 1) * 128]) for c in range(8)],
                           ["h"], ["pt"])
                tt("dve", mnT[:, :, blk * 128:(blk + 1) * 128], pt[:].rearrange("p (c m) -> p c m", c=8),
                   bc(mnwT, 128), ALU.mult, ["pt", "pcols"], ["mixed"])
            for hd in range(4):
                pjt, pjn = next_pj()
                mm(pjt[:, 0:256], [(wkv[:, c, hd * 128:(hd + 1) * 128], mnT[:, c, :]) for c in range(8)],
                   ["wq0", "wq1", "mixed"], [pjn])
                copy("act", mkT[:, hd, :], pjt[:, 0:256], [pjn], ["mkT"])
            for blk in range(2):
                pjt, pjn = next_pj()
                mm(pjt[:], [(mnT[:, c, blk * 128:(blk + 1) * 128], wkv[:, c, 512:1024]) for c in range(8)],
                   ["wq0", "wq1", "mixed"], [pjn])
                copy("act", mv[:, blk, :], pjt[:], [pjn], ["mv"])
            for q4 in range(4):
                dma("pool", wout[:, q4 * 4:(q4 + 1) * 4, :],
                    wout_d[L, q4 * 512:(q4 + 1) * 512, :].rearrange("(c p) n -> p c n", p=128),
                    [], ["wq%d" % q4], "d_wo%d" % q4, max_dma_last_dim=4096)
            WOUT = ["wq%d" % q4 for q4 in range(4)]

            def proj(col0, ncol, dst_names):
                pjt, pjn = next_pj()
                mm(pjt[:, 0:ncol], [(hT[:, c, :], win[:, c, col0:col0 + ncol]) for c in range(8)],
                   ["hT"] + WIN, [pjn])
                return pjt, pjn

            def LOAD(t):
                dma("sp", xt[t % 2][:], src_d[t * 128:(t + 1) * 128, :], ["xmid%d" % t] if li > 0 else [],
                    ["xt%d" % (t % 2)], "d_x%d" % (t % 2))

            def HEAD_a(t):
                xb = xt[t % 2]
                xn = "xt%d" % (t % 2)
                act(h[:], xb[:], AF.Square, [xn], ["h", "ssq"], accum_out=st[:, 0:1])
                rstd_from(st[:, 0:1], st[:, 1:2], st[:, 2:3], 1.0 / D, "ssq")
                ts("dve", h[:], xb[:], st[:, 2:3], None, ALU.mult, None, [xn, "ssq_r"], ["h"])

            def HEAD_b(t):
                transposes([(pt[:, c * 128:(c + 1) * 128], h[:, c * 128:(c + 1) * 128]) for c in range(8)],
                           ["h"], ["pt"])
                tt("dve", hT[:], pt[:].rearrange("p (c m) -> p c m", c=8), bc(nwT, 128), ALU.mult,
                   ["pt", "pcols"], ["hT"])

            def HEAD(t):
                HEAD_a(t)
                HEAD_b(t)

            def qk_transposes():
                transposes([(pt[:, hd * 128:(hd + 1) * 128], qb[:, hd * 128:(hd + 1) * 128]) for hd in range(4)] +
                           [(pt[:, (4 + hd) * 128:(5 + hd) * 128], kb[:, hd * 128:(hd + 1) * 128]) for hd in range(4)],
                           ["qb", "kb"], ["pt"])
                copy("dve", qkT[:], pt[:].rearrange("p (c m) -> p c m", c=8), ["pt"], ["qkT"])
                mm_multi([(pss[:, hd * 128:(hd + 1) * 128], qkT[:, 4 + hd, :], qkT[:, hd, :], True, True, {})
                          for hd in range(4)], ["qkT"], ["pss"])

            def mixed_T(e0, e1, rname):
                n = e1 - e0
                transposes([(pt[:, i_ * 128:(i_ + 1) * 128], mixed[:, (e0 + i_) * 128:(e0 + i_ + 1) * 128])
                            for i_ in range(n)], ["mixed"], ["pt"])
                tt("dve", mT[:, e0:e1, :], pt[:, 0:n * 128].rearrange("p (c m) -> p c m", c=n),
                   bc(gwT[:, e0:e1], 128), ALU.mult, ["pt", "pcols"], [rname])

            ctx = {}

            def s_GT():
                for g4 in range(4):
                    pjt, pjn = proj(C_GATE + g4 * 512, 512, None)
                    act(G[:, g4 * 512:(g4 + 1) * 512], pjt[:], AF.Silu, [pjn], ["G", "mTa", "mTb", "mTc"])

            def s_X1():
                pjt, pjn = proj(C_XQ, 512, None)
                act(xqs[:], pjt[:], AF.Identity, [pjn], ["qb"], scale=float(128 ** -0.5))

            def s_G1a():
                pjt, pjn = next_pj()
                mm(pjt[0:16, 0:128], [(win[:, c, C_GLR:C_GLR + 16], hT[:, c, :]) for c in range(8)],
                   ["hT"] + WIN, [pjn])
                copy("act", glrT[0:16, :], pjt[0:16, 0:128], [pjn], ["glrT"])

            def s_X2():
                transposes([(pt[:, hd * 128:(hd + 1) * 128], xqs[:, hd * 128:(hd + 1) * 128]) for hd in range(4)],
                           ["qb"], ["pt"])
                copy("dve", xqT[:], pt[:, 0:512].rearrange("p (c m) -> p c m", c=4), ["pt"], ["AT"])

            def s_G1b():
                pjt, pjn = next_pj()
                mm(pjt[:], [(glrT[0:17, :], wup[0:17, :])], ["glrT", "wup"], [pjn])
                act(Ft[0][:], pjt[:], AF.Exp, [pjn], [F[0]], scale=-1.0)

            def s_X3():
                mm_multi([(pcu[hd // 2][:, (hd % 2) * 256:(hd % 2 + 1) * 256], xqT[:, hd, :], mkT[:, hd, :],
                           True, True, {}) for hd in range(4)], ["AT", "mkT"], ["pcu0", "pcu1"])
                for half in range(2):
                    P.op("dve", (lambda half: lambda e: e.tensor_reduce(
                        st[:, 48 + 2 * half:50 + 2 * half], pcu[half][:].rearrange("p (a b) -> p a b", a=2),
                        AX.X, ALU.max))(half), ["pcu%d" % half], ["xmax%d" % half])
                ts("dve", st[:, 52:56], st[:, 48:52], -1.0, None, ALU.mult, None, ["xmax0", "xmax1"], ["xnmax"])
                for hd in range(4):
                    act(pb[:, hd, :], pcu[hd // 2][:, (hd % 2) * 256:(hd % 2 + 1) * 256], AF.Exp,
                        ["pcu%d" % (hd // 2), "xnmax"], HIM + ["xZ%d" % hd], bias=st[:, 52 + hd:53 + hd],
                        accum_out=st[:, 56 + hd:57 + hd])

            def s_Hp():
                pjz, pjzn = proj(C_HF, 512, None)
                act(Ft[5][:], pjz[:], AF.Exp, [pjzn], [F[5]], scale=-1.0)

            def s_G2():
                act(Ft[1][:], Ft[0][:], AF.Ln, [F[0]], [F[1]], bias=1.0)
                mm(pcu[0][:], [(triG, Ft[1][:])], ["cst", F[1]], ["pcu0"])
                mm(pcu[1][:], [(triUG, Ft[1][:])], ["cst", F[1]], ["pcu1"])
                pjt, pjn = next_pj()
                mm_multi([(pjt[:, hd:hd + 1], Ft[1][:, hd * 128:(hd + 1) * 128], cind[:, 0:1], True, True, {})
                          for hd in range(4)], ["cst", F[1]], [pjn])
                act(st[:, 8:12], pjt[:, 0:4], AF.Exp, [pjn], ["gdec"], scale=-1.0 / 16)
                act(Ft[2][:], pcu[0][:], AF.Exp, ["pcu0"], [F[2]], scale=-1.0 / 16)
                act(Ft[3][:], pcu[0][:], AF.Exp, ["pcu0"], [F[3]], scale=1.0 / 16)
                act(Ft[4][:], pcu[1][:], AF.Exp, ["pcu1"], [F[4]], scale=-1.0 / 16)

            def s_H1():
                act(Ft[1][:], Ft[5][:], AF.Ln, [F[5]], [F[1]], bias=1.0)

            def s_X4():
                transposes([(pt[:, (hd * 2 + mc) * 128:(hd * 2 + mc + 1) * 128], pb[:, hd, mc * 128:(mc + 1) * 128])
                            for hd in range(4) for mc in range(2)], HIM, ["pt"])
                copy("dve", pT[:], pt[:].rearrange("p (c m) -> p c m", c=8), ["pt"], ["qkT"])

            def s_G3a():
                pjt, pjn = proj(C_GQ, 512, None)
                stt(qb[:], pjt[:], float(128 ** -0.5), Ft[2][:], ALU.mult, ALU.mult, [pjn, F[2]], ["qb"])
                pjt, pjn = proj(C_GK, 512, None)
                tt("dve", kb[:], pjt[:], Ft[3][:], ALU.mult, [pjn, F[3]], ["kb"])
                tt("dve", khb[:], pjt[:], Ft[4][:], ALU.mult, [pjn, F[4]], ["khb"])

            def s_X5():
                items = []
                for hd in range(4):
                    for mc in range(2):
                        items.append((po[1][:, hd * 128:(hd + 1) * 128], pT[:, hd * 2 + mc, :],
                                      mv[:, mc, hd * 128:(hd + 1) * 128], mc == 0, mc == 1, {}))
                mm_multi(items, ["qkT", "mv"], ["po1"])
                for hd in range(4):
                    act(junk[:, 0:128], po[1][:, hd * 128:(hd + 1) * 128], AF.Square, ["po1"], ["xssq%d" % hd],
                        accum_out=st[:, 80 + hd:81 + hd])

            def s_G3b():
                for half in range(2):
                    pjt, pjn = proj(C_GV + half * 512, 512, None)
                    copy("act", v[:, half * 512:(half + 1) * 512], pjt[:], [pjn], ["v"])

            def s_H2():
                tt("dve", Ft[0][:], Ft[5][:], lbL, ALU.mult, [F[5], "lbt"], [F[0]])
                act(Ft[0][:], Ft[0][:], AF.Ln, [F[0]], [F[0]], bias=1.0)
                tt("dve", Ft[5][:], Ft[0][:], Ft[1][:], ALU.subtract, [F[0], F[1]], [F[5]])

            def s_X6():
                tt("dve", st[:, 60:64], st[:, 56:60], st[:, 56:60], ALU.mult, ["xZ%d" % hd for hd in range(4)], ["xz2"])
                ts("dve", st[:, 84:88], st[:, 80:84], 1.0 / 128, None, ALU.mult, None,
                   ["xssq%d" % hd for hd in range(4)], ["xvv"])
                stt(st[:, 84:88], st[:, 60:64], EPS, st[:, 84:88], ALU.mult, ALU.add, ["xz2", "xvv"], ["xvv"])
                act(st[:, 84:88], st[:, 84:88], AF.Ln, ["xvv"], ["xvv"])
                act(st[:, 88:92], st[:, 84:88], AF.Exp, ["xvv"], ["xr"], scale=-0.5)
                for hd in range(4):
                    stt(mixed[:, 1536 + hd * 128:1536 + (hd + 1) * 128], po[1][:, hd * 128:(hd + 1) * 128],
                        st[:, 88 + hd:89 + hd], G[:, 1536 + hd * 128:1536 + (hd + 1) * 128], ALU.mult, ALU.mult,
                        ["po1", "xr", "G"], ["mixed"])

            def s_G4():
                qk_transposes()
                tt("dve", AT[:], pss[:].rearrange("p (c m) -> p c m", c=4), bc_mid(maskG, 4), ALU.mult,
                   ["pss", "mskb"], ["AT"])

            def s_H3a():
                act(Ft[0][:], Ft[5][:], AF.Exp, [F[5]], [F[0]])
                ts("dve", Ft[0][:], Ft[0][:], -1.0, 1.0, ALU.mult, ALU.add, [F[0]], [F[0]])

            def s_H3b():
                pjr, pjrn = next_pj()
                mm(pss[:], [(triH, Ft[5][:])], ["cst", F[5]], ["pss"])
                mm(pjr[:], [(triUH, Ft[5][:])], ["cst", F[5]], [pjrn])
                pjt, pjn = next_pj()
                mm_multi([(pjt[:, hd * 4:hd * 4 + 4], Ft[5][:, hd * 128:(hd + 1) * 128], cind[:, 1:5], True, True, {})
                          for hd in range(4)], ["cst", F[5]], [pjn])
                act(Ft[1][:], pss[:], AF.Exp, ["pss"], [F[1]])
                act(Ft[2][:], pss[:], AF.Exp, ["pss"], [F[2]], scale=-1.0)
                act(Ft[3][:], pjr[:], AF.Exp, [pjrn], [F[3]])
                act(st[:, 32:48], pjt[:, 0:16], AF.Exp, [pjn], ["hdec"])

            def s_G5a():
                items = []
                for hd in range(4):
                    o_ap = po[hd // 2][:, (hd % 2) * 256:(hd % 2 + 1) * 256]
                    items.append((o_ap, AT[:, hd, :], v[:, hd * 256:(hd + 1) * 256], True, False, {}))
                    items.append((o_ap, qkT[:, hd, :], gSb[:, hd, :], False, True, {}))
                mm_multi(items, ["AT", "v", "qkT", "gSb"], ["po0", "po1"])
                for hd in range(4):
                    o_ap = po[hd // 2][:, (hd % 2) * 256:(hd % 2 + 1) * 256]
                    act(junk[:, 0:256], o_ap, AF.Square, ["po%d" % (hd // 2)], ["gssq%d" % hd],
                        accum_out=st[:, 16 + hd:17 + hd])
                rstd_from(st[:, 16:20], st[:, 20:24], st[:, 24:28], 1.0 / 256, "gssq",
                          ["gssq%d" % hd for hd in range(4)])

            def s_G5b():
                mm_multi([(pcu[hd // 2][:, (hd % 2) * 256:(hd % 2 + 1) * 256], khb[:, hd * 128:(hd + 1) * 128],
                           v[:, hd * 256:(hd + 1) * 256], True, True, {}) for hd in range(4)],
                         ["khb", "v"], ["pcu0", "pcu1"])
                for hd in range(4):
                    stt(gS[:, hd, :], gS[:, hd, :], st[:, 8 + hd:9 + hd],
                        pcu[hd // 2][:, (hd % 2) * 256:(hd % 2 + 1) * 256], ALU.mult, ALU.add,
                        ["gS", "gdec", "pcu%d" % (hd // 2)], ["gS"])
                copy("act", gSb[:].rearrange("p a b -> p (a b)"), gS[:].rearrange("p a b -> p (a b)"),
                     ["gS"], ["gSb"])

            def s_G5c():
                for hd in range(4):
                    o_ap = po[hd // 2][:, (hd % 2) * 256:(hd % 2 + 1) * 256]
                    stt(mixed[:, hd * 256:(hd + 1) * 256], o_ap, st[:, 24 + hd:25 + hd],
                        G[:, hd * 256:(hd + 1) * 256], ALU.mult, ALU.mult,
                        ["po%d" % (hd // 2), "gssq_r", "G"], ["mixed"])

            def s_H4():
                pjt, pjn = proj(C_HQ, 512, None)
                tt("dve", qb[:], pjt[:], Ft[1][:], ALU.mult, [pjn, F[1]], ["qb"])
                tt("dve", kb[:], Ft[0][:], Ft[2][:], ALU.mult, [F[0], F[2]], ["kb"])
                tt("dve", khb[:], Ft[0][:], Ft[3][:], ALU.mult, [F[0], F[3]], ["khb"])
                pjt, pjn = proj(C_HI, 512, None)
                copy("act", hi[:], pjt[:], [pjn], ["hi"])
                for c4 in range(4):
                    act(him[:, :, c4, :], hi[:].rearrange("p (a b) -> p a b", a=4), AF.Identity, ["hi", "cst"],
                        ["him%d" % c4], scale=cind[:, 1 + c4:2 + c4])

            ubank = [(pcu[0], "pcu0"), (pcu[1], "pcu1"), (pss, "pss"), (po[1], "po1")]

            def s_H5():
                qk_transposes()
                tt("dve", AT[:], pss[:].rearrange("p (c m) -> p c m", c=4), bc_mid(maskH, 4), ALU.mult,
                   ["pss", "mskb"], ["AT"])
                mm_multi([(po[0][:, hd * 128:(hd + 1) * 128], AT[:, hd, :], hi[:, hd * 128:(hd + 1) * 128],
                           hd == 0, False, {"skip_group_check": True}) for hd in range(4)],
                         ["AT", "hi"], ["po0"])
                for hd in range(4):
                    pu, pun = ubank[hd]
                    mm(pu[:], [(khb[:, hd * 128:(hd + 1) * 128], him[:, hd, :, :].rearrange("p a b -> p (a b)"))],
                       ["khb"] + HIM, [pun])

            def s_O2a(part):
                elist = [0, 1, 2, 3, 4, 5, 6, 7, 12, 13, 14, 15]
                es_ = elist[part * 3:(part + 1) * 3]
                items = []
                for half in range(2):
                    for e_ in es_:
                        items.append((pj[half][:], mT[:, e_, :], wout[:, e_, half * 512:(half + 1) * 512],
                                      e_ == 0, False, {"skip_group_check": True}))
                mm_multi(items, ["mTa", "mTb"] + WOUT, ["pj0", "pj1"])

            def s_H6():
                for c4 in range(4):
                    for hd in range(4):
                        pu, pun = ubank[hd]
                        sbn = "hSb%d_%d" % (hd, c4)
                        mm_multi([(po[0][32 * c4:32 * (c4 + 1), hd * 128:(hd + 1) * 128],
                                   qkT[:, hd, 32 * c4:32 * (c4 + 1)], hSb[:, hd, c4, :], False, c4 == 3,
                                   {"skip_group_check": True, "tile_position": (0, 32 * c4)})],
                                 ["qkT", sbn, "hSb"], ["po0"])
                        stt(hS[:, hd, :], hS[:, hd, :], st[:, 32 + hd * 4 + c4:33 + hd * 4 + c4],
                            pu[:, c4 * 128:(c4 + 1) * 128], ALU.mult, ALU.add,
                            ["hS%d" % hd, "hS", "hdec", pun], ["hS%d" % hd])
                        copy("act", hSb[:, hd, (c4 + 1) % 4, :], hS[:, hd, :], ["hS%d" % hd],
                             ["hSb%d_%d" % (hd, (c4 + 1) % 4)])
                    s_O2a(c4)
                for hd in range(4):
                    act(junk[:, 0:128], po[0][:, hd * 128:(hd + 1) * 128], AF.Square, ["po0"], ["hssq%d" % hd],
                        accum_out=st[:, 64 + hd:65 + hd])
                rstd_from(st[:, 64:68], st[:, 68:72], st[:, 72:76], 1.0 / 128, "hssq",
                          ["hssq%d" % hd for hd in range(4)])
                for hd in range(4):
                    stt(mixed[:, 1024 + hd * 128:1024 + (hd + 1) * 128], po[0][:, hd * 128:(hd + 1) * 128],
                        st[:, 72 + hd:73 + hd], G[:, 1024 + hd * 128:1024 + (hd + 1) * 128], ALU.mult, ALU.mult,
                        ["po0", "hssq_r", "G"], ["mixed"])

            def s_O2(t):
                xb = xt[t % 2]
                xn = "xt%d" % (t % 2)
                rows = slice(t * 128, (t + 1) * 128)
                for half in range(2):
                    pjn = "pj%d" % half
                    mm_multi([(pj[half][:], mT[:, e_, :], wout[:, e_, half * 512:(half + 1) * 512], False, e_ == 11,
                               {"skip_group_check": True}) for e_ in range(8, 12)],
                             ["mTc"] + WOUT, [pjn])
                    tt("dve", xb[:, half * 512:(half + 1) * 512], xb[:, half * 512:(half + 1) * 512], pj[half][:],
                       ALU.add, [xn, pjn], [xn])
                pjc[0] = 0
                if do_final:
                    act(outt[:], xb[:], AF.Square, [xn], ["outt", "fssq"], accum_out=st[:, 4:5])
                    rstd_from(st[:, 4:5], st[:, 5:6], st[:, 6:7], 1.0 / D, "fssq")
                    stt(outt[:], xb[:], st[:, 6:7], fnw[:], ALU.mult, ALU.mult, [xn, "fssq_r", "fnw"], ["outt"])
                    dma("sp", dst_d[rows, :], outt[:], ["outt"], ["xmid%d" % t], "d_o")
                else:
                    dma("sp", dst_d[rows, :], xb[:], [xn], ["xmid%d" % t], "d_x%d" % (t % 2))

            LOAD(0)
            HEAD(0)
            for t in range(NT):
                nxt = t + 1 < NT
                if nxt:
                    LOAD(t + 1)
                s_GT()
                s_X1(); s_G1a(); s_Hp(); s_X2(); s_G1b(); s_X3(); s_G2(); s_H1(); s_X4(); s_G3a(); s_X5()
                s_G3b(); s_H2()
                if nxt:
                    HEAD_a(t + 1)
                s_G4(); s_X6(); s_H3a(); s_H3b(); s_G5a(); s_G5b()
                mixed_T(12, 16, "mTb")
                s_G5c(); s_H4()
                if nxt:
                    HEAD_b(t + 1)
                s_H5()
                mixed_T(0, 8, "mTa")
                s_H6()
                mixed_T(8, 12, "mTc")
                s_O2(t)

        P.emit(nc)
    return nc


def _consts():
    j = np.arange(128)[:, None]
    i = np.arange(128)[None, :]
    triG = (j <= i).astype(np.float32)
    triUG = (j > i).astype(np.float32)
    same = (j // 32) == (i // 32)
    triH = ((j <= i) & same).astype(np.float32)
    triUH = ((j > i) & same).astype(np.float32)
    cind = np.zeros((128, 8), np.float32)
    cind[:, 0] = 1.0
    for c in range(4):
        cind[:, 1 + c] = (np.arange(128) // 32 == c)
    cst = np.concatenate([triG, triUG, triH, triUH, cind, np.zeros((128, 6 * 128 + 8 - 520), np.float32)], axis=1)
    ident = np.eye(128, dtype=np.float32)
    maskG = triG
    maskH = triH
    msk = np.concatenate([ident, maskG, maskH], axis=1)
    return np.ascontiguousarray(cst), np.ascontiguousarray(msk)


def _layout_params(norm_w, gla_w_gate_up, gla_b_gate, gla_norm_w, hgrn_lower_bounds, hgrn_norm_w,
                   mem_norm_w, xattn_norm_w, final_norm_w):
    NL = norm_w.shape[0]
    pcols = np.zeros((NL, 128, 32), np.float32)
    for L in range(NL):
        pcols[L, :, 0:8] = norm_w[L].reshape(8, 128).T
        gw = np.concatenate([np.tile(gla_norm_w[L], 4), np.tile(hgrn_norm_w[L], 4), np.tile(xattn_norm_w[L], 4)])
        pcols[L, :, 8:24] = gw.reshape(16, 128).T
        pcols[L, :, 24:32] = mem_norm_w[L].reshape(8, 128).T
    wup = np.concatenate([gla_w_gate_up, gla_b_gate[:, None, :]], axis=1).astype(np.float32)
    lbraw = np.ascontiguousarray(np.broadcast_to(hgrn_lower_bounds.reshape(1, -1), (128, NL * 512))).astype(np.float32)
    fnw = np.ascontiguousarray(np.broadcast_to(final_norm_w.reshape(1, -1), (128, D))).astype(np.float32)
    return pcols, np.ascontiguousarray(wup), lbraw, fnw


_NC_CACHE = {}


def _get_nc(S, layers, final_norm):
    key = (S, tuple(layers), final_norm)
    if key not in _NC_CACHE:
        _NC_CACHE[key] = build(S, list(layers), final_norm)
    return _NC_CACHE[key]


def kernel(x, mem, norm_w, w_in, gla_w_gate_up, gla_b_gate, gla_norm_w, hgrn_lower_bounds,
           hgrn_norm_w, mem_norm_w, w_mem_kv, xattn_norm_w, w_out, final_norm_w):
    x = np.asarray(x, np.float32)
    mem = np.asarray(mem, np.float32)
    B, S, _ = x.shape
    f = lambda a: np.ascontiguousarray(np.asarray(a, np.float32))
    pcols, wup, lbraw, fnw = _layout_params(f(norm_w), f(gla_w_gate_up), f(gla_b_gate), f(gla_norm_w),
                                            f(hgrn_lower_bounds), f(hgrn_norm_w), f(mem_norm_w),
                                            f(xattn_norm_w), f(final_norm_w))
    cst, msk = _consts()
    nc = _get_nc(S, (0, 1), True)
    shared = {"w_in": f(w_in), "w_out": f(w_out), "w_kv": f(w_mem_kv), "wup": wup, "pcols": pcols,
              "lbraw": lbraw, "fnw": fnw, "cst": cst, "msk": msk}
    in_maps = []
    for b in range(B):
        m = dict(shared)
        m["x"] = np.ascontiguousarray(x[b])
        m["mem"] = np.ascontiguousarray(mem[b])
        in_maps.append(m)
    res = run_bass_kernel_spmd(nc, in_maps, core_ids=list(range(B)))
    return np.stack([np.asarray(r["out"], np.float32) for r in res.results], axis=0)
```

```python
import contextlib
import numpy as np
import ml_dtypes
import concourse.bass as bass
import concourse.mybir as mybir
from concourse.bass_utils import run_bass_kernel_spmd

F32 = mybir.dt.float32
BF16 = mybir.dt.bfloat16
AF = mybir.ActivationFunctionType
ALU = mybir.AluOpType
AX = mybir.AxisListType

D = 1024
DIN = 6160
DMIX = 2048
MEM = 256
EPS = 1e-6
C_GQ, C_GK, C_GV, C_GLR, C_HQ, C_HF, C_HI, C_XQ, C_GATE = 0, 512, 1024, 2048, 2064, 2576, 3088, 3600, 4112


class Prog:
    ENG = ("pe", "act", "dve", "pool", "sp")

    def __init__(self):
        self.q = {e: [] for e in self.ENG}
        self.cnt = {}
        self.res = {}
        self.waited = {e: {} for e in self.ENG}

    def op(self, eng, fn, reads=(), writes=(), dma=None):
        deps = {}

        def add(tok):
            if tok is None:
                return
            k, v = tok
            if deps.get(k, 0) < v:
                deps[k] = v

        for r in reads:
            st = self.res.get(r)
            if st:
                add(st[0])
        for w in writes:
            st = self.res.get(w)
            if st:
                add(st[0])
                for k, v in st[1].items():
                    add((k, v))
        if eng == "pe":
            deps.pop("pe", None)
        waits = []
        wd = self.waited[eng]
        for k, v in deps.items():
            if wd.get(k, 0) < v:
                wd[k] = v
                waits.append((k, v))
        key, amt = (dma, 16) if dma is not None else (eng, 1)
        self.cnt[key] = self.cnt.get(key, 0) + amt
        tok = (key, self.cnt[key])
        self.q[eng].append((waits, fn, key, amt))
        for r in reads:
            st = self.res.setdefault(r, [None, {}])
            if st[1].get(key, 0) < tok[1]:
                st[1][key] = tok[1]
        for w in writes:
            self.res[w] = [tok, {}]
        return tok

    def emit(self, nc):
        with contextlib.ExitStack() as es:
            sems = {k: es.enter_context(nc.semaphore("s_" + k)) for k in self.cnt}
            block = es.enter_context(nc.Block())
            final = [(k, v) for k, v in self.cnt.items()]

            def run(name, e):
                for waits, fn, key, amt in self.q[name]:
                    for k, v in waits:
                        e.wait_ge(sems[k], v)
                    ins = fn(e)
                    ins.then_inc(sems[key], amt)

            @block.tensor
            def _(e):
                run("pe", e)

            @block.scalar
            def _(e):
                run("act", e)

            @block.vector
            def _(e):
                run("dve", e)

            @block.gpsimd
            def _(e):
                run("pool", e)

            @block.sync
            def _(e):
                run("sp", e)
                for k, v in final:
                    e.wait_ge(sems[k], v)


def build(S, layers, final_norm, n_layers_total=2):
    nc = bass.Bass("TRN2", target_bir_lowering=False)
    P = Prog()
    NT = S // 128
    NL = n_layers_total

    def din(name, shape, dt=F32):
        return nc.dram_tensor(name, list(shape), dt, kind="ExternalInput").ap()

    x_d = din("x", [S, D])
    mem_d = din("mem", [MEM, D])
    win_d = din("w_in", [NL, D, DIN])
    wout_d = din("w_out", [NL, DMIX, D])
    wkv_d = din("w_kv", [NL, D, 2 * 512])
    wup_d = din("wup", [NL, 17, 512])
    pcols_d = din("pcols", [NL, 128, 32])
    lbraw_d = din("lbraw", [128, NL * 512])
    fnw_d = din("fnw", [128, D])
    cst_d = din("cst", [128, 6 * 128 + 8])
    msk_d = din("msk", [128, 3 * 128])
    out_d = nc.dram_tensor("out", [S, D], F32, kind="ExternalOutput").ap()
    xmid_d = None
    if len(layers) > 1:
        xmid_d = nc.dram_tensor("xmid", [S, D], F32, kind="Internal").ap()

    es = contextlib.ExitStack()
    with es:
        def sb(name, shape, dt):
            return es.enter_context(nc.sbuf_tensor("sb_" + name, list(shape), dt))

        def ps(name, shape, dt):
            return es.enter_context(nc.psum_tensor("ps_" + name, list(shape), dt))

        win = sb("win", [128, 8, DIN], BF16)
        wout = sb("wout", [128, 16, D], BF16)
        cst = sb("cst", [128, 4 * 128 + 8], F32)
        mskb = sb("mskb", [128, 3 * 128], BF16)
        pcols = sb("pcols", [128, 32], F32)
        wup = sb("wup", [32, 512], BF16)
        lbt = sb("lbt", [128, NL * 512], F32)
        fnw = sb("fnw", [128, D], F32)
        gS = sb("gS", [128, 4, 256], F32)
        gSb = sb("gSb", [128, 4, 256], BF16)
        hS = sb("hS", [128, 4, 128], F32)
        hSb = sb("hSb", [128, 4, 4, 128], BF16)
        mkT = sb("mkT", [128, 4, 256], BF16)
        mv = sb("mv", [128, 2, 512], BF16)
        xt = [sb("xt0", [128, D], F32), sb("xt1", [128, D], F32)]
        h = sb("h", [128, D], BF16)
        hT = sb("hT", [128, 8, 128], BF16)
        Ft = [sb("F%d" % i, [128, 512], F32) for i in range(6)]
        qb = sb("qb", [128, 512], BF16)
        kb = sb("kb", [128, 512], BF16)
        khb = sb("khb", [128, 512], BF16)
        qkT = sb("qkT", [128, 8, 128], BF16)
        v = sb("v", [128, 1024], BF16)
        hi = sb("hi", [128, 512], BF16)
        him = sb("him", [128, 4, 4, 128], BF16)
        AT = sb("AT", [128, 4, 128], BF16)
        glrT = sb("glrT", [32, 128], BF16)
        G = sb("G", [128, DMIX], BF16)
        mixed = sb("mixed", [128, DMIX], BF16)
        outt = sb("outt", [128, D], F32)
        lbraw = outt
        xqs = qb
        xqT = AT
        pb = him[:].rearrange("p a b c -> p (a b c)")[:, 0:1024].rearrange("p (a b) -> p a b", a=4)
        HIM = ["him%d" % c_ for c_ in range(4)]
        pT = qkT
        mT = G[:].rearrange("p (c m) -> p c m", c=16)
        st = sb("st", [128, 128], F32)
        junk = sb("junk", [128, 256], BF16)

        pj = [ps("pj0", [128, 512], F32), ps("pj1", [128, 512], F32)]
        pt = ps("pt", [128, 1024], BF16)
        pcu = [ps("pcu0", [128, 512], F32), ps("pcu1", [128, 512], F32)]
        pss = ps("pss", [128, 512], F32)
        po = [ps("po0", [128, 512], F32), ps("po1", [128, 512], F32)]

        ident = mskb[:, 0:128]
        maskG = mskb[:, 128:256]
        maskH = mskb[:, 256:384]
        triG = cst[:, 0:128]
        triUG = cst[:, 128:256]
        triH = cst[:, 256:384]
        triUH = cst[:, 384:512]
        cind = cst[:, 512:520]
        nwT = pcols[:, 0:8]
        gwT = pcols[:, 8:24]
        mnwT = pcols[:, 24:32]

        pjc = [0]

        def next_pj():
            i = pjc[0] % 2
            pjc[0] += 1
            return pj[i], "pj%d" % i

        def bc(ap2, n):
            return ap2.unsqueeze(2).broadcast_to([128, ap2.shape[1], n])

        def bc_mid(ap2, n):
            return ap2.unsqueeze(1).broadcast_to([128, n, ap2.shape[1]])

        def mm(out_ap, pairs, reads, writes, first_start=True, **kw):
            pairs = list(pairs)

            def fn(e):
                n = len(pairs)
                ins = None
                for i, (a, b) in enumerate(pairs):
                    ins = e.matmul(out_ap, a, b, start=(first_start and i == 0), stop=(i == n - 1), **kw)
                return ins
            P.op("pe", fn, reads, writes)

        def mm_multi(items, reads, writes):
            items = list(items)

            def fn(e):
                ins = None
                for (o, a, b, s0, s1, kw) in items:
                    ins = e.matmul(o, a, b, start=s0, stop=s1, **kw)
                return ins
            P.op("pe", fn, reads, writes)

        def transposes(items, reads, writes):
            items = list(items)

            def fn(e):
                ins = None
                for (o, i_) in items:
                    ins = e.transpose(o, i_, ident)
                return ins
            P.op("pe", fn, list(reads) + ["mskb"], writes)

        def act(out, in_, func, reads, writes, **kw):
            P.op("act", lambda e: e.activation(out, in_, func, **kw), reads, writes)

        def tt(eng, out, in0, in1, op, reads, writes):
            P.op(eng, lambda e: e.tensor_tensor(out, in0, in1, op), reads, writes)

        def ts(eng, out, in0, s1, s2, op0, op1, reads, writes):
            if s2 is None:
                P.op(eng, lambda e: e.tensor_scalar(out, in0, s1, None, op0), reads, writes)
            else:
                P.op(eng, lambda e: e.tensor_scalar(out, in0, s1, s2, op0, op1), reads, writes)

        def stt(out, in0, scalar, in1, op0, op1, reads, writes):
            P.op("dve", lambda e: e.scalar_tensor_tensor(out, in0, scalar, in1, op0, op1), reads, writes)

        def copy(eng, out, in_, reads, writes):
            if eng == "act":
                P.op("act", lambda e: e.activation(out, in_, AF.Identity), reads, writes)
            else:
                P.op(eng, lambda e: e.tensor_copy(out, in_), reads, writes)

        def dma(eng, out, in_, reads, writes, sem, **kw):
            P.op(eng, lambda e: e.dma_start(out=out, in_=in_, **kw), reads, writes, dma=sem)

        def rstd_from(ssq_col, tmp_col, out_col, inv_n, rname, reads=None):
            act(tmp_col, ssq_col, AF.Ln, reads or [rname], [rname + "_t"], scale=inv_n, bias=EPS)
            act(out_col, tmp_col, AF.Exp, [rname + "_t"], [rname + "_r"], scale=-0.5)

        dma("sp", cst[:], cst_d[:, 0:520], [], ["cst"], "d_cst")
        dma("pool", mskb[:], msk_d, [], ["mskb"], "d_msk")
        dma("sp", lbraw[:], lbraw_d, [], ["outt"], "d_lbraw")
        if final_norm:
            dma("sp", fnw[:], fnw_d, [], ["fnw"], "d_fnw")
        P.op("pool", lambda e: e.memset(glrT[:], 1.0), [], ["glrT"])
        lr3 = lbraw[:].rearrange("p (l n) -> p l n", l=NL)
        lb3 = lbt[:].rearrange("p (l n) -> p l n", l=NL)
        mx = Ft[0]
        P.op("dve", lambda e: e.tensor_copy(mx[:], lr3[:, 0, :]), ["outt"], ["F0"])
        for l in range(1, NL):
            tt("dve", mx[:], mx[:], lr3[:, l, :], ALU.max, ["F0", "outt"], ["F0"])
        for l in range(NL):
            tt("dve", lb3[:, l, :], lr3[:, l, :], mx[:], ALU.subtract, ["F0", "outt"], ["lbt"])
        act(lbt[:], lbt[:], AF.Exp, ["lbt"], ["lbt"])
        den = Ft[1]
        P.op("dve", lambda e: e.tensor_copy(den[:], lb3[:, 0, :]), ["lbt"], ["F1"])
        for l in range(1, NL):
            tt("dve", den[:], den[:], lb3[:, l, :], ALU.add, ["F1", "lbt"], ["F1"])
        P.op("dve", lambda e: e.reciprocal(den[:], den[:]), ["F1"], ["F1"])
        for l in range(NL):
            tt("dve", lb3[:, l, :], lb3[:, l, :], den[:], ALU.mult, ["F1", "lbt"], ["lbt"])
        p0 = Ft[2]
        P.op("dve", lambda e: e.tensor_copy(p0[:], lb3[:, 0, :]), ["lbt"], ["F2"])
        for l in range(1, NL):
            tt("dve", lb3[:, l, :], lb3[:, l, :], lb3[:, l - 1, :], ALU.add, ["lbt"], ["lbt"])
        for l in range(NL):
            tt("dve", lb3[:, l, :], lb3[:, l, :], p0[:], ALU.subtract, ["lbt", "F2"], ["lbt"])

        F = ["F%d" % i for i in range(6)]

        for li, L in enumerate(layers):
            last = (li == len(layers) - 1)
            src_d = x_d if li == 0 else xmid_d
            dst_d = out_d if last else xmid_d
            do_final = last and final_norm
            lbL = lbt[:, L * 512:(L + 1) * 512]

            dma("sp", pcols[:], pcols_d[L], [], ["pcols"], "d_pcols")
            dma("pool", wup[0:17, :], wup_d[L], [], ["wup"], "d_wup")
            wkv = wout[:, 0:8, :]
            for hf_ in range(2):
                dma("pool", wout[:, hf_ * 4:(hf_ + 1) * 4, :],
                    wkv_d[L, hf_ * 512:(hf_ + 1) * 512, :].rearrange("(c p) n -> p c n", p=128),
                    [], ["wq%d" % hf_], "d_wkv%d" % hf_, max_dma_last_dim=4096)
            for c in range(8):
                dma("pool", win[:, c, :], win_d[L, c * 128:(c + 1) * 128, :], [], ["win%d" % c], "d_win%d" % c,
                    max_dma_last_dim=4096)
            P.op("dve", lambda e: e.memset(gS[:], 0.0), [], ["gS"])
            P.op("dve", lambda e: e.memset(hS[:], 0.0), [], ["hS"])
            P.op("pool", lambda e: e.memset(gSb[:], 0.0), [], ["gSb"])
            P.op("pool", lambda e: e.memset(hSb[:], 0.0), [], ["hSb"])
            WIN = ["win%d" % c for c in range(8)]

            mnT = mixed[:].rearrange("p (c m) -> p c m", c=8)
            for blk in range(2):
                xb = xt[blk]
                xn = "xt%d" % blk
                dma("sp", xb[:], mem_d[blk * 128:(blk + 1) * 128, :], [], [xn], "d_x%d" % blk)
                act(h[:], xb[:], AF.Square, [xn], ["h", "ssq"], accum_out=st[:, 0:1])
                rstd_from(st[:, 0:1], st[:, 1:2], st[:, 2:3], 1.0 / D, "ssq")
                ts("dve", h[:], xb[:], st[:, 2:3], None, ALU.mult, None, [xn, "ssq_r"], ["h"])
                transposes([(pt[:, c * 128:(c + 1) * 128], h[:, c * 128:(c + 1) * 128]) for c in range(8)],
                           ["h"], ["pt"])
                tt("dve", mnT[:, :, blk * 128:(blk + 1) * 128], pt[:].rearrange("p (c m) -> p c m", c=8),
                   bc(mnwT, 128), ALU.mult, ["pt", "pcols"], ["mixed"])
            for hd in range(4):
                pjt, pjn = next_pj()
                mm(pjt[:, 0:256], [(wkv[:, c, hd * 128:(hd + 1) * 128], mnT[:, c, :]) for c in range(8)],
                   ["wq0", "wq1", "mixed"], [pjn])
                copy("act", mkT[:, hd, :], pjt[:, 0:256], [pjn], ["mkT"])
            for blk in range(2):
                pjt, pjn = next_pj()
                mm(pjt[:], [(mnT[:, c, blk * 128:(blk + 1) * 128], wkv[:, c, 512:1024]) for c in range(8)],
                   ["wq0", "wq1", "mixed"], [pjn])
                copy("act", mv[:, blk, :], pjt[:], [pjn], ["mv"])
            for q4 in range(4):
                dma("pool", wout[:, q4 * 4:(q4 + 1) * 4, :],
                    wout_d[L, q4 * 512:(q4 + 1) * 512, :].rearrange("(c p) n -> p c n", p=128),
                    [], ["wq%d" % q4], "d_wo%d" % q4, max_dma_last_dim=4096)
            WOUT = ["wq%d" % q4 for q4 in range(4)]

            def proj(col0, ncol, dst_names):
                pjt, pjn = next_pj()
                mm(pjt[:, 0:ncol], [(hT[:, c, :], win[:, c, col0:col0 + ncol]) for c in range(8)],
                   ["hT"] + WIN, [pjn])
                return pjt, pjn

            def LOAD(t):
                dma("sp", xt[t % 2][:], src_d[t * 128:(t + 1) * 128, :], ["xmid%d" % t] if li > 0 else [],
                    ["xt%d" % (t % 2)], "d_x%d" % (t % 2))

            def HEAD_a(t):
                xb = xt[t % 2]
                xn = "xt%d" % (t % 2)
                act(h[:], xb[:], AF.Square, [xn], ["h", "ssq"], accum_out=st[:, 0:1])
                rstd_from(st[:, 0:1], st[:, 1:2], st[:, 2:3], 1.0 / D, "ssq")
                ts("dve", h[:], xb[:], st[:, 2:3], None, ALU.mult, None, [xn, "ssq_r"], ["h"])

            def HEAD_b(t):
                transposes([(pt[:, c * 128:(c + 1) * 128], h[:, c * 128:(c + 1) * 128]) for c in range(8)],
                           ["h"], ["pt"])
                tt("dve", hT[:], pt[:].rearrange("p (c m) -> p c m", c=8), bc(nwT, 128), ALU.mult,
                   ["pt", "pcols"], ["hT"])

            def HEAD(t):
                HEAD_a(t)
                HEAD_b(t)

            def qk_transposes():
                transposes([(pt[:, hd * 128:(hd + 1) * 128], qb[:, hd * 128:(hd + 1) * 128]) for hd in range(4)] +
                           [(pt[:, (4 + hd) * 128:(5 + hd) * 128], kb[:, hd * 128:(hd + 1) * 128]) for hd in range(4)],
                           ["qb", "kb"], ["pt"])
                copy("dve", qkT[:], pt[:].rearrange("p (c m) -> p c m", c=8), ["pt"], ["qkT"])
                mm_multi([(pss[:, hd * 128:(hd + 1) * 128], qkT[:, 4 + hd, :], qkT[:, hd, :], True, True, {})
                          for hd in range(4)], ["qkT"], ["pss"])

            def mixed_T(e0, e1, rname):
                n = e1 - e0
                transposes([(pt[:, i_ * 128:(i_ + 1) * 128], mixed[:, (e0 + i_) * 128:(e0 + i_ + 1) * 128])
                            for i_ in range(n)], ["mixed"], ["pt"])
                tt("dve", mT[:, e0:e1, :], pt[:, 0:n * 128].rearrange("p (c m) -> p c m", c=n),
                   bc(gwT[:, e0:e1], 128), ALU.mult, ["pt", "pcols"], [rname])

            ctx = {}

            def s_GT():
                for g4 in range(4):
                    pjt, pjn = proj(C_GATE + g4 * 512, 512, None)
                    act(G[:, g4 * 512:(g4 + 1) * 512], pjt[:], AF.Silu, [pjn], ["G", "mTa", "mTb", "mTc"])

            def s_X1():
                pjt, pjn = proj(C_XQ, 512, None)
                act(xqs[:], pjt[:], AF.Identity, [pjn], ["qb"], scale=float(128 ** -0.5))

            def s_G1a():
                pjt, pjn = next_pj()
                mm(pjt[0:16, 0:128], [(win[:, c, C_GLR:C_GLR + 16], hT[:, c, :]) for c in range(8)],
                   ["hT"] + WIN, [pjn])
                copy("act", glrT[0:16, :], pjt[0:16, 0:128], [pjn], ["glrT"])

            def s_X2():
                transposes([(pt[:, hd * 128:(hd + 1) * 128], xqs[:, hd * 128:(hd + 1) * 128]) for hd in range(4)],
                           ["qb"], ["pt"])
                copy("dve", xqT[:], pt[:, 0:512].rearrange("p (c m) -> p c m", c=4), ["pt"], ["AT"])

            def s_G1b():
                pjt, pjn = next_pj()
                mm(pjt[:], [(glrT[0:17, :], wup[0:17, :])], ["glrT", "wup"], [pjn])
                act(Ft[0][:], pjt[:], AF.Exp, [pjn], [F[0]], scale=-1.0)

            def s_X3():
                mm_multi([(pcu[hd // 2][:, (hd % 2) * 256:(hd % 2 + 1) * 256], xqT[:, hd, :], mkT[:, hd, :],
                           True, True, {}) for hd in range(4)], ["AT", "mkT"], ["pcu0", "pcu1"])
                for half in range(2):
                    P.op("dve", (lambda half: lambda e: e.tensor_reduce(
                        st[:, 48 + 2 * half:50 + 2 * half], pcu[half][:].rearrange("p (a b) -> p a b", a=2),
                        AX.X, ALU.max))(half), ["pcu%d" % half], ["xmax%d" % half])
                ts("dve", st[:, 52:56], st[:, 48:52], -1.0, None, ALU.mult, None, ["xmax0", "xmax1"], ["xnmax"])
                for hd in range(4):
                    act(pb[:, hd, :], pcu[hd // 2][:, (hd % 2) * 256:(hd % 2 + 1) * 256], AF.Exp,
                        ["pcu%d" % (hd // 2), "xnmax"], HIM + ["xZ%d" % hd], bias=st[:, 52 + hd:53 + hd],
                        accum_out=st[:, 56 + hd:57 + hd])

            def s_Hp():
                pjz, pjzn = proj(C_HF, 512, None)
                act(Ft[5][:], pjz[:], AF.Exp, [pjzn], [F[5]], scale=-1.0)

            def s_G2():
                act(Ft[1][:], Ft[0][:], AF.Ln, [F[0]], [F[1]], bias=1.0)
                mm(pcu[0][:], [(triG, Ft[1][:])], ["cst", F[1]], ["pcu0"])
                mm(pcu[1][:], [(triUG, Ft[1][:])], ["cst", F[1]], ["pcu1"])
                pjt, pjn = next_pj()
                mm_multi([(pjt[:, hd:hd + 1], Ft[1][:, hd * 128:(hd + 1) * 128], cind[:, 0:1], True, True, {})
                          for hd in range(4)], ["cst", F[1]], [pjn])
                act(st[:, 8:12], pjt[:, 0:4], AF.Exp, [pjn], ["gdec"], scale=-1.0 / 16)
                act(Ft[2][:], pcu[0][:], AF.Exp, ["pcu0"], [F[2]], scale=-1.0 / 16)
                act(Ft[3][:], pcu[0][:], AF.Exp, ["pcu0"], [F[3]], scale=1.0 / 16)
                act(Ft[4][:], pcu[1][:], AF.Exp, ["pcu1"], [F[4]], scale=-1.0 / 16)

            def s_H1():
                act(Ft[1][:], Ft[5][:], AF.Ln, [F[5]], [F[1]], bias=1.0)

            def s_X4():
                transposes([(pt[:, (hd * 2 + mc) * 128:(hd * 2 + mc + 1) * 128], pb[:, hd, mc * 128:(mc + 1) * 128])
                            for hd in range(4) for mc in range(2)], HIM, ["pt"])
                copy("dve", pT[:], pt[:].rearrange("p (c m) -> p c m", c=8), ["pt"], ["qkT"])

            def s_G3a():
                pjt, pjn = proj(C_GQ, 512, None)
                stt(qb[:], pjt[:], float(128 ** -0.5), Ft[2][:], ALU.mult, ALU.mult, [pjn, F[2]], ["qb"])
                pjt, pjn = proj(C_GK, 512, None)
                tt("dve", kb[:], pjt[:], Ft[3][:], ALU.mult, [pjn, F[3]], ["kb"])
                tt("dve", khb[:], pjt[:], Ft[4][:], ALU.mult, [pjn, F[4]], ["khb"])

            def s_X5():
                items = []
                for hd in range(4):
                    for mc in range(2):
                        items.append((po[1][:, hd * 128:(hd + 1) * 128], pT[:, hd * 2 + mc, :],
                                      mv[:, mc, hd * 128:(hd + 1) * 128], mc == 0, mc == 1, {}))
                mm_multi(items, ["qkT", "mv"], ["po1"])
                for hd in range(4):
                    act(junk[:, 0:128], po[1][:, hd * 128:(hd + 1) * 128], AF.Square, ["po1"], ["xssq%d" % hd],
                        accum_out=st[:, 80 + hd:81 + hd])

            def s_G3b():
                for half in range(2):
                    pjt, pjn = proj(C_GV + half * 512, 512, None)
                    copy("act", v[:, half * 512:(half + 1) * 512], pjt[:], [pjn], ["v"])

            def s_H2():
                tt("dve", Ft[0][:], Ft[5][:], lbL, ALU.mult, [F[5], "lbt"], [F[0]])
                act(Ft[0][:], Ft[0][:], AF.Ln, [F[0]], [F[0]], bias=1.0)
                tt("dve", Ft[5][:], Ft[0][:], Ft[1][:], ALU.subtract, [F[0], F[1]], [F[5]])

            def s_X6():
                tt("dve", st[:, 60:64], st[:, 56:60], st[:, 56:60], ALU.mult, ["xZ%d" % hd for hd in range(4)], ["xz2"])
                ts("dve", st[:, 84:88], st[:, 80:84], 1.0 / 128, None, ALU.mult, None,
                   ["xssq%d" % hd for hd in range(4)], ["xvv"])
                stt(st[:, 84:88], st[:, 60:64], EPS, st[:, 84:88], ALU.mult, ALU.add, ["xz2", "xvv"], ["xvv"])
                act(st[:, 84:88], st[:, 84:88], AF.Ln, ["xvv"], ["xvv"])
                act(st[:, 88:92], st[:, 84:88], AF.Exp, ["xvv"], ["xr"], scale=-0.5)
                for hd in range(4):
                    stt(mixed[:, 1536 + hd * 128:1536 + (hd + 1) * 128], po[1][:, hd * 128:(hd + 1) * 128],
                        st[:, 88 + hd:89 + hd], G[:, 1536 + hd * 128:1536 + (hd + 1) * 128], ALU.mult, ALU.mult,
                        ["po1", "xr", "G"], ["mixed"])

            def s_G4():
                qk_transposes()
                tt("dve", AT[:], pss[:].rearrange("p (c m) -> p c m", c=4), bc_mid(maskG, 4), ALU.mult,
                   ["pss", "mskb"], ["AT"])

            def s_H3a():
                act(Ft[0][:], Ft[5][:], AF.Exp, [F[5]], [F[0]])
                ts("dve", Ft[0][:], Ft[0][:], -1.0, 1.0, ALU.mult, ALU.add, [F[0]], [F[0]])

            def s_H3b():
                pjr, pjrn = next_pj()
                mm(pss[:], [(triH, Ft[5][:])], ["cst", F[5]], ["pss"])
                mm(pjr[:], [(triUH, Ft[5][:])], ["cst", F[5]], [pjrn])
                pjt, pjn = next_pj()
                mm_multi([(pjt[:, hd * 4:hd * 4 + 4], Ft[5][:, hd * 128:(hd + 1) * 128], cind[:, 1:5], True, True, {})
                          for hd in range(4)], ["cst", F[5]], [pjn])
                act(Ft[1][:], pss[:], AF.Exp, ["pss"], [F[1]])
                act(Ft[2][:], pss[:], AF.Exp, ["pss"], [F[2]], scale=-1.0)
                act(Ft[3][:], pjr[:], AF.Exp, [pjrn], [F[3]])
                act(st[:, 32:48], pjt[:, 0:16], AF.Exp, [pjn], ["hdec"])

            def s_G5a():
                items = []
                for hd in range(4):
                    o_ap = po[hd // 2][:, (hd % 2) * 256:(hd % 2 + 1) * 256]
                    items.append((o_ap, AT[:, hd, :], v[:, hd * 256:(hd + 1) * 256], True, False, {}))
                    items.append((o_ap, qkT[:, hd, :], gSb[:, hd, :], False, True, {}))
                mm_multi(items, ["AT", "v", "qkT", "gSb"], ["po0", "po1"])
                for hd in range(4):
                    o_ap = po[hd // 2][:, (hd % 2) * 256:(hd % 2 + 1) * 256]
                    act(junk[:, 0:256], o_ap, AF.Square, ["po%d" % (hd // 2)], ["gssq%d" % hd],
                        accum_out=st[:, 16 + hd:17 + hd])
                rstd_from(st[:, 16:20], st[:, 20:24], st[:, 24:28], 1.0 / 256, "gssq",
                          ["gssq%d" % hd for hd in range(4)])

            def s_G5b():
                mm_multi([(pcu[hd // 2][:, (hd % 2) * 256:(hd % 2 + 1) * 256], khb[:, hd * 128:(hd + 1) * 128],
                           v[:, hd * 256:(hd + 1) * 256], True, True, {}) for hd in range(4)],
                         ["khb", "v"], ["pcu0", "pcu1"])
                for hd in range(4):
                    stt(gS[:, hd, :], gS[:, hd, :], st[:, 8 + hd:9 + hd],
                        pcu[hd // 2][:, (hd % 2) * 256:(hd % 2 + 1) * 256], ALU.mult, ALU.add,
                        ["gS", "gdec", "pcu%d" % (hd // 2)], ["gS"])
                copy("act", gSb[:].rearrange("p a b -> p (a b)"), gS[:].rearrange("p a b -> p (a b)"),
                     ["gS"], ["gSb"])

            def s_G5c():
                for hd in range(4):
                    o_ap = po[hd // 2][:, (hd % 2) * 256:(hd % 2 + 1) * 256]
                    stt(mixed[:, hd * 256:(hd + 1) * 256], o_ap, st[:, 24 + hd:25 + hd],
                        G[:, hd * 256:(hd + 1) * 256], ALU.mult, ALU.mult,
                        ["po%d" % (hd // 2), "gssq_r", "G"], ["mixed"])

            def s_H4():
                pjt, pjn = proj(C_HQ, 512, None)
                tt("dve", qb[:], pjt[:], Ft[1][:], ALU.mult, [pjn, F[1]], ["qb"])
                tt("dve", kb[:], Ft[0][:], Ft[2][:], ALU.mult, [F[0], F[2]], ["kb"])
                tt("dve", khb[:], Ft[0][:], Ft[3][:], ALU.mult, [F[0], F[3]], ["khb"])
                pjt, pjn = proj(C_HI, 512, None)
                copy("act", hi[:], pjt[:], [pjn], ["hi"])
                for c4 in range(4):
                    act(him[:, :, c4, :], hi[:].rearrange("p (a b) -> p a b", a=4), AF.Identity, ["hi", "cst"],
                        ["him%d" % c4], scale=cind[:, 1 + c4:2 + c4])

            ubank = [(pcu[0], "pcu0"), (pcu[1], "pcu1"), (pss, "pss"), (po[1], "po1")]

            def s_H5():
                qk_transposes()
                tt("dve", AT[:], pss[:].rearrange("p (c m) -> p c m", c=4), bc_mid(maskH, 4), ALU.mult,
                   ["pss", "mskb"], ["AT"])
                mm_multi([(po[0][:, hd * 128:(hd + 1) * 128], AT[:, hd, :], hi[:, hd * 128:(hd + 1) * 128],
                           hd == 0, False, {"skip_group_check": True}) for hd in range(4)],
                         ["AT", "hi"], ["po0"])
                for hd in range(4):
                    pu, pun = ubank[hd]
                    mm(pu[:], [(khb[:, hd * 128:(hd + 1) * 128], him[:, hd, :, :].rearrange("p a b -> p (a b)"))],
                       ["khb"] + HIM, [pun])

            def s_O2a(part):
                elist = [0, 1, 2, 3, 4, 5, 6, 7, 12, 13, 14, 15]
                es_ = elist[part * 3:(part + 1) * 3]
                items = []
                for half in range(2):
                    for e_ in es_:
                        items.append((pj[half][:], mT[:, e_, :], wout[:, e_, half * 512:(half + 1) * 512],
                                      e_ == 0, False, {"skip_group_check": True}))
                mm_multi(items, ["mTa", "mTb"] + WOUT, ["pj0", "pj1"])

            def s_H6():
                for c4 in range(4):
                    for hd in range(4):
                        pu, pun = ubank[hd]
                        sbn = "hSb%d_%d" % (hd, c4)
                        mm_multi([(po[0][32 * c4:32 * (c4 + 1), hd * 128:(hd + 1) * 128],
                                   qkT[:, hd, 32 * c4:32 * (c4 + 1)], hSb[:, hd, c4, :], False, c4 == 3,
                                   {"skip_group_check": True, "tile_position": (0, 32 * c4)})],
                                 ["qkT", sbn, "hSb"], ["po0"])
                        stt(hS[:, hd, :], hS[:, hd, :], st[:, 32 + hd * 4 + c4:33 + hd * 4 + c4],
                            pu[:, c4 * 128:(c4 + 1) * 128], ALU.mult, ALU.add,
                            ["hS%d" % hd, "hS", "hdec", pun], ["hS%d" % hd])
                        copy("act", hSb[:, hd, (c4 + 1) % 4, :], hS[:, hd, :], ["hS%d" % hd],
                             ["hSb%d_%d" % (hd, (c4 + 1) % 4)])
                    s_O2a(c4)
                for hd in range(4):
                    act(junk[:, 0:128], po[0][:, hd * 128:(hd + 1) * 128], AF.Square, ["po0"], ["hssq%d" % hd],
                        accum_out=st[:, 64 + hd:65 + hd])
                rstd_from(st[:, 64:68], st[:, 68:72], st[:, 72:76], 1.0 / 128, "hssq",
                          ["hssq%d" % hd for hd in range(4)])
                for hd in range(4):
                    stt(mixed[:, 1024 + hd * 128:1024 + (hd + 1) * 128], po[0][:, hd * 128:(hd + 1) * 128],
                        st[:, 72 + hd:73 + hd], G[:, 1024 + hd * 128:1024 + (hd + 1) * 128], ALU.mult, ALU.mult,
                        ["po0", "hssq_r", "G"], ["mixed"])

            def s_O2(t):
                xb = xt[t % 2]
                xn = "xt%d" % (t % 2)
                rows = slice(t * 128, (t + 1) * 128)
                for half in range(2):
                    pjn = "pj%d" % half
                    mm_multi([(pj[half][:], mT[:, e_, :], wout[:, e_, half * 512:(half + 1) * 512], False, e_ == 11,
                               {"skip_group_check": True}) for e_ in range(8, 12)],
                             ["mTc"] + WOUT, [pjn])
                    tt("dve", xb[:, half * 512:(half + 1) * 512], xb[:, half * 512:(half + 1) * 512], pj[half][:],
                       ALU.add, [xn, pjn], [xn])
                pjc[0] = 0
                if do_final:
                    act(outt[:], xb[:], AF.Square, [xn], ["outt", "fssq"], accum_out=st[:, 4:5])
                    rstd_from(st[:, 4:5], st[:, 5:6], st[:, 6:7], 1.0 / D, "fssq")
                    stt(outt[:], xb[:], st[:, 6:7], fnw[:], ALU.mult, ALU.mult, [xn, "fssq_r", "fnw"], ["outt"])
                    dma("sp", dst_d[rows, :], outt[:], ["outt"], ["xmid%d" % t], "d_o")
                else:
                    dma("sp", dst_d[rows, :], xb[:], [xn], ["xmid%d" % t], "d_x%d" % (t % 2))

            LOAD(0)
            HEAD(0)
            for t in range(NT):
                nxt = t + 1 < NT
                if nxt:
                    LOAD(t + 1)
                s_X1(); s_G1a(); s_G3b(); s_Hp(); s_X2(); s_G1b(); s_X3(); s_G2(); s_H1(); s_X4(); s_G3a(); s_X5()
                s_H2()
                if nxt:
                    HEAD_a(t + 1)
                s_GT(); s_G4(); s_X6(); s_H3a(); s_H3b(); s_G5a(); s_G5b()
                mixed_T(12, 16, "mTb")
                s_G5c(); s_H4()
                if nxt:
                    HEAD_b(t + 1)
                s_H5()
                mixed_T(0, 8, "mTa")
                s_H6()
                mixed_T(8, 12, "mTc")
                s_O2(t)

        P.emit(nc)
    return nc


def _consts():
    j = np.arange(128)[:, None]
    i = np.arange(128)[None, :]
    triG = (j <= i).astype(np.float32)
    triUG = (j > i).astype(np.float32)
    same = (j // 32) == (i // 32)
    triH = ((j <= i) & same).astype(np.float32)
    triUH = ((j > i) & same).astype(np.float32)
    cind = np.zeros((128, 8), np.float32)
    cind[:, 0] = 1.0
    for c in range(4):
        cind[:, 1 + c] = (np.arange(128) // 32 == c)
    cst = np.concatenate([triG, triUG, triH, triUH, cind, np.zeros((128, 6 * 128 + 8 - 520), np.float32)], axis=1)
    ident = np.eye(128, dtype=np.float32)
    maskG = triG
    maskH = triH
    msk = np.concatenate([ident, maskG, maskH], axis=1)
    return np.ascontiguousarray(cst), np.ascontiguousarray(msk)


def _layout_params(norm_w, gla_w_gate_up, gla_b_gate, gla_norm_w, hgrn_lower_bounds, hgrn_norm_w,
                   mem_norm_w, xattn_norm_w, final_norm_w):
    NL = norm_w.shape[0]
    pcols = np.zeros((NL, 128, 32), np.float32)
    for L in range(NL):
        pcols[L, :, 0:8] = norm_w[L].reshape(8, 128).T
        gw = np.concatenate([np.tile(gla_norm_w[L], 4), np.tile(hgrn_norm_w[L], 4), np.tile(xattn_norm_w[L], 4)])
        pcols[L, :, 8:24] = gw.reshape(16, 128).T
        pcols[L, :, 24:32] = mem_norm_w[L].reshape(8, 128).T
    wup = np.concatenate([gla_w_gate_up, gla_b_gate[:, None, :]], axis=1).astype(np.float32)
    lbraw = np.ascontiguousarray(np.broadcast_to(hgrn_lower_bounds.reshape(1, -1), (128, NL * 512))).astype(np.float32)
    fnw = np.ascontiguousarray(np.broadcast_to(final_norm_w.reshape(1, -1), (128, D))).astype(np.float32)
    return pcols, np.ascontiguousarray(wup), lbraw, fnw


_NC_CACHE = {}


def _get_nc(S, layers, final_norm):
    key = (S, tuple(layers), final_norm)
    if key not in _NC_CACHE:
        _NC_CACHE[key] = build(S, list(layers), final_norm)
    return _NC_CACHE[key]


def kernel(x, mem, norm_w, w_in, gla_w_gate_up, gla_b_gate, gla_norm_w, hgrn_lower_bounds,
           hgrn_norm_w, mem_norm_w, w_mem_kv, xattn_norm_w, w_out, final_norm_w):
    x = np.asarray(x, np.float32)
    mem = np.asarray(mem, np.float32)
    B, S, _ = x.shape
    f = lambda a: np.ascontiguousarray(np.asarray(a, np.float32))
    pcols, wup, lbraw, fnw = _layout_params(f(norm_w), f(gla_w_gate_up), f(gla_b_gate), f(gla_norm_w),
                                            f(hgrn_lower_bounds), f(hgrn_norm_w), f(mem_norm_w),
                                            f(xattn_norm_w), f(final_norm_w))
    cst, msk = _consts()
    nc = _get_nc(S, (0, 1), True)
    shared = {"w_in": f(w_in), "w_out": f(w_out), "w_kv": f(w_mem_kv), "wup": wup, "pcols": pcols,
              "lbraw": lbraw, "fnw": fnw, "cst": cst, "msk": msk}
    in_maps = []
    for b in range(B):
        m = dict(shared)
        m["x"] = np.ascontiguousarray(x[b])
        m["mem"] = np.ascontiguousarray(mem[b])
        in_maps.append(m)
    res = run_bass_kernel_spmd(nc, in_maps, core_ids=list(range(B)))
    return np.stack([np.asarray(r["out"], np.float32) for r in res.results], axis=0)
```

```python
import contextlib
import numpy as np
import ml_dtypes
import concourse.bass as bass
import concourse.mybir as mybir
from concourse.bass_utils import run_bass_kernel_spmd

F32 = mybir.dt.float32
BF16 = mybir.dt.bfloat16
AF = mybir.ActivationFunctionType
ALU = mybir.AluOpType
AX = mybir.AxisListType

D = 1024
DIN = 6160
DMIX = 2048
MEM = 256
EPS = 1e-6
C_GQ, C_GK, C_GV, C_GLR, C_HQ, C_HF, C_HI, C_XQ, C_GATE = 0, 512, 1024, 2048, 2064, 2576, 3088, 3600, 4112


class Prog:
    ENG = ("pe", "act", "dve", "pool", "sp")

    def __init__(self):
        self.q = {e: [] for e in self.ENG}
        self.cnt = {}
        self.res = {}
        self.waited = {e: {} for e in self.ENG}

    def op(self, eng, fn, reads=(), writes=(), dma=None):
        deps = {}

        def add(tok):
            if tok is None:
                return
            k, v = tok
            if deps.get(k, 0) < v:
                deps[k] = v

        for r in reads:
            st = self.res.get(r)
            if st:
                add(st[0])
        for w in writes:
            st = self.res.get(w)
            if st:
                add(st[0])
                for k, v in st[1].items():
                    add((k, v))
        if eng == "pe":
            deps.pop("pe", None)
        waits = []
        wd = self.waited[eng]
        for k, v in deps.items():
            if wd.get(k, 0) < v:
                wd[k] = v
                waits.append((k, v))
        key, amt = (dma, 16) if dma is not None else (eng, 1)
        self.cnt[key] = self.cnt.get(key, 0) + amt
        tok = (key, self.cnt[key])
        self.q[eng].append((waits, fn, key, amt))
        for r in reads:
            st = self.res.setdefault(r, [None, {}])
            if st[1].get(key, 0) < tok[1]:
                st[1][key] = tok[1]
        for w in writes:
            self.res[w] = [tok, {}]
        return tok

    def emit(self, nc):
        with contextlib.ExitStack() as es:
            sems = {k: es.enter_context(nc.semaphore("s_" + k)) for k in self.cnt}
            block = es.enter_context(nc.Block())
            final = [(k, v) for k, v in self.cnt.items()]

            def run(name, e):
                for waits, fn, key, amt in self.q[name]:
                    for k, v in waits:
                        e.wait_ge(sems[k], v)
                    ins = fn(e)
                    ins.then_inc(sems[key], amt)

            @block.tensor
            def _(e):
                run("pe", e)

            @block.scalar
            def _(e):
                run("act", e)

            @block.vector
            def _(e):
                run("dve", e)

            @block.gpsimd
            def _(e):
                run("pool", e)

            @block.sync
            def _(e):
                run("sp", e)
                for k, v in final:
                    e.wait_ge(sems[k], v)


def build(S, layers, final_norm, n_layers_total=2):
    nc = bass.Bass("TRN2", target_bir_lowering=False)
    P = Prog()
    NT = S // 128
    NL = n_layers_total

    def din(name, shape, dt=F32):
        return nc.dram_tensor(name, list(shape), dt, kind="ExternalInput").ap()

    x_d = din("x", [S, D])
    mem_d = din("mem", [MEM, D])
    win_d = din("w_in", [NL, D, DIN])
    wout_d = din("w_out", [NL, DMIX, D])
    wkv_d = din("w_kv", [NL, D, 2 * 512])
    wup_d = din("wup", [NL, 17, 512])
    pcols_d = din("pcols", [NL, 128, 32])
    lbraw_d = din("lbraw", [128, NL * 512])
    fnw_d = din("fnw", [128, D])
    cst_d = din("cst", [128, 6 * 128 + 8])
    msk_d = din("msk", [128, 3 * 128])
    out_d = nc.dram_tensor("out", [S, D], F32, kind="ExternalOutput").ap()
    xmid_d = None
    if len(layers) > 1:
        xmid_d = nc.dram_tensor("xmid", [S, D], F32, kind="Internal").ap()

    es = contextlib.ExitStack()
    with es:
        def sb(name, shape, dt):
            return es.enter_context(nc.sbuf_tensor("sb_" + name, list(shape), dt))

        def ps(name, shape, dt):
            return es.enter_context(nc.psum_tensor("ps_" + name, list(shape), dt))

        win = sb("win", [128, 8, DIN], BF16)
        wout = sb("wout", [128, 16, D], BF16)
        cst = sb("cst", [128, 4 * 128 + 8], F32)
        mskb = sb("mskb", [128, 3 * 128], BF16)
        pcols = sb("pcols", [128, 32], F32)
        wup = sb("wup", [32, 512], BF16)
        lbt = sb("lbt", [128, NL * 512], F32)
        fnw = sb("fnw", [128, D], F32)
        gS = sb("gS", [128, 4, 256], F32)
        gSb = sb("gSb", [128, 4, 256], BF16)
        hS = sb("hS", [128, 4, 128], F32)
        hSb = sb("hSb", [128, 4, 4, 128], BF16)
        mkT = sb("mkT", [128, 4, 256], BF16)
        mv = sb("mv", [128, 2, 512], BF16)
        xt = [sb("xt0", [128, D], F32), sb("xt1", [128, D], F32)]
        h = sb("h", [128, D], BF16)
        hT = sb("hT", [128, 8, 128], BF16)
        Ft = [sb("F%d" % i, [128, 512], F32) for i in range(6)]
        qb = sb("qb", [128, 512], BF16)
        kb = sb("kb", [128, 512], BF16)
        khb = sb("khb", [128, 512], BF16)
        qkT = sb("qkT", [128, 8, 128], BF16)
        v = sb("v", [128, 1024], BF16)
        hi = sb("hi", [128, 512], BF16)
        him = sb("him", [128, 4, 4, 128], BF16)
        AT = sb("AT", [128, 4, 128], BF16)
        glrT = sb("glrT", [32, 128], BF16)
        G = sb("G", [128, DMIX], BF16)
        mixed = sb("mixed", [128, DMIX], BF16)
        outt = sb("outt", [128, D], F32)
        lbraw = outt
        xqs = qb
        xqT = AT
        pb = him[:].rearrange("p a b c -> p (a b c)")[:, 0:1024].rearrange("p (a b) -> p a b", a=4)
        HIM = ["him%d" % c_ for c_ in range(4)]
        pT = qkT
        mT = G[:].rearrange("p (c m) -> p c m", c=16)
        st = sb("st", [128, 128], F32)
        junk = sb("junk", [128, 256], BF16)

        pj = [ps("pj0", [128, 512], F32), ps("pj1", [128, 512], F32)]
        pt = ps("pt", [128, 1024], BF16)
        pcu = [ps("pcu0", [128, 512], F32), ps("pcu1", [128, 512], F32)]
        pss = ps("pss", [128, 512], F32)
        po = [ps("po0", [128, 512], F32), ps("po1", [128, 512], F32)]

        ident = mskb[:, 0:128]
        maskG = mskb[:, 128:256]
        maskH = mskb[:, 256:384]
        triG = cst[:, 0:128]
        triUG = cst[:, 128:256]
        triH = cst[:, 256:384]
        triUH = cst[:, 384:512]
        cind = cst[:, 512:520]
        nwT = pcols[:, 0:8]
        gwT = pcols[:, 8:24]
        mnwT = pcols[:, 24:32]

        pjc = [0]

        def next_pj():
            i = pjc[0] % 2
            pjc[0] += 1
            return pj[i], "pj%d" % i

        def bc(ap2, n):
            return ap2.unsqueeze(2).broadcast_to([128, ap2.shape[1], n])

        def bc_mid(ap2, n):
            return ap2.unsqueeze(1).broadcast_to([128, n, ap2.shape[1]])

        def mm(out_ap, pairs, reads, writes, first_start=True, **kw):
            pairs = list(pairs)

            def fn(e):
                n = len(pairs)
                ins = None
                for i, (a, b) in enumerate(pairs):
                    ins = e.matmul(out_ap, a, b, start=(first_start and i == 0), stop=(i == n - 1), **kw)
                return ins
            P.op("pe", fn, reads, writes)

        def mm_multi(items, reads, writes):
            items = list(items)

            def fn(e):
                ins = None
                for (o, a, b, s0, s1, kw) in items:
                    ins = e.matmul(o, a, b, start=s0, stop=s1, **kw)
                return ins
            P.op("pe", fn, reads, writes)

        def transposes(items, reads, writes):
            items = list(items)

            def fn(e):
                ins = None
                for (o, i_) in items:
                    ins = e.transpose(o, i_, ident)
                return ins
            P.op("pe", fn, list(reads) + ["mskb"], writes)

        def act(out, in_, func, reads, writes, **kw):
            P.op("act", lambda e: e.activation(out, in_, func, **kw), reads, writes)

        def tt(eng, out, in0, in1, op, reads, writes):
            P.op(eng, lambda e: e.tensor_tensor(out, in0, in1, op), reads, writes)

        def ts(eng, out, in0, s1, s2, op0, op1, reads, writes):
            if s2 is None:
                P.op(eng, lambda e: e.tensor_scalar(out, in0, s1, None, op0), reads, writes)
            else:
                P.op(eng, lambda e: e.tensor_scalar(out, in0, s1, s2, op0, op1), reads, writes)

        def stt(out, in0, scalar, in1, op0, op1, reads, writes):
            P.op("dve", lambda e: e.scalar_tensor_tensor(out, in0, scalar, in1, op0, op1), reads, writes)

        def copy(eng, out, in_, reads, writes):
            if eng == "act":
                P.op("act", lambda e: e.activation(out, in_, AF.Identity), reads, writes)
            else:
                P.op(eng, lambda e: e.tensor_copy(out, in_), reads, writes)

        def dma(eng, out, in_, reads, writes, sem, **kw):
            P.op(eng, lambda e: e.dma_start(out=out, in_=in_, **kw), reads, writes, dma=sem)

        def rstd_from(ssq_col, tmp_col, out_col, inv_n, rname, reads=None):
            act(tmp_col, ssq_col, AF.Ln, reads or [rname], [rname + "_t"], scale=inv_n, bias=EPS)
            act(out_col, tmp_col, AF.Exp, [rname + "_t"], [rname + "_r"], scale=-0.5)

        dma("sp", cst[:], cst_d[:, 0:520], [], ["cst"], "d_cst")
        dma("pool", mskb[:], msk_d, [], ["mskb"], "d_msk")
        dma("sp", lbraw[:], lbraw_d, [], ["outt"], "d_lbraw")
        if final_norm:
            dma("sp", fnw[:], fnw_d, [], ["fnw"], "d_fnw")
        P.op("pool", lambda e: e.memset(glrT[:], 1.0), [], ["glrT"])
        lr3 = lbraw[:].rearrange("p (l n) -> p l n", l=NL)
        lb3 = lbt[:].rearrange("p (l n) -> p l n", l=NL)
        mx = Ft[0]
        P.op("dve", lambda e: e.tensor_copy(mx[:], lr3[:, 0, :]), ["outt"], ["F0"])
        for l in range(1, NL):
            tt("dve", mx[:], mx[:], lr3[:, l, :], ALU.max, ["F0", "outt"], ["F0"])
        for l in range(NL):
            tt("dve", lb3[:, l, :], lr3[:, l, :], mx[:], ALU.subtract, ["F0", "outt"], ["lbt"])
        act(lbt[:], lbt[:], AF.Exp, ["lbt"], ["lbt"])
        den = Ft[1]
        P.op("dve", lambda e: e.tensor_copy(den[:], lb3[:, 0, :]), ["lbt"], ["F1"])
        for l in range(1, NL):
            tt("dve", den[:], den[:], lb3[:, l, :], ALU.add, ["F1", "lbt"], ["F1"])
        P.op("dve", lambda e: e.reciprocal(den[:], den[:]), ["F1"], ["F1"])
        for l in range(NL):
            tt("dve", lb3[:, l, :], lb3[:, l, :], den[:], ALU.mult, ["F1", "lbt"], ["lbt"])
        p0 = Ft[2]
        P.op("dve", lambda e: e.tensor_copy(p0[:], lb3[:, 0, :]), ["lbt"], ["F2"])
        for l in range(1, NL):
            tt("dve", lb3[:, l, :], lb3[:, l, :], lb3[:, l - 1, :], ALU.add, ["lbt"], ["lbt"])
        for l in range(NL):
            tt("dve", lb3[:, l, :], lb3[:, l, :], p0[:], ALU.subtract, ["lbt", "F2"], ["lbt"])

        F = ["F%d" % i for i in range(6)]

        for li, L in enumerate(layers):
            last = (li == len(layers) - 1)
            src_d = x_d if li == 0 else xmid_d
            dst_d = out_d if last else xmid_d
            do_final = last and final_norm
            lbL = lbt[:, L * 512:(L + 1) * 512]

            dma("sp", pcols[:], pcols_d[L], [], ["pcols"], "d_pcols")
            dma("pool", wup[0:17, :], wup_d[L], [], ["wup"], "d_wup")
            wkv = wout[:, 0:8, :]
            for hf_ in range(2):
                dma("pool", wout[:, hf_ * 4:(hf_ + 1) * 4, :],
                    wkv_d[L, hf_ * 512:(hf_ + 1) * 512, :].rearrange("(c p) n -> p c n", p=128),
                    [], ["wq%d" % hf_], "d_wkv%d" % hf_, max_dma_last_dim=4096)
            for c in range(8):
                dma("pool", win[:, c, :], win_d[L, c * 128:(c + 1) * 128, :], [], ["win%d" % c], "d_win%d" % c,
                    max_dma_last_dim=4096)
            P.op("dve", lambda e: e.memset(gS[:], 0.0), [], ["gS"])
            P.op("dve", lambda e: e.memset(hS[:], 0.0), [], ["hS"])
            P.op("pool", lambda e: e.memset(gSb[:], 0.0), [], ["gSb"])
            P.op("pool", lambda e: e.memset(hSb[:], 0.0), [], ["hSb"])
            WIN = ["win%d" % c for c in range(8)]

            mnT = mixed[:].rearrange("p (c m) -> p c m", c=8)
            for blk in range(2):
                xb = xt[blk]
                xn = "xt%d" % blk
                dma("sp", xb[:], mem_d[blk * 128:(blk + 1) * 128, :], [], [xn], "d_x%d" % blk)
                act(h[:], xb[:], AF.Square, [xn], ["h", "ssq"], accum_out=st[:, 0:1])
                rstd_from(st[:, 0:1], st[:, 1:2], st[:, 2:3], 1.0 / D, "ssq")
                ts("dve", h[:], xb[:], st[:, 2:3], None, ALU.mult, None, [xn, "ssq_r"], ["h"])
                transposes([(pt[:, c * 128:(c + 1) * 128], h[:, c * 128:(c + 1) * 128]) for c in range(8)],
                           ["h"], ["pt"])
                tt("dve", mnT[:, :, blk * 128:(blk + 1) * 128], pt[:].rearrange("p (c m) -> p c m", c=8),
                   bc(mnwT, 128), ALU.mult, ["pt", "pcols"], ["mixed"])
            for hd in range(4):
                pjt, pjn = next_pj()
                mm(pjt[:, 0:256], [(wkv[:, c, hd * 128:(hd + 1) * 128], mnT[:, c, :]) for c in range(8)],
                   ["wq0", "wq1", "mixed"], [pjn])
                copy("act", mkT[:, hd, :], pjt[:, 0:256], [pjn], ["mkT"])
            for blk in range(2):
                pjt, pjn = next_pj()
                mm(pjt[:], [(mnT[:, c, blk * 128:(blk + 1) * 128], wkv[:, c, 512:1024]) for c in range(8)],
                   ["wq0", "wq1", "mixed"], [pjn])
                copy("act", mv[:, blk, :], pjt[:], [pjn], ["mv"])
            for q4 in range(4):
                dma("pool", wout[:, q4 * 4:(q4 + 1) * 4, :],
                    wout_d[L, q4 * 512:(q4 + 1) * 512, :].rearrange("(c p) n -> p c n", p=128),
                    [], ["wq%d" % q4], "d_wo%d" % q4, max_dma_last_dim=4096)
            WOUT = ["wq%d" % q4 for q4 in range(4)]

            def proj(col0, ncol, dst_names):
                pjt, pjn = next_pj()
                mm(pjt[:, 0:ncol], [(hT[:, c, :], win[:, c, col0:col0 + ncol]) for c in range(8)],
                   ["hT"] + WIN, [pjn])
                return pjt, pjn

            def LOAD(t):
                dma("sp", xt[t % 2][:], src_d[t * 128:(t + 1) * 128, :], ["xmid%d" % t] if li > 0 else [],
                    ["xt%d" % (t % 2)], "d_x%d" % (t % 2))

            def HEAD_a(t):
                xb = xt[t % 2]
                xn = "xt%d" % (t % 2)
                act(h[:], xb[:], AF.Square, [xn], ["h", "ssq"], accum_out=st[:, 0:1])
                rstd_from(st[:, 0:1], st[:, 1:2], st[:, 2:3], 1.0 / D, "ssq")
                ts("dve", h[:], xb[:], st[:, 2:3], None, ALU.mult, None, [xn, "ssq_r"], ["h"])

            def HEAD_b(t):
                transposes([(pt[:, c * 128:(c + 1) * 128], h[:, c * 128:(c + 1) * 128]) for c in range(8)],
                           ["h"], ["pt"])
                tt("dve", hT[:], pt[:].rearrange("p (c m) -> p c m", c=8), bc(nwT, 128), ALU.mult,
                   ["pt", "pcols"], ["hT"])

            def HEAD(t):
                HEAD_a(t)
                HEAD_b(t)

            def qk_transposes():
                transposes([(pt[:, hd * 128:(hd + 1) * 128], qb[:, hd * 128:(hd + 1) * 128]) for hd in range(4)] +
                           [(pt[:, (4 + hd) * 128:(5 + hd) * 128], kb[:, hd * 128:(hd + 1) * 128]) for hd in range(4)],
                           ["qb", "kb"], ["pt"])
                copy("dve", qkT[:], pt[:].rearrange("p (c m) -> p c m", c=8), ["pt"], ["qkT"])
                mm_multi([(pss[:, hd * 128:(hd + 1) * 128], qkT[:, 4 + hd, :], qkT[:, hd, :], True, True, {})
                          for hd in range(4)], ["qkT"], ["pss"])

            def mixed_T(e0, e1, rname):
                n = e1 - e0
                transposes([(pt[:, i_ * 128:(i_ + 1) * 128], mixed[:, (e0 + i_) * 128:(e0 + i_ + 1) * 128])
                            for i_ in range(n)], ["mixed"], ["pt"])
                tt("dve", mT[:, e0:e1, :], pt[:, 0:n * 128].rearrange("p (c m) -> p c m", c=n),
                   bc(gwT[:, e0:e1], 128), ALU.mult, ["pt", "pcols"], [rname])

            ctx = {}

            def s_GT():
                for g4 in range(4):
                    pjt, pjn = proj(C_GATE + g4 * 512, 512, None)
                    act(G[:, g4 * 512:(g4 + 1) * 512], pjt[:], AF.Silu, [pjn], ["G", "mTa", "mTb", "mTc"])

            def s_X1():
                pjt, pjn = proj(C_XQ, 512, None)
                act(xqs[:], pjt[:], AF.Identity, [pjn], ["qb"], scale=float(128 ** -0.5))

            def s_G1a():
                pjt, pjn = next_pj()
                mm(pjt[0:16, 0:128], [(win[:, c, C_GLR:C_GLR + 16], hT[:, c, :]) for c in range(8)],
                   ["hT"] + WIN, [pjn])
                copy("act", glrT[0:16, :], pjt[0:16, 0:128], [pjn], ["glrT"])

            def s_X2():
                transposes([(pt[:, hd * 128:(hd + 1) * 128], xqs[:, hd * 128:(hd + 1) * 128]) for hd in range(4)],
                           ["qb"], ["pt"])
                copy("dve", xqT[:], pt[:, 0:512].rearrange("p (c m) -> p c m", c=4), ["pt"], ["AT"])

            def s_G1b():
                pjt, pjn = next_pj()
                mm(pjt[:], [(glrT[0:17, :], wup[0:17, :])], ["glrT", "wup"], [pjn])
                act(Ft[0][:], pjt[:], AF.Exp, [pjn], [F[0]], scale=-1.0)

            def s_X3():
                mm_multi([(po[hd // 2][:, (hd % 2) * 256:(hd % 2 + 1) * 256], xqT[:, hd, :], mkT[:, hd, :],
                           True, True, {}) for hd in range(4)], ["AT", "mkT"], ["po0", "po1"])
                for half in range(2):
                    P.op("dve", (lambda half: lambda e: e.tensor_reduce(
                        st[:, 48 + 2 * half:50 + 2 * half], po[half][:].rearrange("p (a b) -> p a b", a=2),
                        AX.X, ALU.max))(half), ["po%d" % half], ["xmax%d" % half])
                ts("dve", st[:, 52:56], st[:, 48:52], -1.0, None, ALU.mult, None, ["xmax0", "xmax1"], ["xnmax"])
                for hd in range(4):
                    act(pb[:, hd, :], po[hd // 2][:, (hd % 2) * 256:(hd % 2 + 1) * 256], AF.Exp,
                        ["po%d" % (hd // 2), "xnmax"], HIM + ["xZ%d" % hd], bias=st[:, 52 + hd:53 + hd],
                        accum_out=st[:, 56 + hd:57 + hd])

            def s_Hp():
                pjz, pjzn = proj(C_HF, 512, None)
                act(Ft[5][:], pjz[:], AF.Exp, [pjzn], [F[5]], scale=-1.0)

            def s_G2a():
                act(Ft[1][:], Ft[0][:], AF.Ln, [F[0]], [F[1]], bias=1.0)
                mm(pcu[0][:], [(triG, Ft[1][:])], ["cst", F[1]], ["pcu0"])
                mm(pcu[1][:], [(triUG, Ft[1][:])], ["cst", F[1]], ["pcu1"])
                pjt, pjn = next_pj()
                ctx["gdecp"] = (pjt, pjn)
                mm_multi([(pjt[:, hd:hd + 1], Ft[1][:, hd * 128:(hd + 1) * 128], cind[:, 0:1], True, True, {})
                          for hd in range(4)], ["cst", F[1]], [pjn])

            def s_G2b():
                pjt, pjn = ctx["gdecp"]
                act(st[:, 8:12], pjt[:, 0:4], AF.Exp, [pjn], ["gdec"], scale=-1.0 / 16)
                act(Ft[2][:], pcu[0][:], AF.Exp, ["pcu0"], [F[2]], scale=-1.0 / 16)
                act(Ft[3][:], pcu[0][:], AF.Exp, ["pcu0"], [F[3]], scale=1.0 / 16)
                act(Ft[4][:], pcu[1][:], AF.Exp, ["pcu1"], [F[4]], scale=-1.0 / 16)

            def s_H1():
                act(Ft[1][:], Ft[5][:], AF.Ln, [F[5]], [F[1]], bias=1.0)

            def s_X4():
                transposes([(pt[:, (hd * 2 + mc) * 128:(hd * 2 + mc + 1) * 128], pb[:, hd, mc * 128:(mc + 1) * 128])
                            for hd in range(4) for mc in range(2)], HIM, ["pt"])
                copy("dve", pT[:], pt[:].rearrange("p (c m) -> p c m", c=8), ["pt"], ["qkT"])

            def s_G3a():
                pjt, pjn = proj(C_GQ, 512, None)
                stt(qb[:], pjt[:], float(128 ** -0.5), Ft[2][:], ALU.mult, ALU.mult, [pjn, F[2]], ["qb"])
                pjt, pjn = proj(C_GK, 512, None)
                tt("dve", kb[:], pjt[:], Ft[3][:], ALU.mult, [pjn, F[3]], ["kb"])
                tt("dve", khb[:], pjt[:], Ft[4][:], ALU.mult, [pjn, F[4]], ["khb"])

            def s_X5():
                items = []
                for hd in range(4):
                    for mc in range(2):
                        items.append((po[1][:, hd * 128:(hd + 1) * 128], pT[:, hd * 2 + mc, :],
                                      mv[:, mc, hd * 128:(hd + 1) * 128], mc == 0, mc == 1, {}))
                mm_multi(items, ["qkT", "mv"], ["po1"])
                for hd in range(4):
                    act(junk[:, 0:128], po[1][:, hd * 128:(hd + 1) * 128], AF.Square, ["po1"], ["xssq%d" % hd],
                        accum_out=st[:, 80 + hd:81 + hd])

            def s_G3b():
                for half in range(2):
                    pjt, pjn = proj(C_GV + half * 512, 512, None)
                    copy("act", v[:, half * 512:(half + 1) * 512], pjt[:], [pjn], ["v"])

            def s_H2():
                tt("dve", Ft[0][:], Ft[5][:], lbL, ALU.mult, [F[5], "lbt"], [F[0]])
                act(Ft[0][:], Ft[0][:], AF.Ln, [F[0]], [F[0]], bias=1.0)
                tt("dve", Ft[5][:], Ft[0][:], Ft[1][:], ALU.subtract, [F[0], F[1]], [F[5]])

            def s_X6():
                tt("dve", st[:, 60:64], st[:, 56:60], st[:, 56:60], ALU.mult, ["xZ%d" % hd for hd in range(4)], ["xz2"])
                ts("dve", st[:, 84:88], st[:, 80:84], 1.0 / 128, None, ALU.mult, None,
                   ["xssq%d" % hd for hd in range(4)], ["xvv"])
                stt(st[:, 84:88], st[:, 60:64], EPS, st[:, 84:88], ALU.mult, ALU.add, ["xz2", "xvv"], ["xvv"])
                act(st[:, 84:88], st[:, 84:88], AF.Ln, ["xvv"], ["xvv"])
                act(st[:, 88:92], st[:, 84:88], AF.Exp, ["xvv"], ["xr"], scale=-0.5)
                for hd in range(4):
                    stt(mixed[:, 1536 + hd * 128:1536 + (hd + 1) * 128], po[1][:, hd * 128:(hd + 1) * 128],
                        st[:, 88 + hd:89 + hd], G[:, 1536 + hd * 128:1536 + (hd + 1) * 128], ALU.mult, ALU.mult,
                        ["po1", "xr", "G"], ["mixed"])

            def s_G4():
                qk_transposes()
                tt("dve", AT[:], pss[:].rearrange("p (c m) -> p c m", c=4), bc_mid(maskG, 4), ALU.mult,
                   ["pss", "mskb"], ["AT"])

            def s_H3a():
                act(Ft[0][:], Ft[5][:], AF.Exp, [F[5]], [F[0]])
                ts("dve", Ft[0][:], Ft[0][:], -1.0, 1.0, ALU.mult, ALU.add, [F[0]], [F[0]])

            def s_H3b():
                pjr, pjrn = next_pj()
                mm(pss[:], [(triH, Ft[5][:])], ["cst", F[5]], ["pss"])
                mm(pjr[:], [(triUH, Ft[5][:])], ["cst", F[5]], [pjrn])
                pjt, pjn = next_pj()
                mm_multi([(pjt[:, hd * 4:hd * 4 + 4], Ft[5][:, hd * 128:(hd + 1) * 128], cind[:, 1:5], True, True, {})
                          for hd in range(4)], ["cst", F[5]], [pjn])
                act(Ft[1][:], pss[:], AF.Exp, ["pss"], [F[1]])
                act(Ft[2][:], pss[:], AF.Exp, ["pss"], [F[2]], scale=-1.0)
                act(Ft[3][:], pjr[:], AF.Exp, [pjrn], [F[3]])
                act(st[:, 32:48], pjt[:, 0:16], AF.Exp, [pjn], ["hdec"])

            def s_G5a():
                items = []
                for hd in range(4):
                    o_ap = po[hd // 2][:, (hd % 2) * 256:(hd % 2 + 1) * 256]
                    items.append((o_ap, AT[:, hd, :], v[:, hd * 256:(hd + 1) * 256], True, False, {}))
                    items.append((o_ap, qkT[:, hd, :], gSb[:, hd, :], False, True, {}))
                mm_multi(items, ["AT", "v", "qkT", "gSb"], ["po0", "po1"])
                for hd in range(4):
                    o_ap = po[hd // 2][:, (hd % 2) * 256:(hd % 2 + 1) * 256]
                    act(junk[:, 0:256], o_ap, AF.Square, ["po%d" % (hd // 2)], ["gssq%d" % hd],
                        accum_out=st[:, 16 + hd:17 + hd])
                rstd_from(st[:, 16:20], st[:, 20:24], st[:, 24:28], 1.0 / 256, "gssq",
                          ["gssq%d" % hd for hd in range(4)])

            def s_G5b():
                mm_multi([(pcu[hd // 2][:, (hd % 2) * 256:(hd % 2 + 1) * 256], khb[:, hd * 128:(hd + 1) * 128],
                           v[:, hd * 256:(hd + 1) * 256], True, True, {}) for hd in range(4)],
                         ["khb", "v"], ["pcu0", "pcu1"])
                for hd in range(4):
                    stt(gS[:, hd, :], gS[:, hd, :], st[:, 8 + hd:9 + hd],
                        pcu[hd // 2][:, (hd % 2) * 256:(hd % 2 + 1) * 256], ALU.mult, ALU.add,
                        ["gS", "gdec", "pcu%d" % (hd // 2)], ["gS"])
                copy("act", gSb[:].rearrange("p a b -> p (a b)"), gS[:].rearrange("p a b -> p (a b)"),
                     ["gS"], ["gSb"])

            def s_G5c():
                for hd in range(4):
                    o_ap = po[hd // 2][:, (hd % 2) * 256:(hd % 2 + 1) * 256]
                    stt(mixed[:, hd * 256:(hd + 1) * 256], o_ap, st[:, 24 + hd:25 + hd],
                        G[:, hd * 256:(hd + 1) * 256], ALU.mult, ALU.mult,
                        ["po%d" % (hd // 2), "gssq_r", "G"], ["mixed"])

            def s_H4():
                pjt, pjn = proj(C_HQ, 512, None)
                tt("dve", qb[:], pjt[:], Ft[1][:], ALU.mult, [pjn, F[1]], ["qb"])
                tt("dve", kb[:], Ft[0][:], Ft[2][:], ALU.mult, [F[0], F[2]], ["kb"])
                tt("dve", khb[:], Ft[0][:], Ft[3][:], ALU.mult, [F[0], F[3]], ["khb"])
                pjt, pjn = proj(C_HI, 512, None)
                copy("act", hi[:], pjt[:], [pjn], ["hi"])
                for c4 in range(4):
                    act(him[:, :, c4, :], hi[:].rearrange("p (a b) -> p a b", a=4), AF.Identity, ["hi", "cst"],
                        ["him%d" % c4], scale=cind[:, 1 + c4:2 + c4])

            ubank = [(pcu[0], "pcu0"), (pcu[1], "pcu1"), (pss, "pss"), (po[1], "po1")]

            def s_H5():
                qk_transposes()
                tt("dve", AT[:], pss[:].rearrange("p (c m) -> p c m", c=4), bc_mid(maskH, 4), ALU.mult,
                   ["pss", "mskb"], ["AT"])
                mm_multi([(po[0][:, hd * 128:(hd + 1) * 128], AT[:, hd, :], hi[:, hd * 128:(hd + 1) * 128],
                           hd == 0, False, {"skip_group_check": True}) for hd in range(4)],
                         ["AT", "hi"], ["po0"])
                for hd in range(4):
                    pu, pun = ubank[hd]
                    mm(pu[:], [(khb[:, hd * 128:(hd + 1) * 128], him[:, hd, :, :].rearrange("p a b -> p (a b)"))],
                       ["khb"] + HIM, [pun])

            def s_O2a(part):
                elist = [0, 1, 2, 3, 4, 5, 6, 7, 12, 13, 14, 15]
                es_ = elist[part * 3:(part + 1) * 3]
                items = []
                for half in range(2):
                    for e_ in es_:
                        items.append((pj[half][:], mT[:, e_, :], wout[:, e_, half * 512:(half + 1) * 512],
                                      e_ == 0, False, {"skip_group_check": True}))
                mm_multi(items, ["mTa", "mTb"] + WOUT, ["pj0", "pj1"])

            def s_H6():
                for c4 in range(4):
                    for hd in range(4):
                        pu, pun = ubank[hd]
                        sbn = "hSb%d_%d" % (hd, c4)
                        mm_multi([(po[0][32 * c4:32 * (c4 + 1), hd * 128:(hd + 1) * 128],
                                   qkT[:, hd, 32 * c4:32 * (c4 + 1)], hSb[:, hd, c4, :], False, c4 == 3,
                                   {"skip_group_check": True, "tile_position": (0, 32 * c4)})],
                                 ["qkT", sbn, "hSb"], ["po0"])
                        stt(hS[:, hd, :], hS[:, hd, :], st[:, 32 + hd * 4 + c4:33 + hd * 4 + c4],
                            pu[:, c4 * 128:(c4 + 1) * 128], ALU.mult, ALU.add,
                            ["hS%d" % hd, "hS", "hdec", pun], ["hS%d" % hd])
                        copy("act", hSb[:, hd, (c4 + 1) % 4, :], hS[:, hd, :], ["hS%d" % hd],
                             ["hSb%d_%d" % (hd, (c4 + 1) % 4)])
                    s_O2a(c4)
                for hd in range(4):
                    act(junk[:, 0:128], po[0][:, hd * 128:(hd + 1) * 128], AF.Square, ["po0"], ["hssq%d" % hd],
                        accum_out=st[:, 64 + hd:65 + hd])
                rstd_from(st[:, 64:68], st[:, 68:72], st[:, 72:76], 1.0 / 128, "hssq",
                          ["hssq%d" % hd for hd in range(4)])
                for hd in range(4):
                    stt(mixed[:, 1024 + hd * 128:1024 + (hd + 1) * 128], po[0][:, hd * 128:(hd + 1) * 128],
                        st[:, 72 + hd:73 + hd], G[:, 1024 + hd * 128:1024 + (hd + 1) * 128], ALU.mult, ALU.mult,
                        ["po0", "hssq_r", "G"], ["mixed"])

            def s_O2(t):
                xb = xt[t % 2]
                xn = "xt%d" % (t % 2)
                rows = slice(t * 128, (t + 1) * 128)
                for half in range(2):
                    pjn = "pj%d" % half
                    mm_multi([(pj[half][:], mT[:, e_, :], wout[:, e_, half * 512:(half + 1) * 512], False, e_ == 11,
                               {"skip_group_check": True}) for e_ in range(8, 12)],
                             ["mTc"] + WOUT, [pjn])
                    tt("dve", xb[:, half * 512:(half + 1) * 512], xb[:, half * 512:(half + 1) * 512], pj[half][:],
                       ALU.add, [xn, pjn], [xn])
                pjc[0] = 0
                if do_final:
                    act(outt[:], xb[:], AF.Square, [xn], ["outt", "fssq"], accum_out=st[:, 4:5])
                    rstd_from(st[:, 4:5], st[:, 5:6], st[:, 6:7], 1.0 / D, "fssq")
                    stt(outt[:], xb[:], st[:, 6:7], fnw[:], ALU.mult, ALU.mult, [xn, "fssq_r", "fnw"], ["outt"])
                    dma("sp", dst_d[rows, :], outt[:], ["outt"], ["xmid%d" % t], "d_o")
                else:
                    dma("sp", dst_d[rows, :], xb[:], [xn], ["xmid%d" % t], "d_x%d" % (t % 2))

            LOAD(0)
            HEAD(0)
            for t in range(NT):
                nxt = t + 1 < NT
                if nxt:
                    LOAD(t + 1)
                s_X1(); s_G1a(); s_G3b(); s_Hp(); s_X2(); s_G1b(); s_G2a(); s_X3(); s_G2b(); s_H1(); s_X4(); s_G3a(); s_X5()
                s_H2()
                if nxt:
                    HEAD_a(t + 1)
                s_GT(); s_G4(); s_X6(); s_H3a(); s_H3b(); s_G5a(); s_G5b()
                mixed_T(12, 16, "mTb")
                s_G5c(); s_H4()
                if nxt:
                    HEAD_b(t + 1)
                s_H5()
                mixed_T(0, 8, "mTa")
                s_H6()
                mixed_T(8, 12, "mTc")
                s_O2(t)

        P.emit(nc)
    return nc


def _consts():
    j = np.arange(128)[:, None]
    i = np.arange(128)[None, :]
    triG = (j <= i).astype(np.float32)
    triUG = (j > i).astype(np.float32)
    same = (j // 32) == (i // 32)
    triH = ((j <= i) & same).astype(np.float32)
    triUH = ((j > i) & same).astype(np.float32)
    cind = np.zeros((128, 8), np.float32)
    cind[:, 0] = 1.0
    for c in range(4):
        cind[:, 1 + c] = (np.arange(128) // 32 == c)
    cst = np.concatenate([triG, triUG, triH, triUH, cind, np.zeros((128, 6 * 128 + 8 - 520), np.float32)], axis=1)
    ident = np.eye(128, dtype=np.float32)
    maskG = triG
    maskH = triH
    msk = np.concatenate([ident, maskG, maskH], axis=1)
    return np.ascontiguousarray(cst), np.ascontiguousarray(msk)


def _layout_params(norm_w, gla_w_gate_up, gla_b_gate, gla_norm_w, hgrn_lower_bounds, hgrn_norm_w,
                   mem_norm_w, xattn_norm_w, final_norm_w):
    NL = norm_w.shape[0]
    pcols = np.zeros((NL, 128, 32), np.float32)
    for L in range(NL):
        pcols[L, :, 0:8] = norm_w[L].reshape(8, 128).T
        gw = np.concatenate([np.tile(gla_norm_w[L], 4), np.tile(hgrn_norm_w[L], 4), np.tile(xattn_norm_w[L], 4)])
        pcols[L, :, 8:24] = gw.reshape(16, 128).T
        pcols[L, :, 24:32] = mem_norm_w[L].reshape(8, 128).T
    wup = np.concatenate([gla_w_gate_up, gla_b_gate[:, None, :]], axis=1).astype(np.float32)
    lbraw = np.ascontiguousarray(np.broadcast_to(hgrn_lower_bounds.reshape(1, -1), (128, NL * 512))).astype(np.float32)
    fnw = np.ascontiguousarray(np.broadcast_to(final_norm_w.reshape(1, -1), (128, D))).astype(np.float32)
    return pcols, np.ascontiguousarray(wup), lbraw, fnw


_NC_CACHE = {}


def _get_nc(S, layers, final_norm):
    key = (S, tuple(layers), final_norm)
    if key not in _NC_CACHE:
        _NC_CACHE[key] = build(S, list(layers), final_norm)
    return _NC_CACHE[key]


def kernel(x, mem, norm_w, w_in, gla_w_gate_up, gla_b_gate, gla_norm_w, hgrn_lower_bounds,
           hgrn_norm_w, mem_norm_w, w_mem_kv, xattn_norm_w, w_out, final_norm_w):
    x = np.asarray(x, np.float32)
    mem = np.asarray(mem, np.float32)
    B, S, _ = x.shape
    f = lambda a: np.ascontiguousarray(np.asarray(a, np.float32))
    pcols, wup, lbraw, fnw = _layout_params(f(norm_w), f(gla_w_gate_up), f(gla_b_gate), f(gla_norm_w),
                                            f(hgrn_lower_bounds), f(hgrn_norm_w), f(mem_norm_w),
                                            f(xattn_norm_w), f(final_norm_w))
    cst, msk = _consts()
    nc = _get_nc(S, (0, 1), True)
    shared = {"w_in": f(w_in), "w_out": f(w_out), "w_kv": f(w_mem_kv), "wup": wup, "pcols": pcols,
              "lbraw": lbraw, "fnw": fnw, "cst": cst, "msk": msk}
    in_maps = []
    for b in range(B):
        m = dict(shared)
        m["x"] = np.ascontiguousarray(x[b])
        m["mem"] = np.ascontiguousarray(mem[b])
        in_maps.append(m)
    res = run_bass_kernel_spmd(nc, in_maps, core_ids=list(range(B)))
    return np.stack([np.asarray(r["out"], np.float32) for r in res.results], axis=0)
```

```python
import contextlib
import numpy as np
import ml_dtypes
import concourse.bass as bass
import concourse.mybir as mybir
from concourse.bass_utils import run_bass_kernel_spmd

F32 = mybir.dt.float32
BF16 = mybir.dt.bfloat16
AF = mybir.ActivationFunctionType
ALU = mybir.AluOpType
AX = mybir.AxisListType

D = 1024
DIN = 6160
DMIX = 2048
MEM = 256
EPS = 1e-6
C_GQ, C_GK, C_GV, C_GLR, C_HQ, C_HF, C_HI, C_XQ, C_GATE = 0, 512, 1024, 2048, 2064, 2576, 3088, 3600, 4112


class Prog:
    ENG = ("pe", "act", "dve", "pool", "sp")

    def __init__(self):
        self.q = {e: [] for e in self.ENG}
        self.cnt = {}
        self.res = {}
        self.waited = {e: {} for e in self.ENG}

    def op(self, eng, fn, reads=(), writes=(), dma=None):
        deps = {}

        def add(tok):
            if tok is None:
                return
            k, v = tok
            if deps.get(k, 0) < v:
                deps[k] = v

        for r in reads:
            st = self.res.get(r)
            if st:
                add(st[0])
        for w in writes:
            st = self.res.get(w)
            if st:
                add(st[0])
                for k, v in st[1].items():
                    add((k, v))
        if eng == "pe":
            deps.pop("pe", None)
        waits = []
        wd = self.waited[eng]
        for k, v in deps.items():
            if wd.get(k, 0) < v:
                wd[k] = v
                waits.append((k, v))
        key, amt = (dma, 16) if dma is not None else (eng, 1)
        self.cnt[key] = self.cnt.get(key, 0) + amt
        tok = (key, self.cnt[key])
        self.q[eng].append((waits, fn, key, amt))
        for r in reads:
            st = self.res.setdefault(r, [None, {}])
            if st[1].get(key, 0) < tok[1]:
                st[1][key] = tok[1]
        for w in writes:
            self.res[w] = [tok, {}]
        return tok

    def emit(self, nc):
        with contextlib.ExitStack() as es:
            sems = {k: es.enter_context(nc.semaphore("s_" + k)) for k in self.cnt}
            block = es.enter_context(nc.Block())
            final = [(k, v) for k, v in self.cnt.items()]

            def run(name, e):
                for waits, fn, key, amt in self.q[name]:
                    for k, v in waits:
                        e.wait_ge(sems[k], v)
                    ins = fn(e)
                    ins.then_inc(sems[key], amt)

            @block.tensor
            def _(e):
                run("pe", e)

            @block.scalar
            def _(e):
                run("act", e)

            @block.vector
            def _(e):
                run("dve", e)

            @block.gpsimd
            def _(e):
                run("pool", e)

            @block.sync
            def _(e):
                run("sp", e)
                for k, v in final:
                    e.wait_ge(sems[k], v)


def build(S, layers, final_norm, n_layers_total=2):
    nc = bass.Bass("TRN2", target_bir_lowering=False)
    P = Prog()
    NT = S // 128
    NL = n_layers_total

    def din(name, shape, dt=F32):
        return nc.dram_tensor(name, list(shape), dt, kind="ExternalInput").ap()

    x_d = din("x", [S, D])
    mem_d = din("mem", [MEM, D])
    win_d = din("w_in", [NL, D, DIN])
    wout_d = din("w_out", [NL, DMIX, D])
    wkv_d = din("w_kv", [NL, D, 2 * 512])
    wup_d = din("wup", [NL, 17, 512])
    pcols_d = din("pcols", [NL, 128, 32])
    lbraw_d = din("lbraw", [128, NL * 512])
    fnw_d = din("fnw", [128, D])
    cst_d = din("cst", [128, 6 * 128 + 8])
    msk_d = din("msk", [128, 3 * 128])
    out_d = nc.dram_tensor("out", [S, D], F32, kind="ExternalOutput").ap()
    xmid_d = None
    if len(layers) > 1:
        xmid_d = nc.dram_tensor("xmid", [S, D], F32, kind="Internal").ap()

    es = contextlib.ExitStack()
    with es:
        def sb(name, shape, dt):
            return es.enter_context(nc.sbuf_tensor("sb_" + name, list(shape), dt))

        def ps(name, shape, dt):
            return es.enter_context(nc.psum_tensor("ps_" + name, list(shape), dt))

        win = sb("win", [128, 8, DIN], BF16)
        wout = sb("wout", [128, 16, D], BF16)
        cst = sb("cst", [128, 4 * 128 + 8], F32)
        mskb = sb("mskb", [128, 3 * 128], BF16)
        pcols = sb("pcols", [128, 32], F32)
        wup = sb("wup", [32, 512], BF16)
        lbt = sb("lbt", [128, NL * 512], F32)
        fnw = sb("fnw", [128, D], F32)
        gS = sb("gS", [128, 4, 256], F32)
        gSb = sb("gSb", [128, 4, 256], BF16)
        hS = sb("hS", [128, 4, 128], F32)
        hSb = sb("hSb", [128, 4, 4, 128], BF16)
        mkT = sb("mkT", [128, 4, 256], BF16)
        mv = sb("mv", [128, 2, 512], BF16)
        xt = [sb("xt0", [128, D], F32), sb("xt1", [128, D], F32)]
        h = sb("h", [128, D], BF16)
        hT = sb("hT", [128, 8, 128], BF16)
        Ft = [sb("F%d" % i, [128, 512], F32) for i in range(6)]
        qb = sb("qb", [128, 512], BF16)
        kb = sb("kb", [128, 512], BF16)
        khb = sb("khb", [128, 512], BF16)
        qkT = sb("qkT", [128, 8, 128], BF16)
        v = sb("v", [128, 1024], BF16)
        hi = sb("hi", [128, 512], BF16)
        pbt = sb("pbt", [128, 4, 256], BF16)
        AT = sb("AT", [128, 4, 128], BF16)
        glrT = sb("glrT", [32, 128], BF16)
        G = sb("G", [128, DMIX], BF16)
        mixed = sb("mixed", [128, DMIX], BF16)
        outt = sb("outt", [128, D], F32)
        lbraw = outt
        xqs = qb
        xqT = AT
        pb = pbt
        HIM = ["pbt"]
        pT = qkT
        mT = G[:].rearrange("p (c m) -> p c m", c=16)
        st = sb("st", [128, 128], F32)
        junk = sb("junk", [128, 256], BF16)

        pj = [ps("pj0", [128, 512], F32), ps("pj1", [128, 512], F32)]
        pt = ps("pt", [128, 1024], BF16)
        pcu = [ps("pcu0", [128, 512], F32), ps("pcu1", [128, 512], F32)]
        pss = ps("pss", [128, 512], F32)
        po = [ps("po0", [128, 512], F32), ps("po1", [128, 512], F32)]

        ident = mskb[:, 0:128]
        maskG = mskb[:, 128:256]
        maskH = mskb[:, 256:384]
        triG = cst[:, 0:128]
        triUG = cst[:, 128:256]
        triH = cst[:, 256:384]
        triUH = cst[:, 384:512]
        cind = cst[:, 512:520]
        nwT = pcols[:, 0:8]
        gwT = pcols[:, 8:24]
        mnwT = pcols[:, 24:32]

        pjc = [0]

        def next_pj():
            i = pjc[0] % 2
            pjc[0] += 1
            return pj[i], "pj%d" % i

        def bc(ap2, n):
            return ap2.unsqueeze(2).broadcast_to([128, ap2.shape[1], n])

        def bc_mid(ap2, n):
            return ap2.unsqueeze(1).broadcast_to([128, n, ap2.shape[1]])

        def mm(out_ap, pairs, reads, writes, first_start=True, **kw):
            pairs = list(pairs)

            def fn(e):
                n = len(pairs)
                ins = None
                for i, (a, b) in enumerate(pairs):
                    ins = e.matmul(out_ap, a, b, start=(first_start and i == 0), stop=(i == n - 1), **kw)
                return ins
            P.op("pe", fn, reads, writes)

        def mm_multi(items, reads, writes):
            items = list(items)

            def fn(e):
                ins = None
                for (o, a, b, s0, s1, kw) in items:
                    ins = e.matmul(o, a, b, start=s0, stop=s1, **kw)
                return ins
            P.op("pe", fn, reads, writes)

        def transposes(items, reads, writes):
            items = list(items)

            def fn(e):
                ins = None
                for (o, i_) in items:
                    ins = e.transpose(o, i_, ident)
                return ins
            P.op("pe", fn, list(reads) + ["mskb"], writes)

        def act(out, in_, func, reads, writes, **kw):
            P.op("act", lambda e: e.activation(out, in_, func, **kw), reads, writes)

        def tt(eng, out, in0, in1, op, reads, writes):
            P.op(eng, lambda e: e.tensor_tensor(out, in0, in1, op), reads, writes)

        def ts(eng, out, in0, s1, s2, op0, op1, reads, writes):
            if s2 is None:
                P.op(eng, lambda e: e.tensor_scalar(out, in0, s1, None, op0), reads, writes)
            else:
                P.op(eng, lambda e: e.tensor_scalar(out, in0, s1, s2, op0, op1), reads, writes)

        def stt(out, in0, scalar, in1, op0, op1, reads, writes):
            P.op("dve", lambda e: e.scalar_tensor_tensor(out, in0, scalar, in1, op0, op1), reads, writes)

        def copy(eng, out, in_, reads, writes):
            if eng == "act":
                P.op("act", lambda e: e.activation(out, in_, AF.Identity), reads, writes)
            else:
                P.op(eng, lambda e: e.tensor_copy(out, in_), reads, writes)

        def dma(eng, out, in_, reads, writes, sem, **kw):
            P.op(eng, lambda e: e.dma_start(out=out, in_=in_, **kw), reads, writes, dma=sem)

        def rstd_from(ssq_col, tmp_col, out_col, inv_n, rname, reads=None):
            act(tmp_col, ssq_col, AF.Ln, reads or [rname], [rname + "_t"], scale=inv_n, bias=EPS)
            act(out_col, tmp_col, AF.Exp, [rname + "_t"], [rname + "_r"], scale=-0.5)

        dma("sp", cst[:], cst_d[:, 0:520], [], ["cst"], "d_cst")
        dma("pool", mskb[:], msk_d, [], ["mskb"], "d_msk")
        dma("sp", lbraw[:], lbraw_d, [], ["outt"], "d_lbraw")
        if final_norm:
            dma("sp", fnw[:], fnw_d, [], ["fnw"], "d_fnw")
        P.op("pool", lambda e: e.memset(glrT[:], 1.0), [], ["glrT"])
        lr3 = lbraw[:].rearrange("p (l n) -> p l n", l=NL)
        lb3 = lbt[:].rearrange("p (l n) -> p l n", l=NL)
        mx = Ft[0]
        P.op("dve", lambda e: e.tensor_copy(mx[:], lr3[:, 0, :]), ["outt"], ["F0"])
        for l in range(1, NL):
            tt("dve", mx[:], mx[:], lr3[:, l, :], ALU.max, ["F0", "outt"], ["F0"])
        for l in range(NL):
            tt("dve", lb3[:, l, :], lr3[:, l, :], mx[:], ALU.subtract, ["F0", "outt"], ["lbt"])
        act(lbt[:], lbt[:], AF.Exp, ["lbt"], ["lbt"])
        den = Ft[1]
        P.op("dve", lambda e: e.tensor_copy(den[:], lb3[:, 0, :]), ["lbt"], ["F1"])
        for l in range(1, NL):
            tt("dve", den[:], den[:], lb3[:, l, :], ALU.add, ["F1", "lbt"], ["F1"])
        P.op("dve", lambda e: e.reciprocal(den[:], den[:]), ["F1"], ["F1"])
        for l in range(NL):
            tt("dve", lb3[:, l, :], lb3[:, l, :], den[:], ALU.mult, ["F1", "lbt"], ["lbt"])
        p0 = Ft[2]
        P.op("dve", lambda e: e.tensor_copy(p0[:], lb3[:, 0, :]), ["lbt"], ["F2"])
        for l in range(1, NL):
            tt("dve", lb3[:, l, :], lb3[:, l, :], lb3[:, l - 1, :], ALU.add, ["lbt"], ["lbt"])
        for l in range(NL):
            tt("dve", lb3[:, l, :], lb3[:, l, :], p0[:], ALU.subtract, ["lbt", "F2"], ["lbt"])

        F = ["F%d" % i for i in range(6)]

        for li, L in enumerate(layers):
            last = (li == len(layers) - 1)
            src_d = x_d if li == 0 else xmid_d
            dst_d = out_d if last else xmid_d
            do_final = last and final_norm
            lbL = lbt[:, L * 512:(L + 1) * 512]

            dma("sp", pcols[:], pcols_d[L], [], ["pcols"], "d_pcols")
            dma("pool", wup[0:17, :], wup_d[L], [], ["wup"], "d_wup")
            wkv = wout[:, 0:8, :]
            for hf_ in range(2):
                dma("pool", wout[:, hf_ * 4:(hf_ + 1) * 4, :],
                    wkv_d[L, hf_ * 512:(hf_ + 1) * 512, :].rearrange("(c p) n -> p c n", p=128),
                    [], ["wq%d" % hf_], "d_wkv%d" % hf_, max_dma_last_dim=4096)
            for c in range(8):
                dma("pool", win[:, c, :], win_d[L, c * 128:(c + 1) * 128, :], [], ["win%d" % c], "d_win%d" % c,
                    max_dma_last_dim=4096)
            P.op("dve", lambda e: e.memset(gS[:], 0.0), [], ["gS"])
            P.op("dve", lambda e: e.memset(hS[:], 0.0), [], ["hS"])
            P.op("pool", lambda e: e.memset(gSb[:], 0.0), [], ["gSb"])
            P.op("pool", lambda e: e.memset(hSb[:], 0.0), [], ["hSb"])
            WIN = ["win%d" % c for c in range(8)]

            mnT = mixed[:].rearrange("p (c m) -> p c m", c=8)
            for blk in range(2):
                xb = xt[blk]
                xn = "xt%d" % blk
                dma("sp", xb[:], mem_d[blk * 128:(blk + 1) * 128, :], [], [xn], "d_x%d" % blk)
                act(h[:], xb[:], AF.Square, [xn], ["h", "ssq"], accum_out=st[:, 0:1])
                rstd_from(st[:, 0:1], st[:, 1:2], st[:, 2:3], 1.0 / D, "ssq")
                ts("dve", h[:], xb[:], st[:, 2:3], None, ALU.mult, None, [xn, "ssq_r"], ["h"])
                transposes([(pt[:, c * 128:(c + 1) * 128], h[:, c * 128:(c + 1) * 128]) for c in range(8)],
                           ["h"], ["pt"])
                tt("dve", mnT[:, :, blk * 128:(blk + 1) * 128], pt[:].rearrange("p (c m) -> p c m", c=8),
                   bc(mnwT, 128), ALU.mult, ["pt", "pcols"], ["mixed"])
            for hd in range(4):
                pjt, pjn = next_pj()
                mm(pjt[:, 0:256], [(wkv[:, c, hd * 128:(hd + 1) * 128], mnT[:, c, :]) for c in range(8)],
                   ["wq0", "wq1", "mixed"], [pjn])
                copy("act", mkT[:, hd, :], pjt[:, 0:256], [pjn], ["mkT"])
            for blk in range(2):
                pjt, pjn = next_pj()
                mm(pjt[:], [(mnT[:, c, blk * 128:(blk + 1) * 128], wkv[:, c, 512:1024]) for c in range(8)],
                   ["wq0", "wq1", "mixed"], [pjn])
                copy("act", mv[:, blk, :], pjt[:], [pjn], ["mv"])
            for q4 in range(4):
                dma("pool", wout[:, q4 * 4:(q4 + 1) * 4, :],
                    wout_d[L, q4 * 512:(q4 + 1) * 512, :].rearrange("(c p) n -> p c n", p=128),
                    [], ["wq%d" % q4], "d_wo%d" % q4, max_dma_last_dim=4096)
            WOUT = ["wq%d" % q4 for q4 in range(4)]

            def proj(col0, ncol, dst_names):
                pjt, pjn = next_pj()
                mm(pjt[:, 0:ncol], [(hT[:, c, :], win[:, c, col0:col0 + ncol]) for c in range(8)],
                   ["hT"] + WIN, [pjn])
                return pjt, pjn

            def LOAD(t):
                dma("sp", xt[t % 2][:], src_d[t * 128:(t + 1) * 128, :], ["xmid%d" % t] if li > 0 else [],
                    ["xt%d" % (t % 2)], "d_x%d" % (t % 2))

            def HEAD_a(t):
                xb = xt[t % 2]
                xn = "xt%d" % (t % 2)
                act(h[:], xb[:], AF.Square, [xn], ["h", "ssq"], accum_out=st[:, 0:1])
                rstd_from(st[:, 0:1], st[:, 1:2], st[:, 2:3], 1.0 / D, "ssq")
                ts("dve", h[:], xb[:], st[:, 2:3], None, ALU.mult, None, [xn, "ssq_r"], ["h"])

            def HEAD_b(t):
                transposes([(pt[:, c * 128:(c + 1) * 128], h[:, c * 128:(c + 1) * 128]) for c in range(8)],
                           ["h"], ["pt"])
                tt("dve", hT[:], pt[:].rearrange("p (c m) -> p c m", c=8), bc(nwT, 128), ALU.mult,
                   ["pt", "pcols"], ["hT"])

            def HEAD(t):
                HEAD_a(t)
                HEAD_b(t)

            def qk_transposes():
                transposes([(pt[:, hd * 128:(hd + 1) * 128], qb[:, hd * 128:(hd + 1) * 128]) for hd in range(4)] +
                           [(pt[:, (4 + hd) * 128:(5 + hd) * 128], kb[:, hd * 128:(hd + 1) * 128]) for hd in range(4)],
                           ["qb", "kb"], ["pt"])
                copy("dve", qkT[:], pt[:].rearrange("p (c m) -> p c m", c=8), ["pt"], ["qkT"])
                mm_multi([(pss[:, hd * 128:(hd + 1) * 128], qkT[:, 4 + hd, :], qkT[:, hd, :], True, True, {})
                          for hd in range(4)], ["qkT"], ["pss"])

            def mixed_T(e0, e1, rname):
                n = e1 - e0
                transposes([(pt[:, i_ * 128:(i_ + 1) * 128], mixed[:, (e0 + i_) * 128:(e0 + i_ + 1) * 128])
                            for i_ in range(n)], ["mixed"], ["pt"])
                tt("dve", mT[:, e0:e1, :], pt[:, 0:n * 128].rearrange("p (c m) -> p c m", c=n),
                   bc(gwT[:, e0:e1], 128), ALU.mult, ["pt", "pcols"], [rname])

            ctx = {}

            def s_GT():
                for g4 in range(4):
                    pjt, pjn = proj(C_GATE + g4 * 512, 512, None)
                    act(G[:, g4 * 512:(g4 + 1) * 512], pjt[:], AF.Silu, [pjn], ["G", "mTa", "mTb", "mTc"])

            def s_X1():
                pjt, pjn = proj(C_XQ, 512, None)
                act(xqs[:], pjt[:], AF.Identity, [pjn], ["qb"], scale=float(128 ** -0.5))

            def s_G1a():
                pjt, pjn = next_pj()
                mm(pjt[0:16, 0:128], [(win[:, c, C_GLR:C_GLR + 16], hT[:, c, :]) for c in range(8)],
                   ["hT"] + WIN, [pjn])
                copy("act", glrT[0:16, :], pjt[0:16, 0:128], [pjn], ["glrT"])

            def s_X2():
                transposes([(pt[:, hd * 128:(hd + 1) * 128], xqs[:, hd * 128:(hd + 1) * 128]) for hd in range(4)],
                           ["qb"], ["pt"])
                copy("dve", xqT[:], pt[:, 0:512].rearrange("p (c m) -> p c m", c=4), ["pt"], ["AT"])

            def s_G1b():
                pjt, pjn = next_pj()
                mm(pjt[:], [(glrT[0:17, :], wup[0:17, :])], ["glrT", "wup"], [pjn])
                act(Ft[0][:], pjt[:], AF.Exp, [pjn], [F[0]], scale=-1.0)

            def s_X3():
                mm_multi([(pcu[hd // 2][:, (hd % 2) * 256:(hd % 2 + 1) * 256], xqT[:, hd, :], mkT[:, hd, :],
                           True, True, {}) for hd in range(4)], ["AT", "mkT"], ["pcu0", "pcu1"])
                for half in range(2):
                    P.op("dve", (lambda half: lambda e: e.tensor_reduce(
                        st[:, 48 + 2 * half:50 + 2 * half], pcu[half][:].rearrange("p (a b) -> p a b", a=2),
                        AX.X, ALU.max))(half), ["pcu%d" % half], ["xmax%d" % half])
                ts("dve", st[:, 52:56], st[:, 48:52], -1.0, None, ALU.mult, None, ["xmax0", "xmax1"], ["xnmax"])
                for hd in range(4):
                    act(pb[:, hd, :], pcu[hd // 2][:, (hd % 2) * 256:(hd % 2 + 1) * 256], AF.Exp,
                        ["pcu%d" % (hd // 2), "xnmax"], HIM + ["xZ%d" % hd], bias=st[:, 52 + hd:53 + hd],
                        accum_out=st[:, 56 + hd:57 + hd])

            def s_Hp():
                pjz, pjzn = proj(C_HF, 512, None)
                act(Ft[5][:], pjz[:], AF.Exp, [pjzn], [F[5]], scale=-1.0)

            def s_G2():
                act(Ft[1][:], Ft[0][:], AF.Ln, [F[0]], [F[1]], bias=1.0)
                mm(pcu[0][:], [(triG, Ft[1][:])], ["cst", F[1]], ["pcu0"])
                mm(pcu[1][:], [(triUG, Ft[1][:])], ["cst", F[1]], ["pcu1"])
                pjt, pjn = next_pj()
                mm_multi([(pjt[:, hd:hd + 1], Ft[1][:, hd * 128:(hd + 1) * 128], cind[:, 0:1], True, True, {})
                          for hd in range(4)], ["cst", F[1]], [pjn])
                act(st[:, 8:12], pjt[:, 0:4], AF.Exp, [pjn], ["gdec"], scale=-1.0 / 16)
                act(Ft[2][:], pcu[0][:], AF.Exp, ["pcu0"], [F[2]], scale=-1.0 / 16)
                act(Ft[3][:], pcu[0][:], AF.Exp, ["pcu0"], [F[3]], scale=1.0 / 16)
                act(Ft[4][:], pcu[1][:], AF.Exp, ["pcu1"], [F[4]], scale=-1.0 / 16)

            def s_H1():
                act(Ft[1][:], Ft[5][:], AF.Ln, [F[5]], [F[1]], bias=1.0)

            def s_X4():
                transposes([(pt[:, (hd * 2 + mc) * 128:(hd * 2 + mc + 1) * 128], pb[:, hd, mc * 128:(mc + 1) * 128])
                            for hd in range(4) for mc in range(2)], HIM, ["pt"])
                copy("dve", pT[:], pt[:].rearrange("p (c m) -> p c m", c=8), ["pt"], ["qkT"])

            def s_G3a():
                pjt, pjn = proj(C_GQ, 512, None)
                stt(qb[:], pjt[:], float(128 ** -0.5), Ft[2][:], ALU.mult, ALU.mult, [pjn, F[2]], ["qb"])
                pjt, pjn = proj(C_GK, 512, None)
                tt("dve", kb[:], pjt[:], Ft[3][:], ALU.mult, [pjn, F[3]], ["kb"])
                tt("dve", khb[:], pjt[:], Ft[4][:], ALU.mult, [pjn, F[4]], ["khb"])

            def s_X5():
                items = []
                for hd in range(4):
                    for mc in range(2):
                        items.append((po[1][:, hd * 128:(hd + 1) * 128], pT[:, hd * 2 + mc, :],
                                      mv[:, mc, hd * 128:(hd + 1) * 128], mc == 0, mc == 1, {}))
                mm_multi(items, ["qkT", "mv"], ["po1"])
                for hd in range(4):
                    act(junk[:, 0:128], po[1][:, hd * 128:(hd + 1) * 128], AF.Square, ["po1"], ["xssq%d" % hd],
                        accum_out=st[:, 80 + hd:81 + hd])

            def s_G3b():
                for half in range(2):
                    pjt, pjn = proj(C_GV + half * 512, 512, None)
                    copy("act", v[:, half * 512:(half + 1) * 512], pjt[:], [pjn], ["v"])

            def s_H2():
                tt("dve", Ft[0][:], Ft[5][:], lbL, ALU.mult, [F[5], "lbt"], [F[0]])
                act(Ft[0][:], Ft[0][:], AF.Ln, [F[0]], [F[0]], bias=1.0)
                tt("dve", Ft[5][:], Ft[0][:], Ft[1][:], ALU.subtract, [F[0], F[1]], [F[5]])

            def s_X6():
                tt("dve", st[:, 60:64], st[:, 56:60], st[:, 56:60], ALU.mult, ["xZ%d" % hd for hd in range(4)], ["xz2"])
                ts("dve", st[:, 84:88], st[:, 80:84], 1.0 / 128, None, ALU.mult, None,
                   ["xssq%d" % hd for hd in range(4)], ["xvv"])
                stt(st[:, 84:88], st[:, 60:64], EPS, st[:, 84:88], ALU.mult, ALU.add, ["xz2", "xvv"], ["xvv"])
                act(st[:, 84:88], st[:, 84:88], AF.Ln, ["xvv"], ["xvv"])
                act(st[:, 88:92], st[:, 84:88], AF.Exp, ["xvv"], ["xr"], scale=-0.5)
                for hd in range(4):
                    stt(mixed[:, 1536 + hd * 128:1536 + (hd + 1) * 128], po[1][:, hd * 128:(hd + 1) * 128],
                        st[:, 88 + hd:89 + hd], G[:, 1536 + hd * 128:1536 + (hd + 1) * 128], ALU.mult, ALU.mult,
                        ["po1", "xr", "G"], ["mixed"])

            def s_G4():
                qk_transposes()
                tt("dve", AT[:], pss[:].rearrange("p (c m) -> p c m", c=4), bc_mid(maskG, 4), ALU.mult,
                   ["pss", "mskb"], ["AT"])

            def s_H3a():
                act(Ft[0][:], Ft[5][:], AF.Exp, [F[5]], [F[0]])
                ts("pool", Ft[0][:], Ft[0][:], -1.0, 1.0, ALU.mult, ALU.add, [F[0]], [F[0]])

            def s_H3b():
                pjr, pjrn = next_pj()
                mm(pss[:], [(triH, Ft[5][:])], ["cst", F[5]], ["pss"])
                mm(pjr[:], [(triUH, Ft[5][:])], ["cst", F[5]], [pjrn])
                pjt, pjn = next_pj()
                mm_multi([(pjt[:, hd * 4:hd * 4 + 4], Ft[5][:, hd * 128:(hd + 1) * 128], cind[:, 1:5], True, True, {})
                          for hd in range(4)], ["cst", F[5]], [pjn])
                act(Ft[1][:], pss[:], AF.Exp, ["pss"], [F[1]])
                act(Ft[2][:], pss[:], AF.Exp, ["pss"], [F[2]], scale=-1.0)
                act(Ft[3][:], pjr[:], AF.Exp, [pjrn], [F[3]])
                act(st[:, 32:48], pjt[:, 0:16], AF.Exp, [pjn], ["hdec"])

            def s_G5a():
                items = []
                for hd in range(4):
                    o_ap = po[hd // 2][:, (hd % 2) * 256:(hd % 2 + 1) * 256]
                    items.append((o_ap, AT[:, hd, :], v[:, hd * 256:(hd + 1) * 256], True, False, {}))
                    items.append((o_ap, qkT[:, hd, :], gSb[:, hd, :], False, True, {}))
                mm_multi(items, ["AT", "v", "qkT", "gSb"], ["po0", "po1"])
                for hd in range(4):
                    o_ap = po[hd // 2][:, (hd % 2) * 256:(hd % 2 + 1) * 256]
                    act(junk[:, 0:256], o_ap, AF.Square, ["po%d" % (hd // 2)], ["gssq%d" % hd],
                        accum_out=st[:, 16 + hd:17 + hd])
                rstd_from(st[:, 16:20], st[:, 20:24], st[:, 24:28], 1.0 / 256, "gssq",
                          ["gssq%d" % hd for hd in range(4)])

            def s_G5b():
                mm_multi([(pcu[hd // 2][:, (hd % 2) * 256:(hd % 2 + 1) * 256], khb[:, hd * 128:(hd + 1) * 128],
                           v[:, hd * 256:(hd + 1) * 256], True, True, {}) for hd in range(4)],
                         ["khb", "v"], ["pcu0", "pcu1"])
                for hd in range(4):
                    stt(gS[:, hd, :], gS[:, hd, :], st[:, 8 + hd:9 + hd],
                        pcu[hd // 2][:, (hd % 2) * 256:(hd % 2 + 1) * 256], ALU.mult, ALU.add,
                        ["gS", "gdec", "pcu%d" % (hd // 2)], ["gS"])
                copy("act", gSb[:].rearrange("p a b -> p (a b)"), gS[:].rearrange("p a b -> p (a b)"),
                     ["gS"], ["gSb"])

            def s_G5c():
                for hd in range(4):
                    o_ap = po[hd // 2][:, (hd % 2) * 256:(hd % 2 + 1) * 256]
                    stt(mixed[:, hd * 256:(hd + 1) * 256], o_ap, st[:, 24 + hd:25 + hd],
                        G[:, hd * 256:(hd + 1) * 256], ALU.mult, ALU.mult,
                        ["po%d" % (hd // 2), "gssq_r", "G"], ["mixed"])

            def s_H4():
                pjt, pjn = proj(C_HQ, 512, None)
                tt("dve", qb[:], pjt[:], Ft[1][:], ALU.mult, [pjn, F[1]], ["qb"])
                tt("pool", kb[:], Ft[0][:], Ft[2][:], ALU.mult, [F[0], F[2]], ["kb"])
                tt("pool", khb[:], Ft[0][:], Ft[3][:], ALU.mult, [F[0], F[3]], ["khb"])
                pjt, pjn = proj(C_HI, 512, None)
                copy("act", hi[:], pjt[:], [pjn], ["hi"])

            ubank = [(pcu[0], "pcu0"), (pcu[1], "pcu1"), (pss, "pss"), (po[1], "po1")]

            def s_H5():
                qk_transposes()
                tt("dve", AT[:], pss[:].rearrange("p (c m) -> p c m", c=4), bc_mid(maskH, 4), ALU.mult,
                   ["pss", "mskb"], ["AT"])
                mm_multi([(po[0][:, hd * 128:(hd + 1) * 128], AT[:, hd, :], hi[:, hd * 128:(hd + 1) * 128],
                           hd == 0, False, {"skip_group_check": True}) for hd in range(4)],
                         ["AT", "hi"], ["po0"])
                for c4 in range(4):
                    pu, pun = ubank[c4]
                    mm_multi([(pu[:, hd * 128:(hd + 1) * 128], khb[32 * c4:32 * (c4 + 1), hd * 128:(hd + 1) * 128],
                               hi[32 * c4:32 * (c4 + 1), hd * 128:(hd + 1) * 128], True, True,
                               {"tile_position": (32 * c4, 0)}) for hd in range(4)],
                             ["khb", "hi"], [pun])

            def s_O2a(part):
                elist = [0, 1, 2, 3, 4, 5, 6, 7, 12, 13, 14, 15]
                es_ = elist[part * 3:(part + 1) * 3]
                items = []
                for half in range(2):
                    for e_ in es_:
                        items.append((pj[half][:], mT[:, e_, :], wout[:, e_, half * 512:(half + 1) * 512],
                                      e_ == 0, False, {"skip_group_check": True}))
                mm_multi(items, ["mTa", "mTb"] + WOUT, ["pj0", "pj1"])

            def s_H6():
                for c4 in range(4):
                    pu, pun = ubank[c4]
                    for hd in range(4):
                        sbn = "hSb%d_%d" % (hd, c4)
                        mm_multi([(po[0][32 * c4:32 * (c4 + 1), hd * 128:(hd + 1) * 128],
                                   qkT[:, hd, 32 * c4:32 * (c4 + 1)], hSb[:, hd, c4, :], False, c4 == 3,
                                   {"skip_group_check": True, "tile_position": (0, 32 * c4)})],
                                 ["qkT", sbn, "hSb"], ["po0"])
                        stt(hS[:, hd, :], hS[:, hd, :], st[:, 32 + hd * 4 + c4:33 + hd * 4 + c4],
                            pu[:, hd * 128:(hd + 1) * 128], ALU.mult, ALU.add,
                            ["hS%d" % hd, "hS", "hdec", pun], ["hS%d" % hd])
                        copy("act", hSb[:, hd, (c4 + 1) % 4, :], hS[:, hd, :], ["hS%d" % hd],
                             ["hSb%d_%d" % (hd, (c4 + 1) % 4)])
                    s_O2a(c4)
                for hd in range(4):
                    act(junk[:, 0:128], po[0][:, hd * 128:(hd + 1) * 128], AF.Square, ["po0"], ["hssq%d" % hd],
                        accum_out=st[:, 64 + hd:65 + hd])
                rstd_from(st[:, 64:68], st[:, 68:72], st[:, 72:76], 1.0 / 128, "hssq",
                          ["hssq%d" % hd for hd in range(4)])
                for hd in range(4):
                    stt(mixed[:, 1024 + hd * 128:1024 + (hd + 1) * 128], po[0][:, hd * 128:(hd + 1) * 128],
                        st[:, 72 + hd:73 + hd], G[:, 1024 + hd * 128:1024 + (hd + 1) * 128], ALU.mult, ALU.mult,
                        ["po0", "hssq_r", "G"], ["mixed"])

            def s_O2(t):
                xb = xt[t % 2]
                xn = "xt%d" % (t % 2)
                rows = slice(t * 128, (t + 1) * 128)
                for half in range(2):
                    pjn = "pj%d" % half
                    mm_multi([(pj[half][:], mT[:, e_, :], wout[:, e_, half * 512:(half + 1) * 512], False, e_ == 11,
                               {"skip_group_check": True}) for e_ in range(8, 12)],
                             ["mTc"] + WOUT, [pjn])
                    tt("dve", xb[:, half * 512:(half + 1) * 512], xb[:, half * 512:(half + 1) * 512], pj[half][:],
                       ALU.add, [xn, pjn], [xn])
                pjc[0] = 0
                if do_final:
                    act(outt[:], xb[:], AF.Square, [xn], ["outt", "fssq"], accum_out=st[:, 4:5])
                    rstd_from(st[:, 4:5], st[:, 5:6], st[:, 6:7], 1.0 / D, "fssq")
                    stt(outt[:], xb[:], st[:, 6:7], fnw[:], ALU.mult, ALU.mult, [xn, "fssq_r", "fnw"], ["outt"])
                    dma("sp", dst_d[rows, :], outt[:], ["outt"], ["xmid%d" % t], "d_o")
                else:
                    dma("sp", dst_d[rows, :], xb[:], [xn], ["xmid%d" % t], "d_x%d" % (t % 2))

            LOAD(0)
            HEAD(0)
            for t in range(NT):
                nxt = t + 1 < NT
                if nxt:
                    LOAD(t + 1)
                s_X1(); s_G1a(); s_G3b(); s_Hp(); s_X2(); s_G1b(); s_X3(); s_G2(); s_H1(); s_X4(); s_G3a(); s_X5()
                s_H2()
                if nxt:
                    HEAD_a(t + 1)
                s_GT(); s_G4(); s_X6(); s_H3a(); s_H3b(); s_G5a(); s_G5b()
                mixed_T(12, 16, "mTb")
                s_G5c(); s_H4()
                if nxt:
                    HEAD_b(t + 1)
                s_H5()
                mixed_T(0, 8, "mTa")
                s_H6()
                mixed_T(8, 12, "mTc")
                s_O2(t)

        P.emit(nc)
    return nc


def _consts():
    j = np.arange(128)[:, None]
    i = np.arange(128)[None, :]
    triG = (j <= i).astype(np.float32)
    triUG = (j > i).astype(np.float32)
    same = (j // 32) == (i // 32)
    triH = ((j <= i) & same).astype(np.float32)
    triUH = ((j > i) & same).astype(np.float32)
    cind = np.zeros((128, 8), np.float32)
    cind[:, 0] = 1.0
    for c in range(4):
        cind[:, 1 + c] = (np.arange(128) // 32 == c)
    cst = np.concatenate([triG, triUG, triH, triUH, cind, np.zeros((128, 6 * 128 + 8 - 520), np.float32)], axis=1)
    ident = np.eye(128, dtype=np.float32)
    maskG = triG
    maskH = triH
    msk = np.concatenate([ident, maskG, maskH], axis=1)
    return np.ascontiguousarray(cst), np.ascontiguousarray(msk)


def _layout_params(norm_w, gla_w_gate_up, gla_b_gate, gla_norm_w, hgrn_lower_bounds, hgrn_norm_w,
                   mem_norm_w, xattn_norm_w, final_norm_w):
    NL = norm_w.shape[0]
    pcols = np.zeros((NL, 128, 32), np.float32)
    for L in range(NL):
        pcols[L, :, 0:8] = norm_w[L].reshape(8, 128).T
        gw = np.concatenate([np.tile(gla_norm_w[L], 4), np.tile(hgrn_norm_w[L], 4), np.tile(xattn_norm_w[L], 4)])
        pcols[L, :, 8:24] = gw.reshape(16, 128).T
        pcols[L, :, 24:32] = mem_norm_w[L].reshape(8, 128).T
    wup = np.concatenate([gla_w_gate_up, gla_b_gate[:, None, :]], axis=1).astype(np.float32)
    lbraw = np.ascontiguousarray(np.broadcast_to(hgrn_lower_bounds.reshape(1, -1), (128, NL * 512))).astype(np.float32)
    fnw = np.ascontiguousarray(np.broadcast_to(final_norm_w.reshape(1, -1), (128, D))).astype(np.float32)
    return pcols, np.ascontiguousarray(wup), lbraw, fnw


_NC_CACHE = {}


def _get_nc(S, layers, final_norm):
    key = (S, tuple(layers), final_norm)
    if key not in _NC_CACHE:
        _NC_CACHE[key] = build(S, list(layers), final_norm)
    return _NC_CACHE[key]


def kernel(x, mem, norm_w, w_in, gla_w_gate_up, gla_b_gate, gla_norm_w, hgrn_lower_bounds,
           hgrn_norm_w, mem_norm_w, w_mem_kv, xattn_norm_w, w_out, final_norm_w):
    x = np.asarray(x, np.float32)
    mem = np.asarray(mem, np.float32)
    B, S, _ = x.shape
    f = lambda a: np.ascontiguousarray(np.asarray(a, np.float32))
    pcols, wup, lbraw, fnw = _layout_params(f(norm_w), f(gla_w_gate_up), f(gla_b_gate), f(gla_norm_w),
                                            f(hgrn_lower_bounds), f(hgrn_norm_w), f(mem_norm_w),
                                            f(xattn_norm_w), f(final_norm_w))
    cst, msk = _consts()
    nc = _get_nc(S, (0, 1), True)
    shared = {"w_in": f(w_in), "w_out": f(w_out), "w_kv": f(w_mem_kv), "wup": wup, "pcols": pcols,
              "lbraw": lbraw, "fnw": fnw, "cst": cst, "msk": msk}
    in_maps = []
    for b in range(B):
        m = dict(shared)
        m["x"] = np.ascontiguousarray(x[b])
        m["mem"] = np.ascontiguousarray(mem[b])
        in_maps.append(m)
    res = run_bass_kernel_spmd(nc, in_maps, core_ids=list(range(B)))
    return np.stack([np.asarray(r["out"], np.float32) for r in res.results], axis=0)
```

```python
import contextlib
import numpy as np
import ml_dtypes
import concourse.bass as bass
import concourse.mybir as mybir
from concourse.bass_utils import run_bass_kernel_spmd

F32 = mybir.dt.float32
BF16 = mybir.dt.bfloat16
AF = mybir.ActivationFunctionType
ALU = mybir.AluOpType
AX = mybir.AxisListType

D = 1024
DIN = 6160
DMIX = 2048
MEM = 256
EPS = 1e-6
C_GQ, C_GK, C_GV, C_GLR, C_HQ, C_HF, C_HI, C_XQ, C_GATE = 0, 512, 1024, 2048, 2064, 2576, 3088, 3600, 4112


class Prog:
    ENG = ("pe", "act", "dve", "pool", "sp")

    def __init__(self):
        self.q = {e: [] for e in self.ENG}
        self.cnt = {}
        self.res = {}
        self.waited = {e: {} for e in self.ENG}

    def op(self, eng, fn, reads=(), writes=(), dma=None):
        deps = {}

        def add(tok):
            if tok is None:
                return
            k, v = tok
            if deps.get(k, 0) < v:
                deps[k] = v

        for r in reads:
            st = self.res.get(r)
            if st:
                add(st[0])
        for w in writes:
            st = self.res.get(w)
            if st:
                add(st[0])
                for k, v in st[1].items():
                    add((k, v))
        if eng == "pe":
            deps.pop("pe", None)
        waits = []
        wd = self.waited[eng]
        for k, v in deps.items():
            if wd.get(k, 0) < v:
                wd[k] = v
                waits.append((k, v))
        key, amt = (dma, 16) if dma is not None else (eng, 1)
        self.cnt[key] = self.cnt.get(key, 0) + amt
        tok = (key, self.cnt[key])
        self.q[eng].append((waits, fn, key, amt))
        for r in reads:
            st = self.res.setdefault(r, [None, {}])
            if st[1].get(key, 0) < tok[1]:
                st[1][key] = tok[1]
        for w in writes:
            self.res[w] = [tok, {}]
        return tok

    def emit(self, nc):
        with contextlib.ExitStack() as es:
            sems = {k: es.enter_context(nc.semaphore("s_" + k)) for k in self.cnt}
            block = es.enter_context(nc.Block())
            final = [(k, v) for k, v in self.cnt.items()]

            def run(name, e):
                for waits, fn, key, amt in self.q[name]:
                    for k, v in waits:
                        e.wait_ge(sems[k], v)
                    ins = fn(e)
                    ins.then_inc(sems[key], amt)

            @block.tensor
            def _(e):
                run("pe", e)

            @block.scalar
            def _(e):
                run("act", e)

            @block.vector
            def _(e):
                run("dve", e)

            @block.gpsimd
            def _(e):
                run("pool", e)

            @block.sync
            def _(e):
                run("sp", e)
                for k, v in final:
                    e.wait_ge(sems[k], v)


def build(S, layers, final_norm, n_layers_total=2):
    nc = bass.Bass("TRN2", target_bir_lowering=False)
    P = Prog()
    NT = S // 128
    NL = n_layers_total

    def din(name, shape, dt=F32):
        return nc.dram_tensor(name, list(shape), dt, kind="ExternalInput").ap()

    x_d = din("x", [S, D])
    mem_d = din("mem", [MEM, D])
    win_d = din("w_in", [NL, D, DIN])
    wout_d = din("w_out", [NL, DMIX, D])
    wkv_d = din("w_kv", [NL, D, 2 * 512])
    wup_d = din("wup", [NL, 17, 512])
    pcols_d = din("pcols", [NL, 128, 32])
    lbraw_d = din("lbraw", [128, NL * 512])
    fnw_d = din("fnw", [128, D])
    cst_d = din("cst", [128, 6 * 128 + 8])
    msk_d = din("msk", [128, 3 * 128])
    out_d = nc.dram_tensor("out", [S, D], F32, kind="ExternalOutput").ap()
    xmid_d = None
    if len(layers) > 1:
        xmid_d = nc.dram_tensor("xmid", [S, D], F32, kind="Internal").ap()

    es = contextlib.ExitStack()
    with es:
        def sb(name, shape, dt):
            return es.enter_context(nc.sbuf_tensor("sb_" + name, list(shape), dt))

        def ps(name, shape, dt):
            return es.enter_context(nc.psum_tensor("ps_" + name, list(shape), dt))

        win = sb("win", [128, 8, DIN], BF16)
        wout = sb("wout", [128, 16, D], BF16)
        cst = sb("cst", [128, 4 * 128 + 8], F32)
        mskb = sb("mskb", [128, 3 * 128], BF16)
        pcols = sb("pcols", [128, 32], F32)
        wup = sb("wup", [32, 512], BF16)
        lbt = sb("lbt", [128, NL * 512], F32)
        fnw = sb("fnw", [128, D], F32)
        gS = sb("gS", [128, 4, 256], F32)
        gSb = sb("gSb", [128, 4, 256], BF16)
        hS = sb("hS", [128, 4, 128], F32)
        hSb = sb("hSb", [128, 4, 4, 128], BF16)
        mkT = sb("mkT", [128, 4, 256], BF16)
        mv = sb("mv", [128, 2, 512], BF16)
        xt = [sb("xt0", [128, D], F32), sb("xt1", [128, D], F32)]
        h = sb("h", [128, D], BF16)
        hT = sb("hT", [128, 8, 128], BF16)
        Ft = [sb("F%d" % i, [128, 512], F32) for i in range(6)]
        qb = sb("qb", [128, 512], BF16)
        kb = sb("kb", [128, 512], BF16)
        khb = sb("khb", [128, 512], BF16)
        qkT = sb("qkT", [128, 8, 128], BF16)
        v = sb("v", [128, 1024], BF16)
        hi = sb("hi", [128, 512], BF16)
        pbt = sb("pbt", [128, 4, 256], BF16)
        AT = sb("AT", [128, 4, 128], BF16)
        glrT = sb("glrT", [32, 128], BF16)
        G = sb("G", [128, DMIX], BF16)
        mixed = sb("mixed", [128, DMIX], BF16)
        outt = sb("outt", [128, D], F32)
        lbraw = outt
        xqs = qb
        xqT = AT
        pb = pbt
        HIM = ["pbt"]
        pT = qkT
        mT = G[:].rearrange("p (c m) -> p c m", c=16)
        st = sb("st", [128, 128], F32)
        junk = sb("junk", [128, 256], BF16)

        pj = [ps("pj0", [128, 512], F32), ps("pj1", [128, 512], F32)]
        pt = ps("pt", [128, 1024], BF16)
        pcu = [ps("pcu0", [128, 512], F32), ps("pcu1", [128, 512], F32)]
        pss = ps("pss", [128, 512], F32)
        po = [ps("po0", [128, 512], F32), ps("po1", [128, 512], F32)]

        ident = mskb[:, 0:128]
        maskG = mskb[:, 128:256]
        maskH = mskb[:, 256:384]
        triG = cst[:, 0:128]
        triUG = cst[:, 128:256]
        triH = cst[:, 256:384]
        triUH = cst[:, 384:512]
        cind = cst[:, 512:520]
        nwT = pcols[:, 0:8]
        gwT = pcols[:, 8:24]
        mnwT = pcols[:, 24:32]

        pjc = [0]

        def next_pj():
            i = pjc[0] % 2
            pjc[0] += 1
            return pj[i], "pj%d" % i

        def bc(ap2, n):
            return ap2.unsqueeze(2).broadcast_to([128, ap2.shape[1], n])

        def bc_mid(ap2, n):
            return ap2.unsqueeze(1).broadcast_to([128, n, ap2.shape[1]])

        def mm(out_ap, pairs, reads, writes, first_start=True, **kw):
            pairs = list(pairs)

            def fn(e):
                n = len(pairs)
                ins = None
                for i, (a, b) in enumerate(pairs):
                    ins = e.matmul(out_ap, a, b, start=(first_start and i == 0), stop=(i == n - 1), **kw)
                return ins
            P.op("pe", fn, reads, writes)

        def mm_multi(items, reads, writes):
            items = list(items)

            def fn(e):
                ins = None
                for (o, a, b, s0, s1, kw) in items:
                    ins = e.matmul(o, a, b, start=s0, stop=s1, **kw)
                return ins
            P.op("pe", fn, reads, writes)

        def transposes(items, reads, writes):
            items = list(items)

            def fn(e):
                ins = None
                for (o, i_) in items:
                    ins = e.transpose(o, i_, ident)
                return ins
            P.op("pe", fn, list(reads) + ["mskb"], writes)

        def act(out, in_, func, reads, writes, **kw):
            P.op("act", lambda e: e.activation(out, in_, func, **kw), reads, writes)

        def tt(eng, out, in0, in1, op, reads, writes):
            P.op(eng, lambda e: e.tensor_tensor(out, in0, in1, op), reads, writes)

        def ts(eng, out, in0, s1, s2, op0, op1, reads, writes):
            if s2 is None:
                P.op(eng, lambda e: e.tensor_scalar(out, in0, s1, None, op0), reads, writes)
            else:
                P.op(eng, lambda e: e.tensor_scalar(out, in0, s1, s2, op0, op1), reads, writes)

        def stt(out, in0, scalar, in1, op0, op1, reads, writes):
            P.op("dve", lambda e: e.scalar_tensor_tensor(out, in0, scalar, in1, op0, op1), reads, writes)

        def copy(eng, out, in_, reads, writes):
            if eng == "act":
                P.op("act", lambda e: e.activation(out, in_, AF.Identity), reads, writes)
            else:
                P.op(eng, lambda e: e.tensor_copy(out, in_), reads, writes)

        def dma(eng, out, in_, reads, writes, sem, **kw):
            P.op(eng, lambda e: e.dma_start(out=out, in_=in_, **kw), reads, writes, dma=sem)

        def rstd_from(ssq_col, tmp_col, out_col, inv_n, rname, reads=None):
            act(tmp_col, ssq_col, AF.Ln, reads or [rname], [rname + "_t"], scale=inv_n, bias=EPS)
            act(out_col, tmp_col, AF.Exp, [rname + "_t"], [rname + "_r"], scale=-0.5)

        dma("sp", cst[:], cst_d[:, 0:520], [], ["cst"], "d_cst")
        dma("pool", mskb[:], msk_d, [], ["mskb"], "d_msk")
        dma("sp", lbraw[:], lbraw_d, [], ["outt"], "d_lbraw")
        if final_norm:
            dma("sp", fnw[:], fnw_d, [], ["fnw"], "d_fnw")
        P.op("pool", lambda e: e.memset(glrT[:], 1.0), [], ["glrT"])
        lr3 = lbraw[:].rearrange("p (l n) -> p l n", l=NL)
        lb3 = lbt[:].rearrange("p (l n) -> p l n", l=NL)
        mx = Ft[0]
        P.op("dve", lambda e: e.tensor_copy(mx[:], lr3[:, 0, :]), ["outt"], ["F0"])
        for l in range(1, NL):
            tt("dve", mx[:], mx[:], lr3[:, l, :], ALU.max, ["F0", "outt"], ["F0"])
        for l in range(NL):
            tt("dve", lb3[:, l, :], lr3[:, l, :], mx[:], ALU.subtract, ["F0", "outt"], ["lbt"])
        act(lbt[:], lbt[:], AF.Exp, ["lbt"], ["lbt"])
        den = Ft[1]
        P.op("dve", lambda e: e.tensor_copy(den[:], lb3[:, 0, :]), ["lbt"], ["F1"])
        for l in range(1, NL):
            tt("dve", den[:], den[:], lb3[:, l, :], ALU.add, ["F1", "lbt"], ["F1"])
        P.op("dve", lambda e: e.reciprocal(den[:], den[:]), ["F1"], ["F1"])
        for l in range(NL):
            tt("dve", lb3[:, l, :], lb3[:, l, :], den[:], ALU.mult, ["F1", "lbt"], ["lbt"])
        p0 = Ft[2]
        P.op("dve", lambda e: e.tensor_copy(p0[:], lb3[:, 0, :]), ["lbt"], ["F2"])
        for l in range(1, NL):
            tt("dve", lb3[:, l, :], lb3[:, l, :], lb3[:, l - 1, :], ALU.add, ["lbt"], ["lbt"])
        for l in range(NL):
            tt("dve", lb3[:, l, :], lb3[:, l, :], p0[:], ALU.subtract, ["lbt", "F2"], ["lbt"])

        F = ["F%d" % i for i in range(6)]

        for li, L in enumerate(layers):
            last = (li == len(layers) - 1)
            src_d = x_d if li == 0 else xmid_d
            dst_d = out_d if last else xmid_d
            do_final = last and final_norm
            lbL = lbt[:, L * 512:(L + 1) * 512]

            dma("sp", pcols[:], pcols_d[L], [], ["pcols"], "d_pcols")
            dma("pool", wup[0:17, :], wup_d[L], [], ["wup"], "d_wup")
            wkv = wout[:, 0:8, :]
            for hf_ in range(2):
                dma("pool", wout[:, hf_ * 4:(hf_ + 1) * 4, :],
                    wkv_d[L, hf_ * 512:(hf_ + 1) * 512, :].rearrange("(c p) n -> p c n", p=128),
                    [], ["wq%d" % hf_], "d_wkv%d" % hf_, max_dma_last_dim=4096)
            for c in range(8):
                dma("pool", win[:, c, :], win_d[L, c * 128:(c + 1) * 128, :], [], ["win%d" % c], "d_win%d" % c,
                    max_dma_last_dim=4096)
            P.op("dve", lambda e: e.memset(gS[:], 0.0), [], ["gS"])
            P.op("dve", lambda e: e.memset(hS[:], 0.0), [], ["hS"])
            P.op("pool", lambda e: e.memset(gSb[:], 0.0), [], ["gSb"])
            P.op("pool", lambda e: e.memset(hSb[:], 0.0), [], ["hSb"])
            WIN = ["win%d" % c for c in range(8)]

            mnT = mixed[:].rearrange("p (c m) -> p c m", c=8)
            for blk in range(2):
                xb = xt[blk]
                xn = "xt%d" % blk
                dma("sp", xb[:], mem_d[blk * 128:(blk + 1) * 128, :], [], [xn], "d_x%d" % blk)
                act(h[:], xb[:], AF.Square, [xn], ["h", "ssq"], accum_out=st[:, 0:1])
                rstd_from(st[:, 0:1], st[:, 1:2], st[:, 2:3], 1.0 / D, "ssq")
                ts("dve", h[:], xb[:], st[:, 2:3], None, ALU.mult, None, [xn, "ssq_r"], ["h"])
                transposes([(pt[:, c * 128:(c + 1) * 128], h[:, c * 128:(c + 1) * 128]) for c in range(8)],
                           ["h"], ["pt"])
                tt("dve", mnT[:, :, blk * 128:(blk + 1) * 128], pt[:].rearrange("p (c m) -> p c m", c=8),
                   bc(mnwT, 128), ALU.mult, ["pt", "pcols"], ["mixed"])
            for hd in range(4):
                pjt, pjn = next_pj()
                mm(pjt[:, 0:256], [(wkv[:, c, hd * 128:(hd + 1) * 128], mnT[:, c, :]) for c in range(8)],
                   ["wq0", "wq1", "mixed"], [pjn])
                copy("act", mkT[:, hd, :], pjt[:, 0:256], [pjn], ["mkT"])
            for blk in range(2):
                pjt, pjn = next_pj()
                mm(pjt[:], [(mnT[:, c, blk * 128:(blk + 1) * 128], wkv[:, c, 512:1024]) for c in range(8)],
                   ["wq0", "wq1", "mixed"], [pjn])
                copy("act", mv[:, blk, :], pjt[:], [pjn], ["mv"])
            for q4 in range(4):
                dma("pool", wout[:, q4 * 4:(q4 + 1) * 4, :],
                    wout_d[L, q4 * 512:(q4 + 1) * 512, :].rearrange("(c p) n -> p c n", p=128),
                    [], ["wq%d" % q4], "d_wo%d" % q4, max_dma_last_dim=4096)
            WOUT = ["wq%d" % q4 for q4 in range(4)]

            def proj(col0, ncol, dst_names):
                pjt, pjn = next_pj()
                mm(pjt[:, 0:ncol], [(hT[:, c, :], win[:, c, col0:col0 + ncol]) for c in range(8)],
                   ["hT"] + WIN, [pjn])
                return pjt, pjn

            def LOAD(t):
                dma("sp", xt[t % 2][:], src_d[t * 128:(t + 1) * 128, :], ["xmid%d" % t] if li > 0 else [],
                    ["xt%d" % (t % 2)], "d_x%d" % (t % 2))

            def HEAD_a(t):
                xb = xt[t % 2]
                xn = "xt%d" % (t % 2)
                act(h[:], xb[:], AF.Square, [xn], ["h", "ssq"], accum_out=st[:, 0:1])
                rstd_from(st[:, 0:1], st[:, 1:2], st[:, 2:3], 1.0 / D, "ssq")
                ts("dve", h[:], xb[:], st[:, 2:3], None, ALU.mult, None, [xn, "ssq_r"], ["h"])

            def HEAD_b(t):
                transposes([(pt[:, c * 128:(c + 1) * 128], h[:, c * 128:(c + 1) * 128]) for c in range(8)],
                           ["h"], ["pt"])
                tt("dve", hT[:], pt[:].rearrange("p (c m) -> p c m", c=8), bc(nwT, 128), ALU.mult,
                   ["pt", "pcols"], ["hT"])

            def HEAD(t):
                HEAD_a(t)
                HEAD_b(t)

            def qk_transposes():
                transposes([(pt[:, hd * 128:(hd + 1) * 128], qb[:, hd * 128:(hd + 1) * 128]) for hd in range(4)] +
                           [(pt[:, (4 + hd) * 128:(5 + hd) * 128], kb[:, hd * 128:(hd + 1) * 128]) for hd in range(4)],
                           ["qb", "kb"], ["pt"])
                copy("dve", qkT[:], pt[:].rearrange("p (c m) -> p c m", c=8), ["pt"], ["qkT"])
                mm_multi([(pss[:, hd * 128:(hd + 1) * 128], qkT[:, 4 + hd, :], qkT[:, hd, :], True, True, {})
                          for hd in range(4)], ["qkT"], ["pss"])

            def mixed_T(e0, e1, rname):
                n = e1 - e0
                transposes([(pt[:, i_ * 128:(i_ + 1) * 128], mixed[:, (e0 + i_) * 128:(e0 + i_ + 1) * 128])
                            for i_ in range(n)], ["mixed"], ["pt"])
                tt("dve", mT[:, e0:e1, :], pt[:, 0:n * 128].rearrange("p (c m) -> p c m", c=n),
                   bc(gwT[:, e0:e1], 128), ALU.mult, ["pt", "pcols"], [rname])

            ctx = {}

            def s_GT():
                for g4 in range(4):
                    pjt, pjn = proj(C_GATE + g4 * 512, 512, None)
                    act(G[:, g4 * 512:(g4 + 1) * 512], pjt[:], AF.Silu, [pjn], ["G", "mTa", "mTb", "mTc"])

            def s_X1():
                pjt, pjn = proj(C_XQ, 512, None)
                act(xqs[:], pjt[:], AF.Identity, [pjn], ["qb"], scale=float(128 ** -0.5))

            def s_G1a():
                pjt, pjn = next_pj()
                mm(pjt[0:16, 0:128], [(win[:, c, C_GLR:C_GLR + 16], hT[:, c, :]) for c in range(8)],
                   ["hT"] + WIN, [pjn])
                copy("act", glrT[0:16, :], pjt[0:16, 0:128], [pjn], ["glrT"])

            def s_X2():
                transposes([(pt[:, hd * 128:(hd + 1) * 128], xqs[:, hd * 128:(hd + 1) * 128]) for hd in range(4)],
                           ["qb"], ["pt"])
                copy("dve", xqT[:], pt[:, 0:512].rearrange("p (c m) -> p c m", c=4), ["pt"], ["AT"])

            def s_G1b():
                pjt, pjn = next_pj()
                mm(pjt[:], [(glrT[0:17, :], wup[0:17, :])], ["glrT", "wup"], [pjn])
                act(Ft[0][:], pjt[:], AF.Exp, [pjn], [F[0]], scale=-1.0)
                act(Ft[1][:], Ft[0][:], AF.Ln, [F[0]], [F[1]], bias=1.0)

            def s_X3():
                mm_multi([(po[hd // 2][:, (hd % 2) * 256:(hd % 2 + 1) * 256], xqT[:, hd, :], mkT[:, hd, :],
                           True, True, {}) for hd in range(4)], ["AT", "mkT"], ["po0", "po1"])
                for half in range(2):
                    P.op("dve", (lambda half: lambda e: e.tensor_reduce(
                        st[:, 48 + 2 * half:50 + 2 * half], po[half][:].rearrange("p (a b) -> p a b", a=2),
                        AX.X, ALU.max))(half), ["po%d" % half], ["xmax%d" % half])
                ts("dve", st[:, 52:56], st[:, 48:52], -1.0, None, ALU.mult, None, ["xmax0", "xmax1"], ["xnmax"])
                for hd in range(4):
                    act(pb[:, hd, :], po[hd // 2][:, (hd % 2) * 256:(hd % 2 + 1) * 256], AF.Exp,
                        ["po%d" % (hd // 2), "xnmax"], HIM + ["xZ%d" % hd], bias=st[:, 52 + hd:53 + hd],
                        accum_out=st[:, 56 + hd:57 + hd])

            def s_Hp():
                pjz, pjzn = proj(C_HF, 512, None)
                act(Ft[5][:], pjz[:], AF.Exp, [pjzn], [F[5]], scale=-1.0)

            def s_G2():
                mm(pcu[0][:], [(triG, Ft[1][:])], ["cst", F[1]], ["pcu0"])
                mm(pcu[1][:], [(triUG, Ft[1][:])], ["cst", F[1]], ["pcu1"])
                pjt, pjn = next_pj()
                mm_multi([(pjt[:, hd:hd + 1], Ft[1][:, hd * 128:(hd + 1) * 128], cind[:, 0:1], True, True, {})
                          for hd in range(4)], ["cst", F[1]], [pjn])
                act(st[:, 8:12], pjt[:, 0:4], AF.Exp, [pjn], ["gdec"], scale=-1.0 / 16)
                act(Ft[2][:], pcu[0][:], AF.Exp, ["pcu0"], [F[2]], scale=-1.0 / 16)
                act(Ft[3][:], pcu[0][:], AF.Exp, ["pcu0"], [F[3]], scale=1.0 / 16)
                act(Ft[4][:], pcu[1][:], AF.Exp, ["pcu1"], [F[4]], scale=-1.0 / 16)

            def s_H1():
                act(Ft[1][:], Ft[5][:], AF.Ln, [F[5]], [F[1]], bias=1.0)

            def s_X4():
                transposes([(pt[:, (hd * 2 + mc) * 128:(hd * 2 + mc + 1) * 128], pb[:, hd, mc * 128:(mc + 1) * 128])
                            for hd in range(4) for mc in range(2)], HIM, ["pt"])
                copy("dve", pT[:], pt[:].rearrange("p (c m) -> p c m", c=8), ["pt"], ["qkT"])

            def s_G3a():
                pjt, pjn = proj(C_GQ, 512, None)
                stt(qb[:], pjt[:], float(128 ** -0.5), Ft[2][:], ALU.mult, ALU.mult, [pjn, F[2]], ["qb"])
                pjt, pjn = proj(C_GK, 512, None)
                tt("dve", kb[:], pjt[:], Ft[3][:], ALU.mult, [pjn, F[3]], ["kb"])
                tt("dve", khb[:], pjt[:], Ft[4][:], ALU.mult, [pjn, F[4]], ["khb"])

            def s_X5():
                items = []
                for hd in range(4):
                    for mc in range(2):
                        items.append((po[1][:, hd * 128:(hd + 1) * 128], pT[:, hd * 2 + mc, :],
                                      mv[:, mc, hd * 128:(hd + 1) * 128], mc == 0, mc == 1, {}))
                mm_multi(items, ["qkT", "mv"], ["po1"])
                for hd in range(4):
                    act(junk[:, 0:128], po[1][:, hd * 128:(hd + 1) * 128], AF.Square, ["po1"], ["xssq%d" % hd],
                        accum_out=st[:, 80 + hd:81 + hd])

            def s_G3b():
                for half in range(2):
                    pjt, pjn = proj(C_GV + half * 512, 512, None)
                    copy("act", v[:, half * 512:(half + 1) * 512], pjt[:], [pjn], ["v"])

            def s_H2():
                tt("dve", Ft[0][:], Ft[5][:], lbL, ALU.mult, [F[5], "lbt"], [F[0]])
                act(Ft[0][:], Ft[0][:], AF.Ln, [F[0]], [F[0]], bias=1.0)
                tt("dve", Ft[5][:], Ft[0][:], Ft[1][:], ALU.subtract, [F[0], F[1]], [F[5]])

            def s_X6():
                tt("dve", st[:, 60:64], st[:, 56:60], st[:, 56:60], ALU.mult, ["xZ%d" % hd for hd in range(4)], ["xz2"])
                ts("dve", st[:, 84:88], st[:, 80:84], 1.0 / 128, None, ALU.mult, None,
                   ["xssq%d" % hd for hd in range(4)], ["xvv"])
                stt(st[:, 84:88], st[:, 60:64], EPS, st[:, 84:88], ALU.mult, ALU.add, ["xz2", "xvv"], ["xvv"])
                act(st[:, 84:88], st[:, 84:88], AF.Ln, ["xvv"], ["xvv"])
                act(st[:, 88:92], st[:, 84:88], AF.Exp, ["xvv"], ["xr"], scale=-0.5)
                for hd in range(4):
                    stt(mixed[:, 1536 + hd * 128:1536 + (hd + 1) * 128], po[1][:, hd * 128:(hd + 1) * 128],
                        st[:, 88 + hd:89 + hd], G[:, 1536 + hd * 128:1536 + (hd + 1) * 128], ALU.mult, ALU.mult,
                        ["po1", "xr", "G"], ["mixed"])

            def s_G4():
                qk_transposes()
                tt("dve", AT[:], pss[:].rearrange("p (c m) -> p c m", c=4), bc_mid(maskG, 4), ALU.mult,
                   ["pss", "mskb"], ["AT"])

            def s_H3a():
                act(Ft[0][:], Ft[5][:], AF.Exp, [F[5]], [F[0]])
                ts("pool", Ft[0][:], Ft[0][:], -1.0, 1.0, ALU.mult, ALU.add, [F[0]], [F[0]])

            def s_H3b():
                pjr, pjrn = next_pj()
                mm(pss[:], [(triH, Ft[5][:])], ["cst", F[5]], ["pss"])
                mm(pjr[:], [(triUH, Ft[5][:])], ["cst", F[5]], [pjrn])
                pjt, pjn = next_pj()
                mm_multi([(pjt[:, hd * 4:hd * 4 + 4], Ft[5][:, hd * 128:(hd + 1) * 128], cind[:, 1:5], True, True, {})
                          for hd in range(4)], ["cst", F[5]], [pjn])
                act(Ft[1][:], pss[:], AF.Exp, ["pss"], [F[1]])
                act(Ft[2][:], pss[:], AF.Exp, ["pss"], [F[2]], scale=-1.0)
                act(Ft[3][:], pjr[:], AF.Exp, [pjrn], [F[3]])
                act(st[:, 32:48], pjt[:, 0:16], AF.Exp, [pjn], ["hdec"])

            def s_G5a():
                items = []
                for hd in range(4):
                    o_ap = po[hd // 2][:, (hd % 2) * 256:(hd % 2 + 1) * 256]
                    items.append((o_ap, AT[:, hd, :], v[:, hd * 256:(hd + 1) * 256], True, False, {}))
                    items.append((o_ap, qkT[:, hd, :], gSb[:, hd, :], False, True, {}))
                mm_multi(items, ["AT", "v", "qkT", "gSb"], ["po0", "po1"])
                for hd in range(4):
                    o_ap = po[hd // 2][:, (hd % 2) * 256:(hd % 2 + 1) * 256]
                    act(junk[:, 0:256], o_ap, AF.Square, ["po%d" % (hd // 2)], ["gssq%d" % hd],
                        accum_out=st[:, 16 + hd:17 + hd])
                rstd_from(st[:, 16:20], st[:, 20:24], st[:, 24:28], 1.0 / 256, "gssq",
                          ["gssq%d" % hd for hd in range(4)])

            def s_G5b():
                mm_multi([(pcu[hd // 2][:, (hd % 2) * 256:(hd % 2 + 1) * 256], khb[:, hd * 128:(hd + 1) * 128],
                           v[:, hd * 256:(hd + 1) * 256], True, True, {}) for hd in range(4)],
                         ["khb", "v"], ["pcu0", "pcu1"])
                for hd in range(4):
                    stt(gS[:, hd, :], gS[:, hd, :], st[:, 8 + hd:9 + hd],
                        pcu[hd // 2][:, (hd % 2) * 256:(hd % 2 + 1) * 256], ALU.mult, ALU.add,
                        ["gS", "gdec", "pcu%d" % (hd // 2)], ["gS"])
                copy("act", gSb[:].rearrange("p a b -> p (a b)"), gS[:].rearrange("p a b -> p (a b)"),
                     ["gS"], ["gSb"])

            def s_G5c():
                for hd in range(4):
                    o_ap = po[hd // 2][:, (hd % 2) * 256:(hd % 2 + 1) * 256]
                    stt(mixed[:, hd * 256:(hd + 1) * 256], o_ap, st[:, 24 + hd:25 + hd],
                        G[:, hd * 256:(hd + 1) * 256], ALU.mult, ALU.mult,
                        ["po%d" % (hd // 2), "gssq_r", "G"], ["mixed"])

            def s_H4():
                pjt, pjn = proj(C_HQ, 512, None)
                tt("dve", qb[:], pjt[:], Ft[1][:], ALU.mult, [pjn, F[1]], ["qb"])
                tt("pool", kb[:], Ft[0][:], Ft[2][:], ALU.mult, [F[0], F[2]], ["kb"])
                tt("pool", khb[:], Ft[0][:], Ft[3][:], ALU.mult, [F[0], F[3]], ["khb"])
                pjt, pjn = proj(C_HI, 512, None)
                copy("act", hi[:], pjt[:], [pjn], ["hi"])

            ubank = [(pcu[0], "pcu0"), (pcu[1], "pcu1"), (pss, "pss"), (po[1], "po1")]

            def s_H5():
                qk_transposes()
                tt("dve", AT[:], pss[:].rearrange("p (c m) -> p c m", c=4), bc_mid(maskH, 4), ALU.mult,
                   ["pss", "mskb"], ["AT"])
                mm_multi([(po[0][:, hd * 128:(hd + 1) * 128], AT[:, hd, :], hi[:, hd * 128:(hd + 1) * 128],
                           hd == 0, False, {"skip_group_check": True}) for hd in range(4)],
                         ["AT", "hi"], ["po0"])
                for c4 in range(4):
                    pu, pun = ubank[c4]
                    mm_multi([(pu[:, hd * 128:(hd + 1) * 128], khb[32 * c4:32 * (c4 + 1), hd * 128:(hd + 1) * 128],
                               hi[32 * c4:32 * (c4 + 1), hd * 128:(hd + 1) * 128], True, True,
                               {"tile_position": (32 * c4, 0)}) for hd in range(4)],
                             ["khb", "hi"], [pun])

            def s_O2a(part):
                elist = [0, 1, 2, 3, 4, 5, 6, 7, 12, 13, 14, 15]
                es_ = elist[part * 3:(part + 1) * 3]
                items = []
                for half in range(2):
                    for e_ in es_:
                        items.append((pj[half][:], mT[:, e_, :], wout[:, e_, half * 512:(half + 1) * 512],
                                      e_ == 0, False, {"skip_group_check": True}))
                mm_multi(items, ["mTa", "mTb"] + WOUT, ["pj0", "pj1"])

            def s_H6():
                for c4 in range(4):
                    pu, pun = ubank[c4]
                    for hd in range(4):
                        sbn = "hSb%d_%d" % (hd, c4)
                        mm_multi([(po[0][32 * c4:32 * (c4 + 1), hd * 128:(hd + 1) * 128],
                                   qkT[:, hd, 32 * c4:32 * (c4 + 1)], hSb[:, hd, c4, :], False, c4 == 3,
                                   {"skip_group_check": True, "tile_position": (0, 32 * c4)})],
                                 ["qkT", sbn, "hSb"], ["po0"])
                        stt(hS[:, hd, :], hS[:, hd, :], st[:, 32 + hd * 4 + c4:33 + hd * 4 + c4],
                            pu[:, hd * 128:(hd + 1) * 128], ALU.mult, ALU.add,
                            ["hS%d" % hd, "hS", "hdec", pun], ["hS%d" % hd])
                        copy("act", hSb[:, hd, (c4 + 1) % 4, :], hS[:, hd, :], ["hS%d" % hd],
                             ["hSb%d_%d" % (hd, (c4 + 1) % 4)])
                    s_O2a(c4)
                for hd in range(4):
                    act(junk[:, 0:128], po[0][:, hd * 128:(hd + 1) * 128], AF.Square, ["po0"], ["hssq%d" % hd],
                        accum_out=st[:, 64 + hd:65 + hd])
                rstd_from(st[:, 64:68], st[:, 68:72], st[:, 72:76], 1.0 / 128, "hssq",
                          ["hssq%d" % hd for hd in range(4)])
                for hd in range(4):
                    stt(mixed[:, 1024 + hd * 128:1024 + (hd + 1) * 128], po[0][:, hd * 128:(hd + 1) * 128],
                        st[:, 72 + hd:73 + hd], G[:, 1024 + hd * 128:1024 + (hd + 1) * 128], ALU.mult, ALU.mult,
                        ["po0", "hssq_r", "G"], ["mixed"])

            def s_O2(t):
                xb = xt[t % 2]
                xn = "xt%d" % (t % 2)
                rows = slice(t * 128, (t + 1) * 128)
                for half in range(2):
                    pjn = "pj%d" % half
                    mm_multi([(pj[half][:], mT[:, e_, :], wout[:, e_, half * 512:(half + 1) * 512], False, e_ == 11,
                               {"skip_group_check": True}) for e_ in range(8, 12)],
                             ["mTc"] + WOUT, [pjn])
                    tt("dve", xb[:, half * 512:(half + 1) * 512], xb[:, half * 512:(half + 1) * 512], pj[half][:],
                       ALU.add, [xn, pjn], [xn])
                pjc[0] = 0
                if do_final:
                    act(outt[:], xb[:], AF.Square, [xn], ["outt", "fssq"], accum_out=st[:, 4:5])
                    rstd_from(st[:, 4:5], st[:, 5:6], st[:, 6:7], 1.0 / D, "fssq")
                    stt(outt[:], xb[:], st[:, 6:7], fnw[:], ALU.mult, ALU.mult, [xn, "fssq_r", "fnw"], ["outt"])
                    dma("sp", dst_d[rows, :], outt[:], ["outt"], ["xmid%d" % t], "d_o")
                else:
                    dma("sp", dst_d[rows, :], xb[:], [xn], ["xmid%d" % t], "d_x%d" % (t % 2))

            LOAD(0)
            HEAD(0)
            for t in range(NT):
                nxt = t + 1 < NT
                if nxt:
                    LOAD(t + 1)
                s_X1(); s_G1a(); s_G3b(); s_Hp(); s_X2(); s_G1b(); s_X3(); s_G2(); s_H1(); s_X4(); s_G3a(); s_X5()
                s_H2()
                if nxt:
                    HEAD_a(t + 1)
                s_GT(); s_G4(); s_X6(); s_H3a(); s_H3b(); s_G5a(); s_G5b()
                mixed_T(12, 16, "mTb")
                s_G5c(); s_H4()
                if nxt:
                    HEAD_b(t + 1)
                s_H5()
                mixed_T(0, 8, "mTa")
                s_H6()
                mixed_T(8, 12, "mTc")
                s_O2(t)

        P.emit(nc)
    return nc


def _consts():
    j = np.arange(128)[:, None]
    i = np.arange(128)[None, :]
    triG = (j <= i).astype(np.float32)
    triUG = (j > i).astype(np.float32)
    same = (j // 32) == (i // 32)
    triH = ((j <= i) & same).astype(np.float32)
    triUH = ((j > i) & same).astype(np.float32)
    cind = np.zeros((128, 8), np.float32)
    cind[:, 0] = 1.0
    for c in range(4):
        cind[:, 1 + c] = (np.arange(128) // 32 == c)
    cst = np.concatenate([triG, triUG, triH, triUH, cind, np.zeros((128, 6 * 128 + 8 - 520), np.float32)], axis=1)
    ident = np.eye(128, dtype=np.float32)
    maskG = triG
    maskH = triH
    msk = np.concatenate([ident, maskG, maskH], axis=1)
    return np.ascontiguousarray(cst), np.ascontiguousarray(msk)


def _layout_params(norm_w, gla_w_gate_up, gla_b_gate, gla_norm_w, hgrn_lower_bounds, hgrn_norm_w,
                   mem_norm_w, xattn_norm_w, final_norm_w):
    NL = norm_w.shape[0]
    pcols = np.zeros((NL, 128, 32), np.float32)
    for L in range(NL):
        pcols[L, :, 0:8] = norm_w[L].reshape(8, 128).T
        gw = np.concatenate([np.tile(gla_norm_w[L], 4), np.tile(hgrn_norm_w[L], 4), np.tile(xattn_norm_w[L], 4)])
        pcols[L, :, 8:24] = gw.reshape(16, 128).T
        pcols[L, :, 24:32] = mem_norm_w[L].reshape(8, 128).T
    wup = np.concatenate([gla_w_gate_up, gla_b_gate[:, None, :]], axis=1).astype(np.float32)
    lbraw = np.ascontiguousarray(np.broadcast_to(hgrn_lower_bounds.reshape(1, -1), (128, NL * 512))).astype(np.float32)
    fnw = np.ascontiguousarray(np.broadcast_to(final_norm_w.reshape(1, -1), (128, D))).astype(np.float32)
    return pcols, np.ascontiguousarray(wup), lbraw, fnw


_NC_CACHE = {}


def _get_nc(S, layers, final_norm):
    key = (S, tuple(layers), final_norm)
    if key not in _NC_CACHE:
        _NC_CACHE[key] = build(S, list(layers), final_norm)
    return _NC_CACHE[key]


def kernel(x, mem, norm_w, w_in, gla_w_gate_up, gla_b_gate, gla_norm_w, hgrn_lower_bounds,
           hgrn_norm_w, mem_norm_w, w_mem_kv, xattn_norm_w, w_out, final_norm_w):
    x = np.asarray(x, np.float32)
    mem = np.asarray(mem, np.float32)
    B, S, _ = x.shape
    f = lambda a: np.ascontiguousarray(np.asarray(a, np.float32))
    pcols, wup, lbraw, fnw = _layout_params(f(norm_w), f(gla_w_gate_up), f(gla_b_gate), f(gla_norm_w),
                                            f(hgrn_lower_bounds), f(hgrn_norm_w), f(mem_norm_w),
                                            f(xattn_norm_w), f(final_norm_w))
    cst, msk = _consts()
    nc = _get_nc(S, (0, 1), True)
    shared = {"w_in": f(w_in), "w_out": f(w_out), "w_kv": f(w_mem_kv), "wup": wup, "pcols": pcols,
              "lbraw": lbraw, "fnw": fnw, "cst": cst, "msk": msk}
    in_maps = []
    for b in range(B):
        m = dict(shared)
        m["x"] = np.ascontiguousarray(x[b])
        m["mem"] = np.ascontiguousarray(mem[b])
        in_maps.append(m)
    res = run_bass_kernel_spmd(nc, in_maps, core_ids=list(range(B)))
    return np.stack([np.asarray(r["out"], np.float32) for r in res.results], axis=0)
```

```python
import contextlib
import numpy as np
import ml_dtypes
import concourse.bass as bass
import concourse.mybir as mybir
from concourse.bass_utils import run_bass_kernel_spmd

F32 = mybir.dt.float32
BF16 = mybir.dt.bfloat16
AF = mybir.ActivationFunctionType
ALU = mybir.AluOpType
AX = mybir.AxisListType

D = 1024
DIN = 6160
DMIX = 2048
MEM = 256
EPS = 1e-6
C_GQ, C_GK, C_GV, C_GLR, C_HQ, C_HF, C_HI, C_XQ, C_GATE = 0, 512, 1024, 2048, 2064, 2576, 3088, 3600, 4112


class Prog:
    ENG = ("pe", "act", "dve", "pool", "sp")

    def __init__(self):
        self.q = {e: [] for e in self.ENG}
        self.cnt = {}
        self.res = {}
        self.waited = {e: {} for e in self.ENG}

    def op(self, eng, fn, reads=(), writes=(), dma=None):
        deps = {}

        def add(tok):
            if tok is None:
                return
            k, v = tok
            if deps.get(k, 0) < v:
                deps[k] = v

        for r in reads:
            st = self.res.get(r)
            if st:
                add(st[0])
        for w in writes:
            st = self.res.get(w)
            if st:
                add(st[0])
                for k, v in st[1].items():
                    add((k, v))
        if eng == "pe":
            deps.pop("pe", None)
        waits = []
        wd = self.waited[eng]
        for k, v in deps.items():
            if wd.get(k, 0) < v:
                wd[k] = v
                waits.append((k, v))
        key, amt = (dma, 16) if dma is not None else (eng, 1)
        self.cnt[key] = self.cnt.get(key, 0) + amt
        tok = (key, self.cnt[key])
        self.q[eng].append((waits, fn, key, amt))
        for r in reads:
            st = self.res.setdefault(r, [None, {}])
            if st[1].get(key, 0) < tok[1]:
                st[1][key] = tok[1]
        for w in writes:
            self.res[w] = [tok, {}]
        return tok

    def emit(self, nc):
        with contextlib.ExitStack() as es:
            sems = {k: es.enter_context(nc.semaphore("s_" + k)) for k in self.cnt}
            block = es.enter_context(nc.Block())
            final = [(k, v) for k, v in self.cnt.items()]

            def run(name, e):
                for waits, fn, key, amt in self.q[name]:
                    for k, v in waits:
                        e.wait_ge(sems[k], v)
                    ins = fn(e)
                    ins.then_inc(sems[key], amt)

            @block.tensor
            def _(e):
                run("pe", e)

            @block.scalar
            def _(e):
                run("act", e)

            @block.vector
            def _(e):
                run("dve", e)

            @block.gpsimd
            def _(e):
                run("pool", e)

            @block.sync
            def _(e):
                run("sp", e)
                for k, v in final:
                    e.wait_ge(sems[k], v)


def build(S, layers, final_norm, n_layers_total=2):
    nc = bass.Bass("TRN2", target_bir_lowering=False)
    P = Prog()
    NT = S // 128
    NL = n_layers_total

    def din(name, shape, dt=F32):
        return nc.dram_tensor(name, list(shape), dt, kind="ExternalInput").ap()

    x_d = din("x", [S, D])
    mem_d = din("mem", [MEM, D])
    win_d = din("w_in", [NL, D, DIN])
    wout_d = din("w_out", [NL, DMIX, D])
    wkv_d = din("w_kv", [NL, D, 2 * 512])
    wup_d = din("wup", [NL, 17, 512])
    pcols_d = din("pcols", [NL, 128, 32])
    lbraw_d = din("lbraw", [128, NL * 512])
    fnw_d = din("fnw", [128, D])
    cst_d = din("cst", [128, 6 * 128 + 8])
    msk_d = din("msk", [128, 3 * 128])
    out_d = nc.dram_tensor("out", [S, D], F32, kind="ExternalOutput").ap()
    xmid_d = None
    if len(layers) > 1:
        xmid_d = nc.dram_tensor("xmid", [S, D], F32, kind="Internal").ap()

    es = contextlib.ExitStack()
    with es:
        def sb(name, shape, dt):
            return es.enter_context(nc.sbuf_tensor("sb_" + name, list(shape), dt))

        def ps(name, shape, dt):
            return es.enter_context(nc.psum_tensor("ps_" + name, list(shape), dt))

        win = sb("win", [128, 8, DIN], BF16)
        wout = sb("wout", [128, 16, D], BF16)
        cst = sb("cst", [128, 4 * 128 + 8], F32)
        mskb = sb("mskb", [128, 3 * 128], BF16)
        pcols = sb("pcols", [128, 32], F32)
        wup = sb("wup", [32, 512], BF16)
        lbt = sb("lbt", [128, NL * 512], F32)
        fnw = sb("fnw", [128, D], F32)
        gS = sb("gS", [128, 4, 256], F32)
        gSb = sb("gSb", [128, 4, 256], BF16)
        hS = sb("hS", [128, 4, 128], F32)
        hSb = sb("hSb", [128, 4, 4, 128], BF16)
        mkT = sb("mkT", [128, 4, 256], BF16)
        mv = sb("mv", [128, 2, 512], BF16)
        xt = [sb("xt0", [128, D], F32), sb("xt1", [128, D], F32)]
        h = sb("h", [128, D], BF16)
        hT = sb("hT", [128, 8, 128], BF16)
        Ft = [sb("F%d" % i, [128, 512], F32) for i in range(6)]
        qb = sb("qb", [128, 512], BF16)
        kb = sb("kb", [128, 512], BF16)
        khb = sb("khb", [128, 512], BF16)
        qkT = sb("qkT", [128, 8, 128], BF16)
        v = sb("v", [128, 1024], BF16)
        hi = sb("hi", [128, 512], BF16)
        pbt = sb("pbt", [128, 4, 256], BF16)
        AT = sb("AT", [128, 4, 128], BF16)
        glrT = sb("glrT", [32, 128], BF16)
        G = sb("G", [128, DMIX], BF16)
        mixed = sb("mixed", [128, DMIX], BF16)
        outt = sb("outt", [128, D], F32)
        lbraw = outt
        xqs = qb
        xqT = AT
        pb = pbt
        HIM = ["pbt"]
        pT = qkT
        mT = G[:].rearrange("p (c m) -> p c m", c=16)
        st = sb("st", [128, 128], F32)
        junk = sb("junk", [128, 256], BF16)

        pj = [ps("pj0", [128, 512], F32), ps("pj1", [128, 512], F32)]
        pt = ps("pt", [128, 1024], BF16)
        pcu = [ps("pcu0", [128, 512], F32), ps("pcu1", [128, 512], F32)]
        pss = ps("pss", [128, 512], F32)
        po = [ps("po0", [128, 512], F32), ps("po1", [128, 512], F32)]

        ident = mskb[:, 0:128]
        maskG = mskb[:, 128:256]
        maskH = mskb[:, 256:384]
        triG = cst[:, 0:128]
        triUG = cst[:, 128:256]
        triH = cst[:, 256:384]
        triUH = cst[:, 384:512]
        cind = cst[:, 512:520]
        nwT = pcols[:, 0:8]
        gwT = pcols[:, 8:24]
        mnwT = pcols[:, 24:32]

        pjc = [0]

        def next_pj():
            i = pjc[0] % 2
            pjc[0] += 1
            return pj[i], "pj%d" % i

        def bc(ap2, n):
            return ap2.unsqueeze(2).broadcast_to([128, ap2.shape[1], n])

        def bc_mid(ap2, n):
            return ap2.unsqueeze(1).broadcast_to([128, n, ap2.shape[1]])

        def mm(out_ap, pairs, reads, writes, first_start=True, **kw):
            pairs = list(pairs)

            def fn(e):
                n = len(pairs)
                ins = None
                for i, (a, b) in enumerate(pairs):
                    ins = e.matmul(out_ap, a, b, start=(first_start and i == 0), stop=(i == n - 1), **kw)
                return ins
            P.op("pe", fn, reads, writes)

        def mm_multi(items, reads, writes):
            items = list(items)

            def fn(e):
                ins = None
                for (o, a, b, s0, s1, kw) in items:
                    ins = e.matmul(o, a, b, start=s0, stop=s1, **kw)
                return ins
            P.op("pe", fn, reads, writes)

        def transposes(items, reads, writes):
            items = list(items)

            def fn(e):
                ins = None
                for (o, i_) in items:
                    ins = e.transpose(o, i_, ident)
                return ins
            P.op("pe", fn, list(reads) + ["mskb"], writes)

        def act(out, in_, func, reads, writes, **kw):
            P.op("act", lambda e: e.activation(out, in_, func, **kw), reads, writes)

        def tt(eng, out, in0, in1, op, reads, writes):
            P.op(eng, lambda e: e.tensor_tensor(out, in0, in1, op), reads, writes)

        def ts(eng, out, in0, s1, s2, op0, op1, reads, writes):
            if s2 is None:
                P.op(eng, lambda e: e.tensor_scalar(out, in0, s1, None, op0), reads, writes)
            else:
                P.op(eng, lambda e: e.tensor_scalar(out, in0, s1, s2, op0, op1), reads, writes)

        def stt(out, in0, scalar, in1, op0, op1, reads, writes):
            P.op("dve", lambda e: e.scalar_tensor_tensor(out, in0, scalar, in1, op0, op1), reads, writes)

        def copy(eng, out, in_, reads, writes):
            if eng == "act":
                P.op("act", lambda e: e.activation(out, in_, AF.Identity), reads, writes)
            else:
                P.op(eng, lambda e: e.tensor_copy(out, in_), reads, writes)

        def dma(eng, out, in_, reads, writes, sem, **kw):
            P.op(eng, lambda e: e.dma_start(out=out, in_=in_, **kw), reads, writes, dma=sem)

        def rstd_from(ssq_col, tmp_col, out_col, inv_n, rname, reads=None):
            act(tmp_col, ssq_col, AF.Ln, reads or [rname], [rname + "_t"], scale=inv_n, bias=EPS)
            act(out_col, tmp_col, AF.Exp, [rname + "_t"], [rname + "_r"], scale=-0.5)

        dma("sp", cst[:], cst_d[:, 0:520], [], ["cst"], "d_cst")
        dma("pool", mskb[:], msk_d, [], ["mskb"], "d_msk")
        dma("sp", lbraw[:], lbraw_d, [], ["outt"], "d_lbraw")
        if final_norm:
            dma("sp", fnw[:], fnw_d, [], ["fnw"], "d_fnw")
        P.op("pool", lambda e: e.memset(glrT[:], 1.0), [], ["glrT"])
        lr3 = lbraw[:].rearrange("p (l n) -> p l n", l=NL)
        lb3 = lbt[:].rearrange("p (l n) -> p l n", l=NL)
        mx = Ft[0]
        P.op("dve", lambda e: e.tensor_copy(mx[:], lr3[:, 0, :]), ["outt"], ["F0"])
        for l in range(1, NL):
            tt("dve", mx[:], mx[:], lr3[:, l, :], ALU.max, ["F0", "outt"], ["F0"])
        for l in range(NL):
            tt("dve", lb3[:, l, :], lr3[:, l, :], mx[:], ALU.subtract, ["F0", "outt"], ["lbt"])
        act(lbt[:], lbt[:], AF.Exp, ["lbt"], ["lbt"])
        den = Ft[1]
        P.op("dve", lambda e: e.tensor_copy(den[:], lb3[:, 0, :]), ["lbt"], ["F1"])
        for l in range(1, NL):
            tt("dve", den[:], den[:], lb3[:, l, :], ALU.add, ["F1", "lbt"], ["F1"])
        P.op("dve", lambda e: e.reciprocal(den[:], den[:]), ["F1"], ["F1"])
        for l in range(NL):
            tt("dve", lb3[:, l, :], lb3[:, l, :], den[:], ALU.mult, ["F1", "lbt"], ["lbt"])
        p0 = Ft[2]
        P.op("dve", lambda e: e.tensor_copy(p0[:], lb3[:, 0, :]), ["lbt"], ["F2"])
        for l in range(1, NL):
            tt("dve", lb3[:, l, :], lb3[:, l, :], lb3[:, l - 1, :], ALU.add, ["lbt"], ["lbt"])
        for l in range(NL):
            tt("dve", lb3[:, l, :], lb3[:, l, :], p0[:], ALU.subtract, ["lbt", "F2"], ["lbt"])

        F = ["F%d" % i for i in range(6)]

        for li, L in enumerate(layers):
            last = (li == len(layers) - 1)
            src_d = x_d if li == 0 else xmid_d
            dst_d = out_d if last else xmid_d
            do_final = last and final_norm
            lbL = lbt[:, L * 512:(L + 1) * 512]

            dma("sp", pcols[:], pcols_d[L], [], ["pcols"], "d_pcols")
            dma("pool", wup[0:17, :], wup_d[L], [], ["wup"], "d_wup")
            wkv = wout[:, 0:8, :]
            for hf_ in range(2):
                dma("pool", wout[:, hf_ * 4:(hf_ + 1) * 4, :],
                    wkv_d[L, hf_ * 512:(hf_ + 1) * 512, :].rearrange("(c p) n -> p c n", p=128),
                    [], ["wq%d" % hf_], "d_wkv%d" % hf_, max_dma_last_dim=4096)
            for c in range(8):
                dma("pool", win[:, c, :], win_d[L, c * 128:(c + 1) * 128, :], [], ["win%d" % c], "d_win%d" % c,
                    max_dma_last_dim=4096)
            P.op("dve", lambda e: e.memset(gS[:], 0.0), [], ["gS"])
            P.op("dve", lambda e: e.memset(hS[:], 0.0), [], ["hS"])
            P.op("pool", lambda e: e.memset(gSb[:], 0.0), [], ["gSb"])
            P.op("pool", lambda e: e.memset(hSb[:], 0.0), [], ["hSb"])
            WIN = ["win%d" % c for c in range(8)]

            mnT = mixed[:].rearrange("p (c m) -> p c m", c=8)
            for blk in range(2):
                xb = xt[blk]
                xn = "xt%d" % blk
                dma("sp", xb[:], mem_d[blk * 128:(blk + 1) * 128, :], [], [xn], "d_x%d" % blk)
                act(h[:], xb[:], AF.Square, [xn], ["h", "ssq"], accum_out=st[:, 0:1])
                rstd_from(st[:, 0:1], st[:, 1:2], st[:, 2:3], 1.0 / D, "ssq")
                ts("dve", h[:], xb[:], st[:, 2:3], None, ALU.mult, None, [xn, "ssq_r"], ["h"])
                transposes([(pt[:, c * 128:(c + 1) * 128], h[:, c * 128:(c + 1) * 128]) for c in range(8)],
                           ["h"], ["pt"])
                tt("dve", mnT[:, :, blk * 128:(blk + 1) * 128], pt[:].rearrange("p (c m) -> p c m", c=8),
                   bc(mnwT, 128), ALU.mult, ["pt", "pcols"], ["mixed"])
            for hd in range(4):
                pjt, pjn = next_pj()
                mm(pjt[:, 0:256], [(wkv[:, c, hd * 128:(hd + 1) * 128], mnT[:, c, :]) for c in range(8)],
                   ["wq0", "wq1", "mixed"], [pjn])
                copy("act", mkT[:, hd, :], pjt[:, 0:256], [pjn], ["mkT"])
            for blk in range(2):
                pjt, pjn = next_pj()
                mm(pjt[:], [(mnT[:, c, blk * 128:(blk + 1) * 128], wkv[:, c, 512:1024]) for c in range(8)],
                   ["wq0", "wq1", "mixed"], [pjn])
                copy("act", mv[:, blk, :], pjt[:], [pjn], ["mv"])
            for q4 in range(4):
                dma("pool", wout[:, q4 * 4:(q4 + 1) * 4, :],
                    wout_d[L, q4 * 512:(q4 + 1) * 512, :].rearrange("(c p) n -> p c n", p=128),
                    [], ["wq%d" % q4], "d_wo%d" % q4, max_dma_last_dim=4096)
            WOUT = ["wq%d" % q4 for q4 in range(4)]

            def proj(col0, ncol, dst_names):
                pjt, pjn = next_pj()
                mm(pjt[:, 0:ncol], [(hT[:, c, :], win[:, c, col0:col0 + ncol]) for c in range(8)],
                   ["hT"] + WIN, [pjn])
                return pjt, pjn

            def LOAD(t):
                dma("sp", xt[t % 2][:], src_d[t * 128:(t + 1) * 128, :], ["xmid%d" % t] if li > 0 else [],
                    ["xt%d" % (t % 2)], "d_x%d" % (t % 2))

            def HEAD_a(t):
                xb = xt[t % 2]
                xn = "xt%d" % (t % 2)
                act(h[:], xb[:], AF.Square, [xn], ["h", "ssq"], accum_out=st[:, 0:1])
                rstd_from(st[:, 0:1], st[:, 1:2], st[:, 2:3], 1.0 / D, "ssq")
                ts("dve", h[:], xb[:], st[:, 2:3], None, ALU.mult, None, [xn, "ssq_r"], ["h"])

            def HEAD_b(t):
                transposes([(pt[:, c * 128:(c + 1) * 128], h[:, c * 128:(c + 1) * 128]) for c in range(8)],
                           ["h"], ["pt"])
                tt("dve", hT[:], pt[:].rearrange("p (c m) -> p c m", c=8), bc(nwT, 128), ALU.mult,
                   ["pt", "pcols"], ["hT"])

            def HEAD(t):
                HEAD_a(t)
                HEAD_b(t)

            def qk_transposes():
                transposes([(pt[:, hd * 128:(hd + 1) * 128], qb[:, hd * 128:(hd + 1) * 128]) for hd in range(4)] +
                           [(pt[:, (4 + hd) * 128:(5 + hd) * 128], kb[:, hd * 128:(hd + 1) * 128]) for hd in range(4)],
                           ["qb", "kb"], ["pt"])
                copy("dve", qkT[:], pt[:].rearrange("p (c m) -> p c m", c=8), ["pt"], ["qkT"])
                mm_multi([(pss[:, hd * 128:(hd + 1) * 128], qkT[:, 4 + hd, :], qkT[:, hd, :], True, True, {})
                          for hd in range(4)], ["qkT"], ["pss"])

            def mixed_T(e0, e1, rname):
                n = e1 - e0
                transposes([(pt[:, i_ * 128:(i_ + 1) * 128], mixed[:, (e0 + i_) * 128:(e0 + i_ + 1) * 128])
                            for i_ in range(n)], ["mixed"], ["pt"])
                tt("dve", mT[:, e0:e1, :], pt[:, 0:n * 128].rearrange("p (c m) -> p c m", c=n),
                   bc(gwT[:, e0:e1], 128), ALU.mult, ["pt", "pcols"], [rname])

            ctx = {}

            def s_GT():
                for g4 in range(4):
                    pjt, pjn = proj(C_GATE + g4 * 512, 512, None)
                    act(G[:, g4 * 512:(g4 + 1) * 512], pjt[:], AF.Silu, [pjn], ["G", "mTa", "mTb", "mTc"])

            def s_X1():
                pjt, pjn = proj(C_XQ, 512, None)
                act(xqs[:], pjt[:], AF.Identity, [pjn], ["qb"], scale=float(128 ** -0.5))

            def s_G1a():
                pjt, pjn = next_pj()
                mm(pjt[0:16, 0:128], [(win[:, c, C_GLR:C_GLR + 16], hT[:, c, :]) for c in range(8)],
                   ["hT"] + WIN, [pjn])
                copy("act", glrT[0:16, :], pjt[0:16, 0:128], [pjn], ["glrT"])

            def s_X2():
                transposes([(pt[:, hd * 128:(hd + 1) * 128], xqs[:, hd * 128:(hd + 1) * 128]) for hd in range(4)],
                           ["qb"], ["pt"])
                copy("dve", xqT[:], pt[:, 0:512].rearrange("p (c m) -> p c m", c=4), ["pt"], ["AT"])

            def s_G1b():
                pjt, pjn = next_pj()
                mm(pjt[:], [(glrT[0:17, :], wup[0:17, :])], ["glrT", "wup"], [pjn])
                act(Ft[0][:], pjt[:], AF.Exp, [pjn], [F[0]], scale=-1.0)
                act(Ft[1][:], Ft[0][:], AF.Ln, [F[0]], [F[1]], bias=1.0)

            def s_X3():
                mm_multi([(po[hd // 2][:, (hd % 2) * 256:(hd % 2 + 1) * 256], xqT[:, hd, :], mkT[:, hd, :],
                           True, True, {}) for hd in range(4)], ["AT", "mkT"], ["po0", "po1"])
                for half in range(2):
                    P.op("dve", (lambda half: lambda e: e.tensor_reduce(
                        st[:, 48 + 2 * half:50 + 2 * half], po[half][:].rearrange("p (a b) -> p a b", a=2),
                        AX.X, ALU.max))(half), ["po%d" % half], ["xmax%d" % half])
                ts("dve", st[:, 52:56], st[:, 48:52], -1.0, None, ALU.mult, None, ["xmax0", "xmax1"], ["xnmax"])
                for hd in range(4):
                    act(pb[:, hd, :], po[hd // 2][:, (hd % 2) * 256:(hd % 2 + 1) * 256], AF.Exp,
                        ["po%d" % (hd // 2), "xnmax"], HIM + ["xZ%d" % hd], bias=st[:, 52 + hd:53 + hd],
                        accum_out=st[:, 56 + hd:57 + hd])

            def s_Hp():
                pjz, pjzn = proj(C_HF, 512, None)
                act(Ft[5][:], pjz[:], AF.Exp, [pjzn], [F[5]], scale=-1.0)

            def s_G2():
                mm(pcu[0][:], [(triG, Ft[1][:])], ["cst", F[1]], ["pcu0"])
                mm(pcu[1][:], [(triUG, Ft[1][:])], ["cst", F[1]], ["pcu1"])
                pjt, pjn = next_pj()
                mm_multi([(pjt[:, hd:hd + 1], Ft[1][:, hd * 128:(hd + 1) * 128], cind[:, 0:1], True, True, {})
                          for hd in range(4)], ["cst", F[1]], [pjn])
                act(st[:, 8:12], pjt[:, 0:4], AF.Exp, [pjn], ["gdec"], scale=-1.0 / 16)
                act(Ft[2][:], pcu[0][:], AF.Exp, ["pcu0"], [F[2]], scale=-1.0 / 16)
                act(Ft[3][:], pcu[0][:], AF.Exp, ["pcu0"], [F[3]], scale=1.0 / 16)
                act(Ft[4][:], pcu[1][:], AF.Exp, ["pcu1"], [F[4]], scale=-1.0 / 16)

            def s_H1():
                act(Ft[1][:], Ft[5][:], AF.Ln, [F[5]], [F[1]], bias=1.0)

            def s_X4():
                transposes([(pt[:, (hd * 2 + mc) * 128:(hd * 2 + mc + 1) * 128], pb[:, hd, mc * 128:(mc + 1) * 128])
                            for hd in range(4) for mc in range(2)], HIM, ["pt"])
                copy("dve", pT[:], pt[:].rearrange("p (c m) -> p c m", c=8), ["pt"], ["qkT"])

            def s_G3a():
                pjt, pjn = proj(C_GQ, 512, None)
                stt(qb[:], pjt[:], float(128 ** -0.5), Ft[2][:], ALU.mult, ALU.mult, [pjn, F[2]], ["qb"])
                pjt, pjn = proj(C_GK, 512, None)
                tt("dve", kb[:], pjt[:], Ft[3][:], ALU.mult, [pjn, F[3]], ["kb"])
                tt("dve", khb[:], pjt[:], Ft[4][:], ALU.mult, [pjn, F[4]], ["khb"])

            def s_X5():
                items = []
                for hd in range(4):
                    for mc in range(2):
                        items.append((po[1][:, hd * 128:(hd + 1) * 128], pT[:, hd * 2 + mc, :],
                                      mv[:, mc, hd * 128:(hd + 1) * 128], mc == 0, mc == 1, {}))
                mm_multi(items, ["qkT", "mv"], ["po1"])
                for hd in range(4):
                    act(junk[:, 0:128], po[1][:, hd * 128:(hd + 1) * 128], AF.Square, ["po1"], ["xssq%d" % hd],
                        accum_out=st[:, 80 + hd:81 + hd])

            def s_G3b():
                for half in range(2):
                    pjt, pjn = proj(C_GV + half * 512, 512, None)
                    copy("act", v[:, half * 512:(half + 1) * 512], pjt[:], [pjn], ["v"])

            def s_H2():
                tt("dve", Ft[0][:], Ft[5][:], lbL, ALU.mult, [F[5], "lbt"], [F[0]])
                act(Ft[0][:], Ft[0][:], AF.Ln, [F[0]], [F[0]], bias=1.0)
                tt("dve", Ft[5][:], Ft[0][:], Ft[1][:], ALU.subtract, [F[0], F[1]], [F[5]])

            def s_X6():
                tt("dve", st[:, 60:64], st[:, 56:60], st[:, 56:60], ALU.mult, ["xZ%d" % hd for hd in range(4)], ["xz2"])
                ts("dve", st[:, 84:88], st[:, 80:84], 1.0 / 128, None, ALU.mult, None,
                   ["xssq%d" % hd for hd in range(4)], ["xvv"])
                stt(st[:, 84:88], st[:, 60:64], EPS, st[:, 84:88], ALU.mult, ALU.add, ["xz2", "xvv"], ["xvv"])
                act(st[:, 84:88], st[:, 84:88], AF.Ln, ["xvv"], ["xvv"])
                act(st[:, 88:92], st[:, 84:88], AF.Exp, ["xvv"], ["xr"], scale=-0.5)
                for hd in range(4):
                    stt(mixed[:, 1536 + hd * 128:1536 + (hd + 1) * 128], po[1][:, hd * 128:(hd + 1) * 128],
                        st[:, 88 + hd:89 + hd], G[:, 1536 + hd * 128:1536 + (hd + 1) * 128], ALU.mult, ALU.mult,
                        ["po1", "xr", "G"], ["mixed"])

            def s_G4():
                qk_transposes()
                tt("dve", AT[:], pss[:].rearrange("p (c m) -> p c m", c=4), bc_mid(maskG, 4), ALU.mult,
                   ["pss", "mskb"], ["AT"])

            def s_H3a():
                act(Ft[0][:], Ft[5][:], AF.Exp, [F[5]], [F[0]])
                ts("pool", Ft[0][:], Ft[0][:], -1.0, 1.0, ALU.mult, ALU.add, [F[0]], [F[0]])

            def s_H3b():
                pjr, pjrn = next_pj()
                mm(pss[:], [(triH, Ft[5][:])], ["cst", F[5]], ["pss"])
                mm(pjr[:], [(triUH, Ft[5][:])], ["cst", F[5]], [pjrn])
                pjt, pjn = next_pj()
                mm_multi([(pjt[:, hd * 4:hd * 4 + 4], Ft[5][:, hd * 128:(hd + 1) * 128], cind[:, 1:5], True, True, {})
                          for hd in range(4)], ["cst", F[5]], [pjn])
                act(Ft[1][:], pss[:], AF.Exp, ["pss"], [F[1]])
                act(Ft[2][:], pss[:], AF.Exp, ["pss"], [F[2]], scale=-1.0)
                act(Ft[3][:], pjr[:], AF.Exp, [pjrn], [F[3]])
                act(st[:, 32:48], pjt[:, 0:16], AF.Exp, [pjn], ["hdec"])

            def s_G5a():
                items = []
                for hd in range(4):
                    o_ap = po[hd // 2][:, (hd % 2) * 256:(hd % 2 + 1) * 256]
                    items.append((o_ap, AT[:, hd, :], v[:, hd * 256:(hd + 1) * 256], True, False, {}))
                    items.append((o_ap, qkT[:, hd, :], gSb[:, hd, :], False, True, {}))
                mm_multi(items, ["AT", "v", "qkT", "gSb"], ["po0", "po1"])
                for hd in range(4):
                    o_ap = po[hd // 2][:, (hd % 2) * 256:(hd % 2 + 1) * 256]
                    act(junk[:, 0:256], o_ap, AF.Square, ["po%d" % (hd // 2)], ["gssq%d" % hd],
                        accum_out=st[:, 16 + hd:17 + hd])
                rstd_from(st[:, 16:20], st[:, 20:24], st[:, 24:28], 1.0 / 256, "gssq",
                          ["gssq%d" % hd for hd in range(4)])

            def s_G5b():
                mm_multi([(pcu[hd // 2][:, (hd % 2) * 256:(hd % 2 + 1) * 256], khb[:, hd * 128:(hd + 1) * 128],
                           v[:, hd * 256:(hd + 1) * 256], True, True, {}) for hd in range(4)],
                         ["khb", "v"], ["pcu0", "pcu1"])
                for hd in range(4):
                    stt(gS[:, hd, :], gS[:, hd, :], st[:, 8 + hd:9 + hd],
                        pcu[hd // 2][:, (hd % 2) * 256:(hd % 2 + 1) * 256], ALU.mult, ALU.add,
                        ["gS", "gdec", "pcu%d" % (hd // 2)], ["gS"])
                copy("act", gSb[:].rearrange("p a b -> p (a b)"), gS[:].rearrange("p a b -> p (a b)"),
                     ["gS"], ["gSb"])

            def s_G5c():
                for hd in range(4):
                    o_ap = po[hd // 2][:, (hd % 2) * 256:(hd % 2 + 1) * 256]
                    stt(mixed[:, hd * 256:(hd + 1) * 256], o_ap, st[:, 24 + hd:25 + hd],
                        G[:, hd * 256:(hd + 1) * 256], ALU.mult, ALU.mult,
                        ["po%d" % (hd // 2), "gssq_r", "G"], ["mixed"])

            def s_H4():
                pjt, pjn = proj(C_HQ, 512, None)
                tt("dve", qb[:], pjt[:], Ft[1][:], ALU.mult, [pjn, F[1]], ["qb"])
                tt("pool", kb[:], Ft[0][:], Ft[2][:], ALU.mult, [F[0], F[2]], ["kb"])
                tt("pool", khb[:], Ft[0][:], Ft[3][:], ALU.mult, [F[0], F[3]], ["khb"])
                pjt, pjn = proj(C_HI, 512, None)
                copy("act", hi[:], pjt[:], [pjn], ["hi"])

            ubank = [(pcu[0], "pcu0"), (pcu[1], "pcu1"), (pss, "pss"), (po[1], "po1")]

            def s_H5():
                qk_transposes()
                tt("dve", AT[:], pss[:].rearrange("p (c m) -> p c m", c=4), bc_mid(maskH, 4), ALU.mult,
                   ["pss", "mskb"], ["AT"])
                mm_multi([(po[0][:, hd * 128:(hd + 1) * 128], AT[:, hd, :], hi[:, hd * 128:(hd + 1) * 128],
                           hd == 0, False, {"skip_group_check": True}) for hd in range(4)],
                         ["AT", "hi"], ["po0"])
                for c4 in range(4):
                    pu, pun = ubank[c4]
                    mm_multi([(pu[:, hd * 128:(hd + 1) * 128], khb[32 * c4:32 * (c4 + 1), hd * 128:(hd + 1) * 128],
                               hi[32 * c4:32 * (c4 + 1), hd * 128:(hd + 1) * 128], True, True,
                               {"tile_position": (32 * c4, 0)}) for hd in range(4)],
                             ["khb", "hi"], [pun])

            def s_O2a(part):
                elist = [0, 1, 2, 3, 4, 5, 6, 7, 12, 13, 14, 15]
                es_ = elist[part * 3:(part + 1) * 3]
                items = []
                for half in range(2):
                    for e_ in es_:
                        items.append((pj[half][:], mT[:, e_, :], wout[:, e_, half * 512:(half + 1) * 512],
                                      e_ == 0, False, {"skip_group_check": True}))
                mm_multi(items, ["mTa", "mTb"] + WOUT, ["pj0", "pj1"])

            def s_H6():
                for c4 in range(4):
                    pu, pun = ubank[c4]
                    for hd in range(4):
                        sbn = "hSb%d_%d" % (hd, c4)
                        mm_multi([(po[0][32 * c4:32 * (c4 + 1), hd * 128:(hd + 1) * 128],
                                   qkT[:, hd, 32 * c4:32 * (c4 + 1)], hSb[:, hd, c4, :], False, c4 == 3,
                                   {"skip_group_check": True, "tile_position": (0, 32 * c4)})],
                                 ["qkT", sbn, "hSb"], ["po0"])
                        stt(hS[:, hd, :], hS[:, hd, :], st[:, 32 + hd * 4 + c4:33 + hd * 4 + c4],
                            pu[:, hd * 128:(hd + 1) * 128], ALU.mult, ALU.add,
                            ["hS%d" % hd, "hS", "hdec", pun], ["hS%d" % hd])
                        copy("act", hSb[:, hd, (c4 + 1) % 4, :], hS[:, hd, :], ["hS%d" % hd],
                             ["hSb%d_%d" % (hd, (c4 + 1) % 4)])
                    s_O2a(c4)
                for hd in range(4):
                    act(junk[:, 0:128], po[0][:, hd * 128:(hd + 1) * 128], AF.Square, ["po0"], ["hssq%d" % hd],
                        accum_out=st[:, 64 + hd:65 + hd])
                rstd_from(st[:, 64:68], st[:, 68:72], st[:, 72:76], 1.0 / 128, "hssq",
                          ["hssq%d" % hd for hd in range(4)])
                for hd in range(4):
                    stt(mixed[:, 1024 + hd * 128:1024 + (hd + 1) * 128], po[0][:, hd * 128:(hd + 1) * 128],
                        st[:, 72 + hd:73 + hd], G[:, 1024 + hd * 128:1024 + (hd + 1) * 128], ALU.mult, ALU.mult,
                        ["po0", "hssq_r", "G"], ["mixed"])

            def s_O2(t):
                xb = xt[t % 2]
                xn = "xt%d" % (t % 2)
                rows = slice(t * 128, (t + 1) * 128)
                for half in range(2):
                    pjn = "pj%d" % half
                    mm_multi([(pj[half][:], mT[:, e_, :], wout[:, e_, half * 512:(half + 1) * 512], False, e_ == 11,
                               {"skip_group_check": True}) for e_ in range(8, 12)],
                             ["mTc"] + WOUT, [pjn])
                    tt("dve", xb[:, half * 512:(half + 1) * 512], xb[:, half * 512:(half + 1) * 512], pj[half][:],
                       ALU.add, [xn, pjn], [xn])
                pjc[0] = 0
                if do_final:
                    act(outt[:], xb[:], AF.Square, [xn], ["outt", "fssq"], accum_out=st[:, 4:5])
                    rstd_from(st[:, 4:5], st[:, 5:6], st[:, 6:7], 1.0 / D, "fssq")
                    stt(outt[:], xb[:], st[:, 6:7], fnw[:], ALU.mult, ALU.mult, [xn, "fssq_r", "fnw"], ["outt"])
                    dma("sp", dst_d[rows, :], outt[:], ["outt"], ["xmid%d" % t], "d_o")
                else:
                    dma("sp", dst_d[rows, :], xb[:], [xn], ["xmid%d" % t], "d_x%d" % (t % 2))

            LOAD(0)
            HEAD(0)
            for t in range(NT):
                nxt = t + 1 < NT
                if nxt:
                    LOAD(t + 1)
                s_X1(); s_G1a(); s_G3b(); s_Hp(); s_X2(); s_G1b(); s_X3(); s_G2(); s_H1(); s_X4(); s_G3a(); s_X5()
                s_H2()
                s_GT(); s_G4()
                if nxt:
                    HEAD_a(t + 1)
                s_X6(); s_H3a(); s_H3b(); s_G5a(); s_G5b()
                mixed_T(12, 16, "mTb")
                s_G5c(); s_H4()
                if nxt:
                    HEAD_b(t + 1)
                s_H5()
                mixed_T(0, 8, "mTa")
                s_H6()
                mixed_T(8, 12, "mTc")
                s_O2(t)

        P.emit(nc)
    return nc


def _consts():
    j = np.arange(128)[:, None]
    i = np.arange(128)[None, :]
    triG = (j <= i).astype(np.float32)
    triUG = (j > i).astype(np.float32)
    same = (j // 32) == (i // 32)
    triH = ((j <= i) & same).astype(np.float32)
    triUH = ((j > i) & same).astype(np.float32)
    cind = np.zeros((128, 8), np.float32)
    cind[:, 0] = 1.0
    for c in range(4):
        cind[:, 1 + c] = (np.arange(128) // 32 == c)
    cst = np.concatenate([triG, triUG, triH, triUH, cind, np.zeros((128, 6 * 128 + 8 - 520), np.float32)], axis=1)
    ident = np.eye(128, dtype=np.float32)
    maskG = triG
    maskH = triH
    msk = np.concatenate([ident, maskG, maskH], axis=1)
    return np.ascontiguousarray(cst), np.ascontiguousarray(msk)


def _layout_params(norm_w, gla_w_gate_up, gla_b_gate, gla_norm_w, hgrn_lower_bounds, hgrn_norm_w,
                   mem_norm_w, xattn_norm_w, final_norm_w):
    NL = norm_w.shape[0]
    pcols = np.zeros((NL, 128, 32), np.float32)
    for L in range(NL):
        pcols[L, :, 0:8] = norm_w[L].reshape(8, 128).T
        gw = np.concatenate([np.tile(gla_norm_w[L], 4), np.tile(hgrn_norm_w[L], 4), np.tile(xattn_norm_w[L], 4)])
        pcols[L, :, 8:24] = gw.reshape(16, 128).T
        pcols[L, :, 24:32] = mem_norm_w[L].reshape(8, 128).T
    wup = np.concatenate([gla_w_gate_up, gla_b_gate[:, None, :]], axis=1).astype(np.float32)
    lbraw = np.ascontiguousarray(np.broadcast_to(hgrn_lower_bounds.reshape(1, -1), (128, NL * 512))).astype(np.float32)
    fnw = np.ascontiguousarray(np.broadcast_to(final_norm_w.reshape(1, -1), (128, D))).astype(np.float32)
    return pcols, np.ascontiguousarray(wup), lbraw, fnw


_NC_CACHE = {}


def _get_nc(S, layers, final_norm):
    key = (S, tuple(layers), final_norm)
    if key not in _NC_CACHE:
        _NC_CACHE[key] = build(S, list(layers), final_norm)
    return _NC_CACHE[key]


def kernel(x, mem, norm_w, w_in, gla_w_gate_up, gla_b_gate, gla_norm_w, hgrn_lower_bounds,
           hgrn_norm_w, mem_norm_w, w_mem_kv, xattn_norm_w, w_out, final_norm_w):
    x = np.asarray(x, np.float32)
    mem = np.asarray(mem, np.float32)
    B, S, _ = x.shape
    f = lambda a: np.ascontiguousarray(np.asarray(a, np.float32))
    pcols, wup, lbraw, fnw = _layout_params(f(norm_w), f(gla_w_gate_up), f(gla_b_gate), f(gla_norm_w),
                                            f(hgrn_lower_bounds), f(hgrn_norm_w), f(mem_norm_w),
                                            f(xattn_norm_w), f(final_norm_w))
    cst, msk = _consts()
    nc = _get_nc(S, (0, 1), True)
    shared = {"w_in": f(w_in), "w_out": f(w_out), "w_kv": f(w_mem_kv), "wup": wup, "pcols": pcols,
              "lbraw": lbraw, "fnw": fnw, "cst": cst, "msk": msk}
    in_maps = []
    for b in range(B):
        m = dict(shared)
        m["x"] = np.ascontiguousarray(x[b])
        m["mem"] = np.ascontiguousarray(mem[b])
        in_maps.append(m)
    res = run_bass_kernel_spmd(nc, in_maps, core_ids=list(range(B)))
    return np.stack([np.asarray(r["out"], np.float32) for r in res.results], axis=0)
```

```python
import contextlib
import numpy as np
import ml_dtypes
import concourse.bass as bass
import concourse.mybir as mybir
from concourse.bass_utils import run_bass_kernel_spmd

F32 = mybir.dt.float32
BF16 = mybir.dt.bfloat16
AF = mybir.ActivationFunctionType
ALU = mybir.AluOpType
AX = mybir.AxisListType

D = 1024
DIN = 6160
DMIX = 2048
MEM = 256
EPS = 1e-6
C_GQ, C_GK, C_GV, C_GLR, C_HQ, C_HF, C_HI, C_XQ, C_GATE = 0, 512, 1024, 2048, 2064, 2576, 3088, 3600, 4112


class Prog:
    ENG = ("pe", "act", "dve", "pool", "sp")

    def __init__(self):
        self.q = {e: [] for e in self.ENG}
        self.cnt = {}
        self.res = {}
        self.waited = {e: {} for e in self.ENG}

    def op(self, eng, fn, reads=(), writes=(), dma=None):
        deps = {}

        def add(tok):
            if tok is None:
                return
            k, v = tok
            if deps.get(k, 0) < v:
                deps[k] = v

        for r in reads:
            st = self.res.get(r)
            if st:
                add(st[0])
        for w in writes:
            st = self.res.get(w)
            if st:
                add(st[0])
                for k, v in st[1].items():
                    add((k, v))
        if eng == "pe":
            deps.pop("pe", None)
        waits = []
        wd = self.waited[eng]
        for k, v in deps.items():
            if wd.get(k, 0) < v:
                wd[k] = v
                waits.append((k, v))
        key, amt = (dma, 16) if dma is not None else (eng, 1)
        self.cnt[key] = self.cnt.get(key, 0) + amt
        tok = (key, self.cnt[key])
        self.q[eng].append((waits, fn, key, amt))
        for r in reads:
            st = self.res.setdefault(r, [None, {}])
            if st[1].get(key, 0) < tok[1]:
                st[1][key] = tok[1]
        for w in writes:
            self.res[w] = [tok, {}]
        return tok

    def emit(self, nc):
        with contextlib.ExitStack() as es:
            sems = {k: es.enter_context(nc.semaphore("s_" + k)) for k in self.cnt}
            block = es.enter_context(nc.Block())
            final = [(k, v) for k, v in self.cnt.items()]

            def run(name, e):
                for waits, fn, key, amt in self.q[name]:
                    for k, v in waits:
                        e.wait_ge(sems[k], v)
                    ins = fn(e)
                    ins.then_inc(sems[key], amt)

            @block.tensor
            def _(e):
                run("pe", e)

            @block.scalar
            def _(e):
                run("act", e)

            @block.vector
            def _(e):
                run("dve", e)

            @block.gpsimd
            def _(e):
                run("pool", e)

            @block.sync
            def _(e):
                run("sp", e)
                for k, v in final:
                    e.wait_ge(sems[k], v)


def build(S, layers, final_norm, n_layers_total=2):
    nc = bass.Bass("TRN2", target_bir_lowering=False)
    P = Prog()
    NT = S // 128
    NL = n_layers_total

    def din(name, shape, dt=F32):
        return nc.dram_tensor(name, list(shape), dt, kind="ExternalInput").ap()

    x_d = din("x", [S, D])
    mem_d = din("mem", [MEM, D])
    win_d = din("w_in", [NL, D, DIN])
    wout_d = din("w_out", [NL, DMIX, D])
    wkv_d = din("w_kv", [NL, D, 2 * 512])
    wup_d = din("wup", [NL, 17, 512])
    pcols_d = din("pcols", [NL, 128, 32])
    lbraw_d = din("lbraw", [128, NL * 512])
    fnw_d = din("fnw", [128, D])
    cst_d = din("cst", [128, 6 * 128 + 8])
    msk_d = din("msk", [128, 3 * 128])
    out_d = nc.dram_tensor("out", [S, D], F32, kind="ExternalOutput").ap()
    xmid_d = None
    if len(layers) > 1:
        xmid_d = nc.dram_tensor("xmid", [S, D], F32, kind="Internal").ap()

    es = contextlib.ExitStack()
    with es:
        def sb(name, shape, dt):
            return es.enter_context(nc.sbuf_tensor("sb_" + name, list(shape), dt))

        def ps(name, shape, dt):
            return es.enter_context(nc.psum_tensor("ps_" + name, list(shape), dt))

        win = sb("win", [128, 8, DIN], BF16)
        wout = sb("wout", [128, 16, D], BF16)
        cst = sb("cst", [128, 4 * 128 + 8], F32)
        mskb = sb("mskb", [128, 3 * 128], BF16)
        pcols = sb("pcols", [128, 32], F32)
        wup = sb("wup", [32, 512], BF16)
        lbt = sb("lbt", [128, NL * 512], F32)
        fnw = sb("fnw", [128, D], F32)
        gS = sb("gS", [128, 4, 256], F32)
        gSb = sb("gSb", [128, 4, 256], BF16)
        hS = sb("hS", [128, 4, 128], F32)
        hSb = sb("hSb", [128, 4, 4, 128], BF16)
        mkT = sb("mkT", [128, 4, 256], BF16)
        mv = sb("mv", [128, 2, 512], BF16)
        xt = [sb("xt0", [128, D], F32), sb("xt1", [128, D], F32)]
        h = sb("h", [128, D], BF16)
        hT = sb("hT", [128, 8, 128], BF16)
        Ft = [sb("F%d" % i, [128, 512], F32) for i in range(6)]
        qb = sb("qb", [128, 512], BF16)
        kb = sb("kb", [128, 512], BF16)
        khb = sb("khb", [128, 512], BF16)
        qkT = sb("qkT", [128, 8, 128], BF16)
        v = sb("v", [128, 1024], BF16)
        hi = sb("hi", [128, 512], BF16)
        pbt = sb("pbt", [128, 4, 256], BF16)
        AT = sb("AT", [128, 4, 128], BF16)
        glrT = sb("glrT", [32, 128], BF16)
        G = sb("G", [128, DMIX], BF16)
        mixed = sb("mixed", [128, DMIX], BF16)
        outt = sb("outt", [128, D], F32)
        lbraw = outt
        xqs = qb
        xqT = AT
        pb = pbt
        HIM = ["pbt"]
        pT = qkT
        mT = G[:].rearrange("p (c m) -> p c m", c=16)
        st = sb("st", [128, 128], F32)
        junk = sb("junk", [128, 1024], BF16)

        pj = [ps("pj0", [128, 512], F32), ps("pj1", [128, 512], F32)]
        pt = ps("pt", [128, 1024], BF16)
        pcu = [ps("pcu0", [128, 512], F32), ps("pcu1", [128, 512], F32)]
        pss = ps("pss", [128, 512], F32)
        po = [ps("po0", [128, 512], F32), ps("po1", [128, 512], F32)]

        ident = mskb[:, 0:128]
        maskG = mskb[:, 128:256]
        maskH = mskb[:, 256:384]
        triG = cst[:, 0:128]
        triUG = cst[:, 128:256]
        triH = cst[:, 256:384]
        triUH = cst[:, 384:512]
        cind = cst[:, 512:520]
        nwT = pcols[:, 0:8]
        gwT = pcols[:, 8:24]
        mnwT = pcols[:, 24:32]

        pjc = [0]

        def next_pj():
            i = pjc[0] % 2
            pjc[0] += 1
            return pj[i], "pj%d" % i

        def bc(ap2, n):
            return ap2.unsqueeze(2).broadcast_to([128, ap2.shape[1], n])

        def bc_mid(ap2, n):
            return ap2.unsqueeze(1).broadcast_to([128, n, ap2.shape[1]])

        def mm(out_ap, pairs, reads, writes, first_start=True, **kw):
            pairs = list(pairs)

            def fn(e):
                n = len(pairs)
                ins = None
                for i, (a, b) in enumerate(pairs):
                    ins = e.matmul(out_ap, a, b, start=(first_start and i == 0), stop=(i == n - 1), **kw)
                return ins
            P.op("pe", fn, reads, writes)

        def mm_multi(items, reads, writes):
            items = list(items)

            def fn(e):
                ins = None
                for (o, a, b, s0, s1, kw) in items:
                    ins = e.matmul(o, a, b, start=s0, stop=s1, **kw)
                return ins
            P.op("pe", fn, reads, writes)

        def transposes(items, reads, writes):
            items = list(items)

            def fn(e):
                ins = None
                for (o, i_) in items:
                    ins = e.transpose(o, i_, ident)
                return ins
            P.op("pe", fn, list(reads) + ["mskb"], writes)

        def act(out, in_, func, reads, writes, **kw):
            P.op("act", lambda e: e.activation(out, in_, func, **kw), reads, writes)

        def tt(eng, out, in0, in1, op, reads, writes):
            P.op(eng, lambda e: e.tensor_tensor(out, in0, in1, op), reads, writes)

        def ts(eng, out, in0, s1, s2, op0, op1, reads, writes):
            if s2 is None:
                P.op(eng, lambda e: e.tensor_scalar(out, in0, s1, None, op0), reads, writes)
            else:
                P.op(eng, lambda e: e.tensor_scalar(out, in0, s1, s2, op0, op1), reads, writes)

        def stt(out, in0, scalar, in1, op0, op1, reads, writes):
            P.op("dve", lambda e: e.scalar_tensor_tensor(out, in0, scalar, in1, op0, op1), reads, writes)

        def copy(eng, out, in_, reads, writes):
            if eng == "act":
                P.op("act", lambda e: e.activation(out, in_, AF.Identity), reads, writes)
            else:
                P.op(eng, lambda e: e.tensor_copy(out, in_), reads, writes)

        def dma(eng, out, in_, reads, writes, sem, **kw):
            P.op(eng, lambda e: e.dma_start(out=out, in_=in_, **kw), reads, writes, dma=sem)

        def rstd_from(ssq_col, tmp_col, out_col, inv_n, rname, reads=None):
            act(tmp_col, ssq_col, AF.Ln, reads or [rname], [rname + "_t"], scale=inv_n, bias=EPS)
            act(out_col, tmp_col, AF.Exp, [rname + "_t"], [rname + "_r"], scale=-0.5)

        dma("sp", cst[:], cst_d[:, 0:520], [], ["cst"], "d_cst")
        dma("pool", mskb[:], msk_d, [], ["mskb"], "d_msk")
        dma("sp", lbraw[:], lbraw_d, [], ["outt"], "d_lbraw")
        if final_norm:
            dma("sp", fnw[:], fnw_d, [], ["fnw"], "d_fnw")
        P.op("pool", lambda e: e.memset(glrT[:], 1.0), [], ["glrT"])
        lr3 = lbraw[:].rearrange("p (l n) -> p l n", l=NL)
        lb3 = lbt[:].rearrange("p (l n) -> p l n", l=NL)
        mx = Ft[0]
        P.op("dve", lambda e: e.tensor_copy(mx[:], lr3[:, 0, :]), ["outt"], ["F0"])
        for l in range(1, NL):
            tt("dve", mx[:], mx[:], lr3[:, l, :], ALU.max, ["F0", "outt"], ["F0"])
        for l in range(NL):
            tt("dve", lb3[:, l, :], lr3[:, l, :], mx[:], ALU.subtract, ["F0", "outt"], ["lbt"])
        act(lbt[:], lbt[:], AF.Exp, ["lbt"], ["lbt"])
        den = Ft[1]
        P.op("dve", lambda e: e.tensor_copy(den[:], lb3[:, 0, :]), ["lbt"], ["F1"])
        for l in range(1, NL):
            tt("dve", den[:], den[:], lb3[:, l, :], ALU.add, ["F1", "lbt"], ["F1"])
        P.op("dve", lambda e: e.reciprocal(den[:], den[:]), ["F1"], ["F1"])
        for l in range(NL):
            tt("dve", lb3[:, l, :], lb3[:, l, :], den[:], ALU.mult, ["F1", "lbt"], ["lbt"])
        p0 = Ft[2]
        P.op("dve", lambda e: e.tensor_copy(p0[:], lb3[:, 0, :]), ["lbt"], ["F2"])
        for l in range(1, NL):
            tt("dve", lb3[:, l, :], lb3[:, l, :], lb3[:, l - 1, :], ALU.add, ["lbt"], ["lbt"])
        for l in range(NL):
            tt("dve", lb3[:, l, :], lb3[:, l, :], p0[:], ALU.subtract, ["lbt", "F2"], ["lbt"])

        F = ["F%d" % i for i in range(6)]

        for li, L in enumerate(layers):
            last = (li == len(layers) - 1)
            src_d = x_d if li == 0 else xmid_d
            dst_d = out_d if last else xmid_d
            do_final = last and final_norm
            lbL = lbt[:, L * 512:(L + 1) * 512]

            dma("sp", pcols[:], pcols_d[L], [], ["pcols"], "d_pcols")
            dma("pool", wup[0:17, :], wup_d[L], [], ["wup"], "d_wup")
            wkv = wout[:, 0:8, :]
            for hf_ in range(2):
                dma("pool", wout[:, hf_ * 4:(hf_ + 1) * 4, :],
                    wkv_d[L, hf_ * 512:(hf_ + 1) * 512, :].rearrange("(c p) n -> p c n", p=128),
                    [], ["wq%d" % hf_], "d_wkv%d" % hf_, max_dma_last_dim=4096)
            for c in range(8):
                dma("pool", win[:, c, :], win_d[L, c * 128:(c + 1) * 128, :], [], ["win%d" % c], "d_win%d" % c,
                    max_dma_last_dim=4096)
            P.op("dve", lambda e: e.memset(gS[:], 0.0), [], ["gS"])
            P.op("dve", lambda e: e.memset(hS[:], 0.0), [], ["hS"])
            P.op("pool", lambda e: e.memset(gSb[:], 0.0), [], ["gSb"])
            P.op("pool", lambda e: e.memset(hSb[:], 0.0), [], ["hSb"])
            WIN = ["win%d" % c for c in range(8)]

            mnT = mixed[:].rearrange("p (c m) -> p c m", c=8)
            for blk in range(2):
                xb = xt[blk]
                xn = "xt%d" % blk
                dma("sp", xb[:], mem_d[blk * 128:(blk + 1) * 128, :], [], [xn], "d_x%d" % blk)
                act(h[:], xb[:], AF.Square, [xn], ["h", "ssq"], accum_out=st[:, 0:1])
                rstd_from(st[:, 0:1], st[:, 1:2], st[:, 2:3], 1.0 / D, "ssq")
                ts("dve", h[:], xb[:], st[:, 2:3], None, ALU.mult, None, [xn, "ssq_r"], ["h"])
                transposes([(pt[:, c * 128:(c + 1) * 128], h[:, c * 128:(c + 1) * 128]) for c in range(8)],
                           ["h"], ["pt"])
                tt("dve", mnT[:, :, blk * 128:(blk + 1) * 128], pt[:].rearrange("p (c m) -> p c m", c=8),
                   bc(mnwT, 128), ALU.mult, ["pt", "pcols"], ["mixed"])
            for hd in range(4):
                pjt, pjn = next_pj()
                mm(pjt[:, 0:256], [(wkv[:, c, hd * 128:(hd + 1) * 128], mnT[:, c, :]) for c in range(8)],
                   ["wq0", "wq1", "mixed"], [pjn])
                copy("act", mkT[:, hd, :], pjt[:, 0:256], [pjn], ["mkT"])
            for blk in range(2):
                pjt, pjn = next_pj()
                mm(pjt[:], [(mnT[:, c, blk * 128:(blk + 1) * 128], wkv[:, c, 512:1024]) for c in range(8)],
                   ["wq0", "wq1", "mixed"], [pjn])
                copy("act", mv[:, blk, :], pjt[:], [pjn], ["mv"])
            for q4 in range(4):
                dma("pool", wout[:, q4 * 4:(q4 + 1) * 4, :],
                    wout_d[L, q4 * 512:(q4 + 1) * 512, :].rearrange("(c p) n -> p c n", p=128),
                    [], ["wq%d" % q4], "d_wo%d" % q4, max_dma_last_dim=4096)
            WOUT = ["wq%d" % q4 for q4 in range(4)]

            def proj(col0, ncol, dst_names):
                pjt, pjn = next_pj()
                mm(pjt[:, 0:ncol], [(hT[:, c, :], win[:, c, col0:col0 + ncol]) for c in range(8)],
                   ["hT"] + WIN, [pjn])
                return pjt, pjn

            def LOAD(t):
                dma("sp", xt[t % 2][:], src_d[t * 128:(t + 1) * 128, :], ["xmid%d" % t] if li > 0 else [],
                    ["xt%d" % (t % 2)], "d_x%d" % (t % 2))

            def HEAD_a(t):
                xb = xt[t % 2]
                xn = "xt%d" % (t % 2)
                act(h[:], xb[:], AF.Square, [xn], ["h", "ssq"], accum_out=st[:, 0:1])
                rstd_from(st[:, 0:1], st[:, 1:2], st[:, 2:3], 1.0 / D, "ssq")
                ts("dve", h[:], xb[:], st[:, 2:3], None, ALU.mult, None, [xn, "ssq_r"], ["h"])

            def HEAD_b(t):
                transposes([(pt[:, c * 128:(c + 1) * 128], h[:, c * 128:(c + 1) * 128]) for c in range(8)],
                           ["h"], ["pt"])
                tt("dve", hT[:], pt[:].rearrange("p (c m) -> p c m", c=8), bc(nwT, 128), ALU.mult,
                   ["pt", "pcols"], ["hT"])

            def HEAD(t):
                HEAD_a(t)
                HEAD_b(t)

            def qk_transposes():
                transposes([(pt[:, hd * 128:(hd + 1) * 128], qb[:, hd * 128:(hd + 1) * 128]) for hd in range(4)] +
                           [(pt[:, (4 + hd) * 128:(5 + hd) * 128], kb[:, hd * 128:(hd + 1) * 128]) for hd in range(4)],
                           ["qb", "kb"], ["pt"])
                copy("dve", qkT[:], pt[:].rearrange("p (c m) -> p c m", c=8), ["pt"], ["qkT"])
                mm_multi([(pss[:, hd * 128:(hd + 1) * 128], qkT[:, 4 + hd, :], qkT[:, hd, :], True, True, {})
                          for hd in range(4)], ["qkT"], ["pss"])

            def mixed_T(e0, e1, rname):
                n = e1 - e0
                transposes([(pt[:, i_ * 128:(i_ + 1) * 128], mixed[:, (e0 + i_) * 128:(e0 + i_ + 1) * 128])
                            for i_ in range(n)], ["mixed"], ["pt"])
                tt("dve", mT[:, e0:e1, :], pt[:, 0:n * 128].rearrange("p (c m) -> p c m", c=n),
                   bc(gwT[:, e0:e1], 128), ALU.mult, ["pt", "pcols"], [rname])

            ctx = {}

            def s_GT():
                for g4 in range(4):
                    pjt, pjn = proj(C_GATE + g4 * 512, 512, None)
                    act(G[:, g4 * 512:(g4 + 1) * 512], pjt[:], AF.Silu, [pjn], ["G", "mTa", "mTb", "mTc"])

            def s_X1():
                pjt, pjn = proj(C_XQ, 512, None)
                act(xqs[:], pjt[:], AF.Identity, [pjn], ["qb"], scale=float(128 ** -0.5))

            def s_G1a():
                pjt, pjn = next_pj()
                mm(pjt[0:16, 0:128], [(win[:, c, C_GLR:C_GLR + 16], hT[:, c, :]) for c in range(8)],
                   ["hT"] + WIN, [pjn])
                copy("act", glrT[0:16, :], pjt[0:16, 0:128], [pjn], ["glrT"])

            def s_X2():
                transposes([(pt[:, hd * 128:(hd + 1) * 128], xqs[:, hd * 128:(hd + 1) * 128]) for hd in range(4)],
                           ["qb"], ["pt"])
                copy("dve", xqT[:], pt[:, 0:512].rearrange("p (c m) -> p c m", c=4), ["pt"], ["AT"])

            def s_G1b():
                pjt, pjn = next_pj()
                mm(pjt[:], [(glrT[0:17, :], wup[0:17, :])], ["glrT", "wup"], [pjn])
                act(Ft[0][:], pjt[:], AF.Exp, [pjn], [F[0]], scale=-1.0)
                act(Ft[1][:], Ft[0][:], AF.Ln, [F[0]], [F[1]], bias=1.0)

            def s_X3():
                mm_multi([(po[hd // 2][:, (hd % 2) * 256:(hd % 2 + 1) * 256], xqT[:, hd, :], mkT[:, hd, :],
                           True, True, {}) for hd in range(4)], ["AT", "mkT"], ["po0", "po1"])
                for half in range(2):
                    P.op("dve", (lambda half: lambda e: e.tensor_reduce(
                        st[:, 48 + 2 * half:50 + 2 * half], po[half][:].rearrange("p (a b) -> p a b", a=2),
                        AX.X, ALU.max))(half), ["po%d" % half], ["xmax%d" % half])
                ts("dve", st[:, 52:56], st[:, 48:52], -1.0, None, ALU.mult, None, ["xmax0", "xmax1"], ["xnmax"])
                for hd in range(4):
                    act(pb[:, hd, :], po[hd // 2][:, (hd % 2) * 256:(hd % 2 + 1) * 256], AF.Exp,
                        ["po%d" % (hd // 2), "xnmax"], HIM + ["xZ%d" % hd], bias=st[:, 52 + hd:53 + hd],
                        accum_out=st[:, 56 + hd:57 + hd])

            def s_Hp():
                pjz, pjzn = proj(C_HF, 512, None)
                act(Ft[5][:], pjz[:], AF.Exp, [pjzn], [F[5]], scale=-1.0)

            def s_G2():
                mm(pcu[0][:], [(triG, Ft[1][:])], ["cst", F[1]], ["pcu0"])
                mm(pcu[1][:], [(triUG, Ft[1][:])], ["cst", F[1]], ["pcu1"])
                pjt, pjn = next_pj()
                mm_multi([(pjt[:, hd:hd + 1], Ft[1][:, hd * 128:(hd + 1) * 128], cind[:, 0:1], True, True, {})
                          for hd in range(4)], ["cst", F[1]], [pjn])
                act(st[:, 8:12], pjt[:, 0:4], AF.Exp, [pjn], ["gdec"], scale=-1.0 / 16)
                act(Ft[2][:], pcu[0][:], AF.Exp, ["pcu0"], [F[2]], scale=-1.0 / 16)
                act(Ft[3][:], pcu[0][:], AF.Exp, ["pcu0"], [F[3]], scale=1.0 / 16)
                act(Ft[4][:], pcu[1][:], AF.Exp, ["pcu1"], [F[4]], scale=-1.0 / 16)

            def s_H1():
                act(Ft[1][:], Ft[5][:], AF.Ln, [F[5]], [F[1]], bias=1.0)

            def s_X4():
                transposes([(pt[:, (hd * 2 + mc) * 128:(hd * 2 + mc + 1) * 128], pb[:, hd, mc * 128:(mc + 1) * 128])
                            for hd in range(4) for mc in range(2)], HIM, ["pt"])
                copy("dve", pT[:], pt[:].rearrange("p (c m) -> p c m", c=8), ["pt"], ["qkT"])

            def s_G3a():
                pjt, pjn = proj(C_GQ, 512, None)
                stt(qb[:], pjt[:], float(128 ** -0.5), Ft[2][:], ALU.mult, ALU.mult, [pjn, F[2]], ["qb"])
                pjt, pjn = proj(C_GK, 512, None)
                tt("dve", kb[:], pjt[:], Ft[3][:], ALU.mult, [pjn, F[3]], ["kb"])
                tt("dve", khb[:], pjt[:], Ft[4][:], ALU.mult, [pjn, F[4]], ["khb"])

            def s_X5():
                items = []
                for hd in range(4):
                    for mc in range(2):
                        items.append((po[1][:, hd * 128:(hd + 1) * 128], pT[:, hd * 2 + mc, :],
                                      mv[:, mc, hd * 128:(hd + 1) * 128], mc == 0, mc == 1, {}))
                mm_multi(items, ["qkT", "mv"], ["po1"])
                for hd in range(4):
                    act(junk[:, hd * 256 + 128:hd * 256 + 256], po[1][:, hd * 128:(hd + 1) * 128], AF.Square, ["po1"], ["xssq%d" % hd, "jk%d" % hd],
                        accum_out=st[:, 80 + hd:81 + hd])

            def s_G3b():
                for half in range(2):
                    pjt, pjn = proj(C_GV + half * 512, 512, None)
                    copy("act", v[:, half * 512:(half + 1) * 512], pjt[:], [pjn], ["v"])

            def s_H2():
                tt("dve", Ft[0][:], Ft[5][:], lbL, ALU.mult, [F[5], "lbt"], [F[0]])
                act(Ft[0][:], Ft[0][:], AF.Ln, [F[0]], [F[0]], bias=1.0)
                tt("dve", Ft[5][:], Ft[0][:], Ft[1][:], ALU.subtract, [F[0], F[1]], [F[5]])

            def s_X6():
                tt("dve", st[:, 60:64], st[:, 56:60], st[:, 56:60], ALU.mult, ["xZ%d" % hd for hd in range(4)], ["xz2"])
                ts("dve", st[:, 84:88], st[:, 80:84], 1.0 / 128, None, ALU.mult, None,
                   ["xssq%d" % hd for hd in range(4)], ["xvv"])
                stt(st[:, 84:88], st[:, 60:64], EPS, st[:, 84:88], ALU.mult, ALU.add, ["xz2", "xvv"], ["xvv"])
                act(st[:, 84:88], st[:, 84:88], AF.Ln, ["xvv"], ["xvv"])
                act(st[:, 88:92], st[:, 84:88], AF.Exp, ["xvv"], ["xr"], scale=-0.5)
                for hd in range(4):
                    stt(mixed[:, 1536 + hd * 128:1536 + (hd + 1) * 128], po[1][:, hd * 128:(hd + 1) * 128],
                        st[:, 88 + hd:89 + hd], G[:, 1536 + hd * 128:1536 + (hd + 1) * 128], ALU.mult, ALU.mult,
                        ["po1", "xr", "G"], ["mixed"])

            def s_G4():
                qk_transposes()
                tt("dve", AT[:], pss[:].rearrange("p (c m) -> p c m", c=4), bc_mid(maskG, 4), ALU.mult,
                   ["pss", "mskb"], ["AT"])

            def s_H3a():
                act(Ft[0][:], Ft[5][:], AF.Exp, [F[5]], [F[0]])
                ts("dve", Ft[0][:], Ft[0][:], -1.0, 1.0, ALU.mult, ALU.add, [F[0]], [F[0]])

            def s_H3b():
                pjr, pjrn = next_pj()
                mm(pss[:], [(triH, Ft[5][:])], ["cst", F[5]], ["pss"])
                mm(pjr[:], [(triUH, Ft[5][:])], ["cst", F[5]], [pjrn])
                pjt, pjn = next_pj()
                mm_multi([(pjt[:, hd * 4:hd * 4 + 4], Ft[5][:, hd * 128:(hd + 1) * 128], cind[:, 1:5], True, True, {})
                          for hd in range(4)], ["cst", F[5]], [pjn])
                act(Ft[1][:], pss[:], AF.Exp, ["pss"], [F[1]])
                act(Ft[2][:], pss[:], AF.Exp, ["pss"], [F[2]], scale=-1.0)
                act(Ft[3][:], pjr[:], AF.Exp, [pjrn], [F[3]])
                act(st[:, 32:48], pjt[:, 0:16], AF.Exp, [pjn], ["hdec"])

            def s_G5a():
                items = []
                for hd in range(4):
                    o_ap = po[hd // 2][:, (hd % 2) * 256:(hd % 2 + 1) * 256]
                    items.append((o_ap, AT[:, hd, :], v[:, hd * 256:(hd + 1) * 256], True, False, {}))
                    items.append((o_ap, qkT[:, hd, :], gSb[:, hd, :], False, True, {}))
                mm_multi(items, ["AT", "v", "qkT", "gSb"], ["po0", "po1"])
                for hd in range(4):
                    o_ap = po[hd // 2][:, (hd % 2) * 256:(hd % 2 + 1) * 256]
                    act(junk[:, hd * 256:(hd + 1) * 256], o_ap, AF.Square, ["po%d" % (hd // 2)], ["gssq%d" % hd, "jk%d" % hd],
                        accum_out=st[:, 16 + hd:17 + hd])
                rstd_from(st[:, 16:20], st[:, 20:24], st[:, 24:28], 1.0 / 256, "gssq",
                          ["gssq%d" % hd for hd in range(4)])

            def s_G5b():
                mm_multi([(pcu[hd // 2][:, (hd % 2) * 256:(hd % 2 + 1) * 256], khb[:, hd * 128:(hd + 1) * 128],
                           v[:, hd * 256:(hd + 1) * 256], True, True, {}) for hd in range(4)],
                         ["khb", "v"], ["pcu0", "pcu1"])
                for hd in range(4):
                    stt(gS[:, hd, :], gS[:, hd, :], st[:, 8 + hd:9 + hd],
                        pcu[hd // 2][:, (hd % 2) * 256:(hd % 2 + 1) * 256], ALU.mult, ALU.add,
                        ["gS", "gdec", "pcu%d" % (hd // 2)], ["gS"])
                copy("act", gSb[:].rearrange("p a b -> p (a b)"), gS[:].rearrange("p a b -> p (a b)"),
                     ["gS"], ["gSb"])

            def s_G5c():
                for hd in range(4):
                    o_ap = po[hd // 2][:, (hd % 2) * 256:(hd % 2 + 1) * 256]
                    stt(mixed[:, hd * 256:(hd + 1) * 256], o_ap, st[:, 24 + hd:25 + hd],
                        G[:, hd * 256:(hd + 1) * 256], ALU.mult, ALU.mult,
                        ["po%d" % (hd // 2), "gssq_r", "G"], ["mixed"])

            def s_H4():
                pjt, pjn = proj(C_HQ, 512, None)
                tt("dve", qb[:], pjt[:], Ft[1][:], ALU.mult, [pjn, F[1]], ["qb"])
                tt("dve", kb[:], Ft[0][:], Ft[2][:], ALU.mult, [F[0], F[2]], ["kb"])
                tt("pool", khb[:], Ft[0][:], Ft[3][:], ALU.mult, [F[0], F[3]], ["khb"])
                pjt, pjn = proj(C_HI, 512, None)
                copy("act", hi[:], pjt[:], [pjn], ["hi"])

            ubank = [(pcu[0], "pcu0"), (pcu[1], "pcu1"), (pss, "pss"), (po[1], "po1")]

            def s_H5a():
                qk_transposes()
                tt("dve", AT[:], pss[:].rearrange("p (c m) -> p c m", c=4), bc_mid(maskH, 4), ALU.mult,
                   ["pss", "mskb"], ["AT"])

            def s_H5b():
                mm_multi([(po[0][:, hd * 128:(hd + 1) * 128], AT[:, hd, :], hi[:, hd * 128:(hd + 1) * 128],
                           hd == 0, False, {"skip_group_check": True}) for hd in range(4)],
                         ["AT", "hi"], ["po0"])
                for c4 in range(4):
                    pu, pun = ubank[c4]
                    mm_multi([(pu[:, hd * 128:(hd + 1) * 128], khb[32 * c4:32 * (c4 + 1), hd * 128:(hd + 1) * 128],
                               hi[32 * c4:32 * (c4 + 1), hd * 128:(hd + 1) * 128], True, True,
                               {"tile_position": (32 * c4, 0)}) for hd in range(4)],
                             ["khb", "hi"], [pun])

            def s_O2a(part):
                elist = [0, 1, 2, 3, 4, 5, 6, 7, 12, 13, 14, 15]
                es_ = elist[part * 3:(part + 1) * 3]
                items = []
                for half in range(2):
                    for e_ in es_:
                        items.append((pj[half][:], mT[:, e_, :], wout[:, e_, half * 512:(half + 1) * 512],
                                      e_ == 0, False, {"skip_group_check": True}))
                mm_multi(items, ["mTa", "mTb"] + WOUT, ["pj0", "pj1"])

            def s_H6():
                for c4 in range(4):
                    pu, pun = ubank[c4]
                    for hd in range(4):
                        sbn = "hSb%d_%d" % (hd, c4)
                        mm_multi([(po[0][32 * c4:32 * (c4 + 1), hd * 128:(hd + 1) * 128],
                                   qkT[:, hd, 32 * c4:32 * (c4 + 1)], hSb[:, hd, c4, :], False, c4 == 3,
                                   {"skip_group_check": True, "tile_position": (0, 32 * c4)})],
                                 ["qkT", sbn, "hSb"], ["po0"])
                        stt(hS[:, hd, :], hS[:, hd, :], st[:, 32 + hd * 4 + c4:33 + hd * 4 + c4],
                            pu[:, hd * 128:(hd + 1) * 128], ALU.mult, ALU.add,
                            ["hS%d" % hd, "hS", "hdec", pun], ["hS%d" % hd])
                        copy("act", hSb[:, hd, (c4 + 1) % 4, :], hS[:, hd, :], ["hS%d" % hd],
                             ["hSb%d_%d" % (hd, (c4 + 1) % 4)])
                    s_O2a(c4)
                for hd in range(4):
                    act(junk[:, hd * 256:hd * 256 + 128], po[0][:, hd * 128:(hd + 1) * 128], AF.Square, ["po0"], ["hssq%d" % hd, "jk%d" % hd],
                        accum_out=st[:, 64 + hd:65 + hd])
                rstd_from(st[:, 64:68], st[:, 68:72], st[:, 72:76], 1.0 / 128, "hssq",
                          ["hssq%d" % hd for hd in range(4)])
                for hd in range(4):
                    stt(mixed[:, 1024 + hd * 128:1024 + (hd + 1) * 128], po[0][:, hd * 128:(hd + 1) * 128],
                        st[:, 72 + hd:73 + hd], G[:, 1024 + hd * 128:1024 + (hd + 1) * 128], ALU.mult, ALU.mult,
                        ["po0", "hssq_r", "G"], ["mixed"])

            def s_O2(t):
                xb = xt[t % 2]
                xn = "xt%d" % (t % 2)
                rows = slice(t * 128, (t + 1) * 128)
                for half in range(2):
                    pjn = "pj%d" % half
                    mm_multi([(pj[half][:], mT[:, e_, :], wout[:, e_, half * 512:(half + 1) * 512], False, e_ == 11,
                               {"skip_group_check": True}) for e_ in range(8, 12)],
                             ["mTc"] + WOUT, [pjn])
                    tt("dve", xb[:, half * 512:(half + 1) * 512], xb[:, half * 512:(half + 1) * 512], pj[half][:],
                       ALU.add, [xn, pjn], [xn])
                pjc[0] = 0
                if do_final:
                    act(outt[:], xb[:], AF.Square, [xn], ["outt", "fssq"], accum_out=st[:, 4:5])
                    rstd_from(st[:, 4:5], st[:, 5:6], st[:, 6:7], 1.0 / D, "fssq")
                    stt(outt[:], xb[:], st[:, 6:7], fnw[:], ALU.mult, ALU.mult, [xn, "fssq_r", "fnw"], ["outt"])
                    dma("sp", dst_d[rows, :], outt[:], ["outt"], ["xmid%d" % t], "d_o")
                else:
                    dma("sp", dst_d[rows, :], xb[:], [xn], ["xmid%d" % t], "d_x%d" % (t % 2))

            LOAD(0)
            HEAD(0)
            for t in range(NT):
                nxt = t + 1 < NT
                if nxt:
                    LOAD(t + 1)
                s_X1(); s_G1a(); s_G3b(); s_Hp(); s_X2(); s_G1b(); s_X3(); s_G2(); s_H1(); s_X4(); s_G3a(); s_X5()
                s_H2()
                s_GT(); s_G4(); s_X6()
                if nxt:
                    HEAD_a(t + 1)
                s_H3a(); s_H3b(); s_G5a(); s_G5b()
                mixed_T(12, 16, "mTb")
                s_H4()
                if nxt:
                    HEAD_b(t + 1)
                s_H5a()
                s_G5c()
                s_H5b()
                mixed_T(0, 8, "mTa")
                s_H6()
                mixed_T(8, 12, "mTc")
                s_O2(t)

        P.emit(nc)
    return nc


def _consts():
    j = np.arange(128)[:, None]
    i = np.arange(128)[None, :]
    triG = (j <= i).astype(np.float32)
    triUG = (j > i).astype(np.float32)
    same = (j // 32) == (i // 32)
    triH = ((j <= i) & same).astype(np.float32)
    triUH = ((j > i) & same).astype(np.float32)
    cind = np.zeros((128, 8), np.float32)
    cind[:, 0] = 1.0
    for c in range(4):
        cind[:, 1 + c] = (np.arange(128) // 32 == c)
    cst = np.concatenate([triG, triUG, triH, triUH, cind, np.zeros((128, 6 * 128 + 8 - 520), np.float32)], axis=1)
    ident = np.eye(128, dtype=np.float32)
    maskG = triG
    maskH = triH
    msk = np.concatenate([ident, maskG, maskH], axis=1)
    return np.ascontiguousarray(cst), np.ascontiguousarray(msk)


def _layout_params(norm_w, gla_w_gate_up, gla_b_gate, gla_norm_w, hgrn_lower_bounds, hgrn_norm_w,
                   mem_norm_w, xattn_norm_w, final_norm_w):
    NL = norm_w.shape[0]
    pcols = np.zeros((NL, 128, 32), np.float32)
    for L in range(NL):
        pcols[L, :, 0:8] = norm_w[L].reshape(8, 128).T
        gw = np.concatenate([np.tile(gla_norm_w[L], 4), np.tile(hgrn_norm_w[L], 4), np.tile(xattn_norm_w[L], 4)])
        pcols[L, :, 8:24] = gw.reshape(16, 128).T
        pcols[L, :, 24:32] = mem_norm_w[L].reshape(8, 128).T
    wup = np.concatenate([gla_w_gate_up, gla_b_gate[:, None, :]], axis=1).astype(np.float32)
    lbraw = np.ascontiguousarray(np.broadcast_to(hgrn_lower_bounds.reshape(1, -1), (128, NL * 512))).astype(np.float32)
    fnw = np.ascontiguousarray(np.broadcast_to(final_norm_w.reshape(1, -1), (128, D))).astype(np.float32)
    return pcols, np.ascontiguousarray(wup), lbraw, fnw


_NC_CACHE = {}


def _get_nc(S, layers, final_norm):
    key = (S, tuple(layers), final_norm)
    if key not in _NC_CACHE:
        _NC_CACHE[key] = build(S, list(layers), final_norm)
    return _NC_CACHE[key]


def kernel(x, mem, norm_w, w_in, gla_w_gate_up, gla_b_gate, gla_norm_w, hgrn_lower_bounds,
           hgrn_norm_w, mem_norm_w, w_mem_kv, xattn_norm_w, w_out, final_norm_w):
    x = np.asarray(x, np.float32)
    mem = np.asarray(mem, np.float32)
    B, S, _ = x.shape
    f = lambda a: np.ascontiguousarray(np.asarray(a, np.float32))
    pcols, wup, lbraw, fnw = _layout_params(f(norm_w), f(gla_w_gate_up), f(gla_b_gate), f(gla_norm_w),
                                            f(hgrn_lower_bounds), f(hgrn_norm_w), f(mem_norm_w),
                                            f(xattn_norm_w), f(final_norm_w))
    cst, msk = _consts()
    nc = _get_nc(S, (0, 1), True)
    shared = {"w_in": f(w_in), "w_out": f(w_out), "w_kv": f(w_mem_kv), "wup": wup, "pcols": pcols,
              "lbraw": lbraw, "fnw": fnw, "cst": cst, "msk": msk}
    in_maps = []
    for b in range(B):
        m = dict(shared)
        m["x"] = np.ascontiguousarray(x[b])
        m["mem"] = np.ascontiguousarray(mem[b])
        in_maps.append(m)
    res = run_bass_kernel_spmd(nc, in_maps, core_ids=list(range(B)))
    return np.stack([np.asarray(r["out"], np.float32) for r in res.results], axis=0)
```

```python
import contextlib
import numpy as np
import ml_dtypes
import concourse.bass as bass
import concourse.mybir as mybir
from concourse.bass_utils import run_bass_kernel_spmd

F32 = mybir.dt.float32
BF16 = mybir.dt.bfloat16
AF = mybir.ActivationFunctionType
ALU = mybir.AluOpType
AX = mybir.AxisListType

D = 1024
DIN = 6160
DMIX = 2048
MEM = 256
EPS = 1e-6
C_GQ, C_GK, C_GV, C_GLR, C_HQ, C_HF, C_HI, C_XQ, C_GATE = 0, 512, 1024, 2048, 2064, 2576, 3088, 3600, 4112


class Prog:
    ENG = ("pe", "act", "dve", "pool", "sp")

    def __init__(self):
        self.q = {e: [] for e in self.ENG}
        self.cnt = {}
        self.res = {}
        self.waited = {e: {} for e in self.ENG}

    def op(self, eng, fn, reads=(), writes=(), dma=None):
        deps = {}

        def add(tok):
            if tok is None:
                return
            k, v = tok
            if deps.get(k, 0) < v:
                deps[k] = v

        for r in reads:
            st = self.res.get(r)
            if st:
                add(st[0])
        for w in writes:
            st = self.res.get(w)
            if st:
                add(st[0])
                for k, v in st[1].items():
                    add((k, v))
        if eng == "pe":
            deps.pop("pe", None)
        waits = []
        wd = self.waited[eng]
        for k, v in deps.items():
            if wd.get(k, 0) < v:
                wd[k] = v
                waits.append((k, v))
        key, amt = (dma, 16) if dma is not None else (eng, 1)
        self.cnt[key] = self.cnt.get(key, 0) + amt
        tok = (key, self.cnt[key])
        self.q[eng].append((waits, fn, key, amt))
        for r in reads:
            st = self.res.setdefault(r, [None, {}])
            if st[1].get(key, 0) < tok[1]:
                st[1][key] = tok[1]
        for w in writes:
            self.res[w] = [tok, {}]
        return tok

    def emit(self, nc):
        with contextlib.ExitStack() as es:
            sems = {k: es.enter_context(nc.semaphore("s_" + k)) for k in self.cnt}
            block = es.enter_context(nc.Block())
            final = [(k, v) for k, v in self.cnt.items()]

            def run(name, e):
                for waits, fn, key, amt in self.q[name]:
                    for k, v in waits:
                        e.wait_ge(sems[k], v)
                    ins = fn(e)
                    ins.then_inc(sems[key], amt)

            @block.tensor
            def _(e):
                run("pe", e)

            @block.scalar
            def _(e):
                run("act", e)

            @block.vector
            def _(e):
                run("dve", e)

            @block.gpsimd
            def _(e):
                run("pool", e)

            @block.sync
            def _(e):
                run("sp", e)
                for k, v in final:
                    e.wait_ge(sems[k], v)


def build(S, layers, final_norm, n_layers_total=2):
    nc = bass.Bass("TRN2", target_bir_lowering=False)
    P = Prog()
    NT = S // 128
    NL = n_layers_total

    def din(name, shape, dt=F32):
        return nc.dram_tensor(name, list(shape), dt, kind="ExternalInput").ap()

    x_d = din("x", [S, D])
    mem_d = din("mem", [MEM, D])
    win_d = din("w_in", [NL, D, DIN])
    wout_d = din("w_out", [NL, DMIX, D])
    wkv_d = din("w_kv", [NL, D, 2 * 512])
    wup_d = din("wup", [NL, 17, 512])
    pcols_d = din("pcols", [NL, 128, 32])
    lbraw_d = din("lbraw", [128, NL * 512])
    fnw_d = din("fnw", [128, D])
    cst_d = din("cst", [128, 6 * 128 + 8])
    msk_d = din("msk", [128, 3 * 128])
    out_d = nc.dram_tensor("out", [S, D], F32, kind="ExternalOutput").ap()
    xmid_d = None
    if len(layers) > 1:
        xmid_d = nc.dram_tensor("xmid", [S, D], F32, kind="Internal").ap()

    es = contextlib.ExitStack()
    with es:
        def sb(name, shape, dt):
            return es.enter_context(nc.sbuf_tensor("sb_" + name, list(shape), dt))

        def ps(name, shape, dt):
            return es.enter_context(nc.psum_tensor("ps_" + name, list(shape), dt))

        win = sb("win", [128, 8, DIN], BF16)
        wout = sb("wout", [128, 16, D], BF16)
        cst = sb("cst", [128, 4 * 128 + 8], F32)
        mskb = sb("mskb", [128, 3 * 128], BF16)
        pcols = sb("pcols", [128, 32], F32)
        wup = sb("wup", [32, 512], BF16)
        lbt = sb("lbt", [128, NL * 512], F32)
        fnw = sb("fnw", [128, D], F32)
        gS = sb("gS", [128, 4, 256], F32)
        gSb = sb("gSb", [128, 4, 256], BF16)
        hS = sb("hS", [128, 4, 128], F32)
        hSb = sb("hSb", [128, 4, 4, 128], BF16)
        mkT = sb("mkT", [128, 4, 256], BF16)
        mv = sb("mv", [128, 2, 512], BF16)
        xt = [sb("xt0", [128, D], F32), sb("xt1", [128, D], F32)]
        h = sb("h", [128, D], BF16)
        hT = sb("hT", [128, 8, 128], BF16)
        Ft = [sb("F%d" % i, [128, 512], F32) for i in range(6)]
        qb = sb("qb", [128, 512], BF16)
        kb = sb("kb", [128, 512], BF16)
        khb = sb("khb", [128, 512], BF16)
        qkT = sb("qkT", [128, 8, 128], BF16)
        v = sb("v", [128, 1024], BF16)
        hi = sb("hi", [128, 512], BF16)
        pbt = sb("pbt", [128, 4, 256], BF16)
        AT = sb("AT", [128, 4, 128], BF16)
        glrT = sb("glrT", [32, 128], BF16)
        G = sb("G", [128, DMIX], BF16)
        mixed = sb("mixed", [128, DMIX], BF16)
        outt = sb("outt", [128, D], F32)
        lbraw = outt
        xqs = qb
        xqT = AT
        pb = pbt
        HIM = ["pbt"]
        pT = qkT
        mT = G[:].rearrange("p (c m) -> p c m", c=16)
        st = sb("st", [128, 128], F32)
        junk = sb("junk", [128, 1024], BF16)
        dmy = sb("dmy", [128, 4], F32)

        pj = [ps("pj0", [128, 512], F32), ps("pj1", [128, 512], F32)]
        pt = ps("pt", [128, 1024], BF16)
        pcu = [ps("pcu0", [128, 512], F32), ps("pcu1", [128, 512], F32)]
        pss = ps("pss", [128, 512], F32)
        po = [ps("po0", [128, 512], F32), ps("po1", [128, 512], F32)]

        ident = mskb[:, 0:128]
        maskG = mskb[:, 128:256]
        maskH = mskb[:, 256:384]
        triG = cst[:, 0:128]
        triUG = cst[:, 128:256]
        triH = cst[:, 256:384]
        triUH = cst[:, 384:512]
        cind = cst[:, 512:520]
        nwT = pcols[:, 0:8]
        gwT = pcols[:, 8:24]
        mnwT = pcols[:, 24:32]

        pjc = [0]

        def next_pj():
            i = pjc[0] % 2
            pjc[0] += 1
            return pj[i], "pj%d" % i

        def bc(ap2, n):
            return ap2.unsqueeze(2).broadcast_to([128, ap2.shape[1], n])

        def bc_mid(ap2, n):
            return ap2.unsqueeze(1).broadcast_to([128, n, ap2.shape[1]])

        def mm(out_ap, pairs, reads, writes, first_start=True, **kw):
            pairs = list(pairs)

            def fn(e):
                n = len(pairs)
                ins = None
                for i, (a, b) in enumerate(pairs):
                    ins = e.matmul(out_ap, a, b, start=(first_start and i == 0), stop=(i == n - 1), **kw)
                return ins
            P.op("pe", fn, reads, writes)

        def mm_multi(items, reads, writes):
            items = list(items)

            def fn(e):
                ins = None
                for (o, a, b, s0, s1, kw) in items:
                    ins = e.matmul(o, a, b, start=s0, stop=s1, **kw)
                return ins
            P.op("pe", fn, reads, writes)

        def transposes(items, reads, writes):
            items = list(items)

            def fn(e):
                ins = None
                for (o, i_) in items:
                    ins = e.transpose(o, i_, ident)
                return ins
            P.op("pe", fn, list(reads) + ["mskb"], writes)

        def act(out, in_, func, reads, writes, **kw):
            P.op("act", lambda e: e.activation(out, in_, func, **kw), reads, writes)

        def tt(eng, out, in0, in1, op, reads, writes):
            P.op(eng, lambda e: e.tensor_tensor(out, in0, in1, op), reads, writes)

        def ts(eng, out, in0, s1, s2, op0, op1, reads, writes):
            if s2 is None:
                P.op(eng, lambda e: e.tensor_scalar(out, in0, s1, None, op0), reads, writes)
            else:
                P.op(eng, lambda e: e.tensor_scalar(out, in0, s1, s2, op0, op1), reads, writes)

        def stt(out, in0, scalar, in1, op0, op1, reads, writes):
            P.op("dve", lambda e: e.scalar_tensor_tensor(out, in0, scalar, in1, op0, op1), reads, writes)

        def copy(eng, out, in_, reads, writes):
            if eng == "act":
                P.op("act", lambda e: e.activation(out, in_, AF.Identity), reads, writes)
            else:
                P.op(eng, lambda e: e.tensor_copy(out, in_), reads, writes)

        def dma(eng, out, in_, reads, writes, sem, **kw):
            P.op(eng, lambda e: e.dma_start(out=out, in_=in_, **kw), reads, writes, dma=sem)

        def rstd_from(ssq_col, tmp_col, out_col, inv_n, rname, reads=None):
            act(tmp_col, ssq_col, AF.Ln, reads or [rname], [rname + "_t"], scale=inv_n, bias=EPS)
            act(out_col, tmp_col, AF.Exp, [rname + "_t"], [rname + "_r"], scale=-0.5)

        dma("sp", cst[:], cst_d[:, 0:520], [], ["cst"], "d_cst")
        dma("pool", mskb[:], msk_d, [], ["mskb"], "d_msk")
        dma("sp", lbraw[:], lbraw_d, [], ["outt"], "d_lbraw")
        if final_norm:
            dma("sp", fnw[:], fnw_d, [], ["fnw"], "d_fnw")
        P.op("pool", lambda e: e.memset(glrT[:], 1.0), [], ["glrT"])
        P.op("dve", lambda e: e.memset(dmy[:], 0.0), [], ["dmy"])
        lr3 = lbraw[:].rearrange("p (l n) -> p l n", l=NL)
        lb3 = lbt[:].rearrange("p (l n) -> p l n", l=NL)
        mx = Ft[0]
        P.op("dve", lambda e: e.tensor_copy(mx[:], lr3[:, 0, :]), ["outt"], ["F0"])
        for l in range(1, NL):
            tt("dve", mx[:], mx[:], lr3[:, l, :], ALU.max, ["F0", "outt"], ["F0"])
        for l in range(NL):
            tt("dve", lb3[:, l, :], lr3[:, l, :], mx[:], ALU.subtract, ["F0", "outt"], ["lbt"])
        act(lbt[:], lbt[:], AF.Exp, ["lbt"], ["lbt"])
        den = Ft[1]
        P.op("dve", lambda e: e.tensor_copy(den[:], lb3[:, 0, :]), ["lbt"], ["F1"])
        for l in range(1, NL):
            tt("dve", den[:], den[:], lb3[:, l, :], ALU.add, ["F1", "lbt"], ["F1"])
        P.op("dve", lambda e: e.reciprocal(den[:], den[:]), ["F1"], ["F1"])
        for l in range(NL):
            tt("dve", lb3[:, l, :], lb3[:, l, :], den[:], ALU.mult, ["F1", "lbt"], ["lbt"])
        p0 = Ft[2]
        P.op("dve", lambda e: e.tensor_copy(p0[:], lb3[:, 0, :]), ["lbt"], ["F2"])
        for l in range(1, NL):
            tt("dve", lb3[:, l, :], lb3[:, l, :], lb3[:, l - 1, :], ALU.add, ["lbt"], ["lbt"])
        for l in range(NL):
            tt("dve", lb3[:, l, :], lb3[:, l, :], p0[:], ALU.subtract, ["lbt", "F2"], ["lbt"])

        F = ["F%d" % i for i in range(6)]

        for li, L in enumerate(layers):
            last = (li == len(layers) - 1)
            src_d = x_d if li == 0 else xmid_d
            dst_d = out_d if last else xmid_d
            do_final = last and final_norm
            lbL = lbt[:, L * 512:(L + 1) * 512]

            dma("sp", pcols[:], pcols_d[L], [], ["pcols"], "d_pcols")
            dma("pool", wup[0:17, :], wup_d[L], [], ["wup"], "d_wup")
            wkv = wout[:, 0:8, :]
            for hf_ in range(2):
                dma("pool", wout[:, hf_ * 4:(hf_ + 1) * 4, :],
                    wkv_d[L, hf_ * 512:(hf_ + 1) * 512, :].rearrange("(c p) n -> p c n", p=128),
                    [], ["wq%d" % hf_], "d_wkv%d" % hf_, max_dma_last_dim=4096)
            for c in range(8):
                dma("pool", win[:, c, :], win_d[L, c * 128:(c + 1) * 128, :], [], ["win%d" % c], "d_win%d" % c,
                    max_dma_last_dim=4096)
            P.op("dve", lambda e: e.memset(gS[:], 0.0), [], ["gS"])
            P.op("dve", lambda e: e.memset(hS[:], 0.0), [], ["hS"])
            P.op("pool", lambda e: e.memset(gSb[:], 0.0), [], ["gSb"])
            P.op("pool", lambda e: e.memset(hSb[:], 0.0), [], ["hSb"])
            WIN = ["win%d" % c for c in range(8)]

            mnT = mixed[:].rearrange("p (c m) -> p c m", c=8)
            for blk in range(2):
                xb = xt[blk]
                xn = "xt%d" % blk
                dma("sp", xb[:], mem_d[blk * 128:(blk + 1) * 128, :], [], [xn], "d_x%d" % blk)
                act(h[:], xb[:], AF.Square, [xn], ["h", "ssq"], accum_out=st[:, 0:1])
                rstd_from(st[:, 0:1], st[:, 1:2], st[:, 2:3], 1.0 / D, "ssq")
                ts("dve", h[:], xb[:], st[:, 2:3], None, ALU.mult, None, [xn, "ssq_r"], ["h"])
                transposes([(pt[:, c * 128:(c + 1) * 128], h[:, c * 128:(c + 1) * 128]) for c in range(8)],
                           ["h"], ["pt"])
                tt("dve", mnT[:, :, blk * 128:(blk + 1) * 128], pt[:].rearrange("p (c m) -> p c m", c=8),
                   bc(mnwT, 128), ALU.mult, ["pt", "pcols"], ["mixed"])
            for hd in range(4):
                pjt, pjn = next_pj()
                mm(pjt[:, 0:256], [(wkv[:, c, hd * 128:(hd + 1) * 128], mnT[:, c, :]) for c in range(8)],
                   ["wq0", "wq1", "mixed"], [pjn])
                copy("act", mkT[:, hd, :], pjt[:, 0:256], [pjn], ["mkT"])
            for blk in range(2):
                pjt, pjn = next_pj()
                mm(pjt[:], [(mnT[:, c, blk * 128:(blk + 1) * 128], wkv[:, c, 512:1024]) for c in range(8)],
                   ["wq0", "wq1", "mixed"], [pjn])
                copy("act", mv[:, blk, :], pjt[:], [pjn], ["mv"])
            for q4 in range(4):
                dma("pool", wout[:, q4 * 4:(q4 + 1) * 4, :],
                    wout_d[L, q4 * 512:(q4 + 1) * 512, :].rearrange("(c p) n -> p c n", p=128),
                    [], ["wq%d" % q4], "d_wo%d" % q4, max_dma_last_dim=4096)
            WOUT = ["wq%d" % q4 for q4 in range(4)]

            def proj(col0, ncol, dst_names):
                pjt, pjn = next_pj()
                mm(pjt[:, 0:ncol], [(hT[:, c, :], win[:, c, col0:col0 + ncol]) for c in range(8)],
                   ["hT"] + WIN, [pjn])
                return pjt, pjn

            def LOAD(t):
                dma("sp", xt[t % 2][:], src_d[t * 128:(t + 1) * 128, :], ["xmid%d" % t] if li > 0 else [],
                    ["xt%d" % (t % 2)], "d_x%d" % (t % 2))

            def HEAD_a(t):
                xb = xt[t % 2]
                xn = "xt%d" % (t % 2)
                act(h[:], xb[:], AF.Square, [xn], ["h", "ssq"], accum_out=st[:, 0:1])
                rstd_from(st[:, 0:1], st[:, 1:2], st[:, 2:3], 1.0 / D, "ssq")
                ts("dve", h[:], xb[:], st[:, 2:3], None, ALU.mult, None, [xn, "ssq_r"], ["h"])

            def HEAD_b(t):
                transposes([(pt[:, c * 128:(c + 1) * 128], h[:, c * 128:(c + 1) * 128]) for c in range(8)],
                           ["h"], ["pt"])
                tt("dve", hT[:], pt[:].rearrange("p (c m) -> p c m", c=8), bc(nwT, 128), ALU.mult,
                   ["pt", "pcols"], ["hT"])

            def HEAD(t):
                HEAD_a(t)
                HEAD_b(t)

            def qk_transposes():
                transposes([(pt[:, hd * 128:(hd + 1) * 128], qb[:, hd * 128:(hd + 1) * 128]) for hd in range(4)] +
                           [(pt[:, (4 + hd) * 128:(5 + hd) * 128], kb[:, hd * 128:(hd + 1) * 128]) for hd in range(4)],
                           ["qb", "kb"], ["pt"])
                copy("dve", qkT[:], pt[:].rearrange("p (c m) -> p c m", c=8), ["pt"], ["qkT"])
                mm_multi([(pss[:, hd * 128:(hd + 1) * 128], qkT[:, 4 + hd, :], qkT[:, hd, :], True, True, {})
                          for hd in range(4)], ["qkT"], ["pss"])

            def mixed_T(e0, e1, rname):
                n = e1 - e0
                transposes([(pt[:, i_ * 128:(i_ + 1) * 128], mixed[:, (e0 + i_) * 128:(e0 + i_ + 1) * 128])
                            for i_ in range(n)], ["mixed"], ["pt"])
                tt("dve", mT[:, e0:e1, :], pt[:, 0:n * 128].rearrange("p (c m) -> p c m", c=n),
                   bc(gwT[:, e0:e1], 128), ALU.mult, ["pt", "pcols"], [rname])

            ctx = {}

            def s_GT():
                act(dmy[:, 0:1], dmy[:, 1:2], AF.Silu, ["dmy"], ["dmy"])
                for g4 in range(4):
                    pjt, pjn = proj(C_GATE + g4 * 512, 512, None)
                    act(G[:, g4 * 512:(g4 + 1) * 512], pjt[:], AF.Silu, [pjn], ["G", "mTa", "mTb", "mTc"])
                act(dmy[:, 0:1], dmy[:, 1:2], AF.Exp, ["dmy"], ["dmy"])

            def s_X1():
                pjt, pjn = proj(C_XQ, 512, None)
                act(xqs[:], pjt[:], AF.Identity, [pjn], ["qb"], scale=float(128 ** -0.5))

            def s_G1a():
                pjt, pjn = next_pj()
                mm(pjt[0:16, 0:128], [(win[:, c, C_GLR:C_GLR + 16], hT[:, c, :]) for c in range(8)],
                   ["hT"] + WIN, [pjn])
                copy("act", glrT[0:16, :], pjt[0:16, 0:128], [pjn], ["glrT"])

            def s_X2():
                transposes([(pt[:, hd * 128:(hd + 1) * 128], xqs[:, hd * 128:(hd + 1) * 128]) for hd in range(4)],
                           ["qb"], ["pt"])
                copy("dve", xqT[:], pt[:, 0:512].rearrange("p (c m) -> p c m", c=4), ["pt"], ["AT"])

            def s_G1b():
                pjt, pjn = next_pj()
                mm(pjt[:], [(glrT[0:17, :], wup[0:17, :])], ["glrT", "wup"], [pjn])
                act(Ft[0][:], pjt[:], AF.Exp, [pjn], [F[0]], scale=-1.0)
                act(Ft[1][:], Ft[0][:], AF.Ln, [F[0]], [F[1]], bias=1.0)

            def s_X3():
                mm_multi([(po[hd // 2][:, (hd % 2) * 256:(hd % 2 + 1) * 256], xqT[:, hd, :], mkT[:, hd, :],
                           True, True, {}) for hd in range(4)], ["AT", "mkT"], ["po0", "po1"])
                for half in range(2):
                    P.op("dve", (lambda half: lambda e: e.tensor_reduce(
                        st[:, 48 + 2 * half:50 + 2 * half], po[half][:].rearrange("p (a b) -> p a b", a=2),
                        AX.X, ALU.max))(half), ["po%d" % half], ["xmax%d" % half])
                ts("dve", st[:, 52:56], st[:, 48:52], -1.0, None, ALU.mult, None, ["xmax0", "xmax1"], ["xnmax"])
                for hd in range(4):
                    act(pb[:, hd, :], po[hd // 2][:, (hd % 2) * 256:(hd % 2 + 1) * 256], AF.Exp,
                        ["po%d" % (hd // 2), "xnmax"], HIM + ["xZ%d" % hd], bias=st[:, 52 + hd:53 + hd],
                        accum_out=st[:, 56 + hd:57 + hd])

            def s_Hp():
                pjz, pjzn = proj(C_HF, 512, None)
                act(Ft[5][:], pjz[:], AF.Exp, [pjzn], [F[5]], scale=-1.0)

            def s_G2():
                mm(pcu[0][:], [(triG, Ft[1][:])], ["cst", F[1]], ["pcu0"])
                mm(pcu[1][:], [(triUG, Ft[1][:])], ["cst", F[1]], ["pcu1"])
                pjt, pjn = next_pj()
                mm_multi([(pjt[:, hd:hd + 1], Ft[1][:, hd * 128:(hd + 1) * 128], cind[:, 0:1], True, True, {})
                          for hd in range(4)], ["cst", F[1]], [pjn])
                act(st[:, 8:12], pjt[:, 0:4], AF.Exp, [pjn], ["gdec"], scale=-1.0 / 16)
                act(Ft[2][:], pcu[0][:], AF.Exp, ["pcu0"], [F[2]], scale=-1.0 / 16)
                act(Ft[3][:], pcu[0][:], AF.Exp, ["pcu0"], [F[3]], scale=1.0 / 16)
                act(Ft[4][:], pcu[1][:], AF.Exp, ["pcu1"], [F[4]], scale=-1.0 / 16)

            def s_H1():
                act(Ft[1][:], Ft[5][:], AF.Ln, [F[5]], [F[1]], bias=1.0)

            def s_X4():
                transposes([(pt[:, (hd * 2 + mc) * 128:(hd * 2 + mc + 1) * 128], pb[:, hd, mc * 128:(mc + 1) * 128])
                            for hd in range(4) for mc in range(2)], HIM, ["pt"])
                copy("dve", pT[:], pt[:].rearrange("p (c m) -> p c m", c=8), ["pt"], ["qkT"])

            def s_G3a():
                pjt, pjn = proj(C_GQ, 512, None)
                stt(qb[:], pjt[:], float(128 ** -0.5), Ft[2][:], ALU.mult, ALU.mult, [pjn, F[2]], ["qb"])
                pjt, pjn = proj(C_GK, 512, None)
                tt("dve", kb[:], pjt[:], Ft[3][:], ALU.mult, [pjn, F[3]], ["kb"])
                tt("dve", khb[:], pjt[:], Ft[4][:], ALU.mult, [pjn, F[4]], ["khb"])

            def s_X5():
                items = []
                for hd in range(4):
                    for mc in range(2):
                        items.append((po[1][:, hd * 128:(hd + 1) * 128], pT[:, hd * 2 + mc, :],
                                      mv[:, mc, hd * 128:(hd + 1) * 128], mc == 0, mc == 1, {}))
                mm_multi(items, ["qkT", "mv"], ["po1"])
                for hd in range(4):
                    act(junk[:, hd * 256 + 128:hd * 256 + 256], po[1][:, hd * 128:(hd + 1) * 128], AF.Square, ["po1"], ["xssq%d" % hd, "jk%d" % hd],
                        accum_out=st[:, 80 + hd:81 + hd])

            def s_G3b():
                for half in range(2):
                    pjt, pjn = proj(C_GV + half * 512, 512, None)
                    copy("act", v[:, half * 512:(half + 1) * 512], pjt[:], [pjn], ["v"])

            def s_H2():
                tt("dve", Ft[0][:], Ft[5][:], lbL, ALU.mult, [F[5], "lbt"], [F[0]])
                act(Ft[0][:], Ft[0][:], AF.Ln, [F[0]], [F[0]], bias=1.0)
                tt("dve", Ft[5][:], Ft[0][:], Ft[1][:], ALU.subtract, [F[0], F[1]], [F[5]])

            def s_X6():
                tt("dve", st[:, 60:64], st[:, 56:60], st[:, 56:60], ALU.mult, ["xZ%d" % hd for hd in range(4)], ["xz2"])
                ts("dve", st[:, 84:88], st[:, 80:84], 1.0 / 128, None, ALU.mult, None,
                   ["xssq%d" % hd for hd in range(4)], ["xvv"])
                stt(st[:, 84:88], st[:, 60:64], EPS, st[:, 84:88], ALU.mult, ALU.add, ["xz2", "xvv"], ["xvv"])
                act(st[:, 84:88], st[:, 84:88], AF.Ln, ["xvv"], ["xvv"])
                act(st[:, 88:92], st[:, 84:88], AF.Exp, ["xvv"], ["xr"], scale=-0.5)
                for hd in range(4):
                    stt(mixed[:, 1536 + hd * 128:1536 + (hd + 1) * 128], po[1][:, hd * 128:(hd + 1) * 128],
                        st[:, 88 + hd:89 + hd], G[:, 1536 + hd * 128:1536 + (hd + 1) * 128], ALU.mult, ALU.mult,
                        ["po1", "xr", "G"], ["mixed"])

            def s_G4():
                qk_transposes()
                tt("dve", AT[:], pss[:].rearrange("p (c m) -> p c m", c=4), bc_mid(maskG, 4), ALU.mult,
                   ["pss", "mskb"], ["AT"])

            def s_H3a():
                act(Ft[0][:], Ft[5][:], AF.Exp, [F[5]], [F[0]])
                ts("dve", Ft[0][:], Ft[0][:], -1.0, 1.0, ALU.mult, ALU.add, [F[0]], [F[0]])

            def s_H3b():
                pjr, pjrn = next_pj()
                mm(pss[:], [(triH, Ft[5][:])], ["cst", F[5]], ["pss"])
                mm(pjr[:], [(triUH, Ft[5][:])], ["cst", F[5]], [pjrn])
                pjt, pjn = next_pj()
                mm_multi([(pjt[:, hd * 4:hd * 4 + 4], Ft[5][:, hd * 128:(hd + 1) * 128], cind[:, 1:5], True, True, {})
                          for hd in range(4)], ["cst", F[5]], [pjn])
                act(Ft[1][:], pss[:], AF.Exp, ["pss"], [F[1]])
                act(Ft[2][:], pss[:], AF.Exp, ["pss"], [F[2]], scale=-1.0)
                act(Ft[3][:], pjr[:], AF.Exp, [pjrn], [F[3]])
                act(st[:, 32:48], pjt[:, 0:16], AF.Exp, [pjn], ["hdec"])

            def s_G5a():
                items = []
                for hd in range(4):
                    o_ap = po[hd // 2][:, (hd % 2) * 256:(hd % 2 + 1) * 256]
                    items.append((o_ap, AT[:, hd, :], v[:, hd * 256:(hd + 1) * 256], True, False, {}))
                    items.append((o_ap, qkT[:, hd, :], gSb[:, hd, :], False, True, {}))
                mm_multi(items, ["AT", "v", "qkT", "gSb"], ["po0", "po1"])
                for hd in range(4):
                    o_ap = po[hd // 2][:, (hd % 2) * 256:(hd % 2 + 1) * 256]
                    act(junk[:, hd * 256:(hd + 1) * 256], o_ap, AF.Square, ["po%d" % (hd // 2)], ["gssq%d" % hd, "jk%d" % hd],
                        accum_out=st[:, 16 + hd:17 + hd])
                rstd_from(st[:, 16:20], st[:, 20:24], st[:, 24:28], 1.0 / 256, "gssq",
                          ["gssq%d" % hd for hd in range(4)])

            def s_G5b():
                mm_multi([(pcu[hd // 2][:, (hd % 2) * 256:(hd % 2 + 1) * 256], khb[:, hd * 128:(hd + 1) * 128],
                           v[:, hd * 256:(hd + 1) * 256], True, True, {}) for hd in range(4)],
                         ["khb", "v"], ["pcu0", "pcu1"])
                for hd in range(4):
                    stt(gS[:, hd, :], gS[:, hd, :], st[:, 8 + hd:9 + hd],
                        pcu[hd // 2][:, (hd % 2) * 256:(hd % 2 + 1) * 256], ALU.mult, ALU.add,
                        ["gS", "gdec", "pcu%d" % (hd // 2)], ["gS"])
                copy("act", gSb[:].rearrange("p a b -> p (a b)"), gS[:].rearrange("p a b -> p (a b)"),
                     ["gS"], ["gSb"])

            def s_G5c():
                for hd in range(4):
                    o_ap = po[hd // 2][:, (hd % 2) * 256:(hd % 2 + 1) * 256]
                    stt(mixed[:, hd * 256:(hd + 1) * 256], o_ap, st[:, 24 + hd:25 + hd],
                        G[:, hd * 256:(hd + 1) * 256], ALU.mult, ALU.mult,
                        ["po%d" % (hd // 2), "gssq_r", "G"], ["mixed"])

            def s_H4():
                pjt, pjn = proj(C_HQ, 512, None)
                tt("dve", qb[:], pjt[:], Ft[1][:], ALU.mult, [pjn, F[1]], ["qb"])
                tt("dve", kb[:], Ft[0][:], Ft[2][:], ALU.mult, [F[0], F[2]], ["kb"])
                tt("pool", khb[:], Ft[0][:], Ft[3][:], ALU.mult, [F[0], F[3]], ["khb"])
                pjt, pjn = proj(C_HI, 512, None)
                copy("act", hi[:], pjt[:], [pjn], ["hi"])

            ubank = [(pcu[0], "pcu0"), (pcu[1], "pcu1"), (pss, "pss"), (po[1], "po1")]

            def s_H5a():
                qk_transposes()
                tt("dve", AT[:], pss[:].rearrange("p (c m) -> p c m", c=4), bc_mid(maskH, 4), ALU.mult,
                   ["pss", "mskb"], ["AT"])

            def s_H5b():
                mm_multi([(po[0][:, hd * 128:(hd + 1) * 128], AT[:, hd, :], hi[:, hd * 128:(hd + 1) * 128],
                           hd == 0, False, {"skip_group_check": True}) for hd in range(4)],
                         ["AT", "hi"], ["po0"])
                for c4 in range(4):
                    pu, pun = ubank[c4]
                    mm_multi([(pu[:, hd * 128:(hd + 1) * 128], khb[32 * c4:32 * (c4 + 1), hd * 128:(hd + 1) * 128],
                               hi[32 * c4:32 * (c4 + 1), hd * 128:(hd + 1) * 128], True, True,
                               {"tile_position": (32 * c4, 0)}) for hd in range(4)],
                             ["khb", "hi"], [pun])

            def s_O2a(part):
                elist = [0, 1, 2, 3, 4, 5, 6, 7, 12, 13, 14, 15]
                es_ = elist[part * 3:(part + 1) * 3]
                items = []
                for half in range(2):
                    for e_ in es_:
                        items.append((pj[half][:], mT[:, e_, :], wout[:, e_, half * 512:(half + 1) * 512],
                                      e_ == 0, False, {"skip_group_check": True}))
                mm_multi(items, ["mTa", "mTb"] + WOUT, ["pj0", "pj1"])

            def s_H6():
                for c4 in range(4):
                    pu, pun = ubank[c4]
                    for hd in range(4):
                        sbn = "hSb%d_%d" % (hd, c4)
                        mm_multi([(po[0][32 * c4:32 * (c4 + 1), hd * 128:(hd + 1) * 128],
                                   qkT[:, hd, 32 * c4:32 * (c4 + 1)], hSb[:, hd, c4, :], False, c4 == 3,
                                   {"skip_group_check": True, "tile_position": (0, 32 * c4)})],
                                 ["qkT", sbn, "hSb"], ["po0"])
                        stt(hS[:, hd, :], hS[:, hd, :], st[:, 32 + hd * 4 + c4:33 + hd * 4 + c4],
                            pu[:, hd * 128:(hd + 1) * 128], ALU.mult, ALU.add,
                            ["hS%d" % hd, "hS", "hdec", pun], ["hS%d" % hd])
                        copy("act", hSb[:, hd, (c4 + 1) % 4, :], hS[:, hd, :], ["hS%d" % hd],
                             ["hSb%d_%d" % (hd, (c4 + 1) % 4)])
                    s_O2a(c4)
                for hd in range(4):
                    act(junk[:, hd * 256:hd * 256 + 128], po[0][:, hd * 128:(hd + 1) * 128], AF.Square, ["po0"], ["hssq%d" % hd, "jk%d" % hd],
                        accum_out=st[:, 64 + hd:65 + hd])
                rstd_from(st[:, 64:68], st[:, 68:72], st[:, 72:76], 1.0 / 128, "hssq",
                          ["hssq%d" % hd for hd in range(4)])
                for hd in range(4):
                    stt(mixed[:, 1024 + hd * 128:1024 + (hd + 1) * 128], po[0][:, hd * 128:(hd + 1) * 128],
                        st[:, 72 + hd:73 + hd], G[:, 1024 + hd * 128:1024 + (hd + 1) * 128], ALU.mult, ALU.mult,
                        ["po0", "hssq_r", "G"], ["mixed"])

            def s_O2(t):
                xb = xt[t % 2]
                xn = "xt%d" % (t % 2)
                rows = slice(t * 128, (t + 1) * 128)
                for half in range(2):
                    pjn = "pj%d" % half
                    mm_multi([(pj[half][:], mT[:, e_, :], wout[:, e_, half * 512:(half + 1) * 512], False, e_ == 11,
                               {"skip_group_check": True}) for e_ in range(8, 12)],
                             ["mTc"] + WOUT, [pjn])
                    tt("dve", xb[:, half * 512:(half + 1) * 512], xb[:, half * 512:(half + 1) * 512], pj[half][:],
                       ALU.add, [xn, pjn], [xn])
                pjc[0] = 0
                if do_final:
                    act(outt[:], xb[:], AF.Square, [xn], ["outt", "fssq"], accum_out=st[:, 4:5])
                    rstd_from(st[:, 4:5], st[:, 5:6], st[:, 6:7], 1.0 / D, "fssq")
                    stt(outt[:], xb[:], st[:, 6:7], fnw[:], ALU.mult, ALU.mult, [xn, "fssq_r", "fnw"], ["outt"])
                    dma("sp", dst_d[rows, :], outt[:], ["outt"], ["xmid%d" % t], "d_o")
                else:
                    dma("sp", dst_d[rows, :], xb[:], [xn], ["xmid%d" % t], "d_x%d" % (t % 2))

            LOAD(0)
            HEAD(0)
            for t in range(NT):
                nxt = t + 1 < NT
                if nxt:
                    LOAD(t + 1)
                s_X1(); s_G1a(); s_G3b(); s_Hp(); s_X2(); s_G1b(); s_X3(); s_G2(); s_H1(); s_X4(); s_G3a(); s_X5()
                s_H2()
                s_GT(); s_G4(); s_X6()
                if nxt:
                    HEAD_a(t + 1)
                s_H3a(); s_H3b(); s_G5a(); s_G5b()
                mixed_T(12, 16, "mTb")
                s_H4()
                if nxt:
                    HEAD_b(t + 1)
                s_H5a()
                s_G5c()
                s_H5b()
                mixed_T(0, 8, "mTa")
                s_H6()
                mixed_T(8, 12, "mTc")
                s_O2(t)

        P.emit(nc)
    return nc


def _consts():
    j = np.arange(128)[:, None]
    i = np.arange(128)[None, :]
    triG = (j <= i).astype(np.float32)
    triUG = (j > i).astype(np.float32)
    same = (j // 32) == (i // 32)
    triH = ((j <= i) & same).astype(np.float32)
    triUH = ((j > i) & same).astype(np.float32)
    cind = np.zeros((128, 8), np.float32)
    cind[:, 0] = 1.0
    for c in range(4):
        cind[:, 1 + c] = (np.arange(128) // 32 == c)
    cst = np.concatenate([triG, triUG, triH, triUH, cind, np.zeros((128, 6 * 128 + 8 - 520), np.float32)], axis=1)
    ident = np.eye(128, dtype=np.float32)
    maskG = triG
    maskH = triH
    msk = np.concatenate([ident, maskG, maskH], axis=1)
    return np.ascontiguousarray(cst), np.ascontiguousarray(msk)


def _layout_params(norm_w, gla_w_gate_up, gla_b_gate, gla_norm_w, hgrn_lower_bounds, hgrn_norm_w,
                   mem_norm_w, xattn_norm_w, final_norm_w):
    NL = norm_w.shape[0]
    pcols = np.zeros((NL, 128, 32), np.float32)
    for L in range(NL):
        pcols[L, :, 0:8] = norm_w[L].reshape(8, 128).T
        gw = np.concatenate([np.tile(gla_norm_w[L], 4), np.tile(hgrn_norm_w[L], 4), np.tile(xattn_norm_w[L], 4)])
        pcols[L, :, 8:24] = gw.reshape(16, 128).T
        pcols[L, :, 24:32] = mem_norm_w[L].reshape(8, 128).T
    wup = np.concatenate([gla_w_gate_up, gla_b_gate[:, None, :]], axis=1).astype(np.float32)
    lbraw = np.ascontiguousarray(np.broadcast_to(hgrn_lower_bounds.reshape(1, -1), (128, NL * 512))).astype(np.float32)
    fnw = np.ascontiguousarray(np.broadcast_to(final_norm_w.reshape(1, -1), (128, D))).astype(np.float32)
    return pcols, np.ascontiguousarray(wup), lbraw, fnw


_NC_CACHE = {}


def _get_nc(S, layers, final_norm):
    key = (S, tuple(layers), final_norm)
    if key not in _NC_CACHE:
        _NC_CACHE[key] = build(S, list(layers), final_norm)
    return _NC_CACHE[key]


def kernel(x, mem, norm_w, w_in, gla_w_gate_up, gla_b_gate, gla_norm_w, hgrn_lower_bounds,
           hgrn_norm_w, mem_norm_w, w_mem_kv, xattn_norm_w, w_out, final_norm_w):
    x = np.asarray(x, np.float32)
    mem = np.asarray(mem, np.float32)
    B, S, _ = x.shape
    f = lambda a: np.ascontiguousarray(np.asarray(a, np.float32))
    pcols, wup, lbraw, fnw = _layout_params(f(norm_w), f(gla_w_gate_up), f(gla_b_gate), f(gla_norm_w),
                                            f(hgrn_lower_bounds), f(hgrn_norm_w), f(mem_norm_w),
                                            f(xattn_norm_w), f(final_norm_w))
    cst, msk = _consts()
    nc = _get_nc(S, (0, 1), True)
    shared = {"w_in": f(w_in), "w_out": f(w_out), "w_kv": f(w_mem_kv), "wup": wup, "pcols": pcols,
              "lbraw": lbraw, "fnw": fnw, "cst": cst, "msk": msk}
    in_maps = []
    for b in range(B):
        m = dict(shared)
        m["x"] = np.ascontiguousarray(x[b])
        m["mem"] = np.ascontiguousarray(mem[b])
        in_maps.append(m)
    res = run_bass_kernel_spmd(nc, in_maps, core_ids=list(range(B)))
    return np.stack([np.asarray(r["out"], np.float32) for r in res.results], axis=0)
```

```python
import contextlib
import numpy as np
import ml_dtypes
import concourse.bass as bass
import concourse.mybir as mybir
from concourse.bass_utils import run_bass_kernel_spmd

F32 = mybir.dt.float32
BF16 = mybir.dt.bfloat16
AF = mybir.ActivationFunctionType
ALU = mybir.AluOpType
AX = mybir.AxisListType

D = 1024
DIN = 6160
DMIX = 2048
MEM = 256
EPS = 1e-6
C_GQ, C_GK, C_GV, C_GLR, C_HQ, C_HF, C_HI, C_XQ, C_GATE = 0, 512, 1024, 2048, 2064, 2576, 3088, 3600, 4112


class Prog:
    ENG = ("pe", "act", "dve", "pool", "sp")

    def __init__(self):
        self.q = {e: [] for e in self.ENG}
        self.cnt = {}
        self.res = {}
        self.waited = {e: {} for e in self.ENG}

    def op(self, eng, fn, reads=(), writes=(), dma=None):
        deps = {}

        def add(tok):
            if tok is None:
                return
            k, v = tok
            if deps.get(k, 0) < v:
                deps[k] = v

        for r in reads:
            st = self.res.get(r)
            if st:
                add(st[0])
        for w in writes:
            st = self.res.get(w)
            if st:
                add(st[0])
                for k, v in st[1].items():
                    add((k, v))
        if eng == "pe":
            deps.pop("pe", None)
        waits = []
        wd = self.waited[eng]
        for k, v in deps.items():
            if wd.get(k, 0) < v:
                wd[k] = v
                waits.append((k, v))
        key, amt = (dma, 16) if dma is not None else (eng, 1)
        self.cnt[key] = self.cnt.get(key, 0) + amt
        tok = (key, self.cnt[key])
        self.q[eng].append((waits, fn, key, amt))
        for r in reads:
            st = self.res.setdefault(r, [None, {}])
            if st[1].get(key, 0) < tok[1]:
                st[1][key] = tok[1]
        for w in writes:
            self.res[w] = [tok, {}]
        return tok

    def emit(self, nc):
        with contextlib.ExitStack() as es:
            sems = {k: es.enter_context(nc.semaphore("s_" + k)) for k in self.cnt}
            block = es.enter_context(nc.Block())
            final = [(k, v) for k, v in self.cnt.items()]

            def run(name, e):
                for waits, fn, key, amt in self.q[name]:
                    for k, v in waits:
                        e.wait_ge(sems[k], v)
                    ins = fn(e)
                    ins.then_inc(sems[key], amt)

            @block.tensor
            def _(e):
                run("pe", e)

            @block.scalar
            def _(e):
                run("act", e)

            @block.vector
            def _(e):
                run("dve", e)

            @block.gpsimd
            def _(e):
                run("pool", e)

            @block.sync
            def _(e):
                run("sp", e)
                for k, v in final:
                    e.wait_ge(sems[k], v)


def build(S, layers, final_norm, n_layers_total=2):
    nc = bass.Bass("TRN2", target_bir_lowering=False)
    P = Prog()
    NT = S // 128
    NL = n_layers_total

    def din(name, shape, dt=F32):
        return nc.dram_tensor(name, list(shape), dt, kind="ExternalInput").ap()

    x_d = din("x", [S, D])
    mem_d = din("mem", [MEM, D])
    win_d = din("w_in", [NL, D, DIN])
    wout_d = din("w_out", [NL, DMIX, D])
    wkv_d = din("w_kv", [NL, D, 2 * 512])
    wup_d = din("wup", [NL, 17, 512])
    pcols_d = din("pcols", [NL, 128, 32])
    lbraw_d = din("lbraw", [128, NL * 512])
    fnw_d = din("fnw", [128, D])
    cst_d = din("cst", [128, 6 * 128 + 8])
    msk_d = din("msk", [128, 3 * 128])
    out_d = nc.dram_tensor("out", [S, D], F32, kind="ExternalOutput").ap()
    xmid_d = None
    if len(layers) > 1:
        xmid_d = nc.dram_tensor("xmid", [S, D], F32, kind="Internal").ap()

    es = contextlib.ExitStack()
    with es:
        def sb(name, shape, dt):
            return es.enter_context(nc.sbuf_tensor("sb_" + name, list(shape), dt))

        def ps(name, shape, dt):
            return es.enter_context(nc.psum_tensor("ps_" + name, list(shape), dt))

        win = sb("win", [128, 8, DIN], BF16)
        wout = sb("wout", [128, 16, D], BF16)
        cst = sb("cst", [128, 4 * 128 + 8], F32)
        mskb = sb("mskb", [128, 3 * 128], BF16)
        pcols = sb("pcols", [128, 32], F32)
        wup = sb("wup", [32, 512], BF16)
        lbt = sb("lbt", [128, NL * 512], F32)
        fnw = sb("fnw", [128, D], F32)
        gS = sb("gS", [128, 4, 256], F32)
        gSb = sb("gSb", [128, 4, 256], BF16)
        hS = sb("hS", [128, 4, 128], F32)
        hSb = sb("hSb", [128, 4, 4, 128], BF16)
        mkT = sb("mkT", [128, 4, 256], BF16)
        mv = sb("mv", [128, 2, 512], BF16)
        xt = [sb("xt0", [128, D], F32), sb("xt1", [128, D], F32)]
        h = sb("h", [128, D], BF16)
        hT = sb("hT", [128, 8, 128], BF16)
        Ft = [sb("F%d" % i, [128, 512], F32) for i in range(6)]
        qb = sb("qb", [128, 512], BF16)
        kb = sb("kb", [128, 512], BF16)
        khb = sb("khb", [128, 512], BF16)
        qkT = sb("qkT", [128, 8, 128], BF16)
        v = sb("v", [128, 1024], BF16)
        hi = sb("hi", [128, 512], BF16)
        pbt = sb("pbt", [128, 4, 256], BF16)
        AT = sb("AT", [128, 4, 128], BF16)
        glrT = sb("glrT", [32, 128], BF16)
        G = sb("G", [128, DMIX], BF16)
        mixed = sb("mixed", [128, DMIX], BF16)
        outt = sb("outt", [128, D], F32)
        lbraw = outt
        xqs = qb
        xqT = AT
        pb = pbt
        HIM = ["pbt"]
        pT = qkT
        mT = G[:].rearrange("p (c m) -> p c m", c=16)
        st = sb("st", [128, 128], F32)
        junk = sb("junk", [128, 1024], BF16)
        dmy = sb("dmy", [128, 4], F32)

        pj = [ps("pj0", [128, 512], F32), ps("pj1", [128, 512], F32)]
        pt = ps("pt", [128, 1024], BF16)
        pcu = [ps("pcu0", [128, 512], F32), ps("pcu1", [128, 512], F32)]
        pss = ps("pss", [128, 512], F32)
        po = [ps("po0", [128, 512], F32), ps("po1", [128, 512], F32)]

        ident = mskb[:, 0:128]
        maskG = mskb[:, 128:256]
        maskH = mskb[:, 256:384]
        triG = cst[:, 0:128]
        triUG = cst[:, 128:256]
        triH = cst[:, 256:384]
        triUH = cst[:, 384:512]
        cind = cst[:, 512:520]
        nwT = pcols[:, 0:8]
        gwT = pcols[:, 8:24]
        mnwT = pcols[:, 24:32]

        pjc = [0]

        def next_pj():
            i = pjc[0] % 2
            pjc[0] += 1
            return pj[i], "pj%d" % i

        def bc(ap2, n):
            return ap2.unsqueeze(2).broadcast_to([128, ap2.shape[1], n])

        def bc_mid(ap2, n):
            return ap2.unsqueeze(1).broadcast_to([128, n, ap2.shape[1]])

        def mm(out_ap, pairs, reads, writes, first_start=True, **kw):
            pairs = list(pairs)

            def fn(e):
                n = len(pairs)
                ins = None
                for i, (a, b) in enumerate(pairs):
                    ins = e.matmul(out_ap, a, b, start=(first_start and i == 0), stop=(i == n - 1), **kw)
                return ins
            P.op("pe", fn, reads, writes)

        def mm_multi(items, reads, writes):
            items = list(items)

            def fn(e):
                ins = None
                for (o, a, b, s0, s1, kw) in items:
                    ins = e.matmul(o, a, b, start=s0, stop=s1, **kw)
                return ins
            P.op("pe", fn, reads, writes)

        def transposes(items, reads, writes):
            items = list(items)

            def fn(e):
                ins = None
                for (o, i_) in items:
                    ins = e.transpose(o, i_, ident)
                return ins
            P.op("pe", fn, list(reads) + ["mskb"], writes)

        def act(out, in_, func, reads, writes, **kw):
            P.op("act", lambda e: e.activation(out, in_, func, **kw), reads, writes)

        def tt(eng, out, in0, in1, op, reads, writes):
            P.op(eng, lambda e: e.tensor_tensor(out, in0, in1, op), reads, writes)

        def ts(eng, out, in0, s1, s2, op0, op1, reads, writes):
            if s2 is None:
                P.op(eng, lambda e: e.tensor_scalar(out, in0, s1, None, op0), reads, writes)
            else:
                P.op(eng, lambda e: e.tensor_scalar(out, in0, s1, s2, op0, op1), reads, writes)

        def stt(out, in0, scalar, in1, op0, op1, reads, writes):
            P.op("dve", lambda e: e.scalar_tensor_tensor(out, in0, scalar, in1, op0, op1), reads, writes)

        def copy(eng, out, in_, reads, writes):
            if eng == "act":
                P.op("act", lambda e: e.activation(out, in_, AF.Identity), reads, writes)
            else:
                P.op(eng, lambda e: e.tensor_copy(out, in_), reads, writes)

        def dma(eng, out, in_, reads, writes, sem, **kw):
            P.op(eng, lambda e: e.dma_start(out=out, in_=in_, **kw), reads, writes, dma=sem)

        def rstd_from(ssq_col, tmp_col, out_col, inv_n, rname, reads=None):
            act(tmp_col, ssq_col, AF.Ln, reads or [rname], [rname + "_t"], scale=inv_n, bias=EPS)
            act(out_col, tmp_col, AF.Exp, [rname + "_t"], [rname + "_r"], scale=-0.5)

        dma("sp", cst[:], cst_d[:, 0:520], [], ["cst"], "d_cst")
        dma("pool", mskb[:], msk_d, [], ["mskb"], "d_msk")
        dma("sp", lbraw[:], lbraw_d, [], ["outt"], "d_lbraw")
        if final_norm:
            dma("sp", fnw[:], fnw_d, [], ["fnw"], "d_fnw")
        P.op("pool", lambda e: e.memset(glrT[:], 1.0), [], ["glrT"])
        P.op("dve", lambda e: e.memset(dmy[:], 0.0), [], ["dmy"])
        lr3 = lbraw[:].rearrange("p (l n) -> p l n", l=NL)
        lb3 = lbt[:].rearrange("p (l n) -> p l n", l=NL)
        mx = Ft[0]
        P.op("dve", lambda e: e.tensor_copy(mx[:], lr3[:, 0, :]), ["outt"], ["F0"])
        for l in range(1, NL):
            tt("dve", mx[:], mx[:], lr3[:, l, :], ALU.max, ["F0", "outt"], ["F0"])
        for l in range(NL):
            tt("dve", lb3[:, l, :], lr3[:, l, :], mx[:], ALU.subtract, ["F0", "outt"], ["lbt"])
        act(lbt[:], lbt[:], AF.Exp, ["lbt"], ["lbt"])
        den = Ft[1]
        P.op("dve", lambda e: e.tensor_copy(den[:], lb3[:, 0, :]), ["lbt"], ["F1"])
        for l in range(1, NL):
            tt("dve", den[:], den[:], lb3[:, l, :], ALU.add, ["F1", "lbt"], ["F1"])
        P.op("dve", lambda e: e.reciprocal(den[:], den[:]), ["F1"], ["F1"])
        for l in range(NL):
            tt("dve", lb3[:, l, :], lb3[:, l, :], den[:], ALU.mult, ["F1", "lbt"], ["lbt"])
        p0 = Ft[2]
        P.op("dve", lambda e: e.tensor_copy(p0[:], lb3[:, 0, :]), ["lbt"], ["F2"])
        for l in range(1, NL):
            tt("dve", lb3[:, l, :], lb3[:, l, :], lb3[:, l - 1, :], ALU.add, ["lbt"], ["lbt"])
        for l in range(NL):
            tt("dve", lb3[:, l, :], lb3[:, l, :], p0[:], ALU.subtract, ["lbt", "F2"], ["lbt"])

        F = ["F%d" % i for i in range(6)]

        for li, L in enumerate(layers):
            last = (li == len(layers) - 1)
            src_d = x_d if li == 0 else xmid_d
            dst_d = out_d if last else xmid_d
            do_final = last and final_norm
            lbL = lbt[:, L * 512:(L + 1) * 512]

            dma("sp", pcols[:], pcols_d[L], [], ["pcols"], "d_pcols")
            dma("pool", wup[0:17, :], wup_d[L], [], ["wup"], "d_wup")
            wkv = wout[:, 0:8, :]
            for hf_ in range(2):
                dma("pool", wout[:, hf_ * 4:(hf_ + 1) * 4, :],
                    wkv_d[L, hf_ * 512:(hf_ + 1) * 512, :].rearrange("(c p) n -> p c n", p=128),
                    [], ["wq%d" % hf_], "d_wkv%d" % hf_, max_dma_last_dim=4096)
            for c in range(8):
                dma("pool", win[:, c, :], win_d[L, c * 128:(c + 1) * 128, :], [], ["win%d" % c], "d_win%d" % c,
                    max_dma_last_dim=4096)
            P.op("dve", lambda e: e.memset(gS[:], 0.0), [], ["gS"])
            P.op("dve", lambda e: e.memset(hS[:], 0.0), [], ["hS"])
            P.op("pool", lambda e: e.memset(gSb[:], 0.0), [], ["gSb"])
            P.op("pool", lambda e: e.memset(hSb[:], 0.0), [], ["hSb"])
            WIN = ["win%d" % c for c in range(8)]

            mnT = mixed[:].rearrange("p (c m) -> p c m", c=8)
            for blk in range(2):
                xb = xt[blk]
                xn = "xt%d" % blk
                dma("sp", xb[:], mem_d[blk * 128:(blk + 1) * 128, :], [], [xn], "d_x%d" % blk)
                act(h[:], xb[:], AF.Square, [xn], ["h", "ssq"], accum_out=st[:, 0:1])
                rstd_from(st[:, 0:1], st[:, 1:2], st[:, 2:3], 1.0 / D, "ssq")
                ts("dve", h[:], xb[:], st[:, 2:3], None, ALU.mult, None, [xn, "ssq_r"], ["h"])
                transposes([(pt[:, c * 128:(c + 1) * 128], h[:, c * 128:(c + 1) * 128]) for c in range(8)],
                           ["h"], ["pt"])
                tt("dve", mnT[:, :, blk * 128:(blk + 1) * 128], pt[:].rearrange("p (c m) -> p c m", c=8),
                   bc(mnwT, 128), ALU.mult, ["pt", "pcols"], ["mixed"])
            for hd in range(4):
                pjt, pjn = next_pj()
                mm(pjt[:, 0:256], [(wkv[:, c, hd * 128:(hd + 1) * 128], mnT[:, c, :]) for c in range(8)],
                   ["wq0", "wq1", "mixed"], [pjn])
                copy("act", mkT[:, hd, :], pjt[:, 0:256], [pjn], ["mkT"])
            for blk in range(2):
                pjt, pjn = next_pj()
                mm(pjt[:], [(mnT[:, c, blk * 128:(blk + 1) * 128], wkv[:, c, 512:1024]) for c in range(8)],
                   ["wq0", "wq1", "mixed"], [pjn])
                copy("act", mv[:, blk, :], pjt[:], [pjn], ["mv"])
            for q4 in range(4):
                dma("pool", wout[:, q4 * 4:(q4 + 1) * 4, :],
                    wout_d[L, q4 * 512:(q4 + 1) * 512, :].rearrange("(c p) n -> p c n", p=128),
                    [], ["wq%d" % q4], "d_wo%d" % q4, max_dma_last_dim=4096)
            WOUT = ["wq%d" % q4 for q4 in range(4)]

            def proj(col0, ncol, dst_names):
                pjt, pjn = next_pj()
                mm(pjt[:, 0:ncol], [(hT[:, c, :], win[:, c, col0:col0 + ncol]) for c in range(8)],
                   ["hT"] + WIN, [pjn])
                return pjt, pjn

            def LOAD(t):
                dma("sp", xt[t % 2][:], src_d[t * 128:(t + 1) * 128, :], ["xmid%d" % t] if li > 0 else [],
                    ["xt%d" % (t % 2)], "d_x%d" % (t % 2))

            def HEAD_a(t):
                xb = xt[t % 2]
                xn = "xt%d" % (t % 2)
                act(h[:], xb[:], AF.Square, [xn], ["h", "ssq"], accum_out=st[:, 0:1])
                rstd_from(st[:, 0:1], st[:, 1:2], st[:, 2:3], 1.0 / D, "ssq")
                ts("dve", h[:], xb[:], st[:, 2:3], None, ALU.mult, None, [xn, "ssq_r"], ["h"])

            def HEAD_b(t):
                transposes([(pt[:, c * 128:(c + 1) * 128], h[:, c * 128:(c + 1) * 128]) for c in range(8)],
                           ["h"], ["pt"])
                tt("dve", hT[:], pt[:].rearrange("p (c m) -> p c m", c=8), bc(nwT, 128), ALU.mult,
                   ["pt", "pcols"], ["hT"])

            def HEAD(t):
                HEAD_a(t)
                HEAD_b(t)

            def qk_transposes():
                transposes([(pt[:, hd * 128:(hd + 1) * 128], qb[:, hd * 128:(hd + 1) * 128]) for hd in range(4)] +
                           [(pt[:, (4 + hd) * 128:(5 + hd) * 128], kb[:, hd * 128:(hd + 1) * 128]) for hd in range(4)],
                           ["qb", "kb"], ["pt"])
                copy("dve", qkT[:], pt[:].rearrange("p (c m) -> p c m", c=8), ["pt"], ["qkT"])
                mm_multi([(pss[:, hd * 128:(hd + 1) * 128], qkT[:, 4 + hd, :], qkT[:, hd, :], True, True, {})
                          for hd in range(4)], ["qkT"], ["pss"])

            def mixed_T(e0, e1, rname):
                n = e1 - e0
                transposes([(pt[:, i_ * 128:(i_ + 1) * 128], mixed[:, (e0 + i_) * 128:(e0 + i_ + 1) * 128])
                            for i_ in range(n)], ["mixed"], ["pt"])
                tt("dve", mT[:, e0:e1, :], pt[:, 0:n * 128].rearrange("p (c m) -> p c m", c=n),
                   bc(gwT[:, e0:e1], 128), ALU.mult, ["pt", "pcols"], [rname])

            ctx = {}

            def s_GT():
                act(dmy[:, 0:1], dmy[:, 1:2], AF.Silu, ["dmy"], ["dmy"])
                for g4 in range(4):
                    pjt, pjn = proj(C_GATE + g4 * 512, 512, None)
                    act(G[:, g4 * 512:(g4 + 1) * 512], pjt[:], AF.Silu, [pjn], ["G", "mTa", "mTb", "mTc"])
                act(dmy[:, 0:1], dmy[:, 1:2], AF.Exp, ["dmy"], ["dmy"])

            def s_X1():
                pjt, pjn = proj(C_XQ, 512, None)
                act(xqs[:], pjt[:], AF.Identity, [pjn], ["qb"], scale=float(128 ** -0.5))

            def s_G1a():
                pjt, pjn = next_pj()
                mm(pjt[0:16, 0:128], [(win[:, c, C_GLR:C_GLR + 16], hT[:, c, :]) for c in range(8)],
                   ["hT"] + WIN, [pjn])
                copy("act", glrT[0:16, :], pjt[0:16, 0:128], [pjn], ["glrT"])

            def s_X2():
                transposes([(pt[:, hd * 128:(hd + 1) * 128], xqs[:, hd * 128:(hd + 1) * 128]) for hd in range(4)],
                           ["qb"], ["pt"])
                copy("dve", xqT[:], pt[:, 0:512].rearrange("p (c m) -> p c m", c=4), ["pt"], ["AT"])

            def s_G1b():
                pjt, pjn = next_pj()
                mm(pjt[:], [(glrT[0:17, :], wup[0:17, :])], ["glrT", "wup"], [pjn])
                act(Ft[0][:], pjt[:], AF.Exp, [pjn], [F[0]], scale=-1.0)
                act(Ft[1][:], Ft[0][:], AF.Ln, [F[0]], [F[1]], bias=1.0)

            def s_X3():
                mm_multi([(po[hd // 2][:, (hd % 2) * 256:(hd % 2 + 1) * 256], xqT[:, hd, :], mkT[:, hd, :],
                           True, True, {}) for hd in range(4)], ["AT", "mkT"], ["po0", "po1"])
                for half in range(2):
                    P.op("dve", (lambda half: lambda e: e.tensor_reduce(
                        st[:, 48 + 2 * half:50 + 2 * half], po[half][:].rearrange("p (a b) -> p a b", a=2),
                        AX.X, ALU.max))(half), ["po%d" % half], ["xmax%d" % half])
                ts("dve", st[:, 52:56], st[:, 48:52], -1.0, None, ALU.mult, None, ["xmax0", "xmax1"], ["xnmax"])
                for hd in range(4):
                    act(pb[:, hd, :], po[hd // 2][:, (hd % 2) * 256:(hd % 2 + 1) * 256], AF.Exp,
                        ["po%d" % (hd // 2), "xnmax"], HIM + ["xZ%d" % hd], bias=st[:, 52 + hd:53 + hd],
                        accum_out=st[:, 56 + hd:57 + hd])

            def s_Hp():
                pjz, pjzn = proj(C_HF, 512, None)
                act(Ft[5][:], pjz[:], AF.Exp, [pjzn], [F[5]], scale=-1.0)

            def s_G2():
                mm(pcu[0][:], [(triG, Ft[1][:])], ["cst", F[1]], ["pcu0"])
                mm(pcu[1][:], [(triUG, Ft[1][:])], ["cst", F[1]], ["pcu1"])
                pjt, pjn = next_pj()
                mm_multi([(pjt[:, hd:hd + 1], Ft[1][:, hd * 128:(hd + 1) * 128], cind[:, 0:1], True, True, {})
                          for hd in range(4)], ["cst", F[1]], [pjn])
                act(st[:, 8:12], pjt[:, 0:4], AF.Exp, [pjn], ["gdec"], scale=-1.0 / 16)
                act(Ft[2][:], pcu[0][:], AF.Exp, ["pcu0"], [F[2]], scale=-1.0 / 16)
                act(Ft[3][:], pcu[0][:], AF.Exp, ["pcu0"], [F[3]], scale=1.0 / 16)
                act(Ft[4][:], pcu[1][:], AF.Exp, ["pcu1"], [F[4]], scale=-1.0 / 16)

            def s_H1():
                act(Ft[1][:], Ft[5][:], AF.Ln, [F[5]], [F[1]], bias=1.0)

            def s_X4():
                transposes([(pt[:, (hd * 2 + mc) * 128:(hd * 2 + mc + 1) * 128], pb[:, hd, mc * 128:(mc + 1) * 128])
                            for hd in range(4) for mc in range(2)], HIM, ["pt"])
                copy("dve", pT[:], pt[:].rearrange("p (c m) -> p c m", c=8), ["pt"], ["qkT"])

            def s_G3a():
                pjt, pjn = proj(C_GQ, 512, None)
                stt(qb[:], pjt[:], float(128 ** -0.5), Ft[2][:], ALU.mult, ALU.mult, [pjn, F[2]], ["qb"])
                pjt, pjn = proj(C_GK, 512, None)
                tt("dve", kb[:], pjt[:], Ft[3][:], ALU.mult, [pjn, F[3]], ["kb"])
                tt("dve", khb[:], pjt[:], Ft[4][:], ALU.mult, [pjn, F[4]], ["khb"])

            def s_X5():
                items = []
                for hd in range(4):
                    for mc in range(2):
                        items.append((po[1][:, hd * 128:(hd + 1) * 128], pT[:, hd * 2 + mc, :],
                                      mv[:, mc, hd * 128:(hd + 1) * 128], mc == 0, mc == 1, {}))
                mm_multi(items, ["qkT", "mv"], ["po1"])
                for hd in range(4):
                    act(junk[:, hd * 256 + 128:hd * 256 + 256], po[1][:, hd * 128:(hd + 1) * 128], AF.Square, ["po1"], ["xssq%d" % hd, "jk%d" % hd],
                        accum_out=st[:, 80 + hd:81 + hd])

            def s_G3b():
                for half in range(2):
                    pjt, pjn = proj(C_GV + half * 512, 512, None)
                    copy("act", v[:, half * 512:(half + 1) * 512], pjt[:], [pjn], ["v"])

            def s_H2():
                tt("dve", Ft[0][:], Ft[5][:], lbL, ALU.mult, [F[5], "lbt"], [F[0]])
                act(Ft[0][:], Ft[0][:], AF.Ln, [F[0]], [F[0]], bias=1.0)
                tt("dve", Ft[5][:], Ft[0][:], Ft[1][:], ALU.subtract, [F[0], F[1]], [F[5]])

            def s_X6():
                tt("dve", st[:, 60:64], st[:, 56:60], st[:, 56:60], ALU.mult, ["xZ%d" % hd for hd in range(4)], ["xz2"])
                ts("dve", st[:, 84:88], st[:, 80:84], 1.0 / 128, None, ALU.mult, None,
                   ["xssq%d" % hd for hd in range(4)], ["xvv"])
                stt(st[:, 84:88], st[:, 60:64], EPS, st[:, 84:88], ALU.mult, ALU.add, ["xz2", "xvv"], ["xvv"])
                act(st[:, 84:88], st[:, 84:88], AF.Ln, ["xvv"], ["xvv"])
                act(st[:, 88:92], st[:, 84:88], AF.Exp, ["xvv"], ["xr"], scale=-0.5)
                for hd in range(4):
                    stt(mixed[:, 1536 + hd * 128:1536 + (hd + 1) * 128], po[1][:, hd * 128:(hd + 1) * 128],
                        st[:, 88 + hd:89 + hd], G[:, 1536 + hd * 128:1536 + (hd + 1) * 128], ALU.mult, ALU.mult,
                        ["po1", "xr", "G"], ["mixed"])

            def s_G4():
                qk_transposes()
                tt("dve", AT[:], pss[:].rearrange("p (c m) -> p c m", c=4), bc_mid(maskG, 4), ALU.mult,
                   ["pss", "mskb"], ["AT"])

            def s_H3a():
                act(Ft[0][:], Ft[5][:], AF.Exp, [F[5]], [F[0]])
                ts("dve", Ft[0][:], Ft[0][:], -1.0, 1.0, ALU.mult, ALU.add, [F[0]], [F[0]])

            def s_H3b():
                pjr, pjrn = next_pj()
                mm(pss[:], [(triH, Ft[5][:])], ["cst", F[5]], ["pss"])
                mm(pjr[:], [(triUH, Ft[5][:])], ["cst", F[5]], [pjrn])
                pjt, pjn = next_pj()
                mm_multi([(pjt[:, hd * 4:hd * 4 + 4], Ft[5][:, hd * 128:(hd + 1) * 128], cind[:, 1:5], True, True, {})
                          for hd in range(4)], ["cst", F[5]], [pjn])
                act(Ft[1][:], pss[:], AF.Exp, ["pss"], [F[1]])
                act(Ft[2][:], pss[:], AF.Exp, ["pss"], [F[2]], scale=-1.0)
                act(Ft[3][:], pjr[:], AF.Exp, [pjrn], [F[3]])
                act(st[:, 32:48], pjt[:, 0:16], AF.Exp, [pjn], ["hdec"])

            def s_G5a():
                items = []
                for hd in range(4):
                    o_ap = po[hd // 2][:, (hd % 2) * 256:(hd % 2 + 1) * 256]
                    items.append((o_ap, AT[:, hd, :], v[:, hd * 256:(hd + 1) * 256], True, False, {}))
                    items.append((o_ap, qkT[:, hd, :], gSb[:, hd, :], False, True, {}))
                mm_multi(items, ["AT", "v", "qkT", "gSb"], ["po0", "po1"])
                for hd in range(4):
                    o_ap = po[hd // 2][:, (hd % 2) * 256:(hd % 2 + 1) * 256]
                    act(junk[:, hd * 256:(hd + 1) * 256], o_ap, AF.Square, ["po%d" % (hd // 2)], ["gssq%d" % hd, "jk%d" % hd],
                        accum_out=st[:, 16 + hd:17 + hd])
                rstd_from(st[:, 16:20], st[:, 20:24], st[:, 24:28], 1.0 / 256, "gssq",
                          ["gssq%d" % hd for hd in range(4)])

            def s_G5b():
                mm_multi([(pcu[hd // 2][:, (hd % 2) * 256:(hd % 2 + 1) * 256], khb[:, hd * 128:(hd + 1) * 128],
                           v[:, hd * 256:(hd + 1) * 256], True, True, {}) for hd in range(4)],
                         ["khb", "v"], ["pcu0", "pcu1"])
                for hd in range(4):
                    stt(gS[:, hd, :], gS[:, hd, :], st[:, 8 + hd:9 + hd],
                        pcu[hd // 2][:, (hd % 2) * 256:(hd % 2 + 1) * 256], ALU.mult, ALU.add,
                        ["gS", "gdec", "pcu%d" % (hd // 2)], ["gS"])
                copy("act", gSb[:].rearrange("p a b -> p (a b)"), gS[:].rearrange("p a b -> p (a b)"),
                     ["gS"], ["gSb"])

            def s_G5c():
                for hd in range(4):
                    o_ap = po[hd // 2][:, (hd % 2) * 256:(hd % 2 + 1) * 256]
                    act(Ft[2 + hd // 2][:, (hd % 2) * 256:(hd % 2 + 1) * 256], o_ap, AF.Identity,
                        ["po%d" % (hd // 2), "gssq_r"], [F[2 + hd // 2]], scale=st[:, 24 + hd:25 + hd])
                for pr in range(2):
                    tt("pool", mixed[:, pr * 512:(pr + 1) * 512], Ft[2 + pr][:], G[:, pr * 512:(pr + 1) * 512],
                       ALU.mult, [F[2 + pr], "G"], ["mixed"])

            def s_H4():
                pjt, pjn = proj(C_HQ, 512, None)
                tt("dve", qb[:], pjt[:], Ft[1][:], ALU.mult, [pjn, F[1]], ["qb"])
                tt("dve", kb[:], Ft[0][:], Ft[2][:], ALU.mult, [F[0], F[2]], ["kb"])
                tt("pool", khb[:], Ft[0][:], Ft[3][:], ALU.mult, [F[0], F[3]], ["khb"])
                pjt, pjn = proj(C_HI, 512, None)
                copy("act", hi[:], pjt[:], [pjn], ["hi"])

            ubank = [(pcu[0], "pcu0"), (pcu[1], "pcu1"), (pss, "pss"), (po[1], "po1")]

            def s_H5a():
                qk_transposes()
                tt("dve", AT[:], pss[:].rearrange("p (c m) -> p c m", c=4), bc_mid(maskH, 4), ALU.mult,
                   ["pss", "mskb"], ["AT"])

            def s_H5b():
                mm_multi([(po[0][:, hd * 128:(hd + 1) * 128], AT[:, hd, :], hi[:, hd * 128:(hd + 1) * 128],
                           hd == 0, False, {"skip_group_check": True}) for hd in range(4)],
                         ["AT", "hi"], ["po0"])
                for c4 in range(4):
                    pu, pun = ubank[c4]
                    mm_multi([(pu[:, hd * 128:(hd + 1) * 128], khb[32 * c4:32 * (c4 + 1), hd * 128:(hd + 1) * 128],
                               hi[32 * c4:32 * (c4 + 1), hd * 128:(hd + 1) * 128], True, True,
                               {"tile_position": (32 * c4, 0)}) for hd in range(4)],
                             ["khb", "hi"], [pun])

            def s_O2a(part):
                elist = [0, 1, 2, 3, 4, 5, 6, 7, 12, 13, 14, 15]
                es_ = elist[part * 3:(part + 1) * 3]
                items = []
                for half in range(2):
                    for e_ in es_:
                        items.append((pj[half][:], mT[:, e_, :], wout[:, e_, half * 512:(half + 1) * 512],
                                      e_ == 0, False, {"skip_group_check": True}))
                mm_multi(items, ["mTa", "mTb"] + WOUT, ["pj0", "pj1"])

            def s_H6():
                for c4 in range(4):
                    pu, pun = ubank[c4]
                    for hd in range(4):
                        sbn = "hSb%d_%d" % (hd, c4)
                        mm_multi([(po[0][32 * c4:32 * (c4 + 1), hd * 128:(hd + 1) * 128],
                                   qkT[:, hd, 32 * c4:32 * (c4 + 1)], hSb[:, hd, c4, :], False, c4 == 3,
                                   {"skip_group_check": True, "tile_position": (0, 32 * c4)})],
                                 ["qkT", sbn, "hSb"], ["po0"])
                        stt(hS[:, hd, :], hS[:, hd, :], st[:, 32 + hd * 4 + c4:33 + hd * 4 + c4],
                            pu[:, hd * 128:(hd + 1) * 128], ALU.mult, ALU.add,
                            ["hS%d" % hd, "hS", "hdec", pun], ["hS%d" % hd])
                        copy("act", hSb[:, hd, (c4 + 1) % 4, :], hS[:, hd, :], ["hS%d" % hd],
                             ["hSb%d_%d" % (hd, (c4 + 1) % 4)])
                    s_O2a(c4)
                for hd in range(4):
                    act(junk[:, hd * 256:hd * 256 + 128], po[0][:, hd * 128:(hd + 1) * 128], AF.Square, ["po0"], ["hssq%d" % hd, "jk%d" % hd],
                        accum_out=st[:, 64 + hd:65 + hd])
                rstd_from(st[:, 64:68], st[:, 68:72], st[:, 72:76], 1.0 / 128, "hssq",
                          ["hssq%d" % hd for hd in range(4)])
                for hd in range(4):
                    stt(mixed[:, 1024 + hd * 128:1024 + (hd + 1) * 128], po[0][:, hd * 128:(hd + 1) * 128],
                        st[:, 72 + hd:73 + hd], G[:, 1024 + hd * 128:1024 + (hd + 1) * 128], ALU.mult, ALU.mult,
                        ["po0", "hssq_r", "G"], ["mixed"])

            def s_O2(t):
                xb = xt[t % 2]
                xn = "xt%d" % (t % 2)
                rows = slice(t * 128, (t + 1) * 128)
                for half in range(2):
                    pjn = "pj%d" % half
                    mm_multi([(pj[half][:], mT[:, e_, :], wout[:, e_, half * 512:(half + 1) * 512], False, e_ == 11,
                               {"skip_group_check": True}) for e_ in range(8, 12)],
                             ["mTc"] + WOUT, [pjn])
                    tt("dve", xb[:, half * 512:(half + 1) * 512], xb[:, half * 512:(half + 1) * 512], pj[half][:],
                       ALU.add, [xn, pjn], [xn])
                pjc[0] = 0
                if do_final:
                    act(outt[:], xb[:], AF.Square, [xn], ["outt", "fssq"], accum_out=st[:, 4:5])
                    rstd_from(st[:, 4:5], st[:, 5:6], st[:, 6:7], 1.0 / D, "fssq")
                    stt(outt[:], xb[:], st[:, 6:7], fnw[:], ALU.mult, ALU.mult, [xn, "fssq_r", "fnw"], ["outt"])
                    dma("sp", dst_d[rows, :], outt[:], ["outt"], ["xmid%d" % t], "d_o")
                else:
                    dma("sp", dst_d[rows, :], xb[:], [xn], ["xmid%d" % t], "d_x%d" % (t % 2))

            LOAD(0)
            HEAD(0)
            for t in range(NT):
                nxt = t + 1 < NT
                if nxt:
                    LOAD(t + 1)
                s_X1(); s_G1a(); s_G3b(); s_Hp(); s_X2(); s_G1b(); s_X3(); s_G2(); s_H1(); s_X4(); s_G3a(); s_X5()
                s_H2()
                s_GT(); s_G4(); s_X6()
                if nxt:
                    HEAD_a(t + 1)
                s_H3a(); s_H3b(); s_G5a(); s_G5b()
                mixed_T(12, 16, "mTb")
                s_H4()
                if nxt:
                    HEAD_b(t + 1)
                s_H5a()
                s_G5c()
                s_H5b()
                mixed_T(0, 8, "mTa")
                s_H6()
                mixed_T(8, 12, "mTc")
                s_O2(t)

        P.emit(nc)
    return nc


def _consts():
    j = np.arange(128)[:, None]
    i = np.arange(128)[None, :]
    triG = (j <= i).astype(np.float32)
    triUG = (j > i).astype(np.float32)
    same = (j // 32) == (i // 32)
    triH = ((j <= i) & same).astype(np.float32)
    triUH = ((j > i) & same).astype(np.float32)
    cind = np.zeros((128, 8), np.float32)
    cind[:, 0] = 1.0
    for c in range(4):
        cind[:, 1 + c] = (np.arange(128) // 32 == c)
    cst = np.concatenate([triG, triUG, triH, triUH, cind, np.zeros((128, 6 * 128 + 8 - 520), np.float32)], axis=1)
    ident = np.eye(128, dtype=np.float32)
    maskG = triG
    maskH = triH
    msk = np.concatenate([ident, maskG, maskH], axis=1)
    return np.ascontiguousarray(cst), np.ascontiguousarray(msk)


def _layout_params(norm_w, gla_w_gate_up, gla_b_gate, gla_norm_w, hgrn_lower_bounds, hgrn_norm_w,
                   mem_norm_w, xattn_norm_w, final_norm_w):
    NL = norm_w.shape[0]
    pcols = np.zeros((NL, 128, 32), np.float32)
    for L in range(NL):
        pcols[L, :, 0:8] = norm_w[L].reshape(8, 128).T
        gw = np.concatenate([np.tile(gla_norm_w[L], 4), np.tile(hgrn_norm_w[L], 4), np.tile(xattn_norm_w[L], 4)])
        pcols[L, :, 8:24] = gw.reshape(16, 128).T
        pcols[L, :, 24:32] = mem_norm_w[L].reshape(8, 128).T
    wup = np.concatenate([gla_w_gate_up, gla_b_gate[:, None, :]], axis=1).astype(np.float32)
    lbraw = np.ascontiguousarray(np.broadcast_to(hgrn_lower_bounds.reshape(1, -1), (128, NL * 512))).astype(np.float32)
    fnw = np.ascontiguousarray(np.broadcast_to(final_norm_w.reshape(1, -1), (128, D))).astype(np.float32)
    return pcols, np.ascontiguousarray(wup), lbraw, fnw


_NC_CACHE = {}


def _get_nc(S, layers, final_norm):
    key = (S, tuple(layers), final_norm)
    if key not in _NC_CACHE:
        _NC_CACHE[key] = build(S, list(layers), final_norm)
    return _NC_CACHE[key]


def kernel(x, mem, norm_w, w_in, gla_w_gate_up, gla_b_gate, gla_norm_w, hgrn_lower_bounds,
           hgrn_norm_w, mem_norm_w, w_mem_kv, xattn_norm_w, w_out, final_norm_w):
    x = np.asarray(x, np.float32)
    mem = np.asarray(mem, np.float32)
    B, S, _ = x.shape
    f = lambda a: np.ascontiguousarray(np.asarray(a, np.float32))
    pcols, wup, lbraw, fnw = _layout_params(f(norm_w), f(gla_w_gate_up), f(gla_b_gate), f(gla_norm_w),
                                            f(hgrn_lower_bounds), f(hgrn_norm_w), f(mem_norm_w),
                                            f(xattn_norm_w), f(final_norm_w))
    cst, msk = _consts()
    nc = _get_nc(S, (0, 1), True)
    shared = {"w_in": f(w_in), "w_out": f(w_out), "w_kv": f(w_mem_kv), "wup": wup, "pcols": pcols,
              "lbraw": lbraw, "fnw": fnw, "cst": cst, "msk": msk}
    in_maps = []
    for b in range(B):
        m = dict(shared)
        m["x"] = np.ascontiguousarray(x[b])
        m["mem"] = np.ascontiguousarray(mem[b])
        in_maps.append(m)
    res = run_bass_kernel_spmd(nc, in_maps, core_ids=list(range(B)))
    return np.stack([np.asarray(r["out"], np.float32) for r in res.results], axis=0)
```

```python
import contextlib
import numpy as np
import ml_dtypes
import concourse.bass as bass
import concourse.mybir as mybir
from concourse.bass_utils import run_bass_kernel_spmd

F32 = mybir.dt.float32
BF16 = mybir.dt.bfloat16
AF = mybir.ActivationFunctionType
ALU = mybir.AluOpType
AX = mybir.AxisListType

D = 1024
DIN = 6160
DMIX = 2048
MEM = 256
EPS = 1e-6
C_GQ, C_GK, C_GV, C_GLR, C_HQ, C_HF, C_HI, C_XQ, C_GATE = 0, 512, 1024, 2048, 2064, 2576, 3088, 3600, 4112


class Prog:
    ENG = ("pe", "act", "dve", "pool", "sp")

    def __init__(self):
        self.q = {e: [] for e in self.ENG}
        self.cnt = {}
        self.res = {}
        self.waited = {e: {} for e in self.ENG}

    def op(self, eng, fn, reads=(), writes=(), dma=None):
        deps = {}

        def add(tok):
            if tok is None:
                return
            k, v = tok
            if deps.get(k, 0) < v:
                deps[k] = v

        for r in reads:
            st = self.res.get(r)
            if st:
                add(st[0])
        for w in writes:
            st = self.res.get(w)
            if st:
                add(st[0])
                for k, v in st[1].items():
                    add((k, v))
        if eng == "pe":
            deps.pop("pe", None)
        waits = []
        wd = self.waited[eng]
        for k, v in deps.items():
            if wd.get(k, 0) < v:
                wd[k] = v
                waits.append((k, v))
        key, amt = (dma, 16) if dma is not None else (eng, 1)
        self.cnt[key] = self.cnt.get(key, 0) + amt
        tok = (key, self.cnt[key])
        self.q[eng].append((waits, fn, key, amt))
        for r in reads:
            st = self.res.setdefault(r, [None, {}])
            if st[1].get(key, 0) < tok[1]:
                st[1][key] = tok[1]
        for w in writes:
            self.res[w] = [tok, {}]
        return tok

    def emit(self, nc):
        with contextlib.ExitStack() as es:
            sems = {k: es.enter_context(nc.semaphore("s_" + k)) for k in self.cnt}
            block = es.enter_context(nc.Block())
            final = [(k, v) for k, v in self.cnt.items()]

            def run(name, e):
                for waits, fn, key, amt in self.q[name]:
                    for k, v in waits:
                        e.wait_ge(sems[k], v)
                    ins = fn(e)
                    ins.then_inc(sems[key], amt)

            @block.tensor
            def _(e):
                run("pe", e)

            @block.scalar
            def _(e):
                run("act", e)

            @block.vector
            def _(e):
                run("dve", e)

            @block.gpsimd
            def _(e):
                run("pool", e)

            @block.sync
            def _(e):
                run("sp", e)
                for k, v in final:
                    e.wait_ge(sems[k], v)


def build(S, layers, final_norm, n_layers_total=2):
    nc = bass.Bass("TRN2", target_bir_lowering=False)
    P = Prog()
    NT = S // 128
    NL = n_layers_total

    def din(name, shape, dt=F32):
        return nc.dram_tensor(name, list(shape), dt, kind="ExternalInput").ap()

    x_d = din("x", [S, D])
    mem_d = din("mem", [MEM, D])
    win_d = din("w_in", [NL, D, DIN])
    wout_d = din("w_out", [NL, DMIX, D])
    wkv_d = din("w_kv", [NL, D, 2 * 512])
    wup_d = din("wup", [NL, 17, 512])
    pcols_d = din("pcols", [NL, 128, 32])
    lbraw_d = din("lbraw", [128, NL * 512])
    fnw_d = din("fnw", [128, D])
    cst_d = din("cst", [128, 6 * 128 + 8])
    msk_d = din("msk", [128, 3 * 128])
    out_d = nc.dram_tensor("out", [S, D], F32, kind="ExternalOutput").ap()
    xmid_d = None
    if len(layers) > 1:
        xmid_d = nc.dram_tensor("xmid", [S, D], F32, kind="Internal").ap()

    es = contextlib.ExitStack()
    with es:
        def sb(name, shape, dt):
            return es.enter_context(nc.sbuf_tensor("sb_" + name, list(shape), dt))

        def ps(name, shape, dt):
            return es.enter_context(nc.psum_tensor("ps_" + name, list(shape), dt))

        win = sb("win", [128, 8, DIN], BF16)
        wout = sb("wout", [128, 16, D], BF16)
        cst = sb("cst", [128, 4 * 128 + 8], F32)
        mskb = sb("mskb", [128, 3 * 128], BF16)
        pcols = sb("pcols", [128, 32], F32)
        wup = sb("wup", [32, 512], BF16)
        lbt = sb("lbt", [128, NL * 512], F32)
        fnw = sb("fnw", [128, D], F32)
        gS = sb("gS", [128, 4, 256], F32)
        gSb = sb("gSb", [128, 4, 256], BF16)
        hS = sb("hS", [128, 4, 128], F32)
        hSb = sb("hSb", [128, 4, 4, 128], BF16)
        mkT = sb("mkT", [128, 4, 256], BF16)
        mv = sb("mv", [128, 2, 512], BF16)
        xt = [sb("xt0", [128, D], F32), sb("xt1", [128, D], F32)]
        h = sb("h", [128, D], BF16)
        hT = sb("hT", [128, 8, 128], BF16)
        Ft = [sb("F%d" % i, [128, 512], F32) for i in range(6)]
        qb = sb("qb", [128, 512], BF16)
        kb = sb("kb", [128, 512], BF16)
        khb = sb("khb", [128, 512], BF16)
        qkT = sb("qkT", [128, 8, 128], BF16)
        v = sb("v", [128, 1024], BF16)
        hi = sb("hi", [128, 512], BF16)
        pbt = sb("pbt", [128, 4, 256], BF16)
        AT = sb("AT", [128, 4, 128], BF16)
        glrT = sb("glrT", [32, 128], BF16)
        G = sb("G", [128, DMIX], BF16)
        mixed = sb("mixed", [128, DMIX], BF16)
        outt = sb("outt", [128, D], F32)
        lbraw = outt
        xqs = qb
        xqT = AT
        pb = pbt
        HIM = ["pbt"]
        pT = qkT
        mT = G[:].rearrange("p (c m) -> p c m", c=16)
        st = sb("st", [128, 128], F32)
        junk = sb("junk", [128, 1024], BF16)
        dmy = sb("dmy", [128, 4], F32)

        pj = [ps("pj0", [128, 512], F32), ps("pj1", [128, 512], F32)]
        pt = ps("pt", [128, 1024], BF16)
        pcu = [ps("pcu0", [128, 512], F32), ps("pcu1", [128, 512], F32)]
        pss = ps("pss", [128, 512], F32)
        po = [ps("po0", [128, 512], F32), ps("po1", [128, 512], F32)]

        ident = mskb[:, 0:128]
        maskG = mskb[:, 128:256]
        maskH = mskb[:, 256:384]
        triG = cst[:, 0:128]
        triUG = cst[:, 128:256]
        triH = cst[:, 256:384]
        triUH = cst[:, 384:512]
        cind = cst[:, 512:520]
        nwT = pcols[:, 0:8]
        gwT = pcols[:, 8:24]
        mnwT = pcols[:, 24:32]

        pjc = [0]

        def next_pj():
            i = pjc[0] % 2
            pjc[0] += 1
            return pj[i], "pj%d" % i

        def bc(ap2, n):
            return ap2.unsqueeze(2).broadcast_to([128, ap2.shape[1], n])

        def bc_mid(ap2, n):
            return ap2.unsqueeze(1).broadcast_to([128, n, ap2.shape[1]])

        def mm(out_ap, pairs, reads, writes, first_start=True, **kw):
            pairs = list(pairs)

            def fn(e):
                n = len(pairs)
                ins = None
                for i, (a, b) in enumerate(pairs):
                    ins = e.matmul(out_ap, a, b, start=(first_start and i == 0), stop=(i == n - 1), **kw)
                return ins
            P.op("pe", fn, reads, writes)

        def mm_multi(items, reads, writes):
            items = list(items)

            def fn(e):
                ins = None
                for (o, a, b, s0, s1, kw) in items:
                    ins = e.matmul(o, a, b, start=s0, stop=s1, **kw)
                return ins
            P.op("pe", fn, reads, writes)

        def transposes(items, reads, writes):
            items = list(items)

            def fn(e):
                ins = None
                for (o, i_) in items:
                    ins = e.transpose(o, i_, ident)
                return ins
            P.op("pe", fn, list(reads) + ["mskb"], writes)

        def act(out, in_, func, reads, writes, **kw):
            P.op("act", lambda e: e.activation(out, in_, func, **kw), reads, writes)

        def tt(eng, out, in0, in1, op, reads, writes):
            P.op(eng, lambda e: e.tensor_tensor(out, in0, in1, op), reads, writes)

        def ts(eng, out, in0, s1, s2, op0, op1, reads, writes):
            if s2 is None:
                P.op(eng, lambda e: e.tensor_scalar(out, in0, s1, None, op0), reads, writes)
            else:
                P.op(eng, lambda e: e.tensor_scalar(out, in0, s1, s2, op0, op1), reads, writes)

        def stt(out, in0, scalar, in1, op0, op1, reads, writes):
            P.op("dve", lambda e: e.scalar_tensor_tensor(out, in0, scalar, in1, op0, op1), reads, writes)

        def copy(eng, out, in_, reads, writes):
            if eng == "act":
                P.op("act", lambda e: e.activation(out, in_, AF.Identity), reads, writes)
            else:
                P.op(eng, lambda e: e.tensor_copy(out, in_), reads, writes)

        def dma(eng, out, in_, reads, writes, sem, **kw):
            P.op(eng, lambda e: e.dma_start(out=out, in_=in_, **kw), reads, writes, dma=sem)

        def rstd_from(ssq_col, tmp_col, out_col, inv_n, rname, reads=None):
            act(tmp_col, ssq_col, AF.Ln, reads or [rname], [rname + "_t"], scale=inv_n, bias=EPS)
            act(out_col, tmp_col, AF.Exp, [rname + "_t"], [rname + "_r"], scale=-0.5)

        dma("sp", cst[:], cst_d[:, 0:520], [], ["cst"], "d_cst")
        dma("pool", mskb[:], msk_d, [], ["mskb"], "d_msk")
        dma("sp", lbraw[:], lbraw_d, [], ["outt"], "d_lbraw")
        if final_norm:
            dma("sp", fnw[:], fnw_d, [], ["fnw"], "d_fnw")
        P.op("pool", lambda e: e.memset(glrT[:], 1.0), [], ["glrT"])
        P.op("dve", lambda e: e.memset(dmy[:], 0.0), [], ["dmy"])
        lr3 = lbraw[:].rearrange("p (l n) -> p l n", l=NL)
        lb3 = lbt[:].rearrange("p (l n) -> p l n", l=NL)
        mx = Ft[0]
        P.op("dve", lambda e: e.tensor_copy(mx[:], lr3[:, 0, :]), ["outt"], ["F0"])
        for l in range(1, NL):
            tt("dve", mx[:], mx[:], lr3[:, l, :], ALU.max, ["F0", "outt"], ["F0"])
        for l in range(NL):
            tt("dve", lb3[:, l, :], lr3[:, l, :], mx[:], ALU.subtract, ["F0", "outt"], ["lbt"])
        act(lbt[:], lbt[:], AF.Exp, ["lbt"], ["lbt"])
        den = Ft[1]
        P.op("dve", lambda e: e.tensor_copy(den[:], lb3[:, 0, :]), ["lbt"], ["F1"])
        for l in range(1, NL):
            tt("dve", den[:], den[:], lb3[:, l, :], ALU.add, ["F1", "lbt"], ["F1"])
        P.op("dve", lambda e: e.reciprocal(den[:], den[:]), ["F1"], ["F1"])
        for l in range(NL):
            tt("dve", lb3[:, l, :], lb3[:, l, :], den[:], ALU.mult, ["F1", "lbt"], ["lbt"])
        p0 = Ft[2]
        P.op("dve", lambda e: e.tensor_copy(p0[:], lb3[:, 0, :]), ["lbt"], ["F2"])
        for l in range(1, NL):
            tt("dve", lb3[:, l, :], lb3[:, l, :], lb3[:, l - 1, :], ALU.add, ["lbt"], ["lbt"])
        for l in range(NL):
            tt("dve", lb3[:, l, :], lb3[:, l, :], p0[:], ALU.subtract, ["lbt", "F2"], ["lbt"])

        F = ["F%d" % i for i in range(6)]

        for li, L in enumerate(layers):
            last = (li == len(layers) - 1)
            src_d = x_d if li == 0 else xmid_d
            dst_d = out_d if last else xmid_d
            do_final = last and final_norm
            lbL = lbt[:, L * 512:(L + 1) * 512]

            dma("sp", pcols[:], pcols_d[L], [], ["pcols"], "d_pcols")
            dma("pool", wup[0:17, :], wup_d[L], [], ["wup"], "d_wup")
            wkv = wout[:, 0:8, :]
            for hf_ in range(2):
                dma("pool", wout[:, hf_ * 4:(hf_ + 1) * 4, :],
                    wkv_d[L, hf_ * 512:(hf_ + 1) * 512, :].rearrange("(c p) n -> p c n", p=128),
                    [], ["wq%d" % hf_], "d_wkv%d" % hf_, max_dma_last_dim=4096)
            wblocks = [(C_XQ, 512, ["wc%d" % C_XQ]), (C_GLR, 528, ["wc%d" % C_GLR, "wc%d" % C_HQ]),
                       (C_GV, 512, ["wc%d" % C_GV]), (C_GV + 512, 512, ["wc%d" % (C_GV + 512)]),
                       (C_HF, 512, ["wc%d" % C_HF]), (C_GQ, 512, ["wc%d" % C_GQ]), (C_GK, 512, ["wc%d" % C_GK])] + \
                      [(C_GATE + g * 512, 512, ["wc%d" % (C_GATE + g * 512)]) for g in range(4)] + \
                      [(C_HI, 512, ["wc%d" % C_HI])]
            for bi, (c0, ncb, rn) in enumerate(wblocks):
                dma("pool", win[:, :, c0:c0 + ncb],
                    win_d[L, :, c0:c0 + ncb].rearrange("(c p) n -> p c n", p=128), [], rn, "d_win%d" % bi,
                    max_dma_last_dim=4096)
            P.op("dve", lambda e: e.memset(gS[:], 0.0), [], ["gS"])
            P.op("dve", lambda e: e.memset(hS[:], 0.0), [], ["hS"])
            P.op("pool", lambda e: e.memset(gSb[:], 0.0), [], ["gSb"])
            P.op("pool", lambda e: e.memset(hSb[:], 0.0), [], ["hSb"])

            mnT = mixed[:].rearrange("p (c m) -> p c m", c=8)
            for blk in range(2):
                xb = xt[blk]
                xn = "xt%d" % blk
                dma("sp", xb[:], mem_d[blk * 128:(blk + 1) * 128, :], [], [xn], "d_x%d" % blk)
                act(h[:], xb[:], AF.Square, [xn], ["h", "ssq"], accum_out=st[:, 0:1])
                rstd_from(st[:, 0:1], st[:, 1:2], st[:, 2:3], 1.0 / D, "ssq")
                ts("dve", h[:], xb[:], st[:, 2:3], None, ALU.mult, None, [xn, "ssq_r"], ["h"])
                transposes([(pt[:, c * 128:(c + 1) * 128], h[:, c * 128:(c + 1) * 128]) for c in range(8)],
                           ["h"], ["pt"])
                tt("dve", mnT[:, :, blk * 128:(blk + 1) * 128], pt[:].rearrange("p (c m) -> p c m", c=8),
                   bc(mnwT, 128), ALU.mult, ["pt", "pcols"], ["mixed"])
            for hd in range(4):
                pjt, pjn = next_pj()
                mm(pjt[:, 0:256], [(wkv[:, c, hd * 128:(hd + 1) * 128], mnT[:, c, :]) for c in range(8)],
                   ["wq0", "wq1", "mixed"], [pjn])
                copy("act", mkT[:, hd, :], pjt[:, 0:256], [pjn], ["mkT"])
            for blk in range(2):
                pjt, pjn = next_pj()
                mm(pjt[:], [(mnT[:, c, blk * 128:(blk + 1) * 128], wkv[:, c, 512:1024]) for c in range(8)],
                   ["wq0", "wq1", "mixed"], [pjn])
                copy("act", mv[:, blk, :], pjt[:], [pjn], ["mv"])
            for q4 in range(4):
                dma("pool", wout[:, q4 * 4:(q4 + 1) * 4, :],
                    wout_d[L, q4 * 512:(q4 + 1) * 512, :].rearrange("(c p) n -> p c n", p=128),
                    [], ["wq%d" % q4], "d_wo%d" % q4, max_dma_last_dim=4096)
            WOUT = ["wq%d" % q4 for q4 in range(4)]

            def proj(col0, ncol, dst_names):
                pjt, pjn = next_pj()
                mm(pjt[:, 0:ncol], [(hT[:, c, :], win[:, c, col0:col0 + ncol]) for c in range(8)],
                   ["hT", "wc%d" % col0], [pjn])
                return pjt, pjn

            def LOAD(t):
                dma("sp", xt[t % 2][:], src_d[t * 128:(t + 1) * 128, :], ["xmid%d" % t] if li > 0 else [],
                    ["xt%d" % (t % 2)], "d_x%d" % (t % 2))

            def HEAD_a(t):
                xb = xt[t % 2]
                xn = "xt%d" % (t % 2)
                act(h[:], xb[:], AF.Square, [xn], ["h", "ssq"], accum_out=st[:, 0:1])
                rstd_from(st[:, 0:1], st[:, 1:2], st[:, 2:3], 1.0 / D, "ssq")
                ts("dve", h[:], xb[:], st[:, 2:3], None, ALU.mult, None, [xn, "ssq_r"], ["h"])

            def HEAD_b(t):
                transposes([(pt[:, c * 128:(c + 1) * 128], h[:, c * 128:(c + 1) * 128]) for c in range(8)],
                           ["h"], ["pt"])
                tt("dve", hT[:], pt[:].rearrange("p (c m) -> p c m", c=8), bc(nwT, 128), ALU.mult,
                   ["pt", "pcols"], ["hT"])

            def HEAD(t):
                HEAD_a(t)
                HEAD_b(t)

            def qk_transposes():
                transposes([(pt[:, hd * 128:(hd + 1) * 128], qb[:, hd * 128:(hd + 1) * 128]) for hd in range(4)] +
                           [(pt[:, (4 + hd) * 128:(5 + hd) * 128], kb[:, hd * 128:(hd + 1) * 128]) for hd in range(4)],
                           ["qb", "kb"], ["pt"])
                copy("dve", qkT[:], pt[:].rearrange("p (c m) -> p c m", c=8), ["pt"], ["qkT"])
                mm_multi([(pss[:, hd * 128:(hd + 1) * 128], qkT[:, 4 + hd, :], qkT[:, hd, :], True, True, {})
                          for hd in range(4)], ["qkT"], ["pss"])

            def mixed_T(e0, e1, rname):
                n = e1 - e0
                transposes([(pt[:, i_ * 128:(i_ + 1) * 128], mixed[:, (e0 + i_) * 128:(e0 + i_ + 1) * 128])
                            for i_ in range(n)], ["mixed"], ["pt"])
                tt("dve", mT[:, e0:e1, :], pt[:, 0:n * 128].rearrange("p (c m) -> p c m", c=n),
                   bc(gwT[:, e0:e1], 128), ALU.mult, ["pt", "pcols"], [rname])

            ctx = {}

            def s_GT():
                act(dmy[:, 0:1], dmy[:, 1:2], AF.Silu, ["dmy"], ["dmy"])
                for g4 in range(4):
                    pjt, pjn = proj(C_GATE + g4 * 512, 512, None)
                    act(G[:, g4 * 512:(g4 + 1) * 512], pjt[:], AF.Silu, [pjn], ["G", "mTa", "mTb", "mTc"])
                act(dmy[:, 0:1], dmy[:, 1:2], AF.Exp, ["dmy"], ["dmy"])

            def s_X1():
                pjt, pjn = proj(C_XQ, 512, None)
                act(xqs[:], pjt[:], AF.Identity, [pjn], ["qb"], scale=float(128 ** -0.5))

            def s_G1a():
                pjt, pjn = next_pj()
                mm(pjt[0:16, 0:128], [(win[:, c, C_GLR:C_GLR + 16], hT[:, c, :]) for c in range(8)],
                   ["hT", "wc%d" % C_GLR], [pjn])
                copy("act", glrT[0:16, :], pjt[0:16, 0:128], [pjn], ["glrT"])

            def s_X2():
                transposes([(pt[:, hd * 128:(hd + 1) * 128], xqs[:, hd * 128:(hd + 1) * 128]) for hd in range(4)],
                           ["qb"], ["pt"])
                copy("dve", xqT[:], pt[:, 0:512].rearrange("p (c m) -> p c m", c=4), ["pt"], ["AT"])

            def s_G1b():
                pjt, pjn = next_pj()
                mm(pjt[:], [(glrT[0:17, :], wup[0:17, :])], ["glrT", "wup"], [pjn])
                act(Ft[0][:], pjt[:], AF.Exp, [pjn], [F[0]], scale=-1.0)
                act(Ft[1][:], Ft[0][:], AF.Ln, [F[0]], [F[1]], bias=1.0)

            def s_X3():
                mm_multi([(po[hd // 2][:, (hd % 2) * 256:(hd % 2 + 1) * 256], xqT[:, hd, :], mkT[:, hd, :],
                           True, True, {}) for hd in range(4)], ["AT", "mkT"], ["po0", "po1"])
                for half in range(2):
                    P.op("dve", (lambda half: lambda e: e.tensor_reduce(
                        st[:, 48 + 2 * half:50 + 2 * half], po[half][:].rearrange("p (a b) -> p a b", a=2),
                        AX.X, ALU.max))(half), ["po%d" % half], ["xmax%d" % half])
                ts("dve", st[:, 52:56], st[:, 48:52], -1.0, None, ALU.mult, None, ["xmax0", "xmax1"], ["xnmax"])
                for hd in range(4):
                    act(pb[:, hd, :], po[hd // 2][:, (hd % 2) * 256:(hd % 2 + 1) * 256], AF.Exp,
                        ["po%d" % (hd // 2), "xnmax"], HIM + ["xZ%d" % hd], bias=st[:, 52 + hd:53 + hd],
                        accum_out=st[:, 56 + hd:57 + hd])

            def s_Hp():
                pjz, pjzn = proj(C_HF, 512, None)
                act(Ft[5][:], pjz[:], AF.Exp, [pjzn], [F[5]], scale=-1.0)

            def s_G2():
                mm(pcu[0][:], [(triG, Ft[1][:])], ["cst", F[1]], ["pcu0"])
                mm(pcu[1][:], [(triUG, Ft[1][:])], ["cst", F[1]], ["pcu1"])
                pjt, pjn = next_pj()
                mm_multi([(pjt[:, hd:hd + 1], Ft[1][:, hd * 128:(hd + 1) * 128], cind[:, 0:1], True, True, {})
                          for hd in range(4)], ["cst", F[1]], [pjn])
                act(st[:, 8:12], pjt[:, 0:4], AF.Exp, [pjn], ["gdec"], scale=-1.0 / 16)
                act(Ft[2][:], pcu[0][:], AF.Exp, ["pcu0"], [F[2]], scale=-1.0 / 16)
                act(Ft[3][:], pcu[0][:], AF.Exp, ["pcu0"], [F[3]], scale=1.0 / 16)
                act(Ft[4][:], pcu[1][:], AF.Exp, ["pcu1"], [F[4]], scale=-1.0 / 16)

            def s_H1():
                act(Ft[1][:], Ft[5][:], AF.Ln, [F[5]], [F[1]], bias=1.0)

            def s_X4():
                transposes([(pt[:, (hd * 2 + mc) * 128:(hd * 2 + mc + 1) * 128], pb[:, hd, mc * 128:(mc + 1) * 128])
                            for hd in range(4) for mc in range(2)], HIM, ["pt"])
                copy("dve", pT[:], pt[:].rearrange("p (c m) -> p c m", c=8), ["pt"], ["qkT"])

            def s_G3a():
                pjt, pjn = proj(C_GQ, 512, None)
                stt(qb[:], pjt[:], float(128 ** -0.5), Ft[2][:], ALU.mult, ALU.mult, [pjn, F[2]], ["qb"])
                pjt, pjn = proj(C_GK, 512, None)
                tt("dve", kb[:], pjt[:], Ft[3][:], ALU.mult, [pjn, F[3]], ["kb"])
                tt("dve", khb[:], pjt[:], Ft[4][:], ALU.mult, [pjn, F[4]], ["khb"])

            def s_X5():
                items = []
                for hd in range(4):
                    for mc in range(2):
                        items.append((po[1][:, hd * 128:(hd + 1) * 128], pT[:, hd * 2 + mc, :],
                                      mv[:, mc, hd * 128:(hd + 1) * 128], mc == 0, mc == 1, {}))
                mm_multi(items, ["qkT", "mv"], ["po1"])
                for hd in range(4):
                    act(junk[:, hd * 256 + 128:hd * 256 + 256], po[1][:, hd * 128:(hd + 1) * 128], AF.Square, ["po1"], ["xssq%d" % hd, "jk%d" % hd],
                        accum_out=st[:, 80 + hd:81 + hd])

            def s_G3b():
                for half in range(2):
                    pjt, pjn = proj(C_GV + half * 512, 512, None)
                    copy("act", v[:, half * 512:(half + 1) * 512], pjt[:], [pjn], ["v"])

            def s_H2():
                tt("dve", Ft[0][:], Ft[5][:], lbL, ALU.mult, [F[5], "lbt"], [F[0]])
                act(Ft[0][:], Ft[0][:], AF.Ln, [F[0]], [F[0]], bias=1.0)
                tt("dve", Ft[5][:], Ft[0][:], Ft[1][:], ALU.subtract, [F[0], F[1]], [F[5]])

            def s_X6():
                tt("dve", st[:, 60:64], st[:, 56:60], st[:, 56:60], ALU.mult, ["xZ%d" % hd for hd in range(4)], ["xz2"])
                ts("dve", st[:, 84:88], st[:, 80:84], 1.0 / 128, None, ALU.mult, None,
                   ["xssq%d" % hd for hd in range(4)], ["xvv"])
                stt(st[:, 84:88], st[:, 60:64], EPS, st[:, 84:88], ALU.mult, ALU.add, ["xz2", "xvv"], ["xvv"])
                act(st[:, 84:88], st[:, 84:88], AF.Ln, ["xvv"], ["xvv"])
                act(st[:, 88:92], st[:, 84:88], AF.Exp, ["xvv"], ["xr"], scale=-0.5)
                for hd in range(4):
                    stt(mixed[:, 1536 + hd * 128:1536 + (hd + 1) * 128], po[1][:, hd * 128:(hd + 1) * 128],
                        st[:, 88 + hd:89 + hd], G[:, 1536 + hd * 128:1536 + (hd + 1) * 128], ALU.mult, ALU.mult,
                        ["po1", "xr", "G"], ["mixed"])

            def s_G4():
                qk_transposes()
                tt("dve", AT[:], pss[:].rearrange("p (c m) -> p c m", c=4), bc_mid(maskG, 4), ALU.mult,
                   ["pss", "mskb"], ["AT"])

            def s_H3a():
                act(Ft[0][:], Ft[5][:], AF.Exp, [F[5]], [F[0]])
                ts("dve", Ft[0][:], Ft[0][:], -1.0, 1.0, ALU.mult, ALU.add, [F[0]], [F[0]])

            def s_H3b():
                pjr, pjrn = next_pj()
                mm(pss[:], [(triH, Ft[5][:])], ["cst", F[5]], ["pss"])
                mm(pjr[:], [(triUH, Ft[5][:])], ["cst", F[5]], [pjrn])
                pjt, pjn = next_pj()
                mm_multi([(pjt[:, hd * 4:hd * 4 + 4], Ft[5][:, hd * 128:(hd + 1) * 128], cind[:, 1:5], True, True, {})
                          for hd in range(4)], ["cst", F[5]], [pjn])
                act(Ft[1][:], pss[:], AF.Exp, ["pss"], [F[1]])
                act(Ft[2][:], pss[:], AF.Exp, ["pss"], [F[2]], scale=-1.0)
                act(Ft[3][:], pjr[:], AF.Exp, [pjrn], [F[3]])
                act(st[:, 32:48], pjt[:, 0:16], AF.Exp, [pjn], ["hdec"])

            def s_G5a():
                items = []
                for hd in range(4):
                    o_ap = po[hd // 2][:, (hd % 2) * 256:(hd % 2 + 1) * 256]
                    items.append((o_ap, AT[:, hd, :], v[:, hd * 256:(hd + 1) * 256], True, False, {}))
                    items.append((o_ap, qkT[:, hd, :], gSb[:, hd, :], False, True, {}))
                mm_multi(items, ["AT", "v", "qkT", "gSb"], ["po0", "po1"])
                for hd in range(4):
                    o_ap = po[hd // 2][:, (hd % 2) * 256:(hd % 2 + 1) * 256]
                    act(junk[:, hd * 256:(hd + 1) * 256], o_ap, AF.Square, ["po%d" % (hd // 2)], ["gssq%d" % hd, "jk%d" % hd],
                        accum_out=st[:, 16 + hd:17 + hd])
                rstd_from(st[:, 16:20], st[:, 20:24], st[:, 24:28], 1.0 / 256, "gssq",
                          ["gssq%d" % hd for hd in range(4)])

            def s_G5b():
                mm_multi([(pcu[hd // 2][:, (hd % 2) * 256:(hd % 2 + 1) * 256], khb[:, hd * 128:(hd + 1) * 128],
                           v[:, hd * 256:(hd + 1) * 256], True, True, {}) for hd in range(4)],
                         ["khb", "v"], ["pcu0", "pcu1"])
                for hd in range(4):
                    stt(gS[:, hd, :], gS[:, hd, :], st[:, 8 + hd:9 + hd],
                        pcu[hd // 2][:, (hd % 2) * 256:(hd % 2 + 1) * 256], ALU.mult, ALU.add,
                        ["gS", "gdec", "pcu%d" % (hd // 2)], ["gS"])
                copy("act", gSb[:].rearrange("p a b -> p (a b)"), gS[:].rearrange("p a b -> p (a b)"),
                     ["gS"], ["gSb"])

            def s_G5c():
                for hd in range(4):
                    o_ap = po[hd // 2][:, (hd % 2) * 256:(hd % 2 + 1) * 256]
                    act(Ft[2 + hd // 2][:, (hd % 2) * 256:(hd % 2 + 1) * 256], o_ap, AF.Identity,
                        ["po%d" % (hd // 2), "gssq_r"], [F[2 + hd // 2]], scale=st[:, 24 + hd:25 + hd])
                for pr in range(2):
                    tt("pool", mixed[:, pr * 512:(pr + 1) * 512], Ft[2 + pr][:], G[:, pr * 512:(pr + 1) * 512],
                       ALU.mult, [F[2 + pr], "G"], ["mixed"])

            def s_H4():
                pjt, pjn = proj(C_HQ, 512, None)
                tt("dve", qb[:], pjt[:], Ft[1][:], ALU.mult, [pjn, F[1]], ["qb"])
                tt("dve", kb[:], Ft[0][:], Ft[2][:], ALU.mult, [F[0], F[2]], ["kb"])
                tt("pool", khb[:], Ft[0][:], Ft[3][:], ALU.mult, [F[0], F[3]], ["khb"])
                pjt, pjn = proj(C_HI, 512, None)
                copy("act", hi[:], pjt[:], [pjn], ["hi"])

            ubank = [(pcu[0], "pcu0"), (pcu[1], "pcu1"), (pss, "pss"), (po[1], "po1")]

            def s_H5a():
                qk_transposes()
                tt("dve", AT[:], pss[:].rearrange("p (c m) -> p c m", c=4), bc_mid(maskH, 4), ALU.mult,
                   ["pss", "mskb"], ["AT"])

            def s_H5b():
                mm_multi([(po[0][:, hd * 128:(hd + 1) * 128], AT[:, hd, :], hi[:, hd * 128:(hd + 1) * 128],
                           hd == 0, False, {"skip_group_check": True}) for hd in range(4)],
                         ["AT", "hi"], ["po0"])
                for c4 in range(4):
                    pu, pun = ubank[c4]
                    mm_multi([(pu[:, hd * 128:(hd + 1) * 128], khb[32 * c4:32 * (c4 + 1), hd * 128:(hd + 1) * 128],
                               hi[32 * c4:32 * (c4 + 1), hd * 128:(hd + 1) * 128], True, True,
                               {"tile_position": (32 * c4, 0)}) for hd in range(4)],
                             ["khb", "hi"], [pun])

            def s_O2a(part):
                elist = [0, 1, 2, 3, 4, 5, 6, 7, 12, 13, 14, 15]
                es_ = elist[part * 3:(part + 1) * 3]
                items = []
                for half in range(2):
                    for e_ in es_:
                        items.append((pj[half][:], mT[:, e_, :], wout[:, e_, half * 512:(half + 1) * 512],
                                      e_ == 0, False, {"skip_group_check": True}))
                mm_multi(items, ["mTa", "mTb"] + WOUT, ["pj0", "pj1"])

            def s_H6():
                for c4 in range(4):
                    pu, pun = ubank[c4]
                    for hd in range(4):
                        sbn = "hSb%d_%d" % (hd, c4)
                        mm_multi([(po[0][32 * c4:32 * (c4 + 1), hd * 128:(hd + 1) * 128],
                                   qkT[:, hd, 32 * c4:32 * (c4 + 1)], hSb[:, hd, c4, :], False, c4 == 3,
                                   {"skip_group_check": True, "tile_position": (0, 32 * c4)})],
                                 ["qkT", sbn, "hSb"], ["po0"])
                        stt(hS[:, hd, :], hS[:, hd, :], st[:, 32 + hd * 4 + c4:33 + hd * 4 + c4],
                            pu[:, hd * 128:(hd + 1) * 128], ALU.mult, ALU.add,
                            ["hS%d" % hd, "hS", "hdec", pun], ["hS%d" % hd])
                        copy("act", hSb[:, hd, (c4 + 1) % 4, :], hS[:, hd, :], ["hS%d" % hd],
                             ["hSb%d_%d" % (hd, (c4 + 1) % 4)])
                    s_O2a(c4)
                for hd in range(4):
                    act(junk[:, hd * 256:hd * 256 + 128], po[0][:, hd * 128:(hd + 1) * 128], AF.Square, ["po0"], ["hssq%d" % hd, "jk%d" % hd],
                        accum_out=st[:, 64 + hd:65 + hd])
                rstd_from(st[:, 64:68], st[:, 68:72], st[:, 72:76], 1.0 / 128, "hssq",
                          ["hssq%d" % hd for hd in range(4)])
                for hd in range(4):
                    stt(mixed[:, 1024 + hd * 128:1024 + (hd + 1) * 128], po[0][:, hd * 128:(hd + 1) * 128],
                        st[:, 72 + hd:73 + hd], G[:, 1024 + hd * 128:1024 + (hd + 1) * 128], ALU.mult, ALU.mult,
                        ["po0", "hssq_r", "G"], ["mixed"])

            def s_O2(t):
                xb = xt[t % 2]
                xn = "xt%d" % (t % 2)
                rows = slice(t * 128, (t + 1) * 128)
                for half in range(2):
                    pjn = "pj%d" % half
                    mm_multi([(pj[half][:], mT[:, e_, :], wout[:, e_, half * 512:(half + 1) * 512], False, e_ == 11,
                               {"skip_group_check": True}) for e_ in range(8, 12)],
                             ["mTc"] + WOUT, [pjn])
                    tt("dve", xb[:, half * 512:(half + 1) * 512], xb[:, half * 512:(half + 1) * 512], pj[half][:],
                       ALU.add, [xn, pjn], [xn])
                pjc[0] = 0
                if do_final:
                    act(outt[:], xb[:], AF.Square, [xn], ["outt", "fssq"], accum_out=st[:, 4:5])
                    rstd_from(st[:, 4:5], st[:, 5:6], st[:, 6:7], 1.0 / D, "fssq")
                    stt(outt[:], xb[:], st[:, 6:7], fnw[:], ALU.mult, ALU.mult, [xn, "fssq_r", "fnw"], ["outt"])
                    dma("sp", dst_d[rows, :], outt[:], ["outt"], ["xmid%d" % t], "d_o")
                else:
                    dma("sp", dst_d[rows, :], xb[:], [xn], ["xmid%d" % t], "d_x%d" % (t % 2))

            LOAD(0)
            HEAD(0)
            for t in range(NT):
                nxt = t + 1 < NT
                if nxt:
                    LOAD(t + 1)
                s_X1(); s_G1a(); s_G3b(); s_Hp(); s_X2(); s_G1b(); s_X3(); s_G2(); s_H1(); s_X4(); s_G3a(); s_X5()
                s_H2()
                s_GT(); s_G4(); s_X6()
                if nxt:
                    HEAD_a(t + 1)
                s_H3a(); s_H3b(); s_G5a(); s_G5b()
                mixed_T(12, 16, "mTb")
                s_H4()
                if nxt:
                    HEAD_b(t + 1)
                s_H5a()
                s_G5c()
                s_H5b()
                mixed_T(0, 8, "mTa")
                s_H6()
                mixed_T(8, 12, "mTc")
                s_O2(t)

        P.emit(nc)
    return nc


def _consts():
    j = np.arange(128)[:, None]
    i = np.arange(128)[None, :]
    triG = (j <= i).astype(np.float32)
    triUG = (j > i).astype(np.float32)
    same = (j // 32) == (i // 32)
    triH = ((j <= i) & same).astype(np.float32)
    triUH = ((j > i) & same).astype(np.float32)
    cind = np.zeros((128, 8), np.float32)
    cind[:, 0] = 1.0
    for c in range(4):
        cind[:, 1 + c] = (np.arange(128) // 32 == c)
    cst = np.concatenate([triG, triUG, triH, triUH, cind, np.zeros((128, 6 * 128 + 8 - 520), np.float32)], axis=1)
    ident = np.eye(128, dtype=np.float32)
    maskG = triG
    maskH = triH
    msk = np.concatenate([ident, maskG, maskH], axis=1)
    return np.ascontiguousarray(cst), np.ascontiguousarray(msk)


def _layout_params(norm_w, gla_w_gate_up, gla_b_gate, gla_norm_w, hgrn_lower_bounds, hgrn_norm_w,
                   mem_norm_w, xattn_norm_w, final_norm_w):
    NL = norm_w.shape[0]
    pcols = np.zeros((NL, 128, 32), np.float32)
    for L in range(NL):
        pcols[L, :, 0:8] = norm_w[L].reshape(8, 128).T
        gw = np.concatenate([np.tile(gla_norm_w[L], 4), np.tile(hgrn_norm_w[L], 4), np.tile(xattn_norm_w[L], 4)])
        pcols[L, :, 8:24] = gw.reshape(16, 128).T
        pcols[L, :, 24:32] = mem_norm_w[L].reshape(8, 128).T
    wup = np.concatenate([gla_w_gate_up, gla_b_gate[:, None, :]], axis=1).astype(np.float32)
    lbraw = np.ascontiguousarray(np.broadcast_to(hgrn_lower_bounds.reshape(1, -1), (128, NL * 512))).astype(np.float32)
    fnw = np.ascontiguousarray(np.broadcast_to(final_norm_w.reshape(1, -1), (128, D))).astype(np.float32)
    return pcols, np.ascontiguousarray(wup), lbraw, fnw


_NC_CACHE = {}


def _get_nc(S, layers, final_norm):
    key = (S, tuple(layers), final_norm)
    if key not in _NC_CACHE:
        _NC_CACHE[key] = build(S, list(layers), final_norm)
    return _NC_CACHE[key]


def kernel(x, mem, norm_w, w_in, gla_w_gate_up, gla_b_gate, gla_norm_w, hgrn_lower_bounds,
           hgrn_norm_w, mem_norm_w, w_mem_kv, xattn_norm_w, w_out, final_norm_w):
    x = np.asarray(x, np.float32)
    mem = np.asarray(mem, np.float32)
    B, S, _ = x.shape
    f = lambda a: np.ascontiguousarray(np.asarray(a, np.float32))
    pcols, wup, lbraw, fnw = _layout_params(f(norm_w), f(gla_w_gate_up), f(gla_b_gate), f(gla_norm_w),
                                            f(hgrn_lower_bounds), f(hgrn_norm_w), f(mem_norm_w),
                                            f(xattn_norm_w), f(final_norm_w))
    cst, msk = _consts()
    nc = _get_nc(S, (0, 1), True)
    shared = {"w_in": f(w_in), "w_out": f(w_out), "w_kv": f(w_mem_kv), "wup": wup, "pcols": pcols,
              "lbraw": lbraw, "fnw": fnw, "cst": cst, "msk": msk}
    in_maps = []
    for b in range(B):
        m = dict(shared)
        m["x"] = np.ascontiguousarray(x[b])
        m["mem"] = np.ascontiguousarray(mem[b])
        in_maps.append(m)
    res = run_bass_kernel_spmd(nc, in_maps, core_ids=list(range(B)))
    return np.stack([np.asarray(r["out"], np.float32) for r in res.results], axis=0)
```

```python
import contextlib
import numpy as np
import ml_dtypes
import concourse.bass as bass
import concourse.mybir as mybir
from concourse.bass_utils import run_bass_kernel_spmd

F32 = mybir.dt.float32
BF16 = mybir.dt.bfloat16
AF = mybir.ActivationFunctionType
ALU = mybir.AluOpType
AX = mybir.AxisListType

D = 1024
DIN = 6160
DMIX = 2048
MEM = 256
EPS = 1e-6
C_GQ, C_GK, C_GV, C_GLR, C_HQ, C_HF, C_HI, C_XQ, C_GATE = 0, 512, 1024, 2048, 2064, 2576, 3088, 3600, 4112


class Prog:
    ENG = ("pe", "act", "dve", "pool", "sp")

    def __init__(self):
        self.q = {e: [] for e in self.ENG}
        self.cnt = {}
        self.res = {}
        self.waited = {e: {} for e in self.ENG}

    def op(self, eng, fn, reads=(), writes=(), dma=None):
        deps = {}

        def add(tok):
            if tok is None:
                return
            k, v = tok
            if deps.get(k, 0) < v:
                deps[k] = v

        for r in reads:
            st = self.res.get(r)
            if st:
                add(st[0])
        for w in writes:
            st = self.res.get(w)
            if st:
                add(st[0])
                for k, v in st[1].items():
                    add((k, v))
        if eng == "pe":
            deps.pop("pe", None)
        waits = []
        wd = self.waited[eng]
        for k, v in deps.items():
            if wd.get(k, 0) < v:
                wd[k] = v
                waits.append((k, v))
        key, amt = (dma, 16) if dma is not None else (eng, 1)
        self.cnt[key] = self.cnt.get(key, 0) + amt
        tok = (key, self.cnt[key])
        self.q[eng].append((waits, fn, key, amt))
        for r in reads:
            st = self.res.setdefault(r, [None, {}])
            if st[1].get(key, 0) < tok[1]:
                st[1][key] = tok[1]
        for w in writes:
            self.res[w] = [tok, {}]
        return tok

    def emit(self, nc):
        with contextlib.ExitStack() as es:
            sems = {k: es.enter_context(nc.semaphore("s_" + k)) for k in self.cnt}
            block = es.enter_context(nc.Block())
            final = [(k, v) for k, v in self.cnt.items()]

            def run(name, e):
                for waits, fn, key, amt in self.q[name]:
                    for k, v in waits:
                        e.wait_ge(sems[k], v)
                    ins = fn(e)
                    ins.then_inc(sems[key], amt)

            @block.tensor
            def _(e):
                run("pe", e)

            @block.scalar
            def _(e):
                run("act", e)

            @block.vector
            def _(e):
                run("dve", e)

            @block.gpsimd
            def _(e):
                run("pool", e)

            @block.sync
            def _(e):
                run("sp", e)
                for k, v in final:
                    e.wait_ge(sems[k], v)


def build(S, layers, final_norm, n_layers_total=2):
    nc = bass.Bass("TRN2", target_bir_lowering=False)
    P = Prog()
    NT = S // 128
    NL = n_layers_total

    def din(name, shape, dt=F32):
        return nc.dram_tensor(name, list(shape), dt, kind="ExternalInput").ap()

    x_d = din("x", [S, D])
    mem_d = din("mem", [MEM, D])
    win_d = din("w_in", [NL, D, DIN])
    wout_d = din("w_out", [NL, DMIX, D])
    wkv_d = din("w_kv", [NL, D, 2 * 512])
    wup_d = din("wup", [NL, 17, 512])
    pcols_d = din("pcols", [NL, 128, 32])
    lbraw_d = din("lbraw", [128, NL * 512])
    fnw_d = din("fnw", [128, D])
    cst_d = din("cst", [128, 6 * 128 + 8])
    msk_d = din("msk", [128, 3 * 128])
    out_d = nc.dram_tensor("out", [S, D], F32, kind="ExternalOutput").ap()
    xmid_d = None
    if len(layers) > 1:
        xmid_d = nc.dram_tensor("xmid", [S, D], F32, kind="Internal").ap()

    es = contextlib.ExitStack()
    with es:
        def sb(name, shape, dt):
            return es.enter_context(nc.sbuf_tensor("sb_" + name, list(shape), dt))

        def ps(name, shape, dt):
            return es.enter_context(nc.psum_tensor("ps_" + name, list(shape), dt))

        win = sb("win", [128, 8, DIN], BF16)
        wout = sb("wout", [128, 16, D], BF16)
        cst = sb("cst", [128, 4 * 128 + 8], F32)
        mskb = sb("mskb", [128, 3 * 128], BF16)
        pcols = sb("pcols", [128, 32], F32)
        wup = sb("wup", [32, 512], BF16)
        lbt = sb("lbt", [128, NL * 512], F32)
        fnw = sb("fnw", [128, D], F32)
        gS = sb("gS", [128, 4, 256], F32)
        gSb = sb("gSb", [128, 4, 256], BF16)
        hS = sb("hS", [128, 4, 128], F32)
        hSb = sb("hSb", [128, 4, 4, 128], BF16)
        mkT = sb("mkT", [128, 4, 256], BF16)
        mv = sb("mv", [128, 2, 512], BF16)
        xt = [sb("xt0", [128, D], F32), sb("xt1", [128, D], F32)]
        h = sb("h", [128, D], BF16)
        hT = sb("hT", [128, 8, 128], BF16)
        Ft = [sb("F%d" % i, [128, 512], F32) for i in range(6)]
        qb = sb("qb", [128, 512], BF16)
        kb = sb("kb", [128, 512], BF16)
        khb = sb("khb", [128, 512], BF16)
        qkT = sb("qkT", [128, 8, 128], BF16)
        v = sb("v", [128, 1024], BF16)
        hi = sb("hi", [128, 512], BF16)
        pbt = sb("pbt", [128, 4, 256], BF16)
        AT = sb("AT", [128, 4, 128], BF16)
        glrT = sb("glrT", [32, 128], BF16)
        G = sb("G", [128, DMIX], BF16)
        mixed = sb("mixed", [128, DMIX], BF16)
        outt = sb("outt", [128, D], F32)
        lbraw = outt
        xqs = qb
        xqT = AT
        pb = pbt
        HIM = ["pbt"]
        pT = qkT
        mT = G[:].rearrange("p (c m) -> p c m", c=16)
        st = sb("st", [128, 128], F32)
        junk = sb("junk", [128, 1024], BF16)
        dmy = sb("dmy", [128, 4], F32)

        pj = [ps("pj0", [128, 512], F32), ps("pj1", [128, 512], F32)]
        pt = ps("pt", [128, 1024], BF16)
        pcu = [ps("pcu0", [128, 512], F32), ps("pcu1", [128, 512], F32)]
        pss = ps("pss", [128, 512], F32)
        po = [ps("po0", [128, 512], F32), ps("po1", [128, 512], F32)]

        ident = mskb[:, 0:128]
        maskG = mskb[:, 128:256]
        maskH = mskb[:, 256:384]
        triG = cst[:, 0:128]
        triUG = cst[:, 128:256]
        triH = cst[:, 256:384]
        triUH = cst[:, 384:512]
        cind = cst[:, 512:520]
        nwT = pcols[:, 0:8]
        gwT = pcols[:, 8:24]
        mnwT = pcols[:, 24:32]

        pjc = [0]

        def next_pj():
            i = pjc[0] % 2
            pjc[0] += 1
            return pj[i], "pj%d" % i

        def bc(ap2, n):
            return ap2.unsqueeze(2).broadcast_to([128, ap2.shape[1], n])

        def bc_mid(ap2, n):
            return ap2.unsqueeze(1).broadcast_to([128, n, ap2.shape[1]])

        def mm(out_ap, pairs, reads, writes, first_start=True, **kw):
            pairs = list(pairs)

            def fn(e):
                n = len(pairs)
                ins = None
                for i, (a, b) in enumerate(pairs):
                    ins = e.matmul(out_ap, a, b, start=(first_start and i == 0), stop=(i == n - 1), **kw)
                return ins
            P.op("pe", fn, reads, writes)

        def mm_multi(items, reads, writes):
            items = list(items)

            def fn(e):
                ins = None
                for (o, a, b, s0, s1, kw) in items:
                    ins = e.matmul(o, a, b, start=s0, stop=s1, **kw)
                return ins
            P.op("pe", fn, reads, writes)

        def transposes(items, reads, writes):
            items = list(items)

            def fn(e):
                ins = None
                for (o, i_) in items:
                    ins = e.transpose(o, i_, ident)
                return ins
            P.op("pe", fn, list(reads) + ["mskb"], writes)

        def act(out, in_, func, reads, writes, **kw):
            P.op("act", lambda e: e.activation(out, in_, func, **kw), reads, writes)

        def tt(eng, out, in0, in1, op, reads, writes):
            P.op(eng, lambda e: e.tensor_tensor(out, in0, in1, op), reads, writes)

        def ts(eng, out, in0, s1, s2, op0, op1, reads, writes):
            if s2 is None:
                P.op(eng, lambda e: e.tensor_scalar(out, in0, s1, None, op0), reads, writes)
            else:
                P.op(eng, lambda e: e.tensor_scalar(out, in0, s1, s2, op0, op1), reads, writes)

        def stt(out, in0, scalar, in1, op0, op1, reads, writes):
            P.op("dve", lambda e: e.scalar_tensor_tensor(out, in0, scalar, in1, op0, op1), reads, writes)

        def copy(eng, out, in_, reads, writes):
            if eng == "act":
                P.op("act", lambda e: e.activation(out, in_, AF.Identity), reads, writes)
            else:
                P.op(eng, lambda e: e.tensor_copy(out, in_), reads, writes)

        def dma(eng, out, in_, reads, writes, sem, **kw):
            P.op(eng, lambda e: e.dma_start(out=out, in_=in_, **kw), reads, writes, dma=sem)

        def rstd_from(ssq_col, tmp_col, out_col, inv_n, rname, reads=None):
            act(tmp_col, ssq_col, AF.Ln, reads or [rname], [rname + "_t"], scale=inv_n, bias=EPS)
            act(out_col, tmp_col, AF.Exp, [rname + "_t"], [rname + "_r"], scale=-0.5)

        dma("sp", cst[:], cst_d[:, 0:520], [], ["cst"], "d_cst")
        dma("pool", mskb[:], msk_d, [], ["mskb"], "d_msk")
        dma("sp", lbraw[:], lbraw_d, [], ["outt"], "d_lbraw")
        if final_norm:
            dma("sp", fnw[:], fnw_d, [], ["fnw"], "d_fnw")
        P.op("pool", lambda e: e.memset(glrT[:], 1.0), [], ["glrT"])
        P.op("dve", lambda e: e.memset(dmy[:], 0.0), [], ["dmy"])
        lr3 = lbraw[:].rearrange("p (l n) -> p l n", l=NL)
        lb3 = lbt[:].rearrange("p (l n) -> p l n", l=NL)
        mx = Ft[0]
        P.op("dve", lambda e: e.tensor_copy(mx[:], lr3[:, 0, :]), ["outt"], ["F0"])
        for l in range(1, NL):
            tt("dve", mx[:], mx[:], lr3[:, l, :], ALU.max, ["F0", "outt"], ["F0"])
        for l in range(NL):
            tt("dve", lb3[:, l, :], lr3[:, l, :], mx[:], ALU.subtract, ["F0", "outt"], ["lbt"])
        act(lbt[:], lbt[:], AF.Exp, ["lbt"], ["lbt"])
        den = Ft[1]
        P.op("dve", lambda e: e.tensor_copy(den[:], lb3[:, 0, :]), ["lbt"], ["F1"])
        for l in range(1, NL):
            tt("dve", den[:], den[:], lb3[:, l, :], ALU.add, ["F1", "lbt"], ["F1"])
        P.op("dve", lambda e: e.reciprocal(den[:], den[:]), ["F1"], ["F1"])
        for l in range(NL):
            tt("dve", lb3[:, l, :], lb3[:, l, :], den[:], ALU.mult, ["F1", "lbt"], ["lbt"])
        p0 = Ft[2]
        P.op("dve", lambda e: e.tensor_copy(p0[:], lb3[:, 0, :]), ["lbt"], ["F2"])
        for l in range(1, NL):
            tt("dve", lb3[:, l, :], lb3[:, l, :], lb3[:, l - 1, :], ALU.add, ["lbt"], ["lbt"])
        for l in range(NL):
            tt("dve", lb3[:, l, :], lb3[:, l, :], p0[:], ALU.subtract, ["lbt", "F2"], ["lbt"])

        F = ["F%d" % i for i in range(6)]

        for li, L in enumerate(layers):
            last = (li == len(layers) - 1)
            src_d = x_d if li == 0 else xmid_d
            dst_d = out_d if last else xmid_d
            do_final = last and final_norm
            lbL = lbt[:, L * 512:(L + 1) * 512]

            dma("sp", pcols[:], pcols_d[L], [], ["pcols"], "d_pcols")
            dma("pool", wup[0:17, :], wup_d[L], [], ["wup"], "d_wup")
            wkv = wout[:, 0:8, :]
            for hf_ in range(2):
                dma("pool", wout[:, hf_ * 4:(hf_ + 1) * 4, :],
                    wkv_d[L, hf_ * 512:(hf_ + 1) * 512, :].rearrange("(c p) n -> p c n", p=128),
                    [], ["wq%d" % hf_], "d_wkv%d" % hf_, max_dma_last_dim=4096)
            wblocks = [(C_XQ, 512, ["wc%d" % C_XQ]), (C_GLR, 528, ["wc%d" % C_GLR, "wc%d" % C_HQ]),
                       (C_GV, 512, ["wc%d" % C_GV]), (C_GV + 512, 512, ["wc%d" % (C_GV + 512)]),
                       (C_HF, 512, ["wc%d" % C_HF]), (C_GQ, 512, ["wc%d" % C_GQ]), (C_GK, 512, ["wc%d" % C_GK])] + \
                      [(C_GATE + g * 512, 512, ["wc%d" % (C_GATE + g * 512)]) for g in range(4)] + \
                      [(C_HI, 512, ["wc%d" % C_HI])]
            for bi, (c0, ncb, rn) in enumerate(wblocks):
                dma("pool", win[:, :, c0:c0 + ncb],
                    win_d[L, :, c0:c0 + ncb].rearrange("(c p) n -> p c n", p=128), [], rn, "d_win%d" % bi,
                    max_dma_last_dim=4096)
            P.op("dve", lambda e: e.memset(gS[:], 0.0), [], ["gS"])
            P.op("dve", lambda e: e.memset(hS[:], 0.0), [], ["hS"])
            P.op("pool", lambda e: e.memset(gSb[:], 0.0), [], ["gSb"])
            P.op("pool", lambda e: e.memset(hSb[:], 0.0), [], ["hSb"])

            mnT = mixed[:].rearrange("p (c m) -> p c m", c=8)
            for blk in range(2):
                xb = xt[blk]
                xn = "xt%d" % blk
                dma("sp", xb[:], mem_d[blk * 128:(blk + 1) * 128, :], [], [xn], "d_x%d" % blk)
                act(h[:], xb[:], AF.Square, [xn], ["h", "ssq"], accum_out=st[:, 0:1])
                rstd_from(st[:, 0:1], st[:, 1:2], st[:, 2:3], 1.0 / D, "ssq")
                ts("dve", h[:], xb[:], st[:, 2:3], None, ALU.mult, None, [xn, "ssq_r"], ["h"])
                transposes([(pt[:, c * 128:(c + 1) * 128], h[:, c * 128:(c + 1) * 128]) for c in range(8)],
                           ["h"], ["pt"])
                tt("dve", mnT[:, :, blk * 128:(blk + 1) * 128], pt[:].rearrange("p (c m) -> p c m", c=8),
                   bc(mnwT, 128), ALU.mult, ["pt", "pcols"], ["mixed"])
            for hd in range(4):
                pjt, pjn = next_pj()
                mm(pjt[:, 0:256], [(wkv[:, c, hd * 128:(hd + 1) * 128], mnT[:, c, :]) for c in range(8)],
                   ["wq0", "wq1", "mixed"], [pjn])
                copy("act", mkT[:, hd, :], pjt[:, 0:256], [pjn], ["mkT"])
            for blk in range(2):
                pjt, pjn = next_pj()
                mm(pjt[:], [(mnT[:, c, blk * 128:(blk + 1) * 128], wkv[:, c, 512:1024]) for c in range(8)],
                   ["wq0", "wq1", "mixed"], [pjn])
                copy("act", mv[:, blk, :], pjt[:], [pjn], ["mv"])
            for q4 in range(4):
                dma("pool", wout[:, q4 * 4:(q4 + 1) * 4, :],
                    wout_d[L, q4 * 512:(q4 + 1) * 512, :].rearrange("(c p) n -> p c n", p=128),
                    [], ["wq%d" % q4], "d_wo%d" % q4, max_dma_last_dim=4096)
            WOUT = ["wq%d" % q4 for q4 in range(4)]

            def proj(col0, ncol, dst_names):
                pjt, pjn = next_pj()
                mm(pjt[:, 0:ncol], [(hT[:, c, :], win[:, c, col0:col0 + ncol]) for c in range(8)],
                   ["hT", "wc%d" % col0], [pjn])
                return pjt, pjn

            def LOAD(t):
                dma("sp", xt[t % 2][:], src_d[t * 128:(t + 1) * 128, :], ["xmid%d" % t] if li > 0 else [],
                    ["xt%d" % (t % 2)], "d_x%d" % (t % 2))

            def HEAD_a(t):
                xb = xt[t % 2]
                xn = "xt%d" % (t % 2)
                act(h[:], xb[:], AF.Square, [xn], ["h", "ssq"], accum_out=st[:, 0:1])
                rstd_from(st[:, 0:1], st[:, 1:2], st[:, 2:3], 1.0 / D, "ssq")
                ts("dve", h[:], xb[:], st[:, 2:3], None, ALU.mult, None, [xn, "ssq_r"], ["h"])

            def HEAD_b(t):
                transposes([(pt[:, c * 128:(c + 1) * 128], h[:, c * 128:(c + 1) * 128]) for c in range(8)],
                           ["h"], ["pt"])
                tt("dve", hT[:], pt[:].rearrange("p (c m) -> p c m", c=8), bc(nwT, 128), ALU.mult,
                   ["pt", "pcols"], ["hT"])

            def HEAD(t):
                HEAD_a(t)
                HEAD_b(t)

            def qk_transposes():
                transposes([(pt[:, hd * 128:(hd + 1) * 128], qb[:, hd * 128:(hd + 1) * 128]) for hd in range(4)] +
                           [(pt[:, (4 + hd) * 128:(5 + hd) * 128], kb[:, hd * 128:(hd + 1) * 128]) for hd in range(4)],
                           ["qb", "kb"], ["pt"])
                copy("dve", qkT[:], pt[:].rearrange("p (c m) -> p c m", c=8), ["pt"], ["qkT"])
                mm_multi([(pss[:, hd * 128:(hd + 1) * 128], qkT[:, 4 + hd, :], qkT[:, hd, :], True, True, {})
                          for hd in range(4)], ["qkT"], ["pss"])

            def mixed_T(e0, e1, rname):
                n = e1 - e0
                transposes([(pt[:, i_ * 128:(i_ + 1) * 128], mixed[:, (e0 + i_) * 128:(e0 + i_ + 1) * 128])
                            for i_ in range(n)], ["mixed"], ["pt"])
                tt("dve", mT[:, e0:e1, :], pt[:, 0:n * 128].rearrange("p (c m) -> p c m", c=n),
                   bc(gwT[:, e0:e1], 128), ALU.mult, ["pt", "pcols"], [rname])

            ctx = {}

            def s_GT():
                act(dmy[:, 0:1], dmy[:, 1:2], AF.Silu, ["dmy"], ["dmy"])
                for g4 in range(4):
                    pjt, pjn = proj(C_GATE + g4 * 512, 512, None)
                    act(G[:, g4 * 512:(g4 + 1) * 512], pjt[:], AF.Silu, [pjn], ["G", "mTa", "mTb", "mTc"])
                act(dmy[:, 0:1], dmy[:, 1:2], AF.Exp, ["dmy"], ["dmy"])

            def s_X1():
                pjt, pjn = proj(C_XQ, 512, None)
                act(xqs[:], pjt[:], AF.Identity, [pjn], ["qb"], scale=float(128 ** -0.5))

            def s_G1a():
                pjt, pjn = next_pj()
                mm(pjt[0:16, 0:128], [(win[:, c, C_GLR:C_GLR + 16], hT[:, c, :]) for c in range(8)],
                   ["hT", "wc%d" % C_GLR], [pjn])
                copy("act", glrT[0:16, :], pjt[0:16, 0:128], [pjn], ["glrT"])

            def s_X2():
                transposes([(pt[:, hd * 128:(hd + 1) * 128], xqs[:, hd * 128:(hd + 1) * 128]) for hd in range(4)],
                           ["qb"], ["pt"])
                copy("dve", xqT[:], pt[:, 0:512].rearrange("p (c m) -> p c m", c=4), ["pt"], ["AT"])

            def s_G1b():
                pjt, pjn = next_pj()
                mm(pjt[:], [(glrT[0:17, :], wup[0:17, :])], ["glrT", "wup"], [pjn])
                act(Ft[0][:], pjt[:], AF.Exp, [pjn], [F[0]], scale=-1.0)
                act(Ft[1][:], Ft[0][:], AF.Ln, [F[0]], [F[1]], bias=1.0)

            def s_X3():
                mm_multi([(po[hd // 2][:, (hd % 2) * 256:(hd % 2 + 1) * 256], xqT[:, hd, :], mkT[:, hd, :],
                           True, True, {}) for hd in range(4)], ["AT", "mkT"], ["po0", "po1"])
                for half in range(2):
                    P.op("dve", (lambda half: lambda e: e.tensor_reduce(
                        st[:, 48 + 2 * half:50 + 2 * half], po[half][:].rearrange("p (a b) -> p a b", a=2),
                        AX.X, ALU.max))(half), ["po%d" % half], ["xmax%d" % half])
                ts("dve", st[:, 52:56], st[:, 48:52], -1.0, None, ALU.mult, None, ["xmax0", "xmax1"], ["xnmax"])
                for hd in range(4):
                    act(pb[:, hd, :], po[hd // 2][:, (hd % 2) * 256:(hd % 2 + 1) * 256], AF.Exp,
                        ["po%d" % (hd // 2), "xnmax"], HIM + ["xZ%d" % hd], bias=st[:, 52 + hd:53 + hd],
                        accum_out=st[:, 56 + hd:57 + hd])

            def s_Hp():
                pjz, pjzn = proj(C_HF, 512, None)
                act(Ft[5][:], pjz[:], AF.Exp, [pjzn], [F[5]], scale=-1.0)

            def s_G2():
                mm(pcu[0][:], [(triG, Ft[1][:])], ["cst", F[1]], ["pcu0"])
                mm(pcu[1][:], [(triUG, Ft[1][:])], ["cst", F[1]], ["pcu1"])
                pjt, pjn = next_pj()
                mm_multi([(pjt[:, hd:hd + 1], Ft[1][:, hd * 128:(hd + 1) * 128], cind[:, 0:1], True, True, {})
                          for hd in range(4)], ["cst", F[1]], [pjn])
                act(st[:, 8:12], pjt[:, 0:4], AF.Exp, [pjn], ["gdec"], scale=-1.0 / 16)
                act(Ft[2][:], pcu[0][:], AF.Exp, ["pcu0"], [F[2]], scale=-1.0 / 16)
                act(Ft[3][:], pcu[0][:], AF.Exp, ["pcu0"], [F[3]], scale=1.0 / 16)
                act(Ft[4][:], pcu[1][:], AF.Exp, ["pcu1"], [F[4]], scale=-1.0 / 16)

            def s_H1():
                act(Ft[1][:], Ft[5][:], AF.Ln, [F[5]], [F[1]], bias=1.0)

            def s_X4():
                transposes([(pt[:, (hd * 2 + mc) * 128:(hd * 2 + mc + 1) * 128], pb[:, hd, mc * 128:(mc + 1) * 128])
                            for hd in range(4) for mc in range(2)], HIM, ["pt"])
                copy("dve", pT[:], pt[:].rearrange("p (c m) -> p c m", c=8), ["pt"], ["qkT"])

            def s_G3a():
                pjt, pjn = proj(C_GQ, 512, None)
                stt(qb[:], pjt[:], float(128 ** -0.5), Ft[2][:], ALU.mult, ALU.mult, [pjn, F[2]], ["qb"])
                pjt, pjn = proj(C_GK, 512, None)
                tt("dve", kb[:], pjt[:], Ft[3][:], ALU.mult, [pjn, F[3]], ["kb"])
                tt("dve", khb[:], pjt[:], Ft[4][:], ALU.mult, [pjn, F[4]], ["khb"])

            def s_X5():
                items = []
                for hd in range(4):
                    for mc in range(2):
                        items.append((po[1][:, hd * 128:(hd + 1) * 128], pT[:, hd * 2 + mc, :],
                                      mv[:, mc, hd * 128:(hd + 1) * 128], mc == 0, mc == 1, {}))
                mm_multi(items, ["qkT", "mv"], ["po1"])
                for hd in range(4):
                    act(junk[:, hd * 256 + 128:hd * 256 + 256], po[1][:, hd * 128:(hd + 1) * 128], AF.Square, ["po1"], ["xssq%d" % hd, "jk%d" % hd],
                        accum_out=st[:, 80 + hd:81 + hd])

            def s_G3b():
                for half in range(2):
                    pjt, pjn = proj(C_GV + half * 512, 512, None)
                    copy("act", v[:, half * 512:(half + 1) * 512], pjt[:], [pjn], ["v"])

            def s_H2():
                tt("dve", Ft[0][:], Ft[5][:], lbL, ALU.mult, [F[5], "lbt"], [F[0]])
                act(Ft[0][:], Ft[0][:], AF.Ln, [F[0]], [F[0]], bias=1.0)
                tt("dve", Ft[5][:], Ft[0][:], Ft[1][:], ALU.subtract, [F[0], F[1]], [F[5]])

            def s_X6():
                tt("dve", st[:, 60:64], st[:, 56:60], st[:, 56:60], ALU.mult, ["xZ%d" % hd for hd in range(4)], ["xz2"])
                ts("dve", st[:, 84:88], st[:, 80:84], 1.0 / 128, None, ALU.mult, None,
                   ["xssq%d" % hd for hd in range(4)], ["xvv"])
                stt(st[:, 84:88], st[:, 60:64], EPS, st[:, 84:88], ALU.mult, ALU.add, ["xz2", "xvv"], ["xvv"])
                act(st[:, 84:88], st[:, 84:88], AF.Ln, ["xvv"], ["xvv"])
                act(st[:, 88:92], st[:, 84:88], AF.Exp, ["xvv"], ["xr"], scale=-0.5)
                for hd in range(4):
                    stt(mixed[:, 1536 + hd * 128:1536 + (hd + 1) * 128], po[1][:, hd * 128:(hd + 1) * 128],
                        st[:, 88 + hd:89 + hd], G[:, 1536 + hd * 128:1536 + (hd + 1) * 128], ALU.mult, ALU.mult,
                        ["po1", "xr", "G"], ["mixed"])

            def s_G4():
                qk_transposes()
                tt("dve", AT[:], pss[:].rearrange("p (c m) -> p c m", c=4), bc_mid(maskG, 4), ALU.mult,
                   ["pss", "mskb"], ["AT"])

            def s_H3a():
                act(Ft[0][:], Ft[5][:], AF.Exp, [F[5]], [F[0]])
                ts("dve", Ft[0][:], Ft[0][:], -1.0, 1.0, ALU.mult, ALU.add, [F[0]], [F[0]])

            def s_H3b():
                pjr, pjrn = next_pj()
                mm(pss[:], [(triH, Ft[5][:])], ["cst", F[5]], ["pss"])
                mm(pjr[:], [(triUH, Ft[5][:])], ["cst", F[5]], [pjrn])
                pjt, pjn = next_pj()
                mm_multi([(pjt[:, hd * 4:hd * 4 + 4], Ft[5][:, hd * 128:(hd + 1) * 128], cind[:, 1:5], True, True, {})
                          for hd in range(4)], ["cst", F[5]], [pjn])
                act(Ft[1][:], pss[:], AF.Exp, ["pss"], [F[1]])
                act(Ft[2][:], pss[:], AF.Exp, ["pss"], [F[2]], scale=-1.0)
                act(Ft[3][:], pjr[:], AF.Exp, [pjrn], [F[3]])
                act(st[:, 32:48], pjt[:, 0:16], AF.Exp, [pjn], ["hdec"])

            def s_G5a():
                items = []
                for hd in range(4):
                    o_ap = po[hd // 2][:, (hd % 2) * 256:(hd % 2 + 1) * 256]
                    items.append((o_ap, AT[:, hd, :], v[:, hd * 256:(hd + 1) * 256], True, False, {}))
                    items.append((o_ap, qkT[:, hd, :], gSb[:, hd, :], False, True, {}))
                mm_multi(items, ["AT", "v", "qkT", "gSb"], ["po0", "po1"])
                for hd in range(4):
                    o_ap = po[hd // 2][:, (hd % 2) * 256:(hd % 2 + 1) * 256]
                    act(junk[:, hd * 256:(hd + 1) * 256], o_ap, AF.Square, ["po%d" % (hd // 2)], ["gssq%d" % hd, "jk%d" % hd],
                        accum_out=st[:, 16 + hd:17 + hd])
                rstd_from(st[:, 16:20], st[:, 20:24], st[:, 24:28], 1.0 / 256, "gssq",
                          ["gssq%d" % hd for hd in range(4)])

            def s_G5b():
                mm_multi([(pcu[hd // 2][:, (hd % 2) * 256:(hd % 2 + 1) * 256], khb[:, hd * 128:(hd + 1) * 128],
                           v[:, hd * 256:(hd + 1) * 256], True, True, {}) for hd in range(4)],
                         ["khb", "v"], ["pcu0", "pcu1"])
                for hd in range(4):
                    stt(gS[:, hd, :], gS[:, hd, :], st[:, 8 + hd:9 + hd],
                        pcu[hd // 2][:, (hd % 2) * 256:(hd % 2 + 1) * 256], ALU.mult, ALU.add,
                        ["gS", "gdec", "pcu%d" % (hd // 2)], ["gS"])
                copy("act", gSb[:].rearrange("p a b -> p (a b)"), gS[:].rearrange("p a b -> p (a b)"),
                     ["gS"], ["gSb"])

            def s_G5c():
                for hd in range(4):
                    o_ap = po[hd // 2][:, (hd % 2) * 256:(hd % 2 + 1) * 256]
                    act(Ft[2 + hd // 2][:, (hd % 2) * 256:(hd % 2 + 1) * 256], o_ap, AF.Identity,
                        ["po%d" % (hd // 2), "gssq_r"], [F[2 + hd // 2]], scale=st[:, 24 + hd:25 + hd])
                for pr in range(2):
                    tt("pool", mixed[:, pr * 512:(pr + 1) * 512], Ft[2 + pr][:], G[:, pr * 512:(pr + 1) * 512],
                       ALU.mult, [F[2 + pr], "G"], ["mixed"])

            def s_H4():
                pjt, pjn = proj(C_HQ, 512, None)
                tt("dve", qb[:], pjt[:], Ft[1][:], ALU.mult, [pjn, F[1]], ["qb"])
                tt("dve", kb[:], Ft[0][:], Ft[2][:], ALU.mult, [F[0], F[2]], ["kb"])
                tt("pool", khb[:], Ft[0][:], Ft[3][:], ALU.mult, [F[0], F[3]], ["khb"])
                pjt, pjn = proj(C_HI, 512, None)
                copy("act", hi[:], pjt[:], [pjn], ["hi"])

            ubank = [(pcu[0], "pcu0"), (pcu[1], "pcu1"), (pss, "pss"), (po[1], "po1")]

            def s_H5a():
                qk_transposes()
                tt("dve", AT[:], pss[:].rearrange("p (c m) -> p c m", c=4), bc_mid(maskH, 4), ALU.mult,
                   ["pss", "mskb"], ["AT"])

            def s_H5b():
                mm_multi([(po[0][:, hd * 128:(hd + 1) * 128], AT[:, hd, :], hi[:, hd * 128:(hd + 1) * 128],
                           hd == 0, False, {"skip_group_check": True}) for hd in range(4)],
                         ["AT", "hi"], ["po0"])
                for c4 in range(4):
                    pu, pun = ubank[c4]
                    mm_multi([(pu[:, hd * 128:(hd + 1) * 128], khb[32 * c4:32 * (c4 + 1), hd * 128:(hd + 1) * 128],
                               hi[32 * c4:32 * (c4 + 1), hd * 128:(hd + 1) * 128], True, True,
                               {"tile_position": (32 * c4, 0)}) for hd in range(4)],
                             ["khb", "hi"], [pun])

            def s_O2a(part):
                elist = [0, 1, 2, 3, 4, 5, 6, 7, 12, 13, 14, 15]
                es_ = elist[part * 3:(part + 1) * 3]
                items = []
                for half in range(2):
                    for e_ in es_:
                        items.append((pj[half][:], mT[:, e_, :], wout[:, e_, half * 512:(half + 1) * 512],
                                      e_ == 0, False, {"skip_group_check": True}))
                mm_multi(items, ["mTa", "mTb"] + WOUT, ["pj0", "pj1"])

            def s_H6():
                for c4 in range(4):
                    pu, pun = ubank[c4]
                    for hd in range(4):
                        sbn = "hSb%d_%d" % (hd, c4)
                        mm_multi([(po[0][32 * c4:32 * (c4 + 1), hd * 128:(hd + 1) * 128],
                                   qkT[:, hd, 32 * c4:32 * (c4 + 1)], hSb[:, hd, c4, :], False, c4 == 3,
                                   {"skip_group_check": True, "tile_position": (0, 32 * c4)})],
                                 ["qkT", sbn, "hSb"], ["po0"])
                        stt(hS[:, hd, :], hS[:, hd, :], st[:, 32 + hd * 4 + c4:33 + hd * 4 + c4],
                            pu[:, hd * 128:(hd + 1) * 128], ALU.mult, ALU.add,
                            ["hS%d" % hd, "hS", "hdec", pun], ["hS%d" % hd])
                        copy("act", hSb[:, hd, (c4 + 1) % 4, :], hS[:, hd, :], ["hS%d" % hd],
                             ["hSb%d_%d" % (hd, (c4 + 1) % 4)])
                    s_O2a(c4)
                for hd in range(4):
                    act(junk[:, hd * 256:hd * 256 + 128], po[0][:, hd * 128:(hd + 1) * 128], AF.Square, ["po0"], ["hssq%d" % hd, "jk%d" % hd],
                        accum_out=st[:, 64 + hd:65 + hd])
                rstd_from(st[:, 64:68], st[:, 68:72], st[:, 72:76], 1.0 / 128, "hssq",
                          ["hssq%d" % hd for hd in range(4)])
                for hd in range(4):
                    stt(mixed[:, 1024 + hd * 128:1024 + (hd + 1) * 128], po[0][:, hd * 128:(hd + 1) * 128],
                        st[:, 72 + hd:73 + hd], G[:, 1024 + hd * 128:1024 + (hd + 1) * 128], ALU.mult, ALU.mult,
                        ["po0", "hssq_r", "G"], ["mixed"])

            def s_O2(t):
                xb = xt[t % 2]
                xn = "xt%d" % (t % 2)
                rows = slice(t * 128, (t + 1) * 128)
                for half in range(2):
                    pjn = "pj%d" % half
                    mm_multi([(pj[half][:], mT[:, e_, :], wout[:, e_, half * 512:(half + 1) * 512], False, e_ == 11,
                               {"skip_group_check": True}) for e_ in range(8, 12)],
                             ["mTc"] + WOUT, [pjn])
                    tt("dve", xb[:, half * 512:(half + 1) * 512], xb[:, half * 512:(half + 1) * 512], pj[half][:],
                       ALU.add, [xn, pjn], [xn])
                pjc[0] = 0
                if do_final:
                    act(outt[:], xb[:], AF.Square, [xn], ["outt", "fssq"], accum_out=st[:, 4:5])
                    rstd_from(st[:, 4:5], st[:, 5:6], st[:, 6:7], 1.0 / D, "fssq")
                    stt(outt[:], xb[:], st[:, 6:7], fnw[:], ALU.mult, ALU.mult, [xn, "fssq_r", "fnw"], ["outt"])
                    dma("sp", dst_d[rows, :], outt[:], ["outt"], ["xmid%d" % t], "d_o")
                else:
                    dma("sp", dst_d[rows, :], xb[:], [xn], ["xmid%d" % t], "d_x%d" % (t % 2))

            LOAD(0)
            HEAD(0)
            for t in range(NT):
                nxt = t + 1 < NT
                if nxt:
                    LOAD(t + 1)
                s_X1(); s_G1a(); s_G3b(); s_Hp(); s_X2(); s_G1b(); s_X3(); s_G2(); s_H1(); s_X4(); s_G3a(); s_X5()
                s_H2()
                s_GT(); s_G4(); s_X6()
                if nxt:
                    HEAD_a(t + 1)
                s_H3a(); s_H3b(); s_G5a(); s_G5b()
                mixed_T(12, 16, "mTb")
                s_H4()
                s_H5a()
                if nxt:
                    HEAD_b(t + 1)
                s_G5c()
                s_H5b()
                mixed_T(0, 8, "mTa")
                s_H6()
                mixed_T(8, 12, "mTc")
                s_O2(t)

        P.emit(nc)
    return nc


def _consts():
    j = np.arange(128)[:, None]
    i = np.arange(128)[None, :]
    triG = (j <= i).astype(np.float32)
    triUG = (j > i).astype(np.float32)
    same = (j // 32) == (i // 32)
    triH = ((j <= i) & same).astype(np.float32)
    triUH = ((j > i) & same).astype(np.float32)
    cind = np.zeros((128, 8), np.float32)
    cind[:, 0] = 1.0
    for c in range(4):
        cind[:, 1 + c] = (np.arange(128) // 32 == c)
    cst = np.concatenate([triG, triUG, triH, triUH, cind, np.zeros((128, 6 * 128 + 8 - 520), np.float32)], axis=1)
    ident = np.eye(128, dtype=np.float32)
    maskG = triG
    maskH = triH
    msk = np.concatenate([ident, maskG, maskH], axis=1)
    return np.ascontiguousarray(cst), np.ascontiguousarray(msk)


def _layout_params(norm_w, gla_w_gate_up, gla_b_gate, gla_norm_w, hgrn_lower_bounds, hgrn_norm_w,
                   mem_norm_w, xattn_norm_w, final_norm_w):
    NL = norm_w.shape[0]
    pcols = np.zeros((NL, 128, 32), np.float32)
    for L in range(NL):
        pcols[L, :, 0:8] = norm_w[L].reshape(8, 128).T
        gw = np.concatenate([np.tile(gla_norm_w[L], 4), np.tile(hgrn_norm_w[L], 4), np.tile(xattn_norm_w[L], 4)])
        pcols[L, :, 8:24] = gw.reshape(16, 128).T
        pcols[L, :, 24:32] = mem_norm_w[L].reshape(8, 128).T
    wup = np.concatenate([gla_w_gate_up, gla_b_gate[:, None, :]], axis=1).astype(np.float32)
    lbraw = np.ascontiguousarray(np.broadcast_to(hgrn_lower_bounds.reshape(1, -1), (128, NL * 512))).astype(np.float32)
    fnw = np.ascontiguousarray(np.broadcast_to(final_norm_w.reshape(1, -1), (128, D))).astype(np.float32)
    return pcols, np.ascontiguousarray(wup), lbraw, fnw


_NC_CACHE = {}


def _get_nc(S, layers, final_norm):
    key = (S, tuple(layers), final_norm)
    if key not in _NC_CACHE:
        _NC_CACHE[key] = build(S, list(layers), final_norm)
    return _NC_CACHE[key]


def kernel(x, mem, norm_w, w_in, gla_w_gate_up, gla_b_gate, gla_norm_w, hgrn_lower_bounds,
           hgrn_norm_w, mem_norm_w, w_mem_kv, xattn_norm_w, w_out, final_norm_w):
    x = np.asarray(x, np.float32)
    mem = np.asarray(mem, np.float32)
    B, S, _ = x.shape
    f = lambda a: np.ascontiguousarray(np.asarray(a, np.float32))
    pcols, wup, lbraw, fnw = _layout_params(f(norm_w), f(gla_w_gate_up), f(gla_b_gate), f(gla_norm_w),
                                            f(hgrn_lower_bounds), f(hgrn_norm_w), f(mem_norm_w),
                                            f(xattn_norm_w), f(final_norm_w))
    cst, msk = _consts()
    nc = _get_nc(S, (0, 1), True)
    shared = {"w_in": f(w_in), "w_out": f(w_out), "w_kv": f(w_mem_kv), "wup": wup, "pcols": pcols,
              "lbraw": lbraw, "fnw": fnw, "cst": cst, "msk": msk}
    in_maps = []
    for b in range(B):
        m = dict(shared)
        m["x"] = np.ascontiguousarray(x[b])
        m["mem"] = np.ascontiguousarray(mem[b])
        in_maps.append(m)
    res = run_bass_kernel_spmd(nc, in_maps, core_ids=list(range(B)))
    return np.stack([np.asarray(r["out"], np.float32) for r in res.results], axis=0)
```
